# Optimizing a Trainium2 kernel written in Bass

```python
import jax
import jax.numpy as jnp
from jax import lax
import numpy as np

D_MODEL = 1024
BATCH = 4
SEQ = 8192
DEPTH = 2

D_MIX = D_MODEL
SB_HEAD_DIM = 64
D_SB = D_MIX // 2
SB_HEADS = D_SB // SB_HEAD_DIM
BLOCK_Q = 128
D_LRU = D_MIX // 4
LRU_BLOCKS = 4
LRU_BLOCK = D_LRU // LRU_BLOCKS
LRU_CONV_WIDTH = 4
LRU_C = 8.0
D_CONF = D_MIX // 4
CONF_CONV_WIDTH = 31
D_IN = 3 * D_SB + 2 * D_LRU + 2 * D_CONF
N_GROUPS = 4
EXPERTS_PER_GROUP = 8
N_EXPERTS = N_GROUPS * EXPERTS_PER_GROUP
TOP_K_FINE = 2
D_EXPERT = D_MODEL // 2
MOE_BLOCK = 256
EPS = 1e-6

kernel_name = 'hybrid_stickbreak_rglru_conformer_hmoe'


def rms_norm(x, g):
    xf = x.astype(jnp.float32)
    y = xf * lax.rsqrt(jnp.mean(xf * xf, axis=-1, keepdims=True) + EPS)
    return (y * g.astype(jnp.float32)).astype(x.dtype)


def layer_norm(x, g, b):
    xf = x.astype(jnp.float32)
    mu = jnp.mean(xf, axis=-1, keepdims=True)
    xc = xf - mu
    var = jnp.mean(xc * xc, axis=-1, keepdims=True)
    return xc * lax.rsqrt(var + EPS) * g.astype(jnp.float32) + b.astype(jnp.float32)


def causal_depthwise_conv(x, w, b):
    k_width, chans = w.shape
    y = lax.conv_general_dilated(
        x, w.astype(x.dtype)[:, None, :], window_strides=(1,),
        padding=[(k_width - 1, 0)], dimension_numbers=('NWC', 'WIO', 'NWC'),
        feature_group_count=chans)
    return y + b.astype(x.dtype)


def stick_breaking_attention(q, k, v):
    seq = q.shape[2]
    scale = SB_HEAD_DIM ** -0.5
    outs = []
    for i in range(seq // BLOCK_Q):
        q0 = i * BLOCK_Q
        kv_len = q0 + BLOCK_Q
        qb = q[:, :, q0:kv_len]
        kb = k[:, :, :kv_len]
        vb = v[:, :, :kv_len]
        z = jnp.einsum('bhqd,bhkd->bhqk', qb, kb, preferred_element_type=jnp.float32) * scale
        t_pos = q0 + jnp.arange(BLOCK_Q, dtype=jnp.int32)[:, None]
        s_pos = jnp.arange(kv_len, dtype=jnp.int32)[None, :]
        mask = s_pos < t_pos
        log_keep = jnp.where(mask, jax.nn.log_sigmoid(-z), 0.0)
        suffix = lax.cumsum(log_keep, axis=3, reverse=True) - log_keep
        wts = jnp.where(mask, jnp.exp(jax.nn.log_sigmoid(z) + suffix), 0.0)
        outs.append(jnp.einsum('bhqk,bhkd->bhqd', wts.astype(vb.dtype), vb))
    return jnp.concatenate(outs, axis=2)


def _linear_combine(c1, c2):
    a1, b1 = c1
    a2, b2 = c2
    return a1 * a2, a2 * b1 + b2


def rg_lru_branch(xr, xg, conv_w, conv_b, wa, ba, wx, bx, lam):
    f32 = jnp.float32
    bsz, seq, _ = xr.shape
    xr = causal_depthwise_conv(xr, conv_w, conv_b)
    xblk = xr.reshape(bsz, seq, LRU_BLOCKS, LRU_BLOCK)
    gate_r = jnp.einsum('bsnc,ncd->bsnd', xblk, wa).reshape(bsz, seq, D_LRU) + ba
    gate_i = jnp.einsum('bsnc,ncd->bsnd', xblk, wx).reshape(bsz, seq, D_LRU) + bx
    log_a = LRU_C * jax.nn.sigmoid(gate_r.astype(f32)) * jax.nn.log_sigmoid(lam.astype(f32))
    a = jnp.exp(log_a)
    u = jnp.sqrt(-jnp.expm1(2.0 * log_a)) * (jax.nn.sigmoid(gate_i.astype(f32)) * xr.astype(f32))
    _, h = lax.associative_scan(_linear_combine, (a, u), axis=1)
    return (h * jax.nn.gelu(xg.astype(f32))).astype(xr.dtype)


def conformer_conv(xc, dw_w, dw_b, ln_g, ln_b, pw_w, pw_b):
    val, gate = jnp.split(xc, 2, axis=-1)
    u = val * jax.nn.sigmoid(gate)
    u = causal_depthwise_conv(u, dw_w, dw_b)
    u = jax.nn.silu(layer_norm(u, ln_g, ln_b)).astype(xc.dtype)
    return jnp.einsum('bsc,ce->bse', u, pw_w) + pw_b


def hierarchical_moe(h, w_coarse, w_fine, w_gate, w_up, w_down):
    n_tok, d = h.shape
    f32 = jnp.float32
    logits_c = jnp.einsum('nd,dg->ng', h, w_coarse, preferred_element_type=f32)
    probs_c = jax.nn.softmax(logits_c, axis=-1)
    grp = jnp.argmax(logits_c, axis=-1).astype(jnp.int32)
    w_grp = jnp.take_along_axis(probs_c, grp[:, None], axis=-1)
    logits_f = jnp.einsum('nd,de->ne', h, w_fine, preferred_element_type=f32)
    logits_f = logits_f.reshape(n_tok, N_GROUPS, EXPERTS_PER_GROUP)
    logits_f = jnp.take_along_axis(logits_f, grp[:, None, None], axis=1)[:, 0]
    top_val, top_idx = lax.top_k(logits_f, TOP_K_FINE)
    gate = (jax.nn.softmax(top_val, axis=-1) * w_grp).astype(h.dtype)
    expert = grp[:, None] * EXPERTS_PER_GROUP + top_idx.astype(jnp.int32)

    n_asg = n_tok * TOP_K_FINE
    flat_e = expert.reshape(n_asg)
    flat_tok = jnp.repeat(jnp.arange(n_tok, dtype=jnp.int32), TOP_K_FINE)
    flat_gate = gate.reshape(n_asg)
    order = jnp.argsort(flat_e)
    sorted_e = flat_e[order]
    counts = jnp.bincount(flat_e, length=N_EXPERTS).astype(jnp.int32)
    padded = (counts + MOE_BLOCK - 1) // MOE_BLOCK * MOE_BLOCK
    starts = jnp.cumsum(counts) - counts
    padded_ends = jnp.cumsum(padded)
    padded_starts = padded_ends - padded
    dest = padded_starts[sorted_e] + jnp.arange(n_asg, dtype=jnp.int32) - starts[sorted_e]
    n_blocks = -(-n_asg // MOE_BLOCK) + N_EXPERTS
    n_slots = n_blocks * MOE_BLOCK
    slot_tok = jnp.full((n_slots,), n_tok, jnp.int32).at[dest].set(flat_tok[order])
    slot_gate = jnp.zeros((n_slots,), h.dtype).at[dest].set(flat_gate[order])
    block_start = jnp.arange(n_blocks, dtype=jnp.int32) * MOE_BLOCK
    block_e = jnp.minimum(jnp.searchsorted(padded_ends, block_start, side='right'),
                          N_EXPERTS - 1).astype(jnp.int32)
    h_pad = jnp.concatenate([h, jnp.zeros((1, d), h.dtype)], axis=0)
    xb = h_pad[slot_tok].reshape(n_blocks, MOE_BLOCK, d)

    def run_block(args):
        xe, e = args
        return (jax.nn.silu(xe @ w_gate[e]) * (xe @ w_up[e])) @ w_down[e]

    yb = lax.map(run_block, (xb, block_e)).reshape(n_slots, d)
    y = jnp.zeros((n_tok + 1, d), yb.dtype).at[slot_tok].add(yb * slot_gate[:, None])
    return y[:n_tok]


def setup_inputs(seed: int = 0) -> dict:
    key = jax.random.key(seed)
    ks = jax.random.split(key, 28)
    f32 = jnp.float32
    L = DEPTH

    def nrm(k, shape, scale):
        return jax.random.normal(k, shape, f32) * scale

    def gain(k, shape):
        return 1.0 + 0.05 * jax.random.normal(k, shape, f32)

    a_c = jax.random.uniform(ks[11], (L, D_LRU), f32, 0.9, 0.999)
    a0 = a_c ** (1.0 / LRU_C)
    lam = jnp.log(a0) - jnp.log1p(-a0)

    return {
        'x': jax.random.normal(ks[0], (BATCH, SEQ, D_MODEL), f32),
        'norm1_g': gain(ks[1], (L, D_MODEL)),
        'w_in': nrm(ks[2], (L, D_MODEL, D_IN), D_MODEL ** -0.5),
        'q_norm_g': gain(ks[3], (L, SB_HEAD_DIM)),
        'k_norm_g': gain(ks[4], (L, SB_HEAD_DIM)),
        'lru_conv_w': nrm(ks[5], (L, LRU_CONV_WIDTH, D_LRU), LRU_CONV_WIDTH ** -0.5),
        'lru_conv_b': nrm(ks[6], (L, D_LRU), 0.02),
        'lru_wa': nrm(ks[7], (L, LRU_BLOCKS, LRU_BLOCK, LRU_BLOCK), LRU_BLOCK ** -0.5),
        'lru_ba': nrm(ks[8], (L, D_LRU), 0.02),
        'lru_wx': nrm(ks[9], (L, LRU_BLOCKS, LRU_BLOCK, LRU_BLOCK), LRU_BLOCK ** -0.5),
        'lru_bx': nrm(ks[10], (L, D_LRU), 0.02),
        'lru_lambda': lam,
        'conf_dw_w': nrm(ks[12], (L, CONF_CONV_WIDTH, D_CONF), CONF_CONV_WIDTH ** -0.5),
        'conf_dw_b': nrm(ks[13], (L, D_CONF), 0.02),
        'conf_ln_g': gain(ks[14], (L, D_CONF)),
        'conf_ln_b': nrm(ks[15], (L, D_CONF), 0.02),
        'conf_pw_w': nrm(ks[16], (L, D_CONF, D_CONF), D_CONF ** -0.5),
        'conf_pw_b': nrm(ks[17], (L, D_CONF), 0.02),
        'out_norm_g': gain(ks[18], (L, D_MIX)),
        'w_out': nrm(ks[19], (L, D_MIX, D_MODEL), D_MIX ** -0.5),
        'norm2_g': gain(ks[20], (L, D_MODEL)),
        'router_coarse': nrm(ks[21], (L, D_MODEL, N_GROUPS), D_MODEL ** -0.5),
        'router_fine': nrm(ks[22], (L, D_MODEL, N_EXPERTS), D_MODEL ** -0.5),
        'exp_w_gate': nrm(ks[23], (L, N_EXPERTS, D_MODEL, D_EXPERT), D_MODEL ** -0.5),
        'exp_w_up': nrm(ks[24], (L, N_EXPERTS, D_MODEL, D_EXPERT), D_MODEL ** -0.5),
        'exp_w_down': nrm(ks[25], (L, N_EXPERTS, D_EXPERT, D_MODEL), D_EXPERT ** -0.5),
    }


def reference(x, norm1_g, w_in, q_norm_g, k_norm_g, lru_conv_w, lru_conv_b, lru_wa, lru_ba,
              lru_wx, lru_bx, lru_lambda, conf_dw_w, conf_dw_b, conf_ln_g, conf_ln_b,
              conf_pw_w, conf_pw_b, out_norm_g, w_out, norm2_g, router_coarse, router_fine,
              exp_w_gate, exp_w_up, exp_w_down):
    bsz, seq, d = x.shape
    splits = [D_SB, 2 * D_SB, 3 * D_SB, 3 * D_SB + D_LRU, 3 * D_SB + 2 * D_LRU]
    for l in range(DEPTH):
        h = rms_norm(x, norm1_g[l])
        proj = jnp.einsum('bsd,de->bse', h, w_in[l])
        q, k, v, xr, xg, xc = jnp.split(proj, splits, axis=-1)
        q = rms_norm(q.reshape(bsz, seq, SB_HEADS, SB_HEAD_DIM), q_norm_g[l]).transpose(0, 2, 1, 3)
        k = rms_norm(k.reshape(bsz, seq, SB_HEADS, SB_HEAD_DIM), k_norm_g[l]).transpose(0, 2, 1, 3)
        v = v.reshape(bsz, seq, SB_HEADS, SB_HEAD_DIM).transpose(0, 2, 1, 3)
        y_a = stick_breaking_attention(q, k, v).transpose(0, 2, 1, 3).reshape(bsz, seq, D_SB)
        y_b = rg_lru_branch(xr, xg, lru_conv_w[l], lru_conv_b[l], lru_wa[l], lru_ba[l],
                            lru_wx[l], lru_bx[l], lru_lambda[l])
        y_c = conformer_conv(xc, conf_dw_w[l], conf_dw_b[l], conf_ln_g[l], conf_ln_b[l],
                             conf_pw_w[l], conf_pw_b[l])
        g_out = out_norm_g[l]
        y = jnp.concatenate([
            rms_norm(y_a, g_out[:D_SB]),
            rms_norm(y_b, g_out[D_SB:D_SB + D_LRU]),
            rms_norm(y_c, g_out[D_SB + D_LRU:]),
        ], axis=-1)
        x = x + jnp.einsum('bse,ed->bsd', y, w_out[l])
        h2 = rms_norm(x, norm2_g[l]).reshape(bsz * seq, d)
        x = x + hierarchical_moe(h2, router_coarse[l], router_fine[l], exp_w_gate[l],
                                 exp_w_up[l], exp_w_down[l]).reshape(bsz, seq, d)
    return x
```

```python
import numpy as np
import ml_dtypes
from contextlib import ExitStack
import concourse.bass as bass
import concourse.mybir as mybir
from concourse.bass_utils import run_bass_kernel_spmd

F32 = mybir.dt.float32
BF16 = mybir.dt.bfloat16
I32 = mybir.dt.int32
AF = mybir.ActivationFunctionType
ALU = mybir.AluOpType
AX = mybir.AxisListType

ENG_ATTR = {'pe': 'tensor', 'act': 'scalar', 'dve': 'vector', 'pool': 'gpsimd', 'sp': 'sync'}
EPS = 1e-6


class GSync:
    def __init__(self, nc, stack):
        self.nc = nc
        self.stack = stack
        self.sems = {}
        self.cnts = {}
        self.tagmap = {}

    def sem(self, name):
        if name not in self.sems:
            self.sems[name] = self.stack.enter_context(self.nc.semaphore(name))
        return self.sems[name]

    def tagsem(self, tag):
        if tag not in self.tagmap:
            self.tagmap[tag] = 't_' + tag
        return self.tagmap[tag]


class Sched:
    def __init__(self, nc, stack, gs=None):
        self.nc = nc
        self.stack = stack
        self.gs = gs if gs is not None else GSync(nc, stack)
        self.ops = []
        self.wx = {}
        self.wc = {}
        self.rd = {}
        self.tag_last = {}
        self.sems = {}

    @staticmethod
    def _merge(dst, src):
        for s, i in src.items():
            if dst.get(s, -1) < i:
                dst[s] = i

    def add(self, eng, fn, r=(), w=(), cw=(), tag=None, inc=16):
        deps = {}
        for k in r:
            self._merge(deps, self.wx.get(k, {}))
            self._merge(deps, self.wc.get(k, {}))
        for k in w:
            self._merge(deps, self.wx.get(k, {}))
            self._merge(deps, self.wc.get(k, {}))
            self._merge(deps, self.rd.get(k, {}))
        for k in cw:
            self._merge(deps, self.wx.get(k, {}))
            self._merge(deps, self.rd.get(k, {}))
        if tag is not None and tag in self.tag_last:
            self._merge(deps, {('t', tag): self.tag_last[tag]})
        idx = len(self.ops)
        if eng == 'pe':
            deps.pop(('e', 'pe'), None)
        self.ops.append(dict(eng=eng, fn=fn, deps=deps, tag=tag, sem=None, cnt=0, inc=inc))
        sig = ('t', tag) if tag else ('e', eng)
        for k in r:
            self.rd.setdefault(k, {})[sig] = idx
        for k in w:
            self.wx[k] = {sig: idx}
            self.wc[k] = {}
            self.rd[k] = {}
        for k in cw:
            self.wc.setdefault(k, {})[sig] = idx
        if tag is not None:
            self.tag_last[tag] = idx
        return idx

    def _sem(self, name):
        return self.gs.sem(name)

    def emit(self):
        ops = self.ops
        need = set()
        for o in ops:
            for i in o['deps'].values():
                need.add(i)
        cnts = self.gs.cnts
        for i, o in enumerate(ops):
            if o['tag']:
                o['sem'] = self.gs.tagsem(o['tag'])
                cnts[o['sem']] = cnts.get(o['sem'], 0) + o['inc']
                o['cnt'] = cnts[o['sem']]
            elif i in need:
                o['sem'] = 'e_' + o['eng']
                cnts[o['sem']] = cnts.get(o['sem'], 0) + 1
                o['cnt'] = cnts[o['sem']]
        final = {}
        for o in ops:
            if o['sem']:
                final[o['sem']] = max(final.get(o['sem'], 0), o['cnt'])
        for s in final:
            self._sem(s)
        with self.nc.Block() as blk:
            for eng, attr in ENG_ATTR.items():
                mine = [o for o in ops if o['eng'] == eng]

                def body(e, mine=mine, eng=eng):
                    waited = {}
                    for o in mine:
                        for di in sorted(o['deps'].values()):
                            d = ops[di]
                            if waited.get(d['sem'], 0) < d['cnt']:
                                e.wait_ge(self._sem(d['sem']), d['cnt'])
                                waited[d['sem']] = d['cnt']
                        ins = o['fn'](e)
                        if o['sem']:
                            ins.then_inc(self._sem(o['sem']), o['inc'] if o['tag'] else 1)
                    if eng == 'sp':
                        for s, c in final.items():
                            if waited.get(s, 0) < c:
                                e.wait_ge(self._sem(s), c)

                getattr(blk, attr)(body)
        return dict(nops=len(ops), final=final)


class Ctx:
    def __init__(self, nc, st, gs=None):
        self.nc = nc
        self.st = st
        self.S = Sched(nc, st, gs)
        g = self.S.gs
        g.phase = getattr(g, 'phase', 0) + 1
        self.pfx = f"p{g.phase}_"

    def sb(self, name, shape, dt):
        return self.st.enter_context(self.nc.sbuf_tensor(self.pfx + name, shape, dt))

    def ps(self, name, shape, dt):
        return self.st.enter_context(self.nc.psum_tensor(self.pfx + name, shape, dt))

    def dma(self, q, out, in_, r, w, tag, cw=()):
        self.S.add(q, lambda e: e.dma_start(out=out, in_=in_), r=r, w=w, cw=cw, tag=tag)

    def mm(self, out, lhsT, rhs, start, stop, r, w):
        self.S.add('pe', lambda e: e.matmul(out, lhsT=lhsT, rhs=rhs, start=start, stop=stop,
                                            skip_group_check=True), r=r, w=w)

    def tr(self, out, in_, ident, r, w):
        self.S.add('pe', lambda e: e.transpose(out, in_, ident), r=r, w=w)

    def act(self, out, in_, func, r, w, bias=None, scale=None, accum=None):
        kw = {}
        if bias is not None:
            kw['bias'] = bias
        if scale is not None:
            kw['scale'] = scale
        if accum is not None:
            kw['accum_out'] = accum
        self.S.add('act', lambda e: e.activation(out=out, in_=in_, func=func, **kw), r=r, w=w)

    def ts(self, eng, out, in0, s1, s2, op0, op1, r, w):
        if op1 is None:
            self.S.add(eng, lambda e: e.tensor_scalar(out=out, in0=in0, scalar1=s1, scalar2=None, op0=op0), r=r, w=w)
        else:
            self.S.add(eng, lambda e: e.tensor_scalar(out=out, in0=in0, scalar1=s1, scalar2=s2, op0=op0, op1=op1), r=r, w=w)

    def tt(self, eng, out, in0, in1, op, r, w):
        self.S.add(eng, lambda e: e.tensor_tensor(out=out, in0=in0, in1=in1, op=op), r=r, w=w)

    def stt(self, out, in0, scalar, in1, op0, op1, r, w):
        self.S.add('dve', lambda e: e.scalar_tensor_tensor(out=out, in0=in0, scalar=scalar, in1=in1, op0=op0, op1=op1), r=r, w=w)

    def cp(self, eng, out, in_, r, w):
        if eng == 'act':
            self.S.add('act', lambda e: e.copy(out=out, in_=in_), r=r, w=w)
        else:
            self.S.add(eng, lambda e: e.tensor_copy(out=out, in_=in_), r=r, w=w)

    def memset(self, eng, ap, val, w):
        self.S.add(eng, lambda e: e.memset(ap, val), w=w)

    def recip(self, out, in_, r, w):
        self.S.add('dve', lambda e: e.reciprocal(out=out, in_=in_), r=r, w=w)

    def aselect(self, out, in_, pattern, op, fill, base, cm, r, w):
        self.S.add('pool', lambda e: e.affine_select(out=out, in_=in_, pattern=pattern, compare_op=op, fill=fill,
                                                     base=base, channel_multiplier=cm), r=r, w=w)


V_GQ, V_GK, V_LCW, V_LCB, V_BA, V_BX, V_LAM = 0, 1, 2, 6, 7, 8, 9
V_DWB, V_LNG, V_LNB, V_PWB, V_DWW = 10, 12, 14, 16, 17
NV = 17 + 62
NW = 1536


def build_mixer(nc, gs, SL, x, wall, vec, g1, wab, pww, yT, post=None, xtile=None):
    NG = SL // 512
    with ExitStack() as st:
        C = Ctx(nc, st, gs)
        S = C.S
        sb, ps = C.sb, C.ps
        Wb = sb("Wb", [128, 8, NW], BF16)
        wst = [sb(f"wst{i}", [128, 384], F32) for i in range(2)]
        vecs = sb("vecs", [128, NV], F32)
        g1s = sb("g1s", [128, 8], F32)
        wabf = sb("wabf", [128, 2, 128], F32)
        wabb = sb("wabb", [128, 2, 128], BF16)
        pwf = sb("pwf", [128, 2, 128], F32)
        pwb = sb("pwb", [128, 2, 128], BF16)
        identb = sb("identb", [128, 128], BF16)
        blk1 = sb("blk1", [128, 128], BF16)
        onesm = sb("onesm", [128, 128], F32)
        uincl = sb("uincl", [128, 128], BF16)
        negones = sb("negones", [128, 128], BF16)
        tri = sb("tri", [128, 128], BF16)
        dg = sb("dg", [128, 62, 128], BF16)
        cl = sb("cl", [128, 4], F32)
        KT = sb("KT", [128, 2, SL], BF16)
        Vt = sb("Vt", [128, SL // 128, 256], BF16)
        QT = sb("QT", [128, 2, 512], BF16)
        xt = [sb(f"xt{i}", [128, 1024], F32) for i in range(2)]
        xn = [sb(f"xn{i}", [128, 1024], BF16) for i in range(2)]
        junk = sb("junk", [128, 1024], BF16)
        st1 = [sb(f"st1_{i}", [128, 4], F32) for i in range(2)]
        hT = sb("hT", [128, 8, 512], BF16)
        sq = sb("sq", [128, 512], BF16)
        rs = sb("rs", [128, 512], F32)
        xrb = sb("xrb", [128, 515], F32)
        xgb = sb("xgb", [128, 512], F32)
        cv = sb("cv", [128, 512], F32)
        cvb = sb("cvb", [128, 512], BF16)
        lr = sb("lr", [128, 512], F32)
        li = sb("li", [128, 512], F32)
        la = sb("la", [128, 512], F32)
        la2 = sb("la2", [128, 512], F32)
        lu = sb("lu", [128, 512], F32)
        lh = sb("lh", [128, 512], F32)
        hprev = sb("hprev", [128, 1], F32)
        gt = sb("gt", [128, 512], F32)
        ybs = sb("ybs", [128, 512], BF16)
        ub = sb("ub", [128, 2, 542], BF16)
        sg = sb("sg", [128, 512], F32)
        cvo = sb("cvo", [128, 2, 512], F32)
        sqo = sb("sqo", [128, 2, 512], F32)
        means = sb("means", [128, 512], F32)
        m2 = sb("m2", [128, 512], F32)
        crs = sb("crs", [128, 512], F32)
        cn = sb("cn", [128, 2, 512], F32)
        csb = sb("csb", [128, 2, 512], BF16)
        ycs = sb("ycs", [128, 512], BF16)
        Eb = [sb(f"Eb{i}", [128, 512], F32) for i in range(2)]
        Lb = [sb(f"Lb{i}", [128, 512], BF16) for i in range(3)]
        NWB = 3
        Wt = [sb(f"Wt{i}", [128, 512], BF16) for i in range(NWB)]
        LaccP = [sb(f"Lacc{i}", [128, 512], F32) for i in range(2)]
        xC = [sb(f"xC{i}", [128, 512], BF16) for i in range(2)]
        Laccb = [sb(f"Laccb{i}", [128, 512], BF16) for i in range(3)]
        yst = [sb(f"yst{i}", [64, 512], BF16) for i in range(2)]
        pA = [ps(f"pA{i}", [128, 512], F32) for i in range(2)]
        pT = ps("pT", [128, 1024], BF16)
        pZ = [ps(f"pZ{i}", [128, 512], F32) for i in range(2)]
        pC = [ps(f"pC{i}", [128, 512], F32) for i in range(2)]
        pO = ps("pO", [128, 512], F32)

        C.dma('sp', vecs[:], vec, [], ['vecs'], 'c0')
        C.dma('sp', g1s[:], g1, [], ['g1s'], 'c1')
        C.dma('sp', wabf[:], wab, [], ['wabf'], 'c2')
        C.dma('sp', pwf[:], pww, [], ['pwf'], 'c3')
        C.cp('dve', wabb[:], wabf[:], ['wabf'], ['wabb'])
        C.cp('dve', pwb[:], pwf[:], ['pwf'], ['pwb'])
        wv = wall.rearrange("(c p) n -> p c n", p=128)
        k = 0
        for c in range(8):
            for hf in range(4):
                b = k % 2
                C.dma('sp', wst[b][:], wv[:, c, hf * 384:(hf + 1) * 384], [], [f'wst{b}'], f'wst{b}')
                C.ts('dve' if k % 2 == 0 else 'pool', Wb[:, c, hf * 384:(hf + 1) * 384], wst[b][:], g1s[:, c:c + 1], None,
                     ALU.mult, None, [f'wst{b}', 'g1s'], [('Wb', c, hf)])
                k += 1
        WbK = [('Wb', c, hf) for c in range(8) for hf in range(4)]
        C.memset('pool', identb[:], 0.0, ['identb'])
        C.aselect(identb[:], identb[:], [[-1, 128]], ALU.not_equal, 1.0, 0, 1, ['identb'], ['identb'])
        C.memset('pool', blk1[:], 0.0, ['blk1'])
        C.memset('pool', blk1[0:64, 0:64], 1.0, ['blk1'])
        C.memset('pool', blk1[64:128, 64:128], 1.0, ['blk1'])
        C.memset('pool', onesm[:], 1.0 / 256.0, ['onesm'])
        C.memset('pool', negones[:], -1.0, ['negones'])
        C.memset('pool', uincl[:], -1.0, ['uincl'])
        C.aselect(uincl[:], uincl[:], [[-1, 128]], ALU.is_ge, 0.0, 0, 1, ['uincl'], ['uincl'])
        C.memset('pool', tri[:], 1.0, ['tri'])
        C.aselect(tri[:], tri[:], [[1, 128]], ALU.is_ge, 0.0, -1, -1, ['tri'], ['tri'])
        for i in range(62):
            C.ts('pool' if i % 2 else 'dve', dg[:, i, :], identb[:], vecs[:, V_DWW + i:V_DWW + i + 1], None, ALU.mult, None,
                 ['identb', 'vecs'], ['dg'])
        C.act(cl[:, 0:1], vecs[:, V_LAM:V_LAM + 1], AF.Exp, ['vecs'], ['cl0'], scale=-1.0)
        C.act(cl[:, 1:2], cl[:, 0:1], AF.Ln, ['cl0'], ['cl1'], bias=1.0)
        C.ts('dve', cl[:, 2:3], cl[:, 1:2], -8.0, None, ALU.mult, None, ['cl1'], ['cl2'])
        C.ts('dve', cl[:, 3:4], cl[:, 1:2], -16.0, None, ALU.mult, None, ['cl1'], ['cl3'])
        C.memset('pool', hprev[:], 0.0, ['hprev'])
        C.memset('pool', xrb[:, 0:3], 0.0, ['xrb_h'])
        C.memset('pool', ub[:, :, 0:30], 0.0, ['ub_h'])

        xv = x.rearrange("(n p) d -> n p d", p=128)
        wcnt = [0]
        ocnt = [0]
        acnt = [0]
        bcnt = [0]
        lcnt = [0]
        pcnt = [0]
        for g in range(NG):
            t0 = g * 512
            for tt in range(4):
                b = tt % 2
                C.dma('sp', xt[b][:], xv[g * 4 + tt] if xtile is None else xtile(g * 4 + tt), [], [f'xt{b}'], f'x{b}')
                C.act(junk[:], xt[b][:], AF.Square, [f'xt{b}'], [f'ss{b}'], accum=st1[b][:, 0:1])
                C.act(st1[b][:, 1:2], st1[b][:, 0:1], AF.Sqrt, [f'ss{b}'], [f'rt{b}'], scale=1.0 / 1024.0, bias=EPS)
                C.recip(st1[b][:, 2:3], st1[b][:, 1:2], [f'rt{b}'], [f'rstd{b}'])
                C.ts('dve', xn[b][:], xt[b][:], st1[b][:, 2:3], None, ALU.mult, None, [f'xt{b}', f'rstd{b}'], [f'xn{b}'])
                for c in range(8):
                    C.tr(pT[:, c * 128:(c + 1) * 128], xn[b][:, c * 128:(c + 1) * 128], identb[:], [f'xn{b}', 'identb'], ['pT'])
                C.cp('act' if tt % 2 else 'dve', hT[:, :, tt * 128:(tt + 1) * 128], pT[:].rearrange("p (c t) -> p c t", c=8),
                     ['pT'], [('hT', tt)])
            hTK = [('hT', tt) for tt in range(4)]
            pidx = [0]

            def proj(ct):
                bank = pidx[0] % 2
                pidx[0] += 1
                for c in range(8):
                    C.mm(pA[bank][:], Wb[:, c, ct * 128:(ct + 1) * 128], hT[:, c, :], c == 0, c == 7, WbK + hTK, [f'pA{bank}'])
                return bank

            for ct in range(4):
                bk = proj(ct)
                C.act(sq[:], pA[bk][:], AF.Square, [f'pA{bk}'], ['sq'])
                C.mm(pA[1 - bk][:], blk1[:], sq[:], True, True, ['blk1', 'sq'], [f'pA{1 - bk}'])
                pidx[0] += 1
                if ct < 2:
                    C.act(rs[:], pA[1 - bk][:], AF.Sqrt, [f'pA{1 - bk}'], ['rs'], scale=1.0, bias=64.0 * EPS)
                else:
                    C.act(rs[:], pA[1 - bk][:], AF.Sqrt, [f'pA{1 - bk}'], ['rs'], scale=1.0 / 64.0, bias=EPS)
                C.recip(rs[:], rs[:], ['rs'], ['rs'])
                if ct < 2:
                    C.stt(QT[:, ct, :], pA[bk][:], vecs[:, V_GQ:V_GQ + 1], rs[:], ALU.mult, ALU.mult,
                          [f'pA{bk}', 'vecs', 'rs'], [('QT', ct)])
                else:
                    C.stt(KT[:, ct - 2, t0:t0 + 512], pA[bk][:], vecs[:, V_GK:V_GK + 1], rs[:], ALU.mult, ALU.mult,
                          [f'pA{bk}', 'vecs', 'rs'], [('KT', ct - 2, g)])
            bk = proj(4)
            C.cp('act', xrb[:, 3:515], pA[bk][:], [f'pA{bk}'], ['xrb'])
            bk = proj(5)
            C.cp('dve', xgb[:], pA[bk][:], [f'pA{bk}'], ['xgb'])
            for t in range(2):
                bv = proj(6 + t)
                bg = proj(8 + t)
                C.act(sg[:], pA[bg][:], AF.Sigmoid, [f'pA{bg}'], ['sg'])
                C.tt('dve', ub[:, t, 30:542], pA[bv][:], sg[:], ALU.mult, [f'pA{bv}', 'sg'], [('ub', t)])
            for tt in range(4):
                bank = pidx[0] % 2
                pidx[0] += 1
                for c in range(8):
                    C.mm(pA[bank][:, 0:256], hT[:, c, tt * 128:(tt + 1) * 128], Wb[:, c, 1280:1536], c == 0, c == 7,
                         WbK + hTK, [f'pA{bank}'])
                C.cp('act' if tt % 2 else 'dve', Vt[:, g * 4 + tt, :], pA[bank][:, 0:256], [f'pA{bank}'], [('V', g * 4 + tt)])

            vc = lambda i: vecs[:, i:i + 1]
            C.ts('dve', cv[:], xrb[:, 3:515], vc(V_LCW + 3), vc(V_LCB), ALU.mult, ALU.add, ['xrb', 'xrb_h', 'vecs'], ['cv'])
            for kk in range(3):
                C.stt(cv[:], xrb[:, kk:kk + 512], vc(V_LCW + kk), cv[:], ALU.mult, ALU.add, ['xrb', 'xrb_h', 'vecs', 'cv'], ['cv'])
            C.cp('pool', xrb[:, 0:3], xrb[:, 512:515], ['xrb'], ['xrb_h'])
            C.cp('pool', cvb[:], cv[:], ['cv'], ['cvb'])
            b0 = pidx[0] % 2
            pidx[0] += 2
            C.mm(pA[b0][:], wabb[:, 0, :], cvb[:], True, True, ['wabb', 'cvb'], [f'pA{b0}'])
            C.mm(pA[1 - b0][:], wabb[:, 1, :], cvb[:], True, True, ['wabb', 'cvb'], [f'pA{1 - b0}'])
            C.act(lr[:], pA[b0][:], AF.Sigmoid, [f'pA{b0}', 'vecs'], ['lr'], bias=vc(V_BA))
            C.act(li[:], pA[1 - b0][:], AF.Sigmoid, [f'pA{1 - b0}', 'vecs'], ['li'], bias=vc(V_BX))
            C.act(la[:], lr[:], AF.Exp, ['lr', 'cl2'], ['la'], scale=cl[:, 2:3])
            C.act(la2[:], lr[:], AF.Exp, ['lr', 'cl3'], ['la2'], scale=cl[:, 3:4])
            C.act(la2[:], la2[:], AF.Sqrt, ['la2'], ['la2'], scale=-1.0, bias=1.0)
            C.tt('pool', lu[:], li[:], cv[:], ALU.mult, ['li', 'cv'], ['lu'])
            C.tt('pool', lu[:], lu[:], la2[:], ALU.mult, ['lu', 'la2'], ['lu'])
            S.add('dve', lambda e: e.tensor_tensor_scan(out=lh[:], data0=la[:], data1=lu[:], initial=hprev[:, 0:1],
                                                        op0=ALU.mult, op1=ALU.add), r=['la', 'lu', 'hprev'], w=['lh'])
            C.cp('pool', hprev[:], lh[:, 511:512], ['lh'], ['hprev'])
            C.tt('pool', gt[:], xgb[:], xgb[:], ALU.mult, ['xgb'], ['gt'])
            C.ts('pool', gt[:], gt[:], 0.044715, 1.0, ALU.mult, ALU.add, ['gt'], ['gt'])
            C.tt('pool', gt[:], gt[:], xgb[:], ALU.mult, ['gt', 'xgb'], ['gt'])
            C.act(gt[:], gt[:], AF.Sigmoid, ['gt'], ['gt'], scale=1.5957691216)
            C.tt('pool', gt[:], gt[:], xgb[:], ALU.mult, ['gt', 'xgb'], ['gt'])
            C.tt('dve', ybs[:], gt[:], lh[:], ALU.mult, ['gt', 'lh'], ['ybs'])
            C.dma('pool', yT[256:384, t0:t0 + 512], ybs[:], ['ybs'], [], 'yb', cw=['yT'])

            for t in range(2):
                bank = pidx[0] % 2
                pidx[0] += 1
                for kk in range(31):
                    C.mm(pA[bank][:], dg[:, t * 31 + kk, :], ub[:, t, kk:kk + 512], kk == 0, kk == 30,
                         ['dg', ('ub', t), 'ub_h'], [f'pA{bank}'])
                C.act(cvo[:, t, :], pA[bank][:], AF.Identity, [f'pA{bank}', 'vecs'], [('cvo', t)], bias=vc(V_DWB + t))
                C.act(sqo[:, t, :], pA[bank][:], AF.Square, [f'pA{bank}', 'vecs'], [('sqo', t)], bias=vc(V_DWB + t))
            C.cp('pool', ub[:, :, 0:30], ub[:, :, 512:542], [('ub', 0), ('ub', 1)], ['ub_h'])
            bm = pidx[0] % 2
            pidx[0] += 2
            for t in range(2):
                C.mm(pA[bm][:], onesm[:], cvo[:, t, :], t == 0, t == 1, ['onesm', ('cvo', t)], [f'pA{bm}'])
            for t in range(2):
                C.mm(pA[1 - bm][:], onesm[:], sqo[:, t, :], t == 0, t == 1, ['onesm', ('sqo', t)], [f'pA{1 - bm}'])
            C.cp('act', means[:], pA[bm][:], [f'pA{bm}'], ['means'])
            C.tt('pool', m2[:], means[:], means[:], ALU.mult, ['means'], ['m2'])
            C.tt('dve', m2[:], pA[1 - bm][:], m2[:], ALU.subtract, [f'pA{1 - bm}', 'm2'], ['m2'])
            C.act(crs[:], m2[:], AF.Sqrt, ['m2'], ['crs'], scale=1.0, bias=EPS)
            C.recip(crs[:], crs[:], ['crs'], ['crs'])
            for t in range(2):
                C.tt('pool', cn[:, t, :], cvo[:, t, :], means[:], ALU.subtract, [('cvo', t), 'means'], [('cn', t)])
                C.tt('dve' if t else 'pool', cn[:, t, :], cn[:, t, :], crs[:], ALU.mult, [('cn', t), 'crs'], [('cn', t)])
                C.act(csb[:, t, :], cn[:, t, :], AF.Silu, [('cn', t), 'vecs'], [('csb', t)], scale=vc(V_LNG + t), bias=vc(V_LNB + t))
            bank = pidx[0] % 2
            pidx[0] += 1
            for t in range(2):
                C.mm(pA[bank][:], pwb[:, t, :], csb[:, t, :], t == 0, t == 1, ['pwb', ('csb', t)], [f'pA{bank}'])
            C.act(ycs[:], pA[bank][:], AF.Identity, [f'pA{bank}', 'vecs'], ['ycs'], bias=vc(V_PWB))
            C.dma('pool', yT[384:512, t0:t0 + 512], ycs[:], ['ycs'], [], 'yc', cw=['yT'])

            items = []
            for hd in range(4):
                nblk = 4 * g + 4
                for bi, kb in enumerate(range(4 * g + 3, -1, -1)):
                    items.append(dict(hd=hd, bi=bi, kb=kb, nblk=nblk))

            def stageA(it):
                hd, bi, kb = it['hd'], it['bi'], it['kb']
                ct = hd // 2
                pb = 64 * (hd % 2)
                Qh = QT[pb:pb + 64, ct, :]
                j = kb - 4 * g
                c0 = 128 * j if j > 0 else 0
                Kh = KT[pb:pb + 64, ct, kb * 128:(kb + 1) * 128]
                kkey = ('KT', ct, kb // 4)
                zb = acnt[0] % 2
                lb = acnt[0] % 3
                acnt[0] += 1
                it.update(c0=c0, j=j, Kh=Kh, kkey=kkey, Qh=Qh, ct=ct, lb=lb, zb=zb)
                if bi == 0:
                    C.memset('pool', LaccP[0][:], 0.0, ['Lacc0'])
                    C.memset('pool', LaccP[1][:], 0.0, ['Lacc1'])
                    pcnt[0] = 0
                C.mm(pZ[zb][:, c0:], Kh, Qh[:, c0:], True, True, [kkey, ('QT', ct)], [f'pZ{zb}'])
                C.act(Eb[zb][:, c0:], pZ[zb][:, c0:], AF.Exp, [f'pZ{zb}'], [f'Eb{zb}'])
                C.act(Lb[lb][:, c0:], Eb[zb][:, c0:], AF.Ln, [f'Eb{zb}'], [f'Lb{lb}'], bias=1.0)
                if j >= 0:
                    C.tt('dve', Lb[lb][:, c0:c0 + 128], Lb[lb][:, c0:c0 + 128], tri[:], ALU.mult, [f'Lb{lb}', 'tri'], [f'Lb{lb}'])
                if kb > 0:
                    la_ = lcnt[0] % 3
                    lcnt[0] += 1
                    pp = pcnt[0] % 2
                    pcnt[0] += 1
                    Lo, Ln_ = LaccP[pp], LaccP[1 - pp]
                    ko, kn = f'Lacc{pp}', f'Lacc{1 - pp}'
                    if c0 > 0:
                        C.cp('dve', Laccb[la_][:, 0:c0], Lo[:, 0:c0], [ko], [f'Laccb{la_}'])
                        C.tt('dve', Laccb[la_][:, c0:], Lo[:, c0:], Lb[lb][:, c0:], ALU.add, [ko, f'Lb{lb}', f'Laccb{la_}'], [f'Laccb{la_}'])
                    else:
                        C.tt('dve', Laccb[la_][:], Lo[:], Lb[lb][:], ALU.add, [ko, f'Lb{lb}'], [f'Laccb{la_}'])
                    C.tt('pool', Ln_[:, c0:], Lo[:, c0:], Lb[lb][:, c0:], ALU.add, [ko, f'Lb{lb}'], [kn])
                    it['la_out'] = la_

            def stageB(it, prev):
                hd, bi, kb, c0, j = it['hd'], it['bi'], it['kb'], it['c0'], it['j']
                Kh, kkey, Qh, ct, lb = it['Kh'], it['kkey'], it['Qh'], it['ct'], it['lb']
                cb = bcnt[0] % 2
                bcnt[0] += 1
                C.mm(pC[cb][:, c0:], uincl[:], Lb[lb][:, c0:], True, bi == 0, ['uincl', f'Lb{lb}'], [f'pC{cb}'])
                if bi > 0:
                    la_ = prev['la_out']
                    C.mm(pC[cb][:, c0:], negones[:], Laccb[la_][:, c0:], False, True, ['negones', f'Laccb{la_}'], [f'pC{cb}'])
                wi = wcnt[0] % NWB
                wcnt[0] += 1
                zb = it['zb']
                C.act(xC[cb][:, c0:], pC[cb][:, c0:], AF.Exp, [f'pC{cb}'], [f'xC{cb}'])
                C.tt('dve', Wt[wi][:, c0:], xC[cb][:, c0:], Eb[zb][:, c0:], ALU.mult, [f'xC{cb}', f'Eb{zb}'], [f'Wt{wi}'])
                if j >= 0:
                    C.tt('dve', Wt[wi][:, c0:c0 + 128], Wt[wi][:, c0:c0 + 128], tri[:], ALU.mult, [f'Wt{wi}', 'tri'], [f'Wt{wi}'])
                C.mm(pO[0:64, c0:], Vt[:, kb, hd * 64:(hd + 1) * 64], Wt[wi][:, c0:], bi == 0, bi == it['nblk'] - 1,
                     [('V', kb), f'Wt{wi}'], ['pO'])
                if bi == it['nblk'] - 1:
                    ob = ocnt[0] % 2
                    ocnt[0] += 1
                    C.cp('dve', yst[ob][:], pO[0:64, :], ['pO'], [f'yst{ob}'])
                    C.dma('pool', yT[hd * 64:(hd + 1) * 64, t0:t0 + 512], yst[ob][:], [f'yst{ob}'], [], f'ya{ob}', cw=['yT'])

            stageA(items[0])
            for n_ in range(len(items)):
                if n_ + 1 < len(items):
                    stageA(items[n_ + 1])
                stageB(items[n_], items[n_ - 1] if n_ > 0 else None)
        if post is not None:
            post(C)
        info = S.emit()
    return info


def prep_mixer_inputs(inp, l, b, hg, SL=None):
    f = np.float32
    w_in = np.asarray(inp['w_in'][l])
    hs = slice(hg * 256, (hg + 1) * 256)
    q = w_in[:, 0:512][:, hs]
    k = w_in[:, 512:1024][:, hs]
    v = w_in[:, 1024:1536][:, hs]
    cs = slice(hg * 128, (hg + 1) * 128)
    xr = w_in[:, 1536:1792][:, cs]
    xg = w_in[:, 1792:2048][:, cs]
    cval = w_in[:, 2048:2304]
    cgate = w_in[:, 2304:2560]
    wall = np.ascontiguousarray(np.concatenate([q, k, xr, xg, cval, cgate, v], axis=1), dtype=f)
    vec = np.zeros((128, NV), f)
    vec[:, V_GQ] = np.tile(inp['q_norm_g'][l], 2)
    vec[:, V_GK] = np.tile(inp['k_norm_g'][l], 2)
    for kk in range(4):
        vec[:, V_LCW + kk] = inp['lru_conv_w'][l][kk, cs]
    vec[:, V_LCB] = inp['lru_conv_b'][l][cs]
    vec[:, V_BA] = inp['lru_ba'][l][cs]
    vec[:, V_BX] = inp['lru_bx'][l][cs]
    vec[:, V_LAM] = inp['lru_lambda'][l][cs]
    for t in range(2):
        ts_ = slice(t * 128, (t + 1) * 128)
        vec[:, V_DWB + t] = inp['conf_dw_b'][l][ts_]
        vec[:, V_LNG + t] = inp['conf_ln_g'][l][ts_]
        vec[:, V_LNB + t] = inp['conf_ln_b'][l][ts_]
        for kk in range(31):
            vec[:, V_DWW + t * 31 + kk] = inp['conf_dw_w'][l][kk, ts_]
    vec[:, V_PWB] = inp['conf_pw_b'][l][cs]
    g1 = np.ascontiguousarray(np.asarray(inp['norm1_g'][l]).reshape(8, 128).T, dtype=f)
    wab = np.zeros((128, 2, 128), f)
    for i in range(2):
        blk = hg * 2 + i
        wab[i * 64:(i + 1) * 64, 0, i * 64:(i + 1) * 64] = inp['lru_wa'][l][blk]
        wab[i * 64:(i + 1) * 64, 1, i * 64:(i + 1) * 64] = inp['lru_wx'][l][blk]
    pw = np.asarray(inp['conf_pw_w'][l])[:, cs]
    pww = np.ascontiguousarray(pw.reshape(2, 128, 128).transpose(1, 0, 2), dtype=f)
    return dict(wall=wall, vec=vec, g1=g1, wab=wab, pww=pww)


CAP = 640
NSLOT = 32 * CAP
NROWS = NSLOT + 128
TRASH = float(NSLOT)


def build_ffn(nc, gs, NT, xin, yG, msk, wout, gout, g2bd, wr, wg, wu, wd, xout, Xs, Ys, xmid, post=None):
    NTT = NT // 128
    stage = 4
    dbg = False
    with ExitStack() as st:
        C = Ctx(nc, st, gs)
        S = C.S
        sb, ps = C.sb, C.ps
        mks = sb("mks", [128, 2], F32)
        ysc = [[sb(f"ysc{i}_{k}", [128, 8, 128], BF16) for k in range(2)] for i in range(2)]
        Woutb = sb("Woutb", [128, 8, 1024], BF16)
        wstg = [sb(f"wstg{i}", [128, 1024], F32) for i in range(2)]
        gos = sb("gos", [128, 8], F32)
        g2b = sb("g2b", [128, 1024], F32)
        wrs = sb("wrs", [128, 8, 36], F32)
        identf = sb("identf", [128, 128], F32)
        identb = sb("identb", [128, 128], BF16)
        onec = sb("onec", [128, 2], BF16)
        ustr = sb("ustr", [128, 128], BF16)
        ones128 = sb("ones128", [128, 128], BF16)
        basef = sb("basef", [128, 32], F32)
        zt = sb("zt", [128, 4, 1024], BF16)
        gates = sb("gates", [128, NTT, 2], F32)
        dsti = sb("dsti", [128, NTT * 2], I32)
        Macc = sb("Macc", [128, 32], F32)
        Maccb = sb("Maccb", [128, 32], BF16)
        xt = [sb(f"xt{i}", [128, 1024], F32) for i in range(2)]
        ys = [sb(f"ys{i}", [128, 8, 128], BF16) for i in range(2)]
        ysq = sb("ysq", [128, 8, 128], BF16)
        x1 = [sb(f"x1_{i}", [128, 1024], F32) for i in range(2)]
        junk = sb("junk", [128, 1024], BF16)
        h2f = sb("h2f", [128, 1024], F32)
        h2b = [sb(f"h2b{i}", [128, 1024], BF16) for i in range(2)]
        h2T = sb("h2T", [128, 8, 128], F32)
        sm = [sb(f"sm{i}", [128, 16], F32) for i in range(2)]
        lg = sb("lg", [128, 36], F32)
        r1 = sb("r1", [128, 224], F32)
        Mb = sb("Mb", [128, 32], BF16)
        dstf = sb("dstf", [128, 2], F32)
        wgb = [sb(f"wgb{i}", [128, 8, 512], BF16) for i in range(2)]
        wub = [sb(f"wub{i}", [128, 8, 512], BF16) for i in range(2)]
        wdb = [sb(f"wdb{i}", [128, 4, 1024], BF16) for i in range(2)]
        NST = CAP // 128
        xe = [sb(f"xe{i}", [128, NST, 1024], BF16) for i in range(2)]
        xeT = sb("xeT", [128, 8, CAP], BF16)
        sgl = [sb(f"sgl{i}", [128, CAP], F32) for i in range(2)]
        aT = sb("aT", [128, 4, CAP], BF16)
        yo = [sb(f"yo{i}", [128, NST, 1024], BF16) for i in range(2)]
        yg = [[sb(f"yg{i}_{k}", [128, 1024], BF16) for k in range(2)] for i in range(2)]
        pM = ps("pM", [128, 512], F32)
        pP = [ps(f"pP{i}", [128, 512], F32) for i in range(2)]
        pTf = ps("pTf", [128, 1024], F32)
        pUU = ps("pUU", [128, 1024], F32)
        pTb = ps("pTb", [128, 1024], BF16)

        C.dma('sp', gos[:], gout, [], ['gos'], 'c0')
        C.dma('sp', mks[:], msk, [], ['mks'], 'c3')
        C.dma('sp', g2b[:], g2bd, [], ['g2b'], 'c1')
        C.dma('sp', wrs[:], wr.rearrange("(c p) n -> p c n", p=128), [], ['wrs'], 'c2')
        wov = wout.rearrange("(c p) n -> p c n", p=128)
        for c in range(8):
            b = c % 2
            C.dma('sp', wstg[b][:], wov[:, c, :], [], [f'wstg{b}'], f'wstg{b}')
            C.ts('dve' if b else 'pool', Woutb[:, c, :], wstg[b][:], gos[:, c:c + 1], None, ALU.mult, None,
                 [f'wstg{b}', 'gos'], [('Wo', c)])
        WoK = [('Wo', c) for c in range(8)]
        C.memset('pool', identf[:], 0.0, ['identf'])
        C.aselect(identf[:], identf[:], [[-1, 128]], ALU.not_equal, 1.0, 0, 1, ['identf'], ['identf'])
        C.cp('pool', identb[:], identf[:], ['identf'], ['identb'])
        C.memset('pool', onec[:], 1.0, ['onec'])
        C.memset('pool', ones128[:], 1.0, ['ones128'])
        C.memset('pool', ustr[:], 1.0, ['ustr'])
        C.aselect(ustr[:], ustr[:], [[1, 128]], ALU.is_ge, 0.0, -1, -1, ['ustr'], ['ustr'])
        S.add('pool', lambda e: e.iota(basef[:], pattern=[[CAP, 32]], base=0, channel_multiplier=0,
                                       allow_small_or_imprecise_dtypes=True), w=['basef'])
        C.memset('pool', zt[:], 0.0, ['zt'])
        C.memset('pool', Macc[:], 0.0, ['Macc'])
        C.memset('pool', Maccb[:], 0.0, ['Maccb'])
        Xv = Xs.rearrange("(n p) d -> p n d", p=128)
        nrt = NROWS // 128
        zi = 0
        for n0 in range(0, nrt, 4):
            n1 = min(nrt, n0 + 4)
            C.dma('sp', Xv[:, n0:n1, :], zt[:, 0:n1 - n0, :], ['zt'], [], f'z{zi % 2}', cw=['Xs'])
            zi += 1
        C.dma('sp', Ys[NSLOT:NROWS, :], zt[:, 0, :], ['zt'], [], 'zy', cw=['Ys'])
        S.add('sp', lambda e: e.nop(), r=[], w=['Xs'])

        xinv = xin.rearrange("(n p) d -> n p d", p=128)
        xmv = xmid.rearrange("(n p) d -> n p d", p=128)
        xov = xout.rearrange("(n p) d -> n p d", p=128)
        yv = yG.rearrange("(c p) t -> p c t", p=128)

        def loads(i):
            b = i % 2
            C.dma('sp', xt[b][:], xinv[i], [], [f'xt{b}'], f'lx{b}')
            for hh in range(2):
                C.dma('sp', ysc[b][hh][:], yv[:, :, hh * NT + i * 128:hh * NT + (i + 1) * 128], [], [f'ysc{b}{hh}'], f'ly{b}{hh}')
            C.ts('dve', ys[b][:], ysc[b][0][:], mks[:, 0:1], None, ALU.mult, None, [f'ysc{b}0', 'mks'], [f'ys{b}'])
            C.stt(ys[b][:], ysc[b][1][:], mks[:, 1:2], ys[b][:], ALU.mult, ALU.add, [f'ysc{b}1', 'mks', f'ys{b}'], [f'ys{b}'])

        loads(0)
        GRP = [([0, 1, 2, 3], 512.0), ([4, 5], 256.0), ([6, 7], 256.0)]
        for i in range(NTT):
            b = i % 2
            if i + 1 < NTT:
                loads(i + 1)
            s = sm[b]
            C.act(ysq[:], ys[b][:], AF.Square, [f'ys{b}'], ['ysq'])
            for gi, (cl_, n) in enumerate(GRP):
                for c in cl_:
                    C.mm(pM[:, gi:gi + 1], ysq[:, c, :], onec[:, 0:1], c == cl_[0], c == cl_[-1], ['ysq', 'onec'], ['pM'])
            C.act(s[:, 0:1], pM[:, 0:1], AF.Sqrt, ['pM'], [f's0{b}'], scale=1.0 / 512.0, bias=EPS)
            C.act(s[:, 1:3], pM[:, 1:3], AF.Sqrt, ['pM'], [f's1{b}'], scale=1.0 / 256.0, bias=EPS)
            C.recip(s[:, 3:6], s[:, 0:3], [f's0{b}', f's1{b}'], [f'rg{b}'])
            k = 0
            for half in range(2):
                for gi, (cl_, n) in enumerate(GRP):
                    bk = k % 2
                    k += 1
                    for c in cl_:
                        C.mm(pP[bk][:], ys[b][:, c, :], Woutb[:, c, half * 512:(half + 1) * 512], c == cl_[0], c == cl_[-1],
                             [f'ys{b}'] + WoK, [f'pP{bk}'])
                    src = xt[b] if gi == 0 else x1[b]
                    C.stt(x1[b][:, half * 512:(half + 1) * 512], pP[bk][:], s[:, 3 + gi:4 + gi], src[:, half * 512:(half + 1) * 512],
                          ALU.mult, ALU.add, [f'pP{bk}', f'rg{b}', f'xt{b}', ('x1', b, half)], [('x1', b, half)])
            x1k = [('x1', b, 0), ('x1', b, 1)]
            C.dma('sp', xmv[i], x1[b][:], x1k, [], f'sx{b}', cw=['xmid'])
            C.act(junk[:], x1[b][:], AF.Square, x1k, [f'ss{b}'], accum=s[:, 6:7])
            C.act(s[:, 7:8], s[:, 6:7], AF.Sqrt, [f'ss{b}'], [f'rt{b}'], scale=1.0 / 1024.0, bias=EPS)
            C.recip(s[:, 8:9], s[:, 7:8], [f'rt{b}'], [f'r2{b}'])
            C.stt(h2f[:], x1[b][:], s[:, 8:9], g2b[:], ALU.mult, ALU.mult, x1k + [f'r2{b}', 'g2b'], ['h2f'])
            C.cp('act', h2b[b][:], h2f[:], ['h2f'], [f'h2b{b}'])
            for c in range(8):
                C.tr(pTf[:, c * 128:(c + 1) * 128], h2f[:, c * 128:(c + 1) * 128], identf[:], ['h2f', 'identf'], ['pTf'])
            C.cp('dve', h2T[:].rearrange("p c t -> p (c t)"), pTf[:], ['pTf'], ['h2T'])
            for c in range(8):
                C.mm(pM[:, 8:44], h2T[:, c, :], wrs[:, c, :], c == 0, c == 7, ['h2T', 'wrs'], ['pM'])
            C.cp('act', lg[:], pM[:, 8:44], ['pM'], ['lg'])
            R = 'r1'
            S.add('dve', lambda e, s=s: e.tensor_reduce(out=s[:, 9:10], in_=lg[:, 0:4], axis=AX.X, op=ALU.max), r=['lg'], w=[f'mx{b}'])
            C.ts('dve', r1[:, 0:4], lg[:, 0:4], s[:, 9:10], None, ALU.is_equal, None, ['lg', f'mx{b}'], ['ohg'])
            C.ts('dve', s[:, 10:11], s[:, 9:10], -1.0, None, ALU.mult, None, [f'mx{b}'], [f'nmx{b}'])
            C.act(r1[:, 4:8], lg[:, 0:4], AF.Exp, ['lg', f'nmx{b}'], ['ec', f'sc{b}'], bias=s[:, 10:11], accum=s[:, 11:12])
            C.recip(s[:, 12:13], s[:, 11:12], [f'sc{b}'], [f'wgrp{b}'])
            C.ts('dve', r1[:, 8:12], r1[:, 0:4], -1.0, 1e30, ALU.add, ALU.mult, ['ohg'], ['pen'])
            for gq in range(4):
                C.ts('dve', r1[:, 16 + gq * 8:24 + gq * 8], lg[:, 4 + gq * 8:12 + gq * 8], r1[:, 8 + gq:9 + gq], None, ALU.add, None,
                     ['lg', 'pen'], [('msk', gq)])
            mk = [('msk', gq) for gq in range(4)]
            S.add('dve', lambda e: e.max(out=r1[:, 48:56], in_=r1[:, 16:48]), r=mk, w=['top8'])
            C.ts('dve', r1[:, 56:88], r1[:, 16:48], r1[:, 48:49], None, ALU.is_equal, None, mk + ['top8'], ['oh1'])
            C.ts('dve', r1[:, 88:120], r1[:, 16:48], r1[:, 49:50], None, ALU.is_equal, None, mk + ['top8'], ['oh2'])
            C.tt('dve', s[:, 13:14], r1[:, 49:50], r1[:, 48:49], ALU.subtract, ['top8'], [f'dd{b}'])
            C.act(s[:, 14:15], s[:, 13:14], AF.Exp, [f'dd{b}'], [f'ee{b}'])
            C.ts('dve', s[:, 15:16], s[:, 14:15], 1.0, None, ALU.add, None, [f'ee{b}'], [f'den{b}'])
            C.recip(s[:, 15:16], s[:, 15:16], [f'den{b}'], [f'den{b}'])
            C.tt('dve', gates[:, i, 0:1], s[:, 15:16], s[:, 12:13], ALU.mult, [f'den{b}', f'wgrp{b}'], [('g1', i)])
            C.tt('dve', gates[:, i, 1:2], gates[:, i, 0:1], s[:, 14:15], ALU.mult, [('g1', i), f'ee{b}'], [('g2', i)])
            C.tt('dve', r1[:, 120:152], r1[:, 56:88], r1[:, 88:120], ALU.add, ['oh1', 'oh2'], ['Mf'])
            C.cp('dve', Mb[:], r1[:, 120:152], ['Mf'], ['Mb'])
            C.mm(pM[:, 64:96], ustr[:], Mb[:], True, i == 0, ['ustr', 'Mb'], ['pM'])
            if i > 0:
                C.mm(pM[:, 64:96], ones128[:], Maccb[:], False, True, ['ones128', 'Maccb'], ['pM'])
            C.tt('pool', Macc[:], Macc[:], r1[:, 120:152], ALU.add, ['Macc', 'Mf'], ['Macc'])
            C.cp('pool', Maccb[:], Macc[:], ['Macc'], ['Maccb'])
            SLT = r1[:, 152:184]
            OKM = r1[:, 184:216]
            C.tt('dve', SLT, pM[:, 64:96], basef[:], ALU.add, ['pM', 'basef'], ['slot'])
            C.ts('dve', OKM, pM[:, 64:96], float(CAP), None, ALU.is_lt, None, ['pM'], ['okm'])
            C.ts('dve', SLT, SLT, -TRASH, None, ALU.add, None, ['slot'], ['slot'])
            C.tt('dve', SLT, SLT, OKM, ALU.mult, ['slot', 'okm'], ['slot'])
            C.ts('dve', SLT, SLT, TRASH, None, ALU.add, None, ['slot'], ['slot'])
            C.tt('dve', r1[:, 56:88], r1[:, 56:88], SLT, ALU.mult, ['oh1', 'slot'], ['oh1'])
            C.tt('dve', r1[:, 88:120], r1[:, 88:120], SLT, ALU.mult, ['oh2', 'slot'], ['oh2'])
            S.add('dve', lambda e: e.reduce_sum(out=dstf[:, 0:1], in_=r1[:, 56:88], axis=AX.X), r=['oh1'], w=['dstf0'])
            S.add('dve', lambda e: e.reduce_sum(out=dstf[:, 1:2], in_=r1[:, 88:120], axis=AX.X), r=['oh2'], w=['dstf1'])
            C.cp('dve', dsti[:, 2 * i:2 * i + 2], dstf[:], ['dstf0', 'dstf1'], [('dsti', i)])
            for k2 in range(2 if stage >= 2 else 0):
                S.add('pool', lambda e, i=i, k2=k2, b=b: e.indirect_dma_start(
                    out=Xs[:, :], out_offset=bass.IndirectOffsetOnAxis(ap=dsti[:, 2 * i + k2:2 * i + k2 + 1], axis=0),
                    in_=h2b[b][:], in_offset=None, oob_is_err=False),
                    r=[f'h2b{b}', ('dsti', i)], cw=['Xs'], tag=f'sc{b}{k2}')

        S.add('dve', lambda e: e.memset(junk[:, 0:8], 0.0), r=['pTf'], w=['pTfg', 'pTfu'])
        def wloads(e_):
            b = e_ % 2
            C.dma('pool', wgb[b][:], wg[e_].rearrange("(c p) f -> p c f", p=128), [], [f'wgb{b}'], f'wg{b}')
            C.dma('pool', wub[b][:], wu[e_].rearrange("(c p) f -> p c f", p=128), [], [f'wub{b}'], f'wu{b}')
            C.dma('pool', wdb[b][:], wd[e_].rearrange("(c p) f -> p c f", p=128), [], [f'wdb{b}'], f'wd{b}')

        def xloads(e_):
            b = e_ % 2
            C.dma('sp', xe[b][:], Xs[e_ * CAP:(e_ + 1) * CAP, :].rearrange("(t p) d -> p t d", p=128), ['Xs'], [f'xe{b}'], f'xe{b}')

        if stage >= 3:
            wloads(0)
            xloads(0)
        dk = 0
        for e_ in range(32 if stage >= 3 else 0):
            b = e_ % 2
            if e_ + 1 < 32:
                wloads(e_ + 1)
                xloads(e_ + 1)
            for stt_ in range(NST):
                for c in range(8):
                    C.tr(pTb[:, c * 128:(c + 1) * 128], xe[b][:, stt_, c * 128:(c + 1) * 128], identb[:], [f'xe{b}', 'identb'], ['pTb'])
                C.cp('act' if stt_ % 2 else 'dve', xeT[:, :, stt_ * 128:(stt_ + 1) * 128], pTb[:].rearrange("p (c t) -> p c t", c=8),
                     ['pTb'], [('xeT', stt_)])
            xk = [('xeT', t_) for t_ in range(NST)]
            for fc in range(4):
                for (a0, a1) in ((0, 512), (512, CAP)):
                    for c in range(8):
                        C.mm(pTf[:, a0:a1], wgb[b][:, c, fc * 128:(fc + 1) * 128], xeT[:, c, a0:a1], c == 0, c == 7, [f'wgb{b}'] + xk, ['pTfg'])
                for (a0, a1) in ((0, 512), (512, CAP)):
                    for c in range(8):
                        C.mm(pUU[:, a0:a1], wub[b][:, c, fc * 128:(fc + 1) * 128], xeT[:, c, a0:a1], c == 0, c == 7, [f'wub{b}'] + xk, ['pTfu'])
                C.act(sgl[fc % 2][:], pTf[:, 0:CAP], AF.Silu, ['pTfg'], [f'sgl{fc % 2}'])
                C.tt('dve', aT[:, fc, :], pUU[:, 0:CAP], sgl[fc % 2][:], ALU.mult, ['pTfu', f'sgl{fc % 2}'], [('aT', fc)])
            ak = [('aT', fc) for fc in range(4)]
            for stt_ in range(NST):
                for half in range(2):
                    bk = dk % 2
                    dk += 1
                    for fc in range(4):
                        C.mm(pP[bk][:], aT[:, fc, stt_ * 128:(stt_ + 1) * 128], wdb[b][:, fc, half * 512:(half + 1) * 512],
                             fc == 0, fc == 3, ak + [f'wdb{b}'], [f'pP{bk}'])
                    C.cp('act' if dk % 2 else 'dve', yo[b][:, stt_, half * 512:(half + 1) * 512], pP[bk][:], [f'pP{bk}'],
                         [('yo', b, stt_, half)])
            yk = [('yo', b, t_, h_) for t_ in range(NST) for h_ in range(2)]
            C.dma('sp', Ys[e_ * CAP:(e_ + 1) * CAP, :].rearrange("(t p) d -> p t d", p=128), yo[b][:], yk, [], f'yo{b}', cw=['Ys'])

        def gloads(i):
            b = i % 2
            C.dma('sp', xt[b][:], xmv[i], ['xmid'], [f'xt{b}'], f'lx{b}')
            for k2 in range(2 if stage >= 4 else 0):
                S.add('pool', lambda e, i=i, k2=k2, b=b: e.indirect_dma_start(
                    out=yg[b][k2][:], out_offset=None, in_=Ys[:, :],
                    in_offset=bass.IndirectOffsetOnAxis(ap=dsti[:, 2 * i + k2:2 * i + k2 + 1], axis=0),
                    oob_is_err=False),
                    r=['Ys', ('dsti', i)], w=[f'yg{b}{k2}'], tag=f'gy{b}{k2}')

        gloads(0)
        for i in range(NTT):
            b = i % 2
            if i + 1 < NTT:
                gloads(i + 1)
            if stage < 4:
                C.dma('sp', xov[i], xt[b][:], [f'xt{b}'], [], f'so{b}')
                continue
            C.stt(x1[b][:], yg[b][0][:], gates[:, i, 0:1], xt[b][:], ALU.mult, ALU.add,
                  [f'yg{b}0', ('g1', i), f'xt{b}'], [('x1', b, 0), ('x1', b, 1)])
            C.stt(x1[b][:], yg[b][1][:], gates[:, i, 1:2], x1[b][:], ALU.mult, ALU.add,
                  [f'yg{b}1', ('g2', i), ('x1', b, 0), ('x1', b, 1)], [('x1', b, 0), ('x1', b, 1)])
            C.dma('sp', xov[i], x1[b][:], [('x1', b, 0), ('x1', b, 1)], [], f'so{b}', cw=['xout'])
        if post is not None:
            post(C)
        info = S.emit()
    return info


STD_OF_YG = [0, 2, 1, 3, 4, 5, 6, 7]


def prep_ffn_inputs(inp, l):
    f = np.float32
    wr = np.ascontiguousarray(np.concatenate([inp['router_coarse'][l], inp['router_fine'][l]], axis=1), dtype=f)
    wout = np.asarray(inp['w_out'][l], dtype=f).reshape(8, 128, 1024)[STD_OF_YG].reshape(1024, 1024)
    gout = np.asarray(inp['out_norm_g'][l], dtype=f).reshape(8, 128)[STD_OF_YG].T
    return dict(
        wout=np.ascontiguousarray(wout),
        gout=np.ascontiguousarray(gout),
        g2bd=np.ascontiguousarray(np.broadcast_to(np.asarray(inp['norm2_g'][l])[None, :], (128, 1024)), dtype=f),
        wr=wr,
        wg=np.ascontiguousarray(inp['exp_w_gate'][l], dtype=f),
        wu=np.ascontiguousarray(inp['exp_w_up'][l], dtype=f),
        wd=np.ascontiguousarray(inp['exp_w_down'][l], dtype=f),
    )


MIX_KEYS = dict(wall=[1024, NW], vec=[128, NV], g1=[128, 8], wab=[128, 2, 128], pww=[128, 2, 128])
FFN_KEYS = dict(wout=[1024, 1024], gout=[128, 8], g2bd=[128, 1024], wr=[1024, 36], wg=[32, 1024, 512],
                wu=[32, 1024, 512], wd=[32, 512, 1024])
RG_PAIRS = [[0, 1], [2, 3], [4, 5], [6, 7]]


def build_fused(SL, depth=2):
    NT = SL // 2
    nc = bass.Bass("TRN2", target_bir_lowering=False)

    def din(name, shape, dt=F32):
        return nc.dram_tensor(name, shape, dt, kind="ExternalInput").ap()

    x_full = din("x_full", [SL, 1024])
    xin0 = din("xin0", [NT, 1024])
    msk = din("msk", [128, 2])
    W = []
    for l in range(depth):
        d = {k: din(f"{k}{l}", shp) for k, shp in MIX_KEYS.items()}
        d.update({k: din(f"{k}{l}", shp) for k, shp in FFN_KEYS.items()})
        W.append(d)
    xout = nc.dram_tensor("xout", [NT, 1024], F32, kind="ExternalOutput").ap()
    yT = nc.dram_tensor("yT_i", [512, SL], BF16, kind="Internal").ap()
    yG = nc.dram_tensor("yG_i", [1024, SL], BF16, kind="Internal").ap()
    Xs = nc.dram_tensor("Xs_i", [NROWS, 1024], BF16, kind="Internal").ap()
    Ys = nc.dram_tensor("Ys_i", [NROWS, 1024], BF16, kind="Internal").ap()
    xmid = nc.dram_tensor("xmid_i", [NT, 1024], F32, kind="Internal").ap()
    xo = nc.dram_tensor("xo_i", [NT, 1024], F32, kind="Internal").ap()
    xG = nc.dram_tensor("xG_i", [SL, 1024], F32, kind="Internal").ap()
    with ExitStack() as outer:
        gs = GSync(nc, outer)
        for l in range(depth):
            last = l == depth - 1
            w = W[l]

            def post_m(C):
                for k in range(4):
                    C.S.add('pool', lambda e, k=k: e.collective_compute(
                        "AllGather", ALU.bypass, replica_groups=RG_PAIRS,
                        ins=[yT[k * 128:(k + 1) * 128, :]], outs=[yG[k * 256:(k + 1) * 256, :]]),
                        r=['yT'], cw=['yG'], tag='cc', inc=1)

            def xtile(n):
                p = n * 128
                r_, q = p // NT, p % NT
                row = (q // 512) * 1024 + r_ * 512 + q % 512
                return xG[row:row + 128, :]

            build_mixer(nc, gs, SL, x_full if l == 0 else xG, w['wall'], w['vec'], w['g1'], w['wab'], w['pww'], yT, post=post_m,
                        xtile=None if l == 0 else xtile)
            nc.all_engine_barrier()

            def post_f(C, last=last):
                if not last:
                    for k in range(NT // 512):
                        C.S.add('pool', lambda e, k=k: e.collective_compute(
                            "AllGather", ALU.bypass, replica_groups=RG_PAIRS,
                            ins=[xo[k * 512:(k + 1) * 512, :]], outs=[xG[k * 1024:(k + 1) * 1024, :]]),
                            r=['xout'], cw=['xG'], tag='cc', inc=1)

            build_ffn(nc, gs, NT, xin0 if l == 0 else xo, yG, msk, w['wout'], w['gout'], w['g2bd'], w['wr'], w['wg'], w['wu'],
                      w['wd'], xout if last else xo, Xs, Ys, xmid, post=post_f)
            if not last:
                nc.all_engine_barrier()
    return nc


_NC_CACHE = {}


def run_fused(inp, X):
    B, SL, D = X.shape
    NT = SL // 2
    depth = np.asarray(inp['w_in']).shape[0]
    key = (SL, depth)
    if key not in _NC_CACHE:
        _NC_CACHE[key] = build_fused(SL, depth)
    nc = _NC_CACHE[key]
    fw = [prep_ffn_inputs(inp, l) for l in range(depth)]
    maps = []
    for c in range(8):
        b, h = c // 2, c % 2
        m = {'x_full': np.ascontiguousarray(X[b]), 'xin0': np.ascontiguousarray(X[b, h * NT:(h + 1) * NT])}
        mk = np.zeros((128, 2), np.float32)
        mk[:, h] = 1.0
        m['msk'] = mk
        for l in range(depth):
            for k, v in prep_mixer_inputs(inp, l, b, h).items():
                m[f'{k}{l}'] = v
            for k, v in fw[l].items():
                m[f'{k}{l}'] = v
        maps.append(m)
    res = run_bass_kernel_spmd(nc, maps, core_ids=list(range(8)))
    out = np.empty_like(X)
    for c in range(8):
        b, h = c // 2, c % 2
        out[b, h * NT:(h + 1) * NT] = np.asarray(res.results[c]['xout'])
    return out


def kernel(**inp):
    X = np.ascontiguousarray(np.asarray(inp['x'], dtype=np.float32))
    return run_fused(inp, X)
```

```python
import numpy as np
import ml_dtypes
from contextlib import ExitStack
import concourse.bass as bass
import concourse.mybir as mybir
from concourse.bass_utils import run_bass_kernel_spmd

F32 = mybir.dt.float32
BF16 = mybir.dt.bfloat16
I32 = mybir.dt.int32
AF = mybir.ActivationFunctionType
ALU = mybir.AluOpType
AX = mybir.AxisListType

ENG_ATTR = {'pe': 'tensor', 'act': 'scalar', 'dve': 'vector', 'pool': 'gpsimd', 'sp': 'sync'}
EPS = 1e-6


class GSync:
    def __init__(self, nc, stack):
        self.nc = nc
        self.stack = stack
        self.sems = {}
        self.cnts = {}
        self.tagmap = {}

    def sem(self, name):
        if name not in self.sems:
            self.sems[name] = self.stack.enter_context(self.nc.semaphore(name))
        return self.sems[name]

    def tagsem(self, tag):
        if tag not in self.tagmap:
            self.tagmap[tag] = 't_' + tag
        return self.tagmap[tag]


class Sched:
    def __init__(self, nc, stack, gs=None):
        self.nc = nc
        self.stack = stack
        self.gs = gs if gs is not None else GSync(nc, stack)
        self.ops = []
        self.wx = {}
        self.wc = {}
        self.rd = {}
        self.tag_last = {}
        self.sems = {}

    @staticmethod
    def _merge(dst, src):
        for s, i in src.items():
            if dst.get(s, -1) < i:
                dst[s] = i

    def add(self, eng, fn, r=(), w=(), cw=(), tag=None, inc=16):
        deps = {}
        for k in r:
            self._merge(deps, self.wx.get(k, {}))
            self._merge(deps, self.wc.get(k, {}))
        for k in w:
            self._merge(deps, self.wx.get(k, {}))
            self._merge(deps, self.wc.get(k, {}))
            self._merge(deps, self.rd.get(k, {}))
        for k in cw:
            self._merge(deps, self.wx.get(k, {}))
            self._merge(deps, self.rd.get(k, {}))
        if tag is not None and tag in self.tag_last:
            self._merge(deps, {('t', tag): self.tag_last[tag]})
        idx = len(self.ops)
        if eng == 'pe':
            deps.pop(('e', 'pe'), None)
        self.ops.append(dict(eng=eng, fn=fn, deps=deps, tag=tag, sem=None, cnt=0, inc=inc))
        sig = ('t', tag) if tag else ('e', eng)
        for k in r:
            self.rd.setdefault(k, {})[sig] = idx
        for k in w:
            self.wx[k] = {sig: idx}
            self.wc[k] = {}
            self.rd[k] = {}
        for k in cw:
            self.wc.setdefault(k, {})[sig] = idx
        if tag is not None:
            self.tag_last[tag] = idx
        return idx

    def _sem(self, name):
        return self.gs.sem(name)

    def emit(self):
        ops = self.ops
        need = set()
        for o in ops:
            for i in o['deps'].values():
                need.add(i)
        cnts = self.gs.cnts
        for i, o in enumerate(ops):
            if o['tag']:
                o['sem'] = self.gs.tagsem(o['tag'])
                cnts[o['sem']] = cnts.get(o['sem'], 0) + o['inc']
                o['cnt'] = cnts[o['sem']]
            elif i in need:
                o['sem'] = 'e_' + o['eng']
                cnts[o['sem']] = cnts.get(o['sem'], 0) + 1
                o['cnt'] = cnts[o['sem']]
        final = {}
        for o in ops:
            if o['sem']:
                final[o['sem']] = max(final.get(o['sem'], 0), o['cnt'])
        for s in final:
            self._sem(s)
        with self.nc.Block() as blk:
            for eng, attr in ENG_ATTR.items():
                mine = [o for o in ops if o['eng'] == eng]

                def body(e, mine=mine, eng=eng):
                    waited = {}
                    for o in mine:
                        for di in sorted(o['deps'].values()):
                            d = ops[di]
                            if waited.get(d['sem'], 0) < d['cnt']:
                                e.wait_ge(self._sem(d['sem']), d['cnt'])
                                waited[d['sem']] = d['cnt']
                        ins = o['fn'](e)
                        if o['sem']:
                            ins.then_inc(self._sem(o['sem']), o['inc'] if o['tag'] else 1)
                    if eng == 'sp':
                        for s, c in final.items():
                            if waited.get(s, 0) < c:
                                e.wait_ge(self._sem(s), c)

                getattr(blk, attr)(body)
        return dict(nops=len(ops), final=final)


class Ctx:
    def __init__(self, nc, st, gs=None):
        self.nc = nc
        self.st = st
        self.S = Sched(nc, st, gs)
        g = self.S.gs
        g.phase = getattr(g, 'phase', 0) + 1
        self.pfx = f"p{g.phase}_"

    def sb(self, name, shape, dt):
        return self.st.enter_context(self.nc.sbuf_tensor(self.pfx + name, shape, dt))

    def ps(self, name, shape, dt):
        return self.st.enter_context(self.nc.psum_tensor(self.pfx + name, shape, dt))

    def dma(self, q, out, in_, r, w, tag, cw=()):
        self.S.add(q, lambda e: e.dma_start(out=out, in_=in_), r=r, w=w, cw=cw, tag=tag)

    def mm(self, out, lhsT, rhs, start, stop, r, w):
        self.S.add('pe', lambda e: e.matmul(out, lhsT=lhsT, rhs=rhs, start=start, stop=stop,
                                            skip_group_check=True), r=r, w=w)

    def tr(self, out, in_, ident, r, w):
        self.S.add('pe', lambda e: e.transpose(out, in_, ident), r=r, w=w)

    def act(self, out, in_, func, r, w, bias=None, scale=None, accum=None):
        kw = {}
        if bias is not None:
            kw['bias'] = bias
        if scale is not None:
            kw['scale'] = scale
        if accum is not None:
            kw['accum_out'] = accum
        self.S.add('act', lambda e: e.activation(out=out, in_=in_, func=func, **kw), r=r, w=w)

    def ts(self, eng, out, in0, s1, s2, op0, op1, r, w):
        if op1 is None:
            self.S.add(eng, lambda e: e.tensor_scalar(out=out, in0=in0, scalar1=s1, scalar2=None, op0=op0), r=r, w=w)
        else:
            self.S.add(eng, lambda e: e.tensor_scalar(out=out, in0=in0, scalar1=s1, scalar2=s2, op0=op0, op1=op1), r=r, w=w)

    def tt(self, eng, out, in0, in1, op, r, w):
        self.S.add(eng, lambda e: e.tensor_tensor(out=out, in0=in0, in1=in1, op=op), r=r, w=w)

    def stt(self, out, in0, scalar, in1, op0, op1, r, w):
        self.S.add('dve', lambda e: e.scalar_tensor_tensor(out=out, in0=in0, scalar=scalar, in1=in1, op0=op0, op1=op1), r=r, w=w)

    def cp(self, eng, out, in_, r, w):
        if eng == 'act':
            self.S.add('act', lambda e: e.copy(out=out, in_=in_), r=r, w=w)
        else:
            self.S.add(eng, lambda e: e.tensor_copy(out=out, in_=in_), r=r, w=w)

    def memset(self, eng, ap, val, w):
        self.S.add(eng, lambda e: e.memset(ap, val), w=w)

    def recip(self, out, in_, r, w):
        self.S.add('dve', lambda e: e.reciprocal(out=out, in_=in_), r=r, w=w)

    def aselect(self, out, in_, pattern, op, fill, base, cm, r, w):
        self.S.add('pool', lambda e: e.affine_select(out=out, in_=in_, pattern=pattern, compare_op=op, fill=fill,
                                                     base=base, channel_multiplier=cm), r=r, w=w)


V_GQ, V_GK, V_LCW, V_LCB, V_BA, V_BX, V_LAM = 0, 1, 2, 6, 7, 8, 9
V_DWB, V_LNG, V_LNB, V_PWB, V_DWW = 10, 12, 14, 16, 17
NV = 17 + 62
NW = 1536


def build_mixer(nc, gs, SL, x, wall, vec, g1, wab, pww, yT, post=None, xtile=None):
    NG = SL // 512
    with ExitStack() as st:
        C = Ctx(nc, st, gs)
        S = C.S
        sb, ps = C.sb, C.ps
        Wb = sb("Wb", [128, 8, NW], BF16)
        wst = [sb(f"wst{i}", [128, 384], F32) for i in range(2)]
        vecs = sb("vecs", [128, NV], F32)
        g1s = sb("g1s", [128, 8], F32)
        wabf = sb("wabf", [128, 2, 128], F32)
        wabb = sb("wabb", [128, 2, 128], BF16)
        pwf = sb("pwf", [128, 2, 128], F32)
        pwb = sb("pwb", [128, 2, 128], BF16)
        identb = sb("identb", [128, 128], BF16)
        blk1 = sb("blk1", [128, 128], BF16)
        onesm = sb("onesm", [128, 128], F32)
        uincl = sb("uincl", [128, 128], BF16)
        negones = sb("negones", [128, 128], BF16)
        tri = sb("tri", [128, 128], BF16)
        dg = sb("dg", [128, 62, 128], BF16)
        cl = sb("cl", [128, 4], F32)
        KT = sb("KT", [128, 2, SL], BF16)
        Vt = sb("Vt", [128, SL // 128, 256], BF16)
        QT = sb("QT", [128, 2, 512], BF16)
        xt = [sb(f"xt{i}", [128, 1024], F32) for i in range(2)]
        xn = [sb(f"xn{i}", [128, 1024], BF16) for i in range(2)]
        junk = sb("junk", [128, 1024], BF16)
        st1 = [sb(f"st1_{i}", [128, 4], F32) for i in range(2)]
        hT = sb("hT", [128, 8, 512], BF16)
        sq = sb("sq", [128, 512], BF16)
        rs = sb("rs", [128, 512], F32)
        xrb = sb("xrb", [128, 515], F32)
        xgb = sb("xgb", [128, 512], F32)
        cv = sb("cv", [128, 512], F32)
        cvb = sb("cvb", [128, 512], BF16)
        lr = sb("lr", [128, 512], F32)
        li = sb("li", [128, 512], F32)
        la = sb("la", [128, 512], F32)
        la2 = sb("la2", [128, 512], F32)
        lu = sb("lu", [128, 512], F32)
        lh = sb("lh", [128, 512], F32)
        hprev = sb("hprev", [128, 1], F32)
        gt = sb("gt", [128, 512], F32)
        ybs = sb("ybs", [128, 512], BF16)
        ub = sb("ub", [128, 2, 542], BF16)
        sg = sb("sg", [128, 512], F32)
        cvo = sb("cvo", [128, 2, 512], F32)
        sqo = sb("sqo", [128, 2, 512], F32)
        means = sb("means", [128, 512], F32)
        m2 = sb("m2", [128, 512], F32)
        crs = sb("crs", [128, 512], F32)
        cn = sb("cn", [128, 2, 512], F32)
        csb = sb("csb", [128, 2, 512], BF16)
        ycs = sb("ycs", [128, 512], BF16)
        Eb = [sb(f"Eb{i}", [128, 512], BF16) for i in range(2)]
        Lb = [sb(f"Lb{i}", [128, 512], BF16) for i in range(3)]
        NWB = 3
        Wt = [sb(f"Wt{i}", [128, 512], BF16) for i in range(NWB)]
        LaccP = [sb(f"Lacc{i}", [128, 512], F32) for i in range(2)]
        xC = [sb(f"xC{i}", [128, 512], BF16) for i in range(2)]
        Laccb = [sb(f"Laccb{i}", [128, 512], BF16) for i in range(3)]
        yst = [sb(f"yst{i}", [64, 512], BF16) for i in range(2)]
        pA = [ps(f"pA{i}", [128, 512], F32) for i in range(2)]
        pT = ps("pT", [128, 1024], BF16)
        pZ = [ps(f"pZ{i}", [128, 512], F32) for i in range(2)]
        pC = [ps(f"pC{i}", [128, 512], F32) for i in range(2)]
        pO = ps("pO", [128, 512], F32)

        C.dma('sp', vecs[:], vec, [], ['vecs'], 'c0')
        C.dma('sp', g1s[:], g1, [], ['g1s'], 'c1')
        C.dma('sp', wabf[:], wab, [], ['wabf'], 'c2')
        C.dma('sp', pwf[:], pww, [], ['pwf'], 'c3')
        C.cp('dve', wabb[:], wabf[:], ['wabf'], ['wabb'])
        C.cp('dve', pwb[:], pwf[:], ['pwf'], ['pwb'])
        wv = wall.rearrange("(c p) n -> p c n", p=128)
        k = 0
        for c in range(8):
            for hf in range(4):
                b = k % 2
                C.dma('sp', wst[b][:], wv[:, c, hf * 384:(hf + 1) * 384], [], [f'wst{b}'], f'wst{b}')
                C.ts('dve' if k % 2 == 0 else 'pool', Wb[:, c, hf * 384:(hf + 1) * 384], wst[b][:], g1s[:, c:c + 1], None,
                     ALU.mult, None, [f'wst{b}', 'g1s'], [('Wb', c, hf)])
                k += 1
        WbK = [('Wb', c, hf) for c in range(8) for hf in range(4)]
        C.memset('pool', identb[:], 0.0, ['identb'])
        C.aselect(identb[:], identb[:], [[-1, 128]], ALU.not_equal, 1.0, 0, 1, ['identb'], ['identb'])
        C.memset('pool', blk1[:], 0.0, ['blk1'])
        C.memset('pool', blk1[0:64, 0:64], 1.0, ['blk1'])
        C.memset('pool', blk1[64:128, 64:128], 1.0, ['blk1'])
        C.memset('pool', onesm[:], 1.0 / 256.0, ['onesm'])
        C.memset('pool', negones[:], -1.0, ['negones'])
        C.memset('pool', uincl[:], -1.0, ['uincl'])
        C.aselect(uincl[:], uincl[:], [[-1, 128]], ALU.is_ge, 0.0, 0, 1, ['uincl'], ['uincl'])
        C.memset('pool', tri[:], 1.0, ['tri'])
        C.aselect(tri[:], tri[:], [[1, 128]], ALU.is_ge, 0.0, -1, -1, ['tri'], ['tri'])
        for i in range(62):
            C.ts('pool' if i % 2 else 'dve', dg[:, i, :], identb[:], vecs[:, V_DWW + i:V_DWW + i + 1], None, ALU.mult, None,
                 ['identb', 'vecs'], ['dg'])
        C.act(cl[:, 0:1], vecs[:, V_LAM:V_LAM + 1], AF.Exp, ['vecs'], ['cl0'], scale=-1.0)
        C.act(cl[:, 1:2], cl[:, 0:1], AF.Ln, ['cl0'], ['cl1'], bias=1.0)
        C.ts('dve', cl[:, 2:3], cl[:, 1:2], -8.0, None, ALU.mult, None, ['cl1'], ['cl2'])
        C.ts('dve', cl[:, 3:4], cl[:, 1:2], -16.0, None, ALU.mult, None, ['cl1'], ['cl3'])
        C.memset('pool', hprev[:], 0.0, ['hprev'])
        C.memset('pool', xrb[:, 0:3], 0.0, ['xrb_h'])
        C.memset('pool', ub[:, :, 0:30], 0.0, ['ub_h'])

        xv = x.rearrange("(n p) d -> n p d", p=128)
        wcnt = [0]
        ocnt = [0]
        acnt = [0]
        bcnt = [0]
        lcnt = [0]
        pcnt = [0]
        for g in range(NG):
            t0 = g * 512
            for tt in range(4):
                b = tt % 2
                C.dma('sp', xt[b][:], xv[g * 4 + tt] if xtile is None else xtile(g * 4 + tt), [], [f'xt{b}'], f'x{b}')
                C.act(junk[:], xt[b][:], AF.Square, [f'xt{b}'], [f'ss{b}'], accum=st1[b][:, 0:1])
                C.act(st1[b][:, 1:2], st1[b][:, 0:1], AF.Sqrt, [f'ss{b}'], [f'rt{b}'], scale=1.0 / 1024.0, bias=EPS)
                C.recip(st1[b][:, 2:3], st1[b][:, 1:2], [f'rt{b}'], [f'rstd{b}'])
                C.ts('dve', xn[b][:], xt[b][:], st1[b][:, 2:3], None, ALU.mult, None, [f'xt{b}', f'rstd{b}'], [f'xn{b}'])
                for c in range(8):
                    C.tr(pT[:, c * 128:(c + 1) * 128], xn[b][:, c * 128:(c + 1) * 128], identb[:], [f'xn{b}', 'identb'], ['pT'])
                C.cp('act' if tt % 2 else 'dve', hT[:, :, tt * 128:(tt + 1) * 128], pT[:].rearrange("p (c t) -> p c t", c=8),
                     ['pT'], [('hT', tt)])
            hTK = [('hT', tt) for tt in range(4)]
            pidx = [0]

            def proj(ct):
                bank = pidx[0] % 2
                pidx[0] += 1
                for c in range(8):
                    C.mm(pA[bank][:], Wb[:, c, ct * 128:(ct + 1) * 128], hT[:, c, :], c == 0, c == 7, WbK + hTK, [f'pA{bank}'])
                return bank

            for ct in range(4):
                bk = proj(ct)
                C.act(sq[:], pA[bk][:], AF.Square, [f'pA{bk}'], ['sq'])
                C.mm(pA[1 - bk][:], blk1[:], sq[:], True, True, ['blk1', 'sq'], [f'pA{1 - bk}'])
                pidx[0] += 1
                if ct < 2:
                    C.act(rs[:], pA[1 - bk][:], AF.Sqrt, [f'pA{1 - bk}'], ['rs'], scale=1.0, bias=64.0 * EPS)
                else:
                    C.act(rs[:], pA[1 - bk][:], AF.Sqrt, [f'pA{1 - bk}'], ['rs'], scale=1.0 / 64.0, bias=EPS)
                C.recip(rs[:], rs[:], ['rs'], ['rs'])
                if ct < 2:
                    C.stt(QT[:, ct, :], pA[bk][:], vecs[:, V_GQ:V_GQ + 1], rs[:], ALU.mult, ALU.mult,
                          [f'pA{bk}', 'vecs', 'rs'], [('QT', ct)])
                else:
                    C.stt(KT[:, ct - 2, t0:t0 + 512], pA[bk][:], vecs[:, V_GK:V_GK + 1], rs[:], ALU.mult, ALU.mult,
                          [f'pA{bk}', 'vecs', 'rs'], [('KT', ct - 2, g)])
            bk = proj(4)
            C.cp('act', xrb[:, 3:515], pA[bk][:], [f'pA{bk}'], ['xrb'])
            bk = proj(5)
            C.cp('dve', xgb[:], pA[bk][:], [f'pA{bk}'], ['xgb'])
            for t in range(2):
                bv = proj(6 + t)
                bg = proj(8 + t)
                C.act(sg[:], pA[bg][:], AF.Sigmoid, [f'pA{bg}'], ['sg'])
                C.tt('dve', ub[:, t, 30:542], pA[bv][:], sg[:], ALU.mult, [f'pA{bv}', 'sg'], [('ub', t)])
            for tt in range(4):
                bank = pidx[0] % 2
                pidx[0] += 1
                for c in range(8):
                    C.mm(pA[bank][:, 0:256], hT[:, c, tt * 128:(tt + 1) * 128], Wb[:, c, 1280:1536], c == 0, c == 7,
                         WbK + hTK, [f'pA{bank}'])
                C.cp('act' if tt % 2 else 'dve', Vt[:, g * 4 + tt, :], pA[bank][:, 0:256], [f'pA{bank}'], [('V', g * 4 + tt)])

            vc = lambda i: vecs[:, i:i + 1]
            C.ts('dve', cv[:], xrb[:, 3:515], vc(V_LCW + 3), vc(V_LCB), ALU.mult, ALU.add, ['xrb', 'xrb_h', 'vecs'], ['cv'])
            for kk in range(3):
                C.stt(cv[:], xrb[:, kk:kk + 512], vc(V_LCW + kk), cv[:], ALU.mult, ALU.add, ['xrb', 'xrb_h', 'vecs', 'cv'], ['cv'])
            C.cp('pool', xrb[:, 0:3], xrb[:, 512:515], ['xrb'], ['xrb_h'])
            C.cp('pool', cvb[:], cv[:], ['cv'], ['cvb'])
            b0 = pidx[0] % 2
            pidx[0] += 2
            C.mm(pA[b0][:], wabb[:, 0, :], cvb[:], True, True, ['wabb', 'cvb'], [f'pA{b0}'])
            C.mm(pA[1 - b0][:], wabb[:, 1, :], cvb[:], True, True, ['wabb', 'cvb'], [f'pA{1 - b0}'])
            C.act(lr[:], pA[b0][:], AF.Sigmoid, [f'pA{b0}', 'vecs'], ['lr'], bias=vc(V_BA))
            C.act(li[:], pA[1 - b0][:], AF.Sigmoid, [f'pA{1 - b0}', 'vecs'], ['li'], bias=vc(V_BX))
            C.act(la[:], lr[:], AF.Exp, ['lr', 'cl2'], ['la'], scale=cl[:, 2:3])
            C.act(la2[:], lr[:], AF.Exp, ['lr', 'cl3'], ['la2'], scale=cl[:, 3:4])
            C.act(la2[:], la2[:], AF.Sqrt, ['la2'], ['la2'], scale=-1.0, bias=1.0)
            C.tt('pool', lu[:], li[:], cv[:], ALU.mult, ['li', 'cv'], ['lu'])
            C.tt('pool', lu[:], lu[:], la2[:], ALU.mult, ['lu', 'la2'], ['lu'])
            S.add('dve', lambda e: e.tensor_tensor_scan(out=lh[:], data0=la[:], data1=lu[:], initial=hprev[:, 0:1],
                                                        op0=ALU.mult, op1=ALU.add), r=['la', 'lu', 'hprev'], w=['lh'])
            C.cp('pool', hprev[:], lh[:, 511:512], ['lh'], ['hprev'])
            C.tt('pool', gt[:], xgb[:], xgb[:], ALU.mult, ['xgb'], ['gt'])
            C.ts('pool', gt[:], gt[:], 0.044715, 1.0, ALU.mult, ALU.add, ['gt'], ['gt'])
            C.tt('pool', gt[:], gt[:], xgb[:], ALU.mult, ['gt', 'xgb'], ['gt'])
            C.act(gt[:], gt[:], AF.Sigmoid, ['gt'], ['gt'], scale=1.5957691216)
            C.tt('pool', gt[:], gt[:], xgb[:], ALU.mult, ['gt', 'xgb'], ['gt'])
            C.tt('dve', ybs[:], gt[:], lh[:], ALU.mult, ['gt', 'lh'], ['ybs'])
            C.dma('pool', yT[256:384, t0:t0 + 512], ybs[:], ['ybs'], [], 'yb', cw=['yT'])

            for t in range(2):
                bank = pidx[0] % 2
                pidx[0] += 1
                for kk in range(31):
                    C.mm(pA[bank][:], dg[:, t * 31 + kk, :], ub[:, t, kk:kk + 512], kk == 0, kk == 30,
                         ['dg', ('ub', t), 'ub_h'], [f'pA{bank}'])
                C.act(cvo[:, t, :], pA[bank][:], AF.Identity, [f'pA{bank}', 'vecs'], [('cvo', t)], bias=vc(V_DWB + t))
                C.act(sqo[:, t, :], pA[bank][:], AF.Square, [f'pA{bank}', 'vecs'], [('sqo', t)], bias=vc(V_DWB + t))
            C.cp('pool', ub[:, :, 0:30], ub[:, :, 512:542], [('ub', 0), ('ub', 1)], ['ub_h'])
            bm = pidx[0] % 2
            pidx[0] += 2
            for t in range(2):
                C.mm(pA[bm][:], onesm[:], cvo[:, t, :], t == 0, t == 1, ['onesm', ('cvo', t)], [f'pA{bm}'])
            for t in range(2):
                C.mm(pA[1 - bm][:], onesm[:], sqo[:, t, :], t == 0, t == 1, ['onesm', ('sqo', t)], [f'pA{1 - bm}'])
            C.cp('act', means[:], pA[bm][:], [f'pA{bm}'], ['means'])
            C.tt('pool', m2[:], means[:], means[:], ALU.mult, ['means'], ['m2'])
            C.tt('dve', m2[:], pA[1 - bm][:], m2[:], ALU.subtract, [f'pA{1 - bm}', 'm2'], ['m2'])
            C.act(crs[:], m2[:], AF.Sqrt, ['m2'], ['crs'], scale=1.0, bias=EPS)
            C.recip(crs[:], crs[:], ['crs'], ['crs'])
            for t in range(2):
                C.tt('pool', cn[:, t, :], cvo[:, t, :], means[:], ALU.subtract, [('cvo', t), 'means'], [('cn', t)])
                C.tt('dve' if t else 'pool', cn[:, t, :], cn[:, t, :], crs[:], ALU.mult, [('cn', t), 'crs'], [('cn', t)])
                C.act(csb[:, t, :], cn[:, t, :], AF.Silu, [('cn', t), 'vecs'], [('csb', t)], scale=vc(V_LNG + t), bias=vc(V_LNB + t))
            bank = pidx[0] % 2
            pidx[0] += 1
            for t in range(2):
                C.mm(pA[bank][:], pwb[:, t, :], csb[:, t, :], t == 0, t == 1, ['pwb', ('csb', t)], [f'pA{bank}'])
            C.act(ycs[:], pA[bank][:], AF.Identity, [f'pA{bank}', 'vecs'], ['ycs'], bias=vc(V_PWB))
            C.dma('pool', yT[384:512, t0:t0 + 512], ycs[:], ['ycs'], [], 'yc', cw=['yT'])

            items = []
            for hd in range(4):
                nblk = 4 * g + 4
                for bi, kb in enumerate(range(4 * g + 3, -1, -1)):
                    items.append(dict(hd=hd, bi=bi, kb=kb, nblk=nblk))

            def stageA(it):
                hd, bi, kb = it['hd'], it['bi'], it['kb']
                ct = hd // 2
                pb = 64 * (hd % 2)
                Qh = QT[pb:pb + 64, ct, :]
                j = kb - 4 * g
                c0 = 128 * j if j > 0 else 0
                Kh = KT[pb:pb + 64, ct, kb * 128:(kb + 1) * 128]
                kkey = ('KT', ct, kb // 4)
                zb = acnt[0] % 2
                lb = acnt[0] % 3
                acnt[0] += 1
                it.update(c0=c0, j=j, Kh=Kh, kkey=kkey, Qh=Qh, ct=ct, lb=lb, zb=zb)
                if bi == 0:
                    C.memset('pool', LaccP[0][:], 0.0, ['Lacc0'])
                    C.memset('pool', LaccP[1][:], 0.0, ['Lacc1'])
                    pcnt[0] = 0
                C.mm(pZ[zb][:, c0:], Kh, Qh[:, c0:], True, True, [kkey, ('QT', ct)], [f'pZ{zb}'])
                C.act(Eb[zb][:, c0:], pZ[zb][:, c0:], AF.Exp, [f'pZ{zb}'], [f'Eb{zb}'])
                C.act(Lb[lb][:, c0:], Eb[zb][:, c0:], AF.Ln, [f'Eb{zb}'], [f'Lb{lb}'], bias=1.0)
                if j >= 0:
                    C.tt('dve', Lb[lb][:, c0:c0 + 128], Lb[lb][:, c0:c0 + 128], tri[:], ALU.mult, [f'Lb{lb}', 'tri'], [f'Lb{lb}'])
                if kb > 0:
                    la_ = lcnt[0] % 3
                    lcnt[0] += 1
                    pp = pcnt[0] % 2
                    pcnt[0] += 1
                    Lo, Ln_ = LaccP[pp], LaccP[1 - pp]
                    ko, kn = f'Lacc{pp}', f'Lacc{1 - pp}'
                    C.tt('pool', Ln_[:, c0:], Lo[:, c0:], Lb[lb][:, c0:], ALU.add, [ko, f'Lb{lb}'], [kn])
                    C.cp('dve', Laccb[la_][:], Ln_[:], [kn], [f'Laccb{la_}'])
                    it['la_out'] = la_

            def stageB(it, prev):
                hd, bi, kb, c0, j = it['hd'], it['bi'], it['kb'], it['c0'], it['j']
                Kh, kkey, Qh, ct, lb = it['Kh'], it['kkey'], it['Qh'], it['ct'], it['lb']
                cb = bcnt[0] % 2
                bcnt[0] += 1
                C.mm(pC[cb][:, c0:], uincl[:], Lb[lb][:, c0:], True, bi == 0, ['uincl', f'Lb{lb}'], [f'pC{cb}'])
                if bi > 0:
                    la_ = prev['la_out']
                    C.mm(pC[cb][:, c0:], negones[:], Laccb[la_][:, c0:], False, True, ['negones', f'Laccb{la_}'], [f'pC{cb}'])
                wi = wcnt[0] % NWB
                wcnt[0] += 1
                zb = it['zb']
                C.act(xC[cb][:, c0:], pC[cb][:, c0:], AF.Exp, [f'pC{cb}'], [f'xC{cb}'])
                C.tt('dve', Wt[wi][:, c0:], xC[cb][:, c0:], Eb[zb][:, c0:], ALU.mult, [f'xC{cb}', f'Eb{zb}'], [f'Wt{wi}'])
                if j >= 0:
                    C.tt('dve', Wt[wi][:, c0:c0 + 128], Wt[wi][:, c0:c0 + 128], tri[:], ALU.mult, [f'Wt{wi}', 'tri'], [f'Wt{wi}'])
                C.mm(pO[0:64, c0:], Vt[:, kb, hd * 64:(hd + 1) * 64], Wt[wi][:, c0:], bi == 0, bi == it['nblk'] - 1,
                     [('V', kb), f'Wt{wi}'], ['pO'])
                if bi == it['nblk'] - 1:
                    ob = ocnt[0] % 2
                    ocnt[0] += 1
                    C.cp('dve', yst[ob][:], pO[0:64, :], ['pO'], [f'yst{ob}'])
                    C.dma('pool', yT[hd * 64:(hd + 1) * 64, t0:t0 + 512], yst[ob][:], [f'yst{ob}'], [], f'ya{ob}', cw=['yT'])

            stageA(items[0])
            for n_ in range(len(items)):
                if n_ + 1 < len(items):
                    stageA(items[n_ + 1])
                stageB(items[n_], items[n_ - 1] if n_ > 0 else None)
        if post is not None:
            post(C)
        info = S.emit()
    return info


def prep_mixer_inputs(inp, l, b, hg, SL=None):
    f = np.float32
    w_in = np.asarray(inp['w_in'][l])
    hs = slice(hg * 256, (hg + 1) * 256)
    q = w_in[:, 0:512][:, hs]
    k = w_in[:, 512:1024][:, hs]
    v = w_in[:, 1024:1536][:, hs]
    cs = slice(hg * 128, (hg + 1) * 128)
    xr = w_in[:, 1536:1792][:, cs]
    xg = w_in[:, 1792:2048][:, cs]
    cval = w_in[:, 2048:2304]
    cgate = w_in[:, 2304:2560]
    wall = np.ascontiguousarray(np.concatenate([q, k, xr, xg, cval, cgate, v], axis=1), dtype=f)
    vec = np.zeros((128, NV), f)
    vec[:, V_GQ] = np.tile(inp['q_norm_g'][l], 2)
    vec[:, V_GK] = np.tile(inp['k_norm_g'][l], 2)
    for kk in range(4):
        vec[:, V_LCW + kk] = inp['lru_conv_w'][l][kk, cs]
    vec[:, V_LCB] = inp['lru_conv_b'][l][cs]
    vec[:, V_BA] = inp['lru_ba'][l][cs]
    vec[:, V_BX] = inp['lru_bx'][l][cs]
    vec[:, V_LAM] = inp['lru_lambda'][l][cs]
    for t in range(2):
        ts_ = slice(t * 128, (t + 1) * 128)
        vec[:, V_DWB + t] = inp['conf_dw_b'][l][ts_]
        vec[:, V_LNG + t] = inp['conf_ln_g'][l][ts_]
        vec[:, V_LNB + t] = inp['conf_ln_b'][l][ts_]
        for kk in range(31):
            vec[:, V_DWW + t * 31 + kk] = inp['conf_dw_w'][l][kk, ts_]
    vec[:, V_PWB] = inp['conf_pw_b'][l][cs]
    g1 = np.ascontiguousarray(np.asarray(inp['norm1_g'][l]).reshape(8, 128).T, dtype=f)
    wab = np.zeros((128, 2, 128), f)
    for i in range(2):
        blk = hg * 2 + i
        wab[i * 64:(i + 1) * 64, 0, i * 64:(i + 1) * 64] = inp['lru_wa'][l][blk]
        wab[i * 64:(i + 1) * 64, 1, i * 64:(i + 1) * 64] = inp['lru_wx'][l][blk]
    pw = np.asarray(inp['conf_pw_w'][l])[:, cs]
    pww = np.ascontiguousarray(pw.reshape(2, 128, 128).transpose(1, 0, 2), dtype=f)
    return dict(wall=wall, vec=vec, g1=g1, wab=wab, pww=pww)


CAP = 640
NSLOT = 32 * CAP
NROWS = NSLOT + 128
TRASH = float(NSLOT)


def build_ffn(nc, gs, NT, xin, yG, msk, wout, gout, g2bd, wr, wg, wu, wd, xout, Xs, Ys, xmid, post=None):
    NTT = NT // 128
    stage = 4
    dbg = False
    with ExitStack() as st:
        C = Ctx(nc, st, gs)
        S = C.S
        sb, ps = C.sb, C.ps
        mks = sb("mks", [128, 2], F32)
        ysc = [[sb(f"ysc{i}_{k}", [128, 8, 128], BF16) for k in range(2)] for i in range(2)]
        Woutb = sb("Woutb", [128, 8, 1024], BF16)
        wstg = [sb(f"wstg{i}", [128, 1024], F32) for i in range(2)]
        gos = sb("gos", [128, 8], F32)
        g2b = sb("g2b", [128, 1024], F32)
        wrs = sb("wrs", [128, 8, 36], F32)
        identf = sb("identf", [128, 128], F32)
        identb = sb("identb", [128, 128], BF16)
        onec = sb("onec", [128, 2], BF16)
        ustr = sb("ustr", [128, 128], BF16)
        ones128 = sb("ones128", [128, 128], BF16)
        basef = sb("basef", [128, 32], F32)
        zt = sb("zt", [128, 4, 1024], BF16)
        gates = sb("gates", [128, NTT, 2], F32)
        dsti = sb("dsti", [128, NTT * 2], I32)
        Macc = sb("Macc", [128, 32], F32)
        Maccb = sb("Maccb", [128, 32], BF16)
        xt = [sb(f"xt{i}", [128, 1024], F32) for i in range(2)]
        ys = [sb(f"ys{i}", [128, 8, 128], BF16) for i in range(2)]
        ysq = sb("ysq", [128, 8, 128], BF16)
        x1 = [sb(f"x1_{i}", [128, 1024], F32) for i in range(2)]
        junk = sb("junk", [128, 1024], BF16)
        h2f = sb("h2f", [128, 1024], F32)
        h2b = [sb(f"h2b{i}", [128, 1024], BF16) for i in range(2)]
        h2T = sb("h2T", [128, 8, 128], F32)
        sm = [sb(f"sm{i}", [128, 16], F32) for i in range(2)]
        lg = sb("lg", [128, 36], F32)
        r1 = sb("r1", [128, 224], F32)
        Mb = sb("Mb", [128, 32], BF16)
        dstf = sb("dstf", [128, 2], F32)
        wgb = [sb(f"wgb{i}", [128, 8, 512], BF16) for i in range(2)]
        wub = [sb(f"wub{i}", [128, 8, 512], BF16) for i in range(2)]
        wdb = [sb(f"wdb{i}", [128, 4, 1024], BF16) for i in range(2)]
        NST = CAP // 128
        xe = [sb(f"xe{i}", [128, NST, 1024], BF16) for i in range(2)]
        xeT = sb("xeT", [128, 8, CAP], BF16)
        sgl = [sb(f"sgl{i}", [128, CAP], F32) for i in range(2)]
        aT = sb("aT", [128, 4, CAP], BF16)
        yo = [sb(f"yo{i}", [128, NST, 1024], BF16) for i in range(2)]
        yg = [[sb(f"yg{i}_{k}", [128, 1024], BF16) for k in range(2)] for i in range(2)]
        pM = ps("pM", [128, 512], F32)
        pP = [ps(f"pP{i}", [128, 512], F32) for i in range(2)]
        pTf = ps("pTf", [128, 1024], F32)
        pUU = ps("pUU", [128, 1024], F32)
        pTb = ps("pTb", [128, 1024], BF16)

        C.dma('sp', gos[:], gout, [], ['gos'], 'c0')
        C.dma('sp', mks[:], msk, [], ['mks'], 'c3')
        C.dma('sp', g2b[:], g2bd, [], ['g2b'], 'c1')
        C.dma('sp', wrs[:], wr.rearrange("(c p) n -> p c n", p=128), [], ['wrs'], 'c2')
        wov = wout.rearrange("(c p) n -> p c n", p=128)
        for c in range(8):
            b = c % 2
            C.dma('sp', wstg[b][:], wov[:, c, :], [], [f'wstg{b}'], f'wstg{b}')
            C.ts('dve' if b else 'pool', Woutb[:, c, :], wstg[b][:], gos[:, c:c + 1], None, ALU.mult, None,
                 [f'wstg{b}', 'gos'], [('Wo', c)])
        WoK = [('Wo', c) for c in range(8)]
        C.memset('pool', identf[:], 0.0, ['identf'])
        C.aselect(identf[:], identf[:], [[-1, 128]], ALU.not_equal, 1.0, 0, 1, ['identf'], ['identf'])
        C.cp('pool', identb[:], identf[:], ['identf'], ['identb'])
        C.memset('pool', onec[:], 1.0, ['onec'])
        C.memset('pool', ones128[:], 1.0, ['ones128'])
        C.memset('pool', ustr[:], 1.0, ['ustr'])
        C.aselect(ustr[:], ustr[:], [[1, 128]], ALU.is_ge, 0.0, -1, -1, ['ustr'], ['ustr'])
        S.add('pool', lambda e: e.iota(basef[:], pattern=[[CAP, 32]], base=0, channel_multiplier=0,
                                       allow_small_or_imprecise_dtypes=True), w=['basef'])
        C.memset('pool', zt[:], 0.0, ['zt'])
        C.memset('pool', Macc[:], 0.0, ['Macc'])
        C.memset('pool', Maccb[:], 0.0, ['Maccb'])
        Xv = Xs.rearrange("(n p) d -> p n d", p=128)
        nrt = NROWS // 128
        zi = 0
        for n0 in range(0, nrt, 4):
            n1 = min(nrt, n0 + 4)
            C.dma('sp', Xv[:, n0:n1, :], zt[:, 0:n1 - n0, :], ['zt'], [], f'z{zi % 2}', cw=['Xs'])
            zi += 1
        C.dma('sp', Ys[NSLOT:NROWS, :], zt[:, 0, :], ['zt'], [], 'zy', cw=['Ys'])
        S.add('sp', lambda e: e.nop(), r=[], w=['Xs'])

        xinv = xin.rearrange("(n p) d -> n p d", p=128)
        xmv = xmid.rearrange("(n p) d -> n p d", p=128)
        xov = xout.rearrange("(n p) d -> n p d", p=128)
        yv = yG.rearrange("(c p) t -> p c t", p=128)

        def loads(i):
            b = i % 2
            C.dma('sp', xt[b][:], xinv[i], [], [f'xt{b}'], f'lx{b}')
            for hh in range(2):
                C.dma('sp', ysc[b][hh][:], yv[:, :, hh * NT + i * 128:hh * NT + (i + 1) * 128], [], [f'ysc{b}{hh}'], f'ly{b}{hh}')
            C.ts('dve', ys[b][:], ysc[b][0][:], mks[:, 0:1], None, ALU.mult, None, [f'ysc{b}0', 'mks'], [f'ys{b}'])
            C.stt(ys[b][:], ysc[b][1][:], mks[:, 1:2], ys[b][:], ALU.mult, ALU.add, [f'ysc{b}1', 'mks', f'ys{b}'], [f'ys{b}'])

        loads(0)
        GRP = [([0, 1, 2, 3], 512.0), ([4, 5], 256.0), ([6, 7], 256.0)]
        for i in range(NTT):
            b = i % 2
            if i + 1 < NTT:
                loads(i + 1)
            s = sm[b]
            C.act(ysq[:], ys[b][:], AF.Square, [f'ys{b}'], ['ysq'])
            for gi, (cl_, n) in enumerate(GRP):
                for c in cl_:
                    C.mm(pM[:, gi:gi + 1], ysq[:, c, :], onec[:, 0:1], c == cl_[0], c == cl_[-1], ['ysq', 'onec'], ['pM'])
            C.act(s[:, 0:1], pM[:, 0:1], AF.Sqrt, ['pM'], [f's0{b}'], scale=1.0 / 512.0, bias=EPS)
            C.act(s[:, 1:3], pM[:, 1:3], AF.Sqrt, ['pM'], [f's1{b}'], scale=1.0 / 256.0, bias=EPS)
            C.recip(s[:, 3:6], s[:, 0:3], [f's0{b}', f's1{b}'], [f'rg{b}'])
            k = 0
            for half in range(2):
                for gi, (cl_, n) in enumerate(GRP):
                    bk = k % 2
                    k += 1
                    for c in cl_:
                        C.mm(pP[bk][:], ys[b][:, c, :], Woutb[:, c, half * 512:(half + 1) * 512], c == cl_[0], c == cl_[-1],
                             [f'ys{b}'] + WoK, [f'pP{bk}'])
                    src = xt[b] if gi == 0 else x1[b]
                    C.stt(x1[b][:, half * 512:(half + 1) * 512], pP[bk][:], s[:, 3 + gi:4 + gi], src[:, half * 512:(half + 1) * 512],
                          ALU.mult, ALU.add, [f'pP{bk}', f'rg{b}', f'xt{b}', ('x1', b, half)], [('x1', b, half)])
            x1k = [('x1', b, 0), ('x1', b, 1)]
            C.dma('sp', xmv[i], x1[b][:], x1k, [], f'sx{b}', cw=['xmid'])
            C.act(junk[:], x1[b][:], AF.Square, x1k, [f'ss{b}'], accum=s[:, 6:7])
            C.act(s[:, 7:8], s[:, 6:7], AF.Sqrt, [f'ss{b}'], [f'rt{b}'], scale=1.0 / 1024.0, bias=EPS)
            C.recip(s[:, 8:9], s[:, 7:8], [f'rt{b}'], [f'r2{b}'])
            C.stt(h2f[:], x1[b][:], s[:, 8:9], g2b[:], ALU.mult, ALU.mult, x1k + [f'r2{b}', 'g2b'], ['h2f'])
            C.cp('act', h2b[b][:], h2f[:], ['h2f'], [f'h2b{b}'])
            for c in range(8):
                C.tr(pTf[:, c * 128:(c + 1) * 128], h2f[:, c * 128:(c + 1) * 128], identf[:], ['h2f', 'identf'], ['pTf'])
            C.cp('dve', h2T[:].rearrange("p c t -> p (c t)"), pTf[:], ['pTf'], ['h2T'])
            for c in range(8):
                C.mm(pM[:, 8:44], h2T[:, c, :], wrs[:, c, :], c == 0, c == 7, ['h2T', 'wrs'], ['pM'])
            C.cp('act', lg[:], pM[:, 8:44], ['pM'], ['lg'])
            R = 'r1'
            S.add('dve', lambda e, s=s: e.tensor_reduce(out=s[:, 9:10], in_=lg[:, 0:4], axis=AX.X, op=ALU.max), r=['lg'], w=[f'mx{b}'])
            C.ts('dve', r1[:, 0:4], lg[:, 0:4], s[:, 9:10], None, ALU.is_equal, None, ['lg', f'mx{b}'], ['ohg'])
            C.ts('dve', s[:, 10:11], s[:, 9:10], -1.0, None, ALU.mult, None, [f'mx{b}'], [f'nmx{b}'])
            C.act(r1[:, 4:8], lg[:, 0:4], AF.Exp, ['lg', f'nmx{b}'], ['ec', f'sc{b}'], bias=s[:, 10:11], accum=s[:, 11:12])
            C.recip(s[:, 12:13], s[:, 11:12], [f'sc{b}'], [f'wgrp{b}'])
            C.ts('dve', r1[:, 8:12], r1[:, 0:4], -1.0, 1e30, ALU.add, ALU.mult, ['ohg'], ['pen'])
            for gq in range(4):
                C.ts('dve', r1[:, 16 + gq * 8:24 + gq * 8], lg[:, 4 + gq * 8:12 + gq * 8], r1[:, 8 + gq:9 + gq], None, ALU.add, None,
                     ['lg', 'pen'], [('msk', gq)])
            mk = [('msk', gq) for gq in range(4)]
            S.add('dve', lambda e: e.max(out=r1[:, 48:56], in_=r1[:, 16:48]), r=mk, w=['top8'])
            C.ts('dve', r1[:, 56:88], r1[:, 16:48], r1[:, 48:49], None, ALU.is_equal, None, mk + ['top8'], ['oh1'])
            C.ts('dve', r1[:, 88:120], r1[:, 16:48], r1[:, 49:50], None, ALU.is_equal, None, mk + ['top8'], ['oh2'])
            C.tt('dve', s[:, 13:14], r1[:, 49:50], r1[:, 48:49], ALU.subtract, ['top8'], [f'dd{b}'])
            C.act(s[:, 14:15], s[:, 13:14], AF.Exp, [f'dd{b}'], [f'ee{b}'])
            C.ts('dve', s[:, 15:16], s[:, 14:15], 1.0, None, ALU.add, None, [f'ee{b}'], [f'den{b}'])
            C.recip(s[:, 15:16], s[:, 15:16], [f'den{b}'], [f'den{b}'])
            C.tt('dve', gates[:, i, 0:1], s[:, 15:16], s[:, 12:13], ALU.mult, [f'den{b}', f'wgrp{b}'], [('g1', i)])
            C.tt('dve', gates[:, i, 1:2], gates[:, i, 0:1], s[:, 14:15], ALU.mult, [('g1', i), f'ee{b}'], [('g2', i)])
            C.tt('dve', r1[:, 120:152], r1[:, 56:88], r1[:, 88:120], ALU.add, ['oh1', 'oh2'], ['Mf'])
            C.cp('dve', Mb[:], r1[:, 120:152], ['Mf'], ['Mb'])
            C.mm(pM[:, 64:96], ustr[:], Mb[:], True, i == 0, ['ustr', 'Mb'], ['pM'])
            if i > 0:
                C.mm(pM[:, 64:96], ones128[:], Maccb[:], False, True, ['ones128', 'Maccb'], ['pM'])
            C.tt('pool', Macc[:], Macc[:], r1[:, 120:152], ALU.add, ['Macc', 'Mf'], ['Macc'])
            C.cp('pool', Maccb[:], Macc[:], ['Macc'], ['Maccb'])
            SLT = r1[:, 152:184]
            OKM = r1[:, 184:216]
            C.tt('dve', SLT, pM[:, 64:96], basef[:], ALU.add, ['pM', 'basef'], ['slot'])
            C.ts('dve', OKM, pM[:, 64:96], float(CAP), None, ALU.is_lt, None, ['pM'], ['okm'])
            C.ts('dve', SLT, SLT, -TRASH, None, ALU.add, None, ['slot'], ['slot'])
            C.tt('dve', SLT, SLT, OKM, ALU.mult, ['slot', 'okm'], ['slot'])
            C.ts('dve', SLT, SLT, TRASH, None, ALU.add, None, ['slot'], ['slot'])
            C.tt('dve', r1[:, 56:88], r1[:, 56:88], SLT, ALU.mult, ['oh1', 'slot'], ['oh1'])
            C.tt('dve', r1[:, 88:120], r1[:, 88:120], SLT, ALU.mult, ['oh2', 'slot'], ['oh2'])
            S.add('dve', lambda e: e.reduce_sum(out=dstf[:, 0:1], in_=r1[:, 56:88], axis=AX.X), r=['oh1'], w=['dstf0'])
            S.add('dve', lambda e: e.reduce_sum(out=dstf[:, 1:2], in_=r1[:, 88:120], axis=AX.X), r=['oh2'], w=['dstf1'])
            C.cp('dve', dsti[:, 2 * i:2 * i + 2], dstf[:], ['dstf0', 'dstf1'], [('dsti', i)])
            for k2 in range(2 if stage >= 2 else 0):
                S.add('pool', lambda e, i=i, k2=k2, b=b: e.indirect_dma_start(
                    out=Xs[:, :], out_offset=bass.IndirectOffsetOnAxis(ap=dsti[:, 2 * i + k2:2 * i + k2 + 1], axis=0),
                    in_=h2b[b][:], in_offset=None, oob_is_err=False),
                    r=[f'h2b{b}', ('dsti', i)], cw=['Xs'], tag=f'sc{b}{k2}')

        S.add('dve', lambda e: e.memset(junk[:, 0:8], 0.0), r=['pTf'], w=['pTfg', 'pTfu'])
        def wloads(e_):
            b = e_ % 2
            C.dma('pool', wgb[b][:], wg[e_].rearrange("(c p) f -> p c f", p=128), [], [f'wgb{b}'], f'wg{b}')
            C.dma('pool', wub[b][:], wu[e_].rearrange("(c p) f -> p c f", p=128), [], [f'wub{b}'], f'wu{b}')
            C.dma('pool', wdb[b][:], wd[e_].rearrange("(c p) f -> p c f", p=128), [], [f'wdb{b}'], f'wd{b}')

        def xloads(e_):
            b = e_ % 2
            C.dma('sp', xe[b][:], Xs[e_ * CAP:(e_ + 1) * CAP, :].rearrange("(t p) d -> p t d", p=128), ['Xs'], [f'xe{b}'], f'xe{b}')

        if stage >= 3:
            wloads(0)
            xloads(0)
        dk = 0
        for e_ in range(32 if stage >= 3 else 0):
            b = e_ % 2
            if e_ + 1 < 32:
                wloads(e_ + 1)
                xloads(e_ + 1)
            for stt_ in range(NST):
                for c in range(8):
                    C.tr(pTb[:, c * 128:(c + 1) * 128], xe[b][:, stt_, c * 128:(c + 1) * 128], identb[:], [f'xe{b}', 'identb'], ['pTb'])
                C.cp('act' if stt_ % 2 else 'dve', xeT[:, :, stt_ * 128:(stt_ + 1) * 128], pTb[:].rearrange("p (c t) -> p c t", c=8),
                     ['pTb'], [('xeT', stt_)])
            xk = [('xeT', t_) for t_ in range(NST)]
            for fc in range(4):
                for (a0, a1) in ((0, 512), (512, CAP)):
                    for c in range(8):
                        C.mm(pTf[:, a0:a1], wgb[b][:, c, fc * 128:(fc + 1) * 128], xeT[:, c, a0:a1], c == 0, c == 7, [f'wgb{b}'] + xk, ['pTfg'])
                for (a0, a1) in ((0, 512), (512, CAP)):
                    for c in range(8):
                        C.mm(pUU[:, a0:a1], wub[b][:, c, fc * 128:(fc + 1) * 128], xeT[:, c, a0:a1], c == 0, c == 7, [f'wub{b}'] + xk, ['pTfu'])
                C.act(sgl[fc % 2][:], pTf[:, 0:CAP], AF.Silu, ['pTfg'], [f'sgl{fc % 2}'])
                C.tt('dve', aT[:, fc, :], pUU[:, 0:CAP], sgl[fc % 2][:], ALU.mult, ['pTfu', f'sgl{fc % 2}'], [('aT', fc)])
            ak = [('aT', fc) for fc in range(4)]
            for stt_ in range(NST):
                for half in range(2):
                    bk = dk % 2
                    dk += 1
                    for fc in range(4):
                        C.mm(pP[bk][:], aT[:, fc, stt_ * 128:(stt_ + 1) * 128], wdb[b][:, fc, half * 512:(half + 1) * 512],
                             fc == 0, fc == 3, ak + [f'wdb{b}'], [f'pP{bk}'])
                    C.cp('act' if dk % 2 else 'dve', yo[b][:, stt_, half * 512:(half + 1) * 512], pP[bk][:], [f'pP{bk}'],
                         [('yo', b, stt_, half)])
            yk = [('yo', b, t_, h_) for t_ in range(NST) for h_ in range(2)]
            C.dma('sp', Ys[e_ * CAP:(e_ + 1) * CAP, :].rearrange("(t p) d -> p t d", p=128), yo[b][:], yk, [], f'yo{b}', cw=['Ys'])

        def gloads(i):
            b = i % 2
            C.dma('sp', xt[b][:], xmv[i], ['xmid'], [f'xt{b}'], f'lx{b}')
            for k2 in range(2 if stage >= 4 else 0):
                S.add('pool', lambda e, i=i, k2=k2, b=b: e.indirect_dma_start(
                    out=yg[b][k2][:], out_offset=None, in_=Ys[:, :],
                    in_offset=bass.IndirectOffsetOnAxis(ap=dsti[:, 2 * i + k2:2 * i + k2 + 1], axis=0),
                    oob_is_err=False),
                    r=['Ys', ('dsti', i)], w=[f'yg{b}{k2}'], tag=f'gy{b}{k2}')

        gloads(0)
        for i in range(NTT):
            b = i % 2
            if i + 1 < NTT:
                gloads(i + 1)
            if stage < 4:
                C.dma('sp', xov[i], xt[b][:], [f'xt{b}'], [], f'so{b}')
                continue
            C.stt(x1[b][:], yg[b][0][:], gates[:, i, 0:1], xt[b][:], ALU.mult, ALU.add,
                  [f'yg{b}0', ('g1', i), f'xt{b}'], [('x1', b, 0), ('x1', b, 1)])
            C.stt(x1[b][:], yg[b][1][:], gates[:, i, 1:2], x1[b][:], ALU.mult, ALU.add,
                  [f'yg{b}1', ('g2', i), ('x1', b, 0), ('x1', b, 1)], [('x1', b, 0), ('x1', b, 1)])
            C.dma('sp', xov[i], x1[b][:], [('x1', b, 0), ('x1', b, 1)], [], f'so{b}', cw=['xout'])
        if post is not None:
            post(C)
        info = S.emit()
    return info


STD_OF_YG = [0, 2, 1, 3, 4, 5, 6, 7]


def prep_ffn_inputs(inp, l):
    f = np.float32
    wr = np.ascontiguousarray(np.concatenate([inp['router_coarse'][l], inp['router_fine'][l]], axis=1), dtype=f)
    wout = np.asarray(inp['w_out'][l], dtype=f).reshape(8, 128, 1024)[STD_OF_YG].reshape(1024, 1024)
    gout = np.asarray(inp['out_norm_g'][l], dtype=f).reshape(8, 128)[STD_OF_YG].T
    return dict(
        wout=np.ascontiguousarray(wout),
        gout=np.ascontiguousarray(gout),
        g2bd=np.ascontiguousarray(np.broadcast_to(np.asarray(inp['norm2_g'][l])[None, :], (128, 1024)), dtype=f),
        wr=wr,
        wg=np.ascontiguousarray(inp['exp_w_gate'][l], dtype=f),
        wu=np.ascontiguousarray(inp['exp_w_up'][l], dtype=f),
        wd=np.ascontiguousarray(inp['exp_w_down'][l], dtype=f),
    )


MIX_KEYS = dict(wall=[1024, NW], vec=[128, NV], g1=[128, 8], wab=[128, 2, 128], pww=[128, 2, 128])
FFN_KEYS = dict(wout=[1024, 1024], gout=[128, 8], g2bd=[128, 1024], wr=[1024, 36], wg=[32, 1024, 512],
                wu=[32, 1024, 512], wd=[32, 512, 1024])
RG_PAIRS = [[0, 1], [2, 3], [4, 5], [6, 7]]


def build_fused(SL, depth=2):
    NT = SL // 2
    nc = bass.Bass("TRN2", target_bir_lowering=False)

    def din(name, shape, dt=F32):
        return nc.dram_tensor(name, shape, dt, kind="ExternalInput").ap()

    x_full = din("x_full", [SL, 1024])
    xin0 = din("xin0", [NT, 1024])
    msk = din("msk", [128, 2])
    W = []
    for l in range(depth):
        d = {k: din(f"{k}{l}", shp) for k, shp in MIX_KEYS.items()}
        d.update({k: din(f"{k}{l}", shp) for k, shp in FFN_KEYS.items()})
        W.append(d)
    xout = nc.dram_tensor("xout", [NT, 1024], F32, kind="ExternalOutput").ap()
    yT = nc.dram_tensor("yT_i", [512, SL], BF16, kind="Internal").ap()
    yG = nc.dram_tensor("yG_i", [1024, SL], BF16, kind="Internal").ap()
    Xs = nc.dram_tensor("Xs_i", [NROWS, 1024], BF16, kind="Internal").ap()
    Ys = nc.dram_tensor("Ys_i", [NROWS, 1024], BF16, kind="Internal").ap()
    xmid = nc.dram_tensor("xmid_i", [NT, 1024], F32, kind="Internal").ap()
    xo = nc.dram_tensor("xo_i", [NT, 1024], F32, kind="Internal").ap()
    xG = nc.dram_tensor("xG_i", [SL, 1024], F32, kind="Internal").ap()
    with ExitStack() as outer:
        gs = GSync(nc, outer)
        for l in range(depth):
            last = l == depth - 1
            w = W[l]

            def post_m(C):
                for k in range(4):
                    C.S.add('pool', lambda e, k=k: e.collective_compute(
                        "AllGather", ALU.bypass, replica_groups=RG_PAIRS,
                        ins=[yT[k * 128:(k + 1) * 128, :]], outs=[yG[k * 256:(k + 1) * 256, :]]),
                        r=['yT'], cw=['yG'], tag='cc', inc=1)

            def xtile(n):
                p = n * 128
                r_, q = p // NT, p % NT
                row = (q // 512) * 1024 + r_ * 512 + q % 512
                return xG[row:row + 128, :]

            build_mixer(nc, gs, SL, x_full if l == 0 else xG, w['wall'], w['vec'], w['g1'], w['wab'], w['pww'], yT, post=post_m,
                        xtile=None if l == 0 else xtile)
            nc.all_engine_barrier()

            def post_f(C, last=last):
                if not last:
                    for k in range(NT // 512):
                        C.S.add('pool', lambda e, k=k: e.collective_compute(
                            "AllGather", ALU.bypass, replica_groups=RG_PAIRS,
                            ins=[xo[k * 512:(k + 1) * 512, :]], outs=[xG[k * 1024:(k + 1) * 1024, :]]),
                            r=['xout'], cw=['xG'], tag='cc', inc=1)

            build_ffn(nc, gs, NT, xin0 if l == 0 else xo, yG, msk, w['wout'], w['gout'], w['g2bd'], w['wr'], w['wg'], w['wu'],
                      w['wd'], xout if last else xo, Xs, Ys, xmid, post=post_f)
            if not last:
                nc.all_engine_barrier()
    return nc


_NC_CACHE = {}


def run_fused(inp, X):
    B, SL, D = X.shape
    NT = SL // 2
    depth = np.asarray(inp['w_in']).shape[0]
    key = (SL, depth)
    if key not in _NC_CACHE:
        _NC_CACHE[key] = build_fused(SL, depth)
    nc = _NC_CACHE[key]
    fw = [prep_ffn_inputs(inp, l) for l in range(depth)]
    maps = []
    for c in range(8):
        b, h = c // 2, c % 2
        m = {'x_full': np.ascontiguousarray(X[b]), 'xin0': np.ascontiguousarray(X[b, h * NT:(h + 1) * NT])}
        mk = np.zeros((128, 2), np.float32)
        mk[:, h] = 1.0
        m['msk'] = mk
        for l in range(depth):
            for k, v in prep_mixer_inputs(inp, l, b, h).items():
                m[f'{k}{l}'] = v
            for k, v in fw[l].items():
                m[f'{k}{l}'] = v
        maps.append(m)
    res = run_bass_kernel_spmd(nc, maps, core_ids=list(range(8)))
    out = np.empty_like(X)
    for c in range(8):
        b, h = c // 2, c % 2
        out[b, h * NT:(h + 1) * NT] = np.asarray(res.results[c]['xout'])
    return out


def kernel(**inp):
    X = np.ascontiguousarray(np.asarray(inp['x'], dtype=np.float32))
    return run_fused(inp, X)
```

```python
import numpy as np
import ml_dtypes
from contextlib import ExitStack
import concourse.bass as bass
import concourse.mybir as mybir
from concourse.bass_utils import run_bass_kernel_spmd

F32 = mybir.dt.float32
BF16 = mybir.dt.bfloat16
I32 = mybir.dt.int32
AF = mybir.ActivationFunctionType
ALU = mybir.AluOpType
AX = mybir.AxisListType

ENG_ATTR = {'pe': 'tensor', 'act': 'scalar', 'dve': 'vector', 'pool': 'gpsimd', 'sp': 'sync'}
EPS = 1e-6


class GSync:
    def __init__(self, nc, stack):
        self.nc = nc
        self.stack = stack
        self.sems = {}
        self.cnts = {}
        self.tagmap = {}

    def sem(self, name):
        if name not in self.sems:
            self.sems[name] = self.stack.enter_context(self.nc.semaphore(name))
        return self.sems[name]

    def tagsem(self, tag):
        if tag not in self.tagmap:
            self.tagmap[tag] = 't_' + tag
        return self.tagmap[tag]


class Sched:
    def __init__(self, nc, stack, gs=None):
        self.nc = nc
        self.stack = stack
        self.gs = gs if gs is not None else GSync(nc, stack)
        self.ops = []
        self.wx = {}
        self.wc = {}
        self.rd = {}
        self.tag_last = {}
        self.sems = {}

    @staticmethod
    def _merge(dst, src):
        for s, i in src.items():
            if dst.get(s, -1) < i:
                dst[s] = i

    def add(self, eng, fn, r=(), w=(), cw=(), tag=None, inc=16):
        deps = {}
        for k in r:
            self._merge(deps, self.wx.get(k, {}))
            self._merge(deps, self.wc.get(k, {}))
        for k in w:
            self._merge(deps, self.wx.get(k, {}))
            self._merge(deps, self.wc.get(k, {}))
            self._merge(deps, self.rd.get(k, {}))
        for k in cw:
            self._merge(deps, self.wx.get(k, {}))
            self._merge(deps, self.rd.get(k, {}))
        if tag is not None and tag in self.tag_last:
            self._merge(deps, {('t', tag): self.tag_last[tag]})
        idx = len(self.ops)
        if eng == 'pe':
            deps.pop(('e', 'pe'), None)
        self.ops.append(dict(eng=eng, fn=fn, deps=deps, tag=tag, sem=None, cnt=0, inc=inc))
        sig = ('t', tag) if tag else ('e', eng)
        for k in r:
            self.rd.setdefault(k, {})[sig] = idx
        for k in w:
            self.wx[k] = {sig: idx}
            self.wc[k] = {}
            self.rd[k] = {}
        for k in cw:
            self.wc.setdefault(k, {})[sig] = idx
        if tag is not None:
            self.tag_last[tag] = idx
        return idx

    def _sem(self, name):
        return self.gs.sem(name)

    def emit(self):
        ops = self.ops
        need = set()
        for o in ops:
            for i in o['deps'].values():
                need.add(i)
        cnts = self.gs.cnts
        for i, o in enumerate(ops):
            if o['tag']:
                o['sem'] = self.gs.tagsem(o['tag'])
                cnts[o['sem']] = cnts.get(o['sem'], 0) + o['inc']
                o['cnt'] = cnts[o['sem']]
            elif i in need:
                o['sem'] = 'e_' + o['eng']
                cnts[o['sem']] = cnts.get(o['sem'], 0) + 1
                o['cnt'] = cnts[o['sem']]
        final = {}
        for o in ops:
            if o['sem']:
                final[o['sem']] = max(final.get(o['sem'], 0), o['cnt'])
        for s in final:
            self._sem(s)
        with self.nc.Block() as blk:
            for eng, attr in ENG_ATTR.items():
                mine = [o for o in ops if o['eng'] == eng]

                def body(e, mine=mine, eng=eng):
                    waited = {}
                    for o in mine:
                        for di in sorted(o['deps'].values()):
                            d = ops[di]
                            if waited.get(d['sem'], 0) < d['cnt']:
                                e.wait_ge(self._sem(d['sem']), d['cnt'])
                                waited[d['sem']] = d['cnt']
                        ins = o['fn'](e)
                        if o['sem']:
                            ins.then_inc(self._sem(o['sem']), o['inc'] if o['tag'] else 1)
                    if eng == 'sp':
                        for s, c in final.items():
                            if waited.get(s, 0) < c:
                                e.wait_ge(self._sem(s), c)

                getattr(blk, attr)(body)
        return dict(nops=len(ops), final=final)


class Ctx:
    def __init__(self, nc, st, gs=None):
        self.nc = nc
        self.st = st
        self.S = Sched(nc, st, gs)
        g = self.S.gs
        g.phase = getattr(g, 'phase', 0) + 1
        self.pfx = f"p{g.phase}_"

    def sb(self, name, shape, dt):
        return self.st.enter_context(self.nc.sbuf_tensor(self.pfx + name, shape, dt))

    def ps(self, name, shape, dt):
        return self.st.enter_context(self.nc.psum_tensor(self.pfx + name, shape, dt))

    def dma(self, q, out, in_, r, w, tag, cw=()):
        self.S.add(q, lambda e: e.dma_start(out=out, in_=in_), r=r, w=w, cw=cw, tag=tag)

    def mm(self, out, lhsT, rhs, start, stop, r, w):
        self.S.add('pe', lambda e: e.matmul(out, lhsT=lhsT, rhs=rhs, start=start, stop=stop,
                                            skip_group_check=True), r=r, w=w)

    def tr(self, out, in_, ident, r, w):
        self.S.add('pe', lambda e: e.transpose(out, in_, ident), r=r, w=w)

    def act(self, out, in_, func, r, w, bias=None, scale=None, accum=None):
        kw = {}
        if bias is not None:
            kw['bias'] = bias
        if scale is not None:
            kw['scale'] = scale
        if accum is not None:
            kw['accum_out'] = accum
        self.S.add('act', lambda e: e.activation(out=out, in_=in_, func=func, **kw), r=r, w=w)

    def ts(self, eng, out, in0, s1, s2, op0, op1, r, w):
        if op1 is None:
            self.S.add(eng, lambda e: e.tensor_scalar(out=out, in0=in0, scalar1=s1, scalar2=None, op0=op0), r=r, w=w)
        else:
            self.S.add(eng, lambda e: e.tensor_scalar(out=out, in0=in0, scalar1=s1, scalar2=s2, op0=op0, op1=op1), r=r, w=w)

    def tt(self, eng, out, in0, in1, op, r, w):
        self.S.add(eng, lambda e: e.tensor_tensor(out=out, in0=in0, in1=in1, op=op), r=r, w=w)

    def stt(self, out, in0, scalar, in1, op0, op1, r, w):
        self.S.add('dve', lambda e: e.scalar_tensor_tensor(out=out, in0=in0, scalar=scalar, in1=in1, op0=op0, op1=op1), r=r, w=w)

    def cp(self, eng, out, in_, r, w):
        if eng == 'act':
            self.S.add('act', lambda e: e.copy(out=out, in_=in_), r=r, w=w)
        else:
            self.S.add(eng, lambda e: e.tensor_copy(out=out, in_=in_), r=r, w=w)

    def memset(self, eng, ap, val, w):
        self.S.add(eng, lambda e: e.memset(ap, val), w=w)

    def recip(self, out, in_, r, w):
        self.S.add('dve', lambda e: e.reciprocal(out=out, in_=in_), r=r, w=w)

    def aselect(self, out, in_, pattern, op, fill, base, cm, r, w):
        self.S.add('pool', lambda e: e.affine_select(out=out, in_=in_, pattern=pattern, compare_op=op, fill=fill,
                                                     base=base, channel_multiplier=cm), r=r, w=w)


V_GQ, V_GK, V_LCW, V_LCB, V_BA, V_BX, V_LAM = 0, 1, 2, 6, 7, 8, 9
V_DWB, V_LNG, V_LNB, V_PWB, V_DWW = 10, 12, 14, 16, 17
NV = 17 + 62
NW = 1536


def build_mixer(nc, gs, SL, x, wall, vec, g1, wab, pww, yT, post=None, xtile=None):
    NG = SL // 512
    with ExitStack() as st:
        C = Ctx(nc, st, gs)
        S = C.S
        sb, ps = C.sb, C.ps
        Wb = sb("Wb", [128, 8, NW], BF16)
        wst = [sb(f"wst{i}", [128, 384], F32) for i in range(2)]
        vecs = sb("vecs", [128, NV], F32)
        g1s = sb("g1s", [128, 8], F32)
        wabf = sb("wabf", [128, 2, 128], F32)
        wabb = sb("wabb", [128, 2, 128], BF16)
        pwf = sb("pwf", [128, 2, 128], F32)
        pwb = sb("pwb", [128, 2, 128], BF16)
        identb = sb("identb", [128, 128], BF16)
        blk1 = sb("blk1", [128, 128], BF16)
        onesm = sb("onesm", [128, 128], F32)
        uincl = sb("uincl", [128, 128], BF16)
        negones = sb("negones", [128, 128], BF16)
        tri = sb("tri", [128, 128], BF16)
        dg = sb("dg", [128, 62, 128], BF16)
        cl = sb("cl", [128, 4], F32)
        KT = sb("KT", [128, 2, SL], BF16)
        Vt = sb("Vt", [128, SL // 128, 256], BF16)
        QT = sb("QT", [128, 2, 512], BF16)
        xt = [sb(f"xt{i}", [128, 1024], F32) for i in range(2)]
        xn = [sb(f"xn{i}", [128, 1024], BF16) for i in range(2)]
        st1 = [sb(f"st1_{i}", [128, 4], F32) for i in range(2)]
        hT = sb("hT", [128, 8, 512], BF16)
        sq = sb("sq", [128, 512], BF16)
        rs = sb("rs", [128, 512], F32)
        xrb = sb("xrb", [128, 515], F32)
        xgb = sb("xgb", [128, 512], F32)
        cv = sb("cv", [128, 512], F32)
        cvb = sb("cvb", [128, 512], BF16)
        lr = sb("lr", [128, 512], F32)
        li = sb("li", [128, 512], F32)
        la = sb("la", [128, 512], F32)
        la2 = sb("la2", [128, 512], F32)
        lu = sb("lu", [128, 512], F32)
        lh = sb("lh", [128, 512], F32)
        hprev = sb("hprev", [128, 1], F32)
        gt = sb("gt", [128, 512], F32)
        ybs = sb("ybs", [128, 512], BF16)
        ub = sb("ub", [128, 2, 542], BF16)
        sg = sb("sg", [128, 512], F32)
        cvo = sb("cvo", [128, 2, 512], F32)
        sqo = sb("sqo", [128, 2, 512], F32)
        means = sb("means", [128, 512], F32)
        m2 = sb("m2", [128, 512], F32)
        crs = sb("crs", [128, 512], F32)
        cn = sb("cn", [128, 2, 512], F32)
        csb = sb("csb", [128, 2, 512], BF16)
        ycs = sb("ycs", [128, 512], BF16)
        LA = 2
        NRB = LA + 2
        Eb = [sb(f"Eb{i}", [128, 512], BF16) for i in range(NRB)]
        Lb = [sb(f"Lb{i}", [128, 512], BF16) for i in range(NRB)]
        NWB = 3
        Wt = [sb(f"Wt{i}", [128, 512], BF16) for i in range(NWB)]
        LaccP = [sb(f"Lacc{i}", [128, 512], F32) for i in range(2)]
        xC = [sb(f"xC{i}", [128, 512], BF16) for i in range(2)]
        Laccb = [sb(f"Laccb{i}", [128, 512], BF16) for i in range(NRB)]
        yst = [sb(f"yst{i}", [64, 512], BF16) for i in range(2)]
        pA = [ps(f"pA{i}", [128, 512], F32) for i in range(2)]
        pT = ps("pT", [128, 1024], BF16)
        pZ = [ps(f"pZ{i}", [128, 512], F32) for i in range(2)]
        pC = [ps(f"pC{i}", [128, 512], F32) for i in range(2)]
        pO = ps("pO", [128, 512], F32)

        C.dma('sp', vecs[:], vec, [], ['vecs'], 'c0')
        C.dma('sp', g1s[:], g1, [], ['g1s'], 'c1')
        C.dma('sp', wabf[:], wab, [], ['wabf'], 'c2')
        C.dma('sp', pwf[:], pww, [], ['pwf'], 'c3')
        C.cp('dve', wabb[:], wabf[:], ['wabf'], ['wabb'])
        C.cp('dve', pwb[:], pwf[:], ['pwf'], ['pwb'])
        wv = wall.rearrange("(c p) n -> p c n", p=128)
        k = 0
        for c in range(8):
            for hf in range(4):
                b = k % 2
                C.dma('sp', wst[b][:], wv[:, c, hf * 384:(hf + 1) * 384], [], [f'wst{b}'], f'wst{b}')
                C.ts('dve' if k % 2 == 0 else 'pool', Wb[:, c, hf * 384:(hf + 1) * 384], wst[b][:], g1s[:, c:c + 1], None,
                     ALU.mult, None, [f'wst{b}', 'g1s'], [('Wb', c, hf)])
                k += 1
        WbK = [('Wb', c, hf) for c in range(8) for hf in range(4)]
        C.memset('pool', identb[:], 0.0, ['identb'])
        C.aselect(identb[:], identb[:], [[-1, 128]], ALU.not_equal, 1.0, 0, 1, ['identb'], ['identb'])
        C.memset('pool', blk1[:], 0.0, ['blk1'])
        C.memset('pool', blk1[0:64, 0:64], 1.0, ['blk1'])
        C.memset('pool', blk1[64:128, 64:128], 1.0, ['blk1'])
        C.memset('pool', onesm[:], 1.0 / 256.0, ['onesm'])
        C.memset('pool', negones[:], -1.0, ['negones'])
        C.memset('pool', uincl[:], -1.0, ['uincl'])
        C.aselect(uincl[:], uincl[:], [[-1, 128]], ALU.is_ge, 0.0, 0, 1, ['uincl'], ['uincl'])
        C.memset('pool', tri[:], 1.0, ['tri'])
        C.aselect(tri[:], tri[:], [[1, 128]], ALU.is_ge, 0.0, -1, -1, ['tri'], ['tri'])
        for i in range(62):
            C.ts('pool' if i % 2 else 'dve', dg[:, i, :], identb[:], vecs[:, V_DWW + i:V_DWW + i + 1], None, ALU.mult, None,
                 ['identb', 'vecs'], ['dg'])
        C.act(cl[:, 0:1], vecs[:, V_LAM:V_LAM + 1], AF.Exp, ['vecs'], ['cl0'], scale=-1.0)
        C.act(cl[:, 1:2], cl[:, 0:1], AF.Ln, ['cl0'], ['cl1'], bias=1.0)
        C.ts('dve', cl[:, 2:3], cl[:, 1:2], -8.0, None, ALU.mult, None, ['cl1'], ['cl2'])
        C.ts('dve', cl[:, 3:4], cl[:, 1:2], -16.0, None, ALU.mult, None, ['cl1'], ['cl3'])
        C.memset('pool', hprev[:], 0.0, ['hprev'])
        C.memset('pool', xrb[:, 0:3], 0.0, ['xrb_h'])
        C.memset('pool', ub[:, :, 0:30], 0.0, ['ub_h'])

        xv = x.rearrange("(n p) d -> n p d", p=128)
        wcnt = [0]
        ocnt = [0]
        acnt = [0]
        bcnt = [0]
        lcnt = [0]
        pcnt = [0]
        for g in range(NG):
            t0 = g * 512
            for tt in range(4):
                b = tt % 2
                C.dma('sp', xt[b][:], xv[g * 4 + tt] if xtile is None else xtile(g * 4 + tt), [], [f'xt{b}'], f'x{b}')
                C.act(xn[b][:], xt[b][:], AF.Square, [f'xt{b}'], [f'ss{b}', f'xn{b}'], accum=st1[b][:, 0:1])
                C.act(st1[b][:, 1:2], st1[b][:, 0:1], AF.Sqrt, [f'ss{b}'], [f'rt{b}'], scale=1.0 / 1024.0, bias=EPS)
                C.recip(st1[b][:, 2:3], st1[b][:, 1:2], [f'rt{b}'], [f'rstd{b}'])
                C.ts('dve', xn[b][:], xt[b][:], st1[b][:, 2:3], None, ALU.mult, None, [f'xt{b}', f'rstd{b}'], [f'xn{b}'])
                for c in range(8):
                    C.tr(pT[:, c * 128:(c + 1) * 128], xn[b][:, c * 128:(c + 1) * 128], identb[:], [f'xn{b}', 'identb'], ['pT'])
                C.cp('act' if tt % 2 else 'dve', hT[:, :, tt * 128:(tt + 1) * 128], pT[:].rearrange("p (c t) -> p c t", c=8),
                     ['pT'], [('hT', tt)])
            hTK = [('hT', tt) for tt in range(4)]
            pidx = [0]

            def proj(ct):
                bank = pidx[0] % 2
                pidx[0] += 1
                for c in range(8):
                    C.mm(pA[bank][:], Wb[:, c, ct * 128:(ct + 1) * 128], hT[:, c, :], c == 0, c == 7, WbK + hTK, [f'pA{bank}'])
                return bank

            for ct in range(4):
                bk = proj(ct)
                C.act(sq[:], pA[bk][:], AF.Square, [f'pA{bk}'], ['sq'])
                C.mm(pA[1 - bk][:], blk1[:], sq[:], True, True, ['blk1', 'sq'], [f'pA{1 - bk}'])
                pidx[0] += 1
                if ct < 2:
                    C.act(rs[:], pA[1 - bk][:], AF.Sqrt, [f'pA{1 - bk}'], ['rs'], scale=1.0, bias=64.0 * EPS)
                else:
                    C.act(rs[:], pA[1 - bk][:], AF.Sqrt, [f'pA{1 - bk}'], ['rs'], scale=1.0 / 64.0, bias=EPS)
                C.recip(rs[:], rs[:], ['rs'], ['rs'])
                if ct < 2:
                    C.stt(QT[:, ct, :], pA[bk][:], vecs[:, V_GQ:V_GQ + 1], rs[:], ALU.mult, ALU.mult,
                          [f'pA{bk}', 'vecs', 'rs'], [('QT', ct)])
                else:
                    C.stt(KT[:, ct - 2, t0:t0 + 512], pA[bk][:], vecs[:, V_GK:V_GK + 1], rs[:], ALU.mult, ALU.mult,
                          [f'pA{bk}', 'vecs', 'rs'], [('KT', ct - 2, g)])
            bk = proj(4)
            C.cp('act', xrb[:, 3:515], pA[bk][:], [f'pA{bk}'], ['xrb'])
            bk = proj(5)
            C.cp('dve', xgb[:], pA[bk][:], [f'pA{bk}'], ['xgb'])
            for t in range(2):
                bv = proj(6 + t)
                bg = proj(8 + t)
                C.act(sg[:], pA[bg][:], AF.Sigmoid, [f'pA{bg}'], ['sg'])
                C.tt('dve', ub[:, t, 30:542], pA[bv][:], sg[:], ALU.mult, [f'pA{bv}', 'sg'], [('ub', t)])
            for tt in range(4):
                bank = pidx[0] % 2
                pidx[0] += 1
                for c in range(8):
                    C.mm(pA[bank][:, 0:256], hT[:, c, tt * 128:(tt + 1) * 128], Wb[:, c, 1280:1536], c == 0, c == 7,
                         WbK + hTK, [f'pA{bank}'])
                C.cp('act' if tt % 2 else 'dve', Vt[:, g * 4 + tt, :], pA[bank][:, 0:256], [f'pA{bank}'], [('V', g * 4 + tt)])

            vc = lambda i: vecs[:, i:i + 1]
            C.ts('dve', cv[:], xrb[:, 3:515], vc(V_LCW + 3), vc(V_LCB), ALU.mult, ALU.add, ['xrb', 'xrb_h', 'vecs'], ['cv'])
            for kk in range(3):
                C.stt(cv[:], xrb[:, kk:kk + 512], vc(V_LCW + kk), cv[:], ALU.mult, ALU.add, ['xrb', 'xrb_h', 'vecs', 'cv'], ['cv'])
            C.cp('pool', xrb[:, 0:3], xrb[:, 512:515], ['xrb'], ['xrb_h'])
            C.cp('pool', cvb[:], cv[:], ['cv'], ['cvb'])
            b0 = pidx[0] % 2
            pidx[0] += 2
            C.mm(pA[b0][:], wabb[:, 0, :], cvb[:], True, True, ['wabb', 'cvb'], [f'pA{b0}'])
            C.mm(pA[1 - b0][:], wabb[:, 1, :], cvb[:], True, True, ['wabb', 'cvb'], [f'pA{1 - b0}'])
            C.act(lr[:], pA[b0][:], AF.Sigmoid, [f'pA{b0}', 'vecs'], ['lr'], bias=vc(V_BA))
            C.act(li[:], pA[1 - b0][:], AF.Sigmoid, [f'pA{1 - b0}', 'vecs'], ['li'], bias=vc(V_BX))
            C.act(la[:], lr[:], AF.Exp, ['lr', 'cl2'], ['la'], scale=cl[:, 2:3])
            C.act(la2[:], lr[:], AF.Exp, ['lr', 'cl3'], ['la2'], scale=cl[:, 3:4])
            C.act(la2[:], la2[:], AF.Sqrt, ['la2'], ['la2'], scale=-1.0, bias=1.0)
            C.tt('pool', lu[:], li[:], cv[:], ALU.mult, ['li', 'cv'], ['lu'])
            C.tt('pool', lu[:], lu[:], la2[:], ALU.mult, ['lu', 'la2'], ['lu'])
            S.add('dve', lambda e: e.tensor_tensor_scan(out=lh[:], data0=la[:], data1=lu[:], initial=hprev[:, 0:1],
                                                        op0=ALU.mult, op1=ALU.add), r=['la', 'lu', 'hprev'], w=['lh'])
            C.cp('pool', hprev[:], lh[:, 511:512], ['lh'], ['hprev'])
            C.tt('pool', gt[:], xgb[:], xgb[:], ALU.mult, ['xgb'], ['gt'])
            C.ts('pool', gt[:], gt[:], 0.044715, 1.0, ALU.mult, ALU.add, ['gt'], ['gt'])
            C.tt('pool', gt[:], gt[:], xgb[:], ALU.mult, ['gt', 'xgb'], ['gt'])
            C.act(gt[:], gt[:], AF.Sigmoid, ['gt'], ['gt'], scale=1.5957691216)
            C.tt('pool', gt[:], gt[:], xgb[:], ALU.mult, ['gt', 'xgb'], ['gt'])
            C.tt('dve', ybs[:], gt[:], lh[:], ALU.mult, ['gt', 'lh'], ['ybs'])
            C.dma('pool', yT[256:384, t0:t0 + 512], ybs[:], ['ybs'], [], 'yb', cw=['yT'])

            for t in range(2):
                bank = pidx[0] % 2
                pidx[0] += 1
                for kk in range(31):
                    C.mm(pA[bank][:], dg[:, t * 31 + kk, :], ub[:, t, kk:kk + 512], kk == 0, kk == 30,
                         ['dg', ('ub', t), 'ub_h'], [f'pA{bank}'])
                C.act(cvo[:, t, :], pA[bank][:], AF.Identity, [f'pA{bank}', 'vecs'], [('cvo', t)], bias=vc(V_DWB + t))
                C.act(sqo[:, t, :], pA[bank][:], AF.Square, [f'pA{bank}', 'vecs'], [('sqo', t)], bias=vc(V_DWB + t))
            C.cp('pool', ub[:, :, 0:30], ub[:, :, 512:542], [('ub', 0), ('ub', 1)], ['ub_h'])
            bm = pidx[0] % 2
            pidx[0] += 2
            for t in range(2):
                C.mm(pA[bm][:], onesm[:], cvo[:, t, :], t == 0, t == 1, ['onesm', ('cvo', t)], [f'pA{bm}'])
            for t in range(2):
                C.mm(pA[1 - bm][:], onesm[:], sqo[:, t, :], t == 0, t == 1, ['onesm', ('sqo', t)], [f'pA{1 - bm}'])
            C.cp('act', means[:], pA[bm][:], [f'pA{bm}'], ['means'])
            C.tt('pool', m2[:], means[:], means[:], ALU.mult, ['means'], ['m2'])
            C.tt('dve', m2[:], pA[1 - bm][:], m2[:], ALU.subtract, [f'pA{1 - bm}', 'm2'], ['m2'])
            C.act(crs[:], m2[:], AF.Sqrt, ['m2'], ['crs'], scale=1.0, bias=EPS)
            C.recip(crs[:], crs[:], ['crs'], ['crs'])
            for t in range(2):
                C.tt('pool', cn[:, t, :], cvo[:, t, :], means[:], ALU.subtract, [('cvo', t), 'means'], [('cn', t)])
                C.tt('dve' if t else 'pool', cn[:, t, :], cn[:, t, :], crs[:], ALU.mult, [('cn', t), 'crs'], [('cn', t)])
                C.act(csb[:, t, :], cn[:, t, :], AF.Silu, [('cn', t), 'vecs'], [('csb', t)], scale=vc(V_LNG + t), bias=vc(V_LNB + t))
            bank = pidx[0] % 2
            pidx[0] += 1
            for t in range(2):
                C.mm(pA[bank][:], pwb[:, t, :], csb[:, t, :], t == 0, t == 1, ['pwb', ('csb', t)], [f'pA{bank}'])
            C.act(ycs[:], pA[bank][:], AF.Identity, [f'pA{bank}', 'vecs'], ['ycs'], bias=vc(V_PWB))
            C.dma('pool', yT[384:512, t0:t0 + 512], ycs[:], ['ycs'], [], 'yc', cw=['yT'])

            items = []
            for hd in range(4):
                nblk = 4 * g + 4
                for bi, kb in enumerate(range(4 * g + 3, -1, -1)):
                    items.append(dict(hd=hd, bi=bi, kb=kb, nblk=nblk))

            def stageA(it):
                hd, bi, kb = it['hd'], it['bi'], it['kb']
                ct = hd // 2
                pb = 64 * (hd % 2)
                Qh = QT[pb:pb + 64, ct, :]
                j = kb - 4 * g
                c0 = 128 * j if j > 0 else 0
                Kh = KT[pb:pb + 64, ct, kb * 128:(kb + 1) * 128]
                kkey = ('KT', ct, kb // 4)
                zb = acnt[0] % 2
                lb = acnt[0] % NRB
                eb = acnt[0] % NRB
                acnt[0] += 1
                it.update(c0=c0, j=j, Kh=Kh, kkey=kkey, Qh=Qh, ct=ct, lb=lb, eb=eb)
                if bi == 0:
                    C.memset('pool', LaccP[0][:], 0.0, ['Lacc0'])
                    C.memset('pool', LaccP[1][:], 0.0, ['Lacc1'])
                    pcnt[0] = 0
                C.mm(pZ[zb][:, c0:], Kh, Qh[:, c0:], True, True, [kkey, ('QT', ct)], [f'pZ{zb}'])
                C.act(Eb[eb][:, c0:], pZ[zb][:, c0:], AF.Exp, [f'pZ{zb}'], [f'Eb{eb}'])
                C.act(Lb[lb][:, c0:], Eb[eb][:, c0:], AF.Ln, [f'Eb{eb}'], [f'Lb{lb}'], bias=1.0)
                if j >= 0:
                    C.tt('dve', Lb[lb][:, c0:c0 + 128], Lb[lb][:, c0:c0 + 128], tri[:], ALU.mult, [f'Lb{lb}', 'tri'], [f'Lb{lb}'])
                if kb > 0:
                    la_ = lcnt[0] % NRB
                    lcnt[0] += 1
                    pp = pcnt[0] % 2
                    pcnt[0] += 1
                    Lo, Ln_ = LaccP[pp], LaccP[1 - pp]
                    ko, kn = f'Lacc{pp}', f'Lacc{1 - pp}'
                    C.tt('pool', Ln_[:, c0:], Lo[:, c0:], Lb[lb][:, c0:], ALU.add, [ko, f'Lb{lb}'], [kn])
                    C.cp('dve', Laccb[la_][:], Ln_[:], [kn], [f'Laccb{la_}'])
                    it['la_out'] = la_

            def stageB(it, prev):
                hd, bi, kb, c0, j = it['hd'], it['bi'], it['kb'], it['c0'], it['j']
                Kh, kkey, Qh, ct, lb = it['Kh'], it['kkey'], it['Qh'], it['ct'], it['lb']
                cb = bcnt[0] % 2
                bcnt[0] += 1
                C.mm(pC[cb][:, c0:], uincl[:], Lb[lb][:, c0:], True, bi == 0, ['uincl', f'Lb{lb}'], [f'pC{cb}'])
                if bi > 0:
                    la_ = prev['la_out']
                    C.mm(pC[cb][:, c0:], negones[:], Laccb[la_][:, c0:], False, True, ['negones', f'Laccb{la_}'], [f'pC{cb}'])
                wi = wcnt[0] % NWB
                wcnt[0] += 1
                eb = it['eb']
                C.act(xC[cb][:, c0:], pC[cb][:, c0:], AF.Exp, [f'pC{cb}'], [f'xC{cb}'])
                C.tt('dve', Wt[wi][:, c0:], xC[cb][:, c0:], Eb[eb][:, c0:], ALU.mult, [f'xC{cb}', f'Eb{eb}'], [f'Wt{wi}'])
                if j >= 0:
                    C.tt('dve', Wt[wi][:, c0:c0 + 128], Wt[wi][:, c0:c0 + 128], tri[:], ALU.mult, [f'Wt{wi}', 'tri'], [f'Wt{wi}'])
                C.mm(pO[0:64, c0:], Vt[:, kb, hd * 64:(hd + 1) * 64], Wt[wi][:, c0:], bi == 0, bi == it['nblk'] - 1,
                     [('V', kb), f'Wt{wi}'], ['pO'])
                if bi == it['nblk'] - 1:
                    ob = ocnt[0] % 2
                    ocnt[0] += 1
                    C.cp('dve', yst[ob][:], pO[0:64, :], ['pO'], [f'yst{ob}'])
                    C.dma('pool', yT[hd * 64:(hd + 1) * 64, t0:t0 + 512], yst[ob][:], [f'yst{ob}'], [], f'ya{ob}', cw=['yT'])

            for n_ in range(min(LA, len(items))):
                stageA(items[n_])
            for n_ in range(len(items)):
                if n_ + LA < len(items):
                    stageA(items[n_ + LA])
                stageB(items[n_], items[n_ - 1] if n_ > 0 else None)
        if post is not None:
            post(C)
        info = S.emit()
    return info


def prep_mixer_inputs(inp, l, b, hg, SL=None):
    f = np.float32
    w_in = np.asarray(inp['w_in'][l])
    hs = slice(hg * 256, (hg + 1) * 256)
    q = w_in[:, 0:512][:, hs]
    k = w_in[:, 512:1024][:, hs]
    v = w_in[:, 1024:1536][:, hs]
    cs = slice(hg * 128, (hg + 1) * 128)
    xr = w_in[:, 1536:1792][:, cs]
    xg = w_in[:, 1792:2048][:, cs]
    cval = w_in[:, 2048:2304]
    cgate = w_in[:, 2304:2560]
    wall = np.ascontiguousarray(np.concatenate([q, k, xr, xg, cval, cgate, v], axis=1), dtype=f)
    vec = np.zeros((128, NV), f)
    vec[:, V_GQ] = np.tile(inp['q_norm_g'][l], 2)
    vec[:, V_GK] = np.tile(inp['k_norm_g'][l], 2)
    for kk in range(4):
        vec[:, V_LCW + kk] = inp['lru_conv_w'][l][kk, cs]
    vec[:, V_LCB] = inp['lru_conv_b'][l][cs]
    vec[:, V_BA] = inp['lru_ba'][l][cs]
    vec[:, V_BX] = inp['lru_bx'][l][cs]
    vec[:, V_LAM] = inp['lru_lambda'][l][cs]
    for t in range(2):
        ts_ = slice(t * 128, (t + 1) * 128)
        vec[:, V_DWB + t] = inp['conf_dw_b'][l][ts_]
        vec[:, V_LNG + t] = inp['conf_ln_g'][l][ts_]
        vec[:, V_LNB + t] = inp['conf_ln_b'][l][ts_]
        for kk in range(31):
            vec[:, V_DWW + t * 31 + kk] = inp['conf_dw_w'][l][kk, ts_]
    vec[:, V_PWB] = inp['conf_pw_b'][l][cs]
    g1 = np.ascontiguousarray(np.asarray(inp['norm1_g'][l]).reshape(8, 128).T, dtype=f)
    wab = np.zeros((128, 2, 128), f)
    for i in range(2):
        blk = hg * 2 + i
        wab[i * 64:(i + 1) * 64, 0, i * 64:(i + 1) * 64] = inp['lru_wa'][l][blk]
        wab[i * 64:(i + 1) * 64, 1, i * 64:(i + 1) * 64] = inp['lru_wx'][l][blk]
    pw = np.asarray(inp['conf_pw_w'][l])[:, cs]
    pww = np.ascontiguousarray(pw.reshape(2, 128, 128).transpose(1, 0, 2), dtype=f)
    return dict(wall=wall, vec=vec, g1=g1, wab=wab, pww=pww)


CAP = 640
NSLOT = 32 * CAP
NROWS = NSLOT + 128
TRASH = float(NSLOT)


def build_ffn(nc, gs, NT, xin, yG, msk, wout, gout, g2bd, wr, wg, wu, wd, xout, Xs, Ys, xmid, post=None):
    NTT = NT // 128
    stage = 4
    dbg = False
    with ExitStack() as st:
        C = Ctx(nc, st, gs)
        S = C.S
        sb, ps = C.sb, C.ps
        mks = sb("mks", [128, 2], F32)
        ysc = [[sb(f"ysc{i}_{k}", [128, 8, 128], BF16) for k in range(2)] for i in range(2)]
        Woutb = sb("Woutb", [128, 8, 1024], BF16)
        wstg = [sb(f"wstg{i}", [128, 1024], F32) for i in range(2)]
        gos = sb("gos", [128, 8], F32)
        g2b = sb("g2b", [128, 1024], F32)
        wrs = sb("wrs", [128, 8, 36], F32)
        identf = sb("identf", [128, 128], F32)
        identb = sb("identb", [128, 128], BF16)
        onec = sb("onec", [128, 2], BF16)
        ustr = sb("ustr", [128, 128], BF16)
        ones128 = sb("ones128", [128, 128], BF16)
        basef = sb("basef", [128, 32], F32)
        zt = sb("zt", [128, 4, 1024], BF16)
        gates = sb("gates", [128, NTT, 2], F32)
        dsti = sb("dsti", [128, NTT * 2], I32)
        Macc = sb("Macc", [128, 32], F32)
        Maccb = sb("Maccb", [128, 32], BF16)
        xt = [sb(f"xt{i}", [128, 1024], F32) for i in range(2)]
        ys = [sb(f"ys{i}", [128, 8, 128], BF16) for i in range(2)]
        ysq = sb("ysq", [128, 8, 128], BF16)
        x1 = [sb(f"x1_{i}", [128, 1024], F32) for i in range(2)]
        junk = sb("junk", [128, 1024], BF16)
        h2f = sb("h2f", [128, 1024], F32)
        h2b = [sb(f"h2b{i}", [128, 1024], BF16) for i in range(2)]
        h2T = sb("h2T", [128, 8, 128], F32)
        sm = [sb(f"sm{i}", [128, 16], F32) for i in range(2)]
        lg = sb("lg", [128, 36], F32)
        r1 = sb("r1", [128, 224], F32)
        Mb = sb("Mb", [128, 32], BF16)
        dstf = sb("dstf", [128, 2], F32)
        wgb = [sb(f"wgb{i}", [128, 8, 512], BF16) for i in range(2)]
        wub = [sb(f"wub{i}", [128, 8, 512], BF16) for i in range(2)]
        wdb = [sb(f"wdb{i}", [128, 4, 1024], BF16) for i in range(2)]
        NST = CAP // 128
        xe = [sb(f"xe{i}", [128, NST, 1024], BF16) for i in range(2)]
        xeT = sb("xeT", [128, 8, CAP], BF16)
        sgl = [sb(f"sgl{i}", [128, CAP], F32) for i in range(2)]
        aT = sb("aT", [128, 4, CAP], BF16)
        yo = [sb(f"yo{i}", [128, NST, 1024], BF16) for i in range(2)]
        yg = [[sb(f"yg{i}_{k}", [128, 1024], BF16) for k in range(2)] for i in range(2)]
        pM = ps("pM", [128, 512], F32)
        pP = [ps(f"pP{i}", [128, 512], F32) for i in range(2)]
        pTf = ps("pTf", [128, 1024], F32)
        pUU = ps("pUU", [128, 1024], F32)
        pTb = ps("pTb", [128, 1024], BF16)

        C.dma('sp', gos[:], gout, [], ['gos'], 'c0')
        C.dma('sp', mks[:], msk, [], ['mks'], 'c3')
        C.dma('sp', g2b[:], g2bd, [], ['g2b'], 'c1')
        C.dma('sp', wrs[:], wr.rearrange("(c p) n -> p c n", p=128), [], ['wrs'], 'c2')
        wov = wout.rearrange("(c p) n -> p c n", p=128)
        for c in range(8):
            b = c % 2
            C.dma('sp', wstg[b][:], wov[:, c, :], [], [f'wstg{b}'], f'wstg{b}')
            C.ts('dve' if b else 'pool', Woutb[:, c, :], wstg[b][:], gos[:, c:c + 1], None, ALU.mult, None,
                 [f'wstg{b}', 'gos'], [('Wo', c)])
        WoK = [('Wo', c) for c in range(8)]
        C.memset('pool', identf[:], 0.0, ['identf'])
        C.aselect(identf[:], identf[:], [[-1, 128]], ALU.not_equal, 1.0, 0, 1, ['identf'], ['identf'])
        C.cp('pool', identb[:], identf[:], ['identf'], ['identb'])
        C.memset('pool', onec[:], 1.0, ['onec'])
        C.memset('pool', ones128[:], 1.0, ['ones128'])
        C.memset('pool', ustr[:], 1.0, ['ustr'])
        C.aselect(ustr[:], ustr[:], [[1, 128]], ALU.is_ge, 0.0, -1, -1, ['ustr'], ['ustr'])
        S.add('pool', lambda e: e.iota(basef[:], pattern=[[CAP, 32]], base=0, channel_multiplier=0,
                                       allow_small_or_imprecise_dtypes=True), w=['basef'])
        C.memset('pool', zt[:], 0.0, ['zt'])
        C.memset('pool', Macc[:], 0.0, ['Macc'])
        C.memset('pool', Maccb[:], 0.0, ['Maccb'])
        Xv = Xs.rearrange("(n p) d -> p n d", p=128)
        nrt = NROWS // 128
        zi = 0
        for n0 in range(0, nrt, 4):
            n1 = min(nrt, n0 + 4)
            C.dma('sp', Xv[:, n0:n1, :], zt[:, 0:n1 - n0, :], ['zt'], [], f'z{zi % 2}', cw=['Xs'])
            zi += 1
        C.dma('sp', Ys[NSLOT:NROWS, :], zt[:, 0, :], ['zt'], [], 'zy', cw=['Ys'])
        S.add('sp', lambda e: e.nop(), r=[], w=['Xs'])

        xinv = xin.rearrange("(n p) d -> n p d", p=128)
        xmv = xmid.rearrange("(n p) d -> n p d", p=128)
        xov = xout.rearrange("(n p) d -> n p d", p=128)
        yv = yG.rearrange("(c p) t -> p c t", p=128)

        def loads(i):
            b = i % 2
            C.dma('sp', xt[b][:], xinv[i], [], [f'xt{b}'], f'lx{b}')
            for hh in range(2):
                C.dma('sp', ysc[b][hh][:], yv[:, :, hh * NT + i * 128:hh * NT + (i + 1) * 128], [], [f'ysc{b}{hh}'], f'ly{b}{hh}')
            C.ts('dve', ys[b][:], ysc[b][0][:], mks[:, 0:1], None, ALU.mult, None, [f'ysc{b}0', 'mks'], [f'ys{b}'])
            C.stt(ys[b][:], ysc[b][1][:], mks[:, 1:2], ys[b][:], ALU.mult, ALU.add, [f'ysc{b}1', 'mks', f'ys{b}'], [f'ys{b}'])

        loads(0)
        GRP = [([0, 1, 2, 3], 512.0), ([4, 5], 256.0), ([6, 7], 256.0)]
        for i in range(NTT):
            b = i % 2
            if i + 1 < NTT:
                loads(i + 1)
            s = sm[b]
            C.act(ysq[:], ys[b][:], AF.Square, [f'ys{b}'], ['ysq'])
            for gi, (cl_, n) in enumerate(GRP):
                for c in cl_:
                    C.mm(pM[:, gi:gi + 1], ysq[:, c, :], onec[:, 0:1], c == cl_[0], c == cl_[-1], ['ysq', 'onec'], ['pM'])
            C.act(s[:, 0:1], pM[:, 0:1], AF.Sqrt, ['pM'], [f's0{b}'], scale=1.0 / 512.0, bias=EPS)
            C.act(s[:, 1:3], pM[:, 1:3], AF.Sqrt, ['pM'], [f's1{b}'], scale=1.0 / 256.0, bias=EPS)
            C.recip(s[:, 3:6], s[:, 0:3], [f's0{b}', f's1{b}'], [f'rg{b}'])
            k = 0
            for half in range(2):
                for gi, (cl_, n) in enumerate(GRP):
                    bk = k % 2
                    k += 1
                    for c in cl_:
                        C.mm(pP[bk][:], ys[b][:, c, :], Woutb[:, c, half * 512:(half + 1) * 512], c == cl_[0], c == cl_[-1],
                             [f'ys{b}'] + WoK, [f'pP{bk}'])
                    src = xt[b] if gi == 0 else x1[b]
                    C.stt(x1[b][:, half * 512:(half + 1) * 512], pP[bk][:], s[:, 3 + gi:4 + gi], src[:, half * 512:(half + 1) * 512],
                          ALU.mult, ALU.add, [f'pP{bk}', f'rg{b}', f'xt{b}', ('x1', b, half)], [('x1', b, half)])
            x1k = [('x1', b, 0), ('x1', b, 1)]
            C.dma('sp', xmv[i], x1[b][:], x1k, [], f'sx{b}', cw=['xmid'])
            C.act(junk[:], x1[b][:], AF.Square, x1k, [f'ss{b}'], accum=s[:, 6:7])
            C.act(s[:, 7:8], s[:, 6:7], AF.Sqrt, [f'ss{b}'], [f'rt{b}'], scale=1.0 / 1024.0, bias=EPS)
            C.recip(s[:, 8:9], s[:, 7:8], [f'rt{b}'], [f'r2{b}'])
            C.stt(h2f[:], x1[b][:], s[:, 8:9], g2b[:], ALU.mult, ALU.mult, x1k + [f'r2{b}', 'g2b'], ['h2f'])
            C.cp('act', h2b[b][:], h2f[:], ['h2f'], [f'h2b{b}'])
            for c in range(8):
                C.tr(pTf[:, c * 128:(c + 1) * 128], h2f[:, c * 128:(c + 1) * 128], identf[:], ['h2f', 'identf'], ['pTf'])
            C.cp('dve', h2T[:].rearrange("p c t -> p (c t)"), pTf[:], ['pTf'], ['h2T'])
            for c in range(8):
                C.mm(pM[:, 8:44], h2T[:, c, :], wrs[:, c, :], c == 0, c == 7, ['h2T', 'wrs'], ['pM'])
            C.cp('act', lg[:], pM[:, 8:44], ['pM'], ['lg'])
            R = 'r1'
            S.add('dve', lambda e, s=s: e.tensor_reduce(out=s[:, 9:10], in_=lg[:, 0:4], axis=AX.X, op=ALU.max), r=['lg'], w=[f'mx{b}'])
            C.ts('dve', r1[:, 0:4], lg[:, 0:4], s[:, 9:10], None, ALU.is_equal, None, ['lg', f'mx{b}'], ['ohg'])
            C.ts('dve', s[:, 10:11], s[:, 9:10], -1.0, None, ALU.mult, None, [f'mx{b}'], [f'nmx{b}'])
            C.act(r1[:, 4:8], lg[:, 0:4], AF.Exp, ['lg', f'nmx{b}'], ['ec', f'sc{b}'], bias=s[:, 10:11], accum=s[:, 11:12])
            C.recip(s[:, 12:13], s[:, 11:12], [f'sc{b}'], [f'wgrp{b}'])
            C.ts('dve', r1[:, 8:12], r1[:, 0:4], -1.0, 1e30, ALU.add, ALU.mult, ['ohg'], ['pen'])
            for gq in range(4):
                C.ts('dve', r1[:, 16 + gq * 8:24 + gq * 8], lg[:, 4 + gq * 8:12 + gq * 8], r1[:, 8 + gq:9 + gq], None, ALU.add, None,
                     ['lg', 'pen'], [('msk', gq)])
            mk = [('msk', gq) for gq in range(4)]
            S.add('dve', lambda e: e.max(out=r1[:, 48:56], in_=r1[:, 16:48]), r=mk, w=['top8'])
            C.ts('dve', r1[:, 56:88], r1[:, 16:48], r1[:, 48:49], None, ALU.is_equal, None, mk + ['top8'], ['oh1'])
            C.ts('dve', r1[:, 88:120], r1[:, 16:48], r1[:, 49:50], None, ALU.is_equal, None, mk + ['top8'], ['oh2'])
            C.tt('dve', s[:, 13:14], r1[:, 49:50], r1[:, 48:49], ALU.subtract, ['top8'], [f'dd{b}'])
            C.act(s[:, 14:15], s[:, 13:14], AF.Exp, [f'dd{b}'], [f'ee{b}'])
            C.ts('dve', s[:, 15:16], s[:, 14:15], 1.0, None, ALU.add, None, [f'ee{b}'], [f'den{b}'])
            C.recip(s[:, 15:16], s[:, 15:16], [f'den{b}'], [f'den{b}'])
            C.tt('dve', gates[:, i, 0:1], s[:, 15:16], s[:, 12:13], ALU.mult, [f'den{b}', f'wgrp{b}'], [('g1', i)])
            C.tt('dve', gates[:, i, 1:2], gates[:, i, 0:1], s[:, 14:15], ALU.mult, [('g1', i), f'ee{b}'], [('g2', i)])
            C.tt('dve', r1[:, 120:152], r1[:, 56:88], r1[:, 88:120], ALU.add, ['oh1', 'oh2'], ['Mf'])
            C.cp('dve', Mb[:], r1[:, 120:152], ['Mf'], ['Mb'])
            C.mm(pM[:, 64:96], ustr[:], Mb[:], True, i == 0, ['ustr', 'Mb'], ['pM'])
            if i > 0:
                C.mm(pM[:, 64:96], ones128[:], Maccb[:], False, True, ['ones128', 'Maccb'], ['pM'])
            C.tt('pool', Macc[:], Macc[:], r1[:, 120:152], ALU.add, ['Macc', 'Mf'], ['Macc'])
            C.cp('pool', Maccb[:], Macc[:], ['Macc'], ['Maccb'])
            SLT = r1[:, 152:184]
            OKM = r1[:, 184:216]
            C.tt('dve', SLT, pM[:, 64:96], basef[:], ALU.add, ['pM', 'basef'], ['slot'])
            C.ts('dve', OKM, pM[:, 64:96], float(CAP), None, ALU.is_lt, None, ['pM'], ['okm'])
            C.ts('dve', SLT, SLT, -TRASH, None, ALU.add, None, ['slot'], ['slot'])
            C.tt('dve', SLT, SLT, OKM, ALU.mult, ['slot', 'okm'], ['slot'])
            C.ts('dve', SLT, SLT, TRASH, None, ALU.add, None, ['slot'], ['slot'])
            C.tt('dve', r1[:, 56:88], r1[:, 56:88], SLT, ALU.mult, ['oh1', 'slot'], ['oh1'])
            C.tt('dve', r1[:, 88:120], r1[:, 88:120], SLT, ALU.mult, ['oh2', 'slot'], ['oh2'])
            S.add('dve', lambda e: e.reduce_sum(out=dstf[:, 0:1], in_=r1[:, 56:88], axis=AX.X), r=['oh1'], w=['dstf0'])
            S.add('dve', lambda e: e.reduce_sum(out=dstf[:, 1:2], in_=r1[:, 88:120], axis=AX.X), r=['oh2'], w=['dstf1'])
            C.cp('dve', dsti[:, 2 * i:2 * i + 2], dstf[:], ['dstf0', 'dstf1'], [('dsti', i)])
            for k2 in range(2 if stage >= 2 else 0):
                S.add('pool', lambda e, i=i, k2=k2, b=b: e.indirect_dma_start(
                    out=Xs[:, :], out_offset=bass.IndirectOffsetOnAxis(ap=dsti[:, 2 * i + k2:2 * i + k2 + 1], axis=0),
                    in_=h2b[b][:], in_offset=None, oob_is_err=False),
                    r=[f'h2b{b}', ('dsti', i)], cw=['Xs'], tag=f'sc{b}{k2}')

        S.add('dve', lambda e: e.memset(junk[:, 0:8], 0.0), r=['pTf'], w=['pTfg', 'pTfu'])
        def wloads(e_):
            b = e_ % 2
            C.dma('pool', wgb[b][:], wg[e_].rearrange("(c p) f -> p c f", p=128), [], [f'wgb{b}'], f'wg{b}')
            C.dma('pool', wub[b][:], wu[e_].rearrange("(c p) f -> p c f", p=128), [], [f'wub{b}'], f'wu{b}')
            C.dma('pool', wdb[b][:], wd[e_].rearrange("(c p) f -> p c f", p=128), [], [f'wdb{b}'], f'wd{b}')

        def xloads(e_):
            b = e_ % 2
            C.dma('sp', xe[b][:], Xs[e_ * CAP:(e_ + 1) * CAP, :].rearrange("(t p) d -> p t d", p=128), ['Xs'], [f'xe{b}'], f'xe{b}')

        if stage >= 3:
            wloads(0)
            xloads(0)
        dk = 0
        for e_ in range(32 if stage >= 3 else 0):
            b = e_ % 2
            if e_ + 1 < 32:
                wloads(e_ + 1)
                xloads(e_ + 1)
            for stt_ in range(NST):
                for c in range(8):
                    C.tr(pTb[:, c * 128:(c + 1) * 128], xe[b][:, stt_, c * 128:(c + 1) * 128], identb[:], [f'xe{b}', 'identb'], ['pTb'])
                C.cp('act' if stt_ % 2 else 'dve', xeT[:, :, stt_ * 128:(stt_ + 1) * 128], pTb[:].rearrange("p (c t) -> p c t", c=8),
                     ['pTb'], [('xeT', stt_)])
            xk = [('xeT', t_) for t_ in range(NST)]
            for fc in range(4):
                for (a0, a1) in ((0, 512), (512, CAP)):
                    for c in range(8):
                        C.mm(pTf[:, a0:a1], wgb[b][:, c, fc * 128:(fc + 1) * 128], xeT[:, c, a0:a1], c == 0, c == 7, [f'wgb{b}'] + xk, ['pTfg'])
                for (a0, a1) in ((0, 512), (512, CAP)):
                    for c in range(8):
                        C.mm(pUU[:, a0:a1], wub[b][:, c, fc * 128:(fc + 1) * 128], xeT[:, c, a0:a1], c == 0, c == 7, [f'wub{b}'] + xk, ['pTfu'])
                C.act(sgl[fc % 2][:], pTf[:, 0:CAP], AF.Silu, ['pTfg'], [f'sgl{fc % 2}'])
                C.tt('dve', aT[:, fc, :], pUU[:, 0:CAP], sgl[fc % 2][:], ALU.mult, ['pTfu', f'sgl{fc % 2}'], [('aT', fc)])
            ak = [('aT', fc) for fc in range(4)]
            for stt_ in range(NST):
                for half in range(2):
                    bk = dk % 2
                    dk += 1
                    for fc in range(4):
                        C.mm(pP[bk][:], aT[:, fc, stt_ * 128:(stt_ + 1) * 128], wdb[b][:, fc, half * 512:(half + 1) * 512],
                             fc == 0, fc == 3, ak + [f'wdb{b}'], [f'pP{bk}'])
                    C.cp('act' if dk % 2 else 'dve', yo[b][:, stt_, half * 512:(half + 1) * 512], pP[bk][:], [f'pP{bk}'],
                         [('yo', b, stt_, half)])
            yk = [('yo', b, t_, h_) for t_ in range(NST) for h_ in range(2)]
            C.dma('sp', Ys[e_ * CAP:(e_ + 1) * CAP, :].rearrange("(t p) d -> p t d", p=128), yo[b][:], yk, [], f'yo{b}', cw=['Ys'])

        def gloads(i):
            b = i % 2
            C.dma('sp', xt[b][:], xmv[i], ['xmid'], [f'xt{b}'], f'lx{b}')
            for k2 in range(2 if stage >= 4 else 0):
                S.add('pool', lambda e, i=i, k2=k2, b=b: e.indirect_dma_start(
                    out=yg[b][k2][:], out_offset=None, in_=Ys[:, :],
                    in_offset=bass.IndirectOffsetOnAxis(ap=dsti[:, 2 * i + k2:2 * i + k2 + 1], axis=0),
                    oob_is_err=False),
                    r=['Ys', ('dsti', i)], w=[f'yg{b}{k2}'], tag=f'gy{b}{k2}')

        gloads(0)
        for i in range(NTT):
            b = i % 2
            if i + 1 < NTT:
                gloads(i + 1)
            if stage < 4:
                C.dma('sp', xov[i], xt[b][:], [f'xt{b}'], [], f'so{b}')
                continue
            C.stt(x1[b][:], yg[b][0][:], gates[:, i, 0:1], xt[b][:], ALU.mult, ALU.add,
                  [f'yg{b}0', ('g1', i), f'xt{b}'], [('x1', b, 0), ('x1', b, 1)])
            C.stt(x1[b][:], yg[b][1][:], gates[:, i, 1:2], x1[b][:], ALU.mult, ALU.add,
                  [f'yg{b}1', ('g2', i), ('x1', b, 0), ('x1', b, 1)], [('x1', b, 0), ('x1', b, 1)])
            C.dma('sp', xov[i], x1[b][:], [('x1', b, 0), ('x1', b, 1)], [], f'so{b}', cw=['xout'])
        if post is not None:
            post(C)
        info = S.emit()
    return info


STD_OF_YG = [0, 2, 1, 3, 4, 5, 6, 7]


def prep_ffn_inputs(inp, l):
    f = np.float32
    wr = np.ascontiguousarray(np.concatenate([inp['router_coarse'][l], inp['router_fine'][l]], axis=1), dtype=f)
    wout = np.asarray(inp['w_out'][l], dtype=f).reshape(8, 128, 1024)[STD_OF_YG].reshape(1024, 1024)
    gout = np.asarray(inp['out_norm_g'][l], dtype=f).reshape(8, 128)[STD_OF_YG].T
    return dict(
        wout=np.ascontiguousarray(wout),
        gout=np.ascontiguousarray(gout),
        g2bd=np.ascontiguousarray(np.broadcast_to(np.asarray(inp['norm2_g'][l])[None, :], (128, 1024)), dtype=f),
        wr=wr,
        wg=np.ascontiguousarray(inp['exp_w_gate'][l], dtype=f),
        wu=np.ascontiguousarray(inp['exp_w_up'][l], dtype=f),
        wd=np.ascontiguousarray(inp['exp_w_down'][l], dtype=f),
    )


MIX_KEYS = dict(wall=[1024, NW], vec=[128, NV], g1=[128, 8], wab=[128, 2, 128], pww=[128, 2, 128])
FFN_KEYS = dict(wout=[1024, 1024], gout=[128, 8], g2bd=[128, 1024], wr=[1024, 36], wg=[32, 1024, 512],
                wu=[32, 1024, 512], wd=[32, 512, 1024])
RG_PAIRS = [[0, 1], [2, 3], [4, 5], [6, 7]]


def build_fused(SL, depth=2):
    NT = SL // 2
    nc = bass.Bass("TRN2", target_bir_lowering=False)

    def din(name, shape, dt=F32):
        return nc.dram_tensor(name, shape, dt, kind="ExternalInput").ap()

    x_full = din("x_full", [SL, 1024])
    xin0 = din("xin0", [NT, 1024])
    msk = din("msk", [128, 2])
    W = []
    for l in range(depth):
        d = {k: din(f"{k}{l}", shp) for k, shp in MIX_KEYS.items()}
        d.update({k: din(f"{k}{l}", shp) for k, shp in FFN_KEYS.items()})
        W.append(d)
    xout = nc.dram_tensor("xout", [NT, 1024], F32, kind="ExternalOutput").ap()
    yT = nc.dram_tensor("yT_i", [512, SL], BF16, kind="Internal").ap()
    yG = nc.dram_tensor("yG_i", [1024, SL], BF16, kind="Internal").ap()
    Xs = nc.dram_tensor("Xs_i", [NROWS, 1024], BF16, kind="Internal").ap()
    Ys = nc.dram_tensor("Ys_i", [NROWS, 1024], BF16, kind="Internal").ap()
    xmid = nc.dram_tensor("xmid_i", [NT, 1024], F32, kind="Internal").ap()
    xo = nc.dram_tensor("xo_i", [NT, 1024], F32, kind="Internal").ap()
    xG = nc.dram_tensor("xG_i", [SL, 1024], F32, kind="Internal").ap()
    with ExitStack() as outer:
        gs = GSync(nc, outer)
        for l in range(depth):
            last = l == depth - 1
            w = W[l]

            def post_m(C):
                for k in range(4):
                    C.S.add('pool', lambda e, k=k: e.collective_compute(
                        "AllGather", ALU.bypass, replica_groups=RG_PAIRS,
                        ins=[yT[k * 128:(k + 1) * 128, :]], outs=[yG[k * 256:(k + 1) * 256, :]]),
                        r=['yT'], cw=['yG'], tag='cc', inc=1)

            def xtile(n):
                p = n * 128
                r_, q = p // NT, p % NT
                row = (q // 512) * 1024 + r_ * 512 + q % 512
                return xG[row:row + 128, :]

            build_mixer(nc, gs, SL, x_full if l == 0 else xG, w['wall'], w['vec'], w['g1'], w['wab'], w['pww'], yT, post=post_m,
                        xtile=None if l == 0 else xtile)
            nc.all_engine_barrier()

            def post_f(C, last=last):
                if not last:
                    for k in range(NT // 512):
                        C.S.add('pool', lambda e, k=k: e.collective_compute(
                            "AllGather", ALU.bypass, replica_groups=RG_PAIRS,
                            ins=[xo[k * 512:(k + 1) * 512, :]], outs=[xG[k * 1024:(k + 1) * 1024, :]]),
                            r=['xout'], cw=['xG'], tag='cc', inc=1)

            build_ffn(nc, gs, NT, xin0 if l == 0 else xo, yG, msk, w['wout'], w['gout'], w['g2bd'], w['wr'], w['wg'], w['wu'],
                      w['wd'], xout if last else xo, Xs, Ys, xmid, post=post_f)
            if not last:
                nc.all_engine_barrier()
    return nc


_NC_CACHE = {}


def run_fused(inp, X):
    B, SL, D = X.shape
    NT = SL // 2
    depth = np.asarray(inp['w_in']).shape[0]
    key = (SL, depth)
    if key not in _NC_CACHE:
        _NC_CACHE[key] = build_fused(SL, depth)
    nc = _NC_CACHE[key]
    fw = [prep_ffn_inputs(inp, l) for l in range(depth)]
    maps = []
    for c in range(8):
        b, h = c // 2, c % 2
        m = {'x_full': np.ascontiguousarray(X[b]), 'xin0': np.ascontiguousarray(X[b, h * NT:(h + 1) * NT])}
        mk = np.zeros((128, 2), np.float32)
        mk[:, h] = 1.0
        m['msk'] = mk
        for l in range(depth):
            for k, v in prep_mixer_inputs(inp, l, b, h).items():
                m[f'{k}{l}'] = v
            for k, v in fw[l].items():
                m[f'{k}{l}'] = v
        maps.append(m)
    res = run_bass_kernel_spmd(nc, maps, core_ids=list(range(8)))
    out = np.empty_like(X)
    for c in range(8):
        b, h = c // 2, c % 2
        out[b, h * NT:(h + 1) * NT] = np.asarray(res.results[c]['xout'])
    return out


def kernel(**inp):
    X = np.ascontiguousarray(np.asarray(inp['x'], dtype=np.float32))
    return run_fused(inp, X)
```

```python
import numpy as np
import ml_dtypes
from contextlib import ExitStack
import concourse.bass as bass
import concourse.mybir as mybir
from concourse.bass_utils import run_bass_kernel_spmd

F32 = mybir.dt.float32
BF16 = mybir.dt.bfloat16
I32 = mybir.dt.int32
AF = mybir.ActivationFunctionType
ALU = mybir.AluOpType
AX = mybir.AxisListType

ENG_ATTR = {'pe': 'tensor', 'act': 'scalar', 'dve': 'vector', 'pool': 'gpsimd', 'sp': 'sync'}
EPS = 1e-6


class GSync:
    def __init__(self, nc, stack):
        self.nc = nc
        self.stack = stack
        self.sems = {}
        self.cnts = {}
        self.tagmap = {}

    def sem(self, name):
        if name not in self.sems:
            self.sems[name] = self.stack.enter_context(self.nc.semaphore(name))
        return self.sems[name]

    def tagsem(self, tag):
        if tag not in self.tagmap:
            self.tagmap[tag] = 't_' + tag
        return self.tagmap[tag]


class Sched:
    def __init__(self, nc, stack, gs=None):
        self.nc = nc
        self.stack = stack
        self.gs = gs if gs is not None else GSync(nc, stack)
        self.ops = []
        self.wx = {}
        self.wc = {}
        self.rd = {}
        self.tag_last = {}
        self.sems = {}

    @staticmethod
    def _merge(dst, src):
        for s, i in src.items():
            if dst.get(s, -1) < i:
                dst[s] = i

    def add(self, eng, fn, r=(), w=(), cw=(), tag=None, inc=16):
        deps = {}
        for k in r:
            self._merge(deps, self.wx.get(k, {}))
            self._merge(deps, self.wc.get(k, {}))
        for k in w:
            self._merge(deps, self.wx.get(k, {}))
            self._merge(deps, self.wc.get(k, {}))
            self._merge(deps, self.rd.get(k, {}))
        for k in cw:
            self._merge(deps, self.wx.get(k, {}))
            self._merge(deps, self.rd.get(k, {}))
        if tag is not None and tag in self.tag_last:
            self._merge(deps, {('t', tag): self.tag_last[tag]})
        idx = len(self.ops)
        if eng == 'pe':
            deps.pop(('e', 'pe'), None)
        self.ops.append(dict(eng=eng, fn=fn, deps=deps, tag=tag, sem=None, cnt=0, inc=inc))
        sig = ('t', tag) if tag else ('e', eng)
        for k in r:
            self.rd.setdefault(k, {})[sig] = idx
        for k in w:
            self.wx[k] = {sig: idx}
            self.wc[k] = {}
            self.rd[k] = {}
        for k in cw:
            self.wc.setdefault(k, {})[sig] = idx
        if tag is not None:
            self.tag_last[tag] = idx
        return idx

    def _sem(self, name):
        return self.gs.sem(name)

    def emit(self):
        ops = self.ops
        need = set()
        for o in ops:
            for i in o['deps'].values():
                need.add(i)
        cnts = self.gs.cnts
        for i, o in enumerate(ops):
            if o['tag']:
                o['sem'] = self.gs.tagsem(o['tag'])
                cnts[o['sem']] = cnts.get(o['sem'], 0) + o['inc']
                o['cnt'] = cnts[o['sem']]
            elif i in need:
                o['sem'] = 'e_' + o['eng']
                cnts[o['sem']] = cnts.get(o['sem'], 0) + 1
                o['cnt'] = cnts[o['sem']]
        final = {}
        for o in ops:
            if o['sem']:
                final[o['sem']] = max(final.get(o['sem'], 0), o['cnt'])
        for s in final:
            self._sem(s)
        with self.nc.Block() as blk:
            for eng, attr in ENG_ATTR.items():
                mine = [o for o in ops if o['eng'] == eng]

                def body(e, mine=mine, eng=eng):
                    waited = {}
                    for o in mine:
                        for di in sorted(o['deps'].values()):
                            d = ops[di]
                            if waited.get(d['sem'], 0) < d['cnt']:
                                e.wait_ge(self._sem(d['sem']), d['cnt'])
                                waited[d['sem']] = d['cnt']
                        ins = o['fn'](e)
                        if o['sem']:
                            ins.then_inc(self._sem(o['sem']), o['inc'] if o['tag'] else 1)
                    if eng == 'sp':
                        for s, c in final.items():
                            if waited.get(s, 0) < c:
                                e.wait_ge(self._sem(s), c)

                getattr(blk, attr)(body)
        return dict(nops=len(ops), final=final)


class Ctx:
    def __init__(self, nc, st, gs=None):
        self.nc = nc
        self.st = st
        self.S = Sched(nc, st, gs)
        g = self.S.gs
        g.phase = getattr(g, 'phase', 0) + 1
        self.pfx = f"p{g.phase}_"

    def sb(self, name, shape, dt):
        return self.st.enter_context(self.nc.sbuf_tensor(self.pfx + name, shape, dt))

    def ps(self, name, shape, dt):
        return self.st.enter_context(self.nc.psum_tensor(self.pfx + name, shape, dt))

    def dma(self, q, out, in_, r, w, tag, cw=()):
        self.S.add(q, lambda e: e.dma_start(out=out, in_=in_), r=r, w=w, cw=cw, tag=tag)

    def mm(self, out, lhsT, rhs, start, stop, r, w):
        self.S.add('pe', lambda e: e.matmul(out, lhsT=lhsT, rhs=rhs, start=start, stop=stop,
                                            skip_group_check=True), r=r, w=w)

    def tr(self, out, in_, ident, r, w):
        self.S.add('pe', lambda e: e.transpose(out, in_, ident), r=r, w=w)

    def act(self, out, in_, func, r, w, bias=None, scale=None, accum=None):
        kw = {}
        if bias is not None:
            kw['bias'] = bias
        if scale is not None:
            kw['scale'] = scale
        if accum is not None:
            kw['accum_out'] = accum
        self.S.add('act', lambda e: e.activation(out=out, in_=in_, func=func, **kw), r=r, w=w)

    def ts(self, eng, out, in0, s1, s2, op0, op1, r, w):
        if op1 is None:
            self.S.add(eng, lambda e: e.tensor_scalar(out=out, in0=in0, scalar1=s1, scalar2=None, op0=op0), r=r, w=w)
        else:
            self.S.add(eng, lambda e: e.tensor_scalar(out=out, in0=in0, scalar1=s1, scalar2=s2, op0=op0, op1=op1), r=r, w=w)

    def tt(self, eng, out, in0, in1, op, r, w):
        self.S.add(eng, lambda e: e.tensor_tensor(out=out, in0=in0, in1=in1, op=op), r=r, w=w)

    def stt(self, out, in0, scalar, in1, op0, op1, r, w):
        self.S.add('dve', lambda e: e.scalar_tensor_tensor(out=out, in0=in0, scalar=scalar, in1=in1, op0=op0, op1=op1), r=r, w=w)

    def cp(self, eng, out, in_, r, w):
        if eng == 'act':
            self.S.add('act', lambda e: e.copy(out=out, in_=in_), r=r, w=w)
        else:
            self.S.add(eng, lambda e: e.tensor_copy(out=out, in_=in_), r=r, w=w)

    def memset(self, eng, ap, val, w):
        self.S.add(eng, lambda e: e.memset(ap, val), w=w)

    def recip(self, out, in_, r, w):
        self.S.add('dve', lambda e: e.reciprocal(out=out, in_=in_), r=r, w=w)

    def aselect(self, out, in_, pattern, op, fill, base, cm, r, w):
        self.S.add('pool', lambda e: e.affine_select(out=out, in_=in_, pattern=pattern, compare_op=op, fill=fill,
                                                     base=base, channel_multiplier=cm), r=r, w=w)


V_GQ, V_GK, V_LCW, V_LCB, V_BA, V_BX, V_LAM = 0, 1, 2, 6, 7, 8, 9
V_DWB, V_LNG, V_LNB, V_PWB, V_DWW = 10, 12, 14, 16, 17
NV = 17 + 62
NW = 1536


def build_mixer(nc, gs, SL, x, wall, vec, g1, wab, pww, yT, post=None, xtile=None):
    NG = SL // 512
    with ExitStack() as st:
        C = Ctx(nc, st, gs)
        S = C.S
        sb, ps = C.sb, C.ps
        Wb = sb("Wb", [128, 8, NW], BF16)
        wst = [sb(f"wst{i}", [128, 384], F32) for i in range(2)]
        vecs = sb("vecs", [128, NV], F32)
        g1s = sb("g1s", [128, 8], F32)
        wabf = sb("wabf", [128, 2, 128], F32)
        wabb = sb("wabb", [128, 2, 128], BF16)
        pwf = sb("pwf", [128, 2, 128], F32)
        pwb = sb("pwb", [128, 2, 128], BF16)
        identb = sb("identb", [128, 128], BF16)
        blk1 = sb("blk1", [128, 128], BF16)
        onesm = sb("onesm", [128, 128], F32)
        uincl = sb("uincl", [128, 128], BF16)
        negones = sb("negones", [128, 128], BF16)
        tri = sb("tri", [128, 128], BF16)
        dg = sb("dg", [128, 62, 128], BF16)
        cl = sb("cl", [128, 4], F32)
        KT = sb("KT", [128, 2, SL], BF16)
        Vt = sb("Vt", [128, SL // 128, 256], BF16)
        QT = sb("QT", [128, 2, 512], BF16)
        xt = [sb(f"xt{i}", [128, 1024], F32) for i in range(2)]
        xn = [sb(f"xn{i}", [128, 1024], BF16) for i in range(2)]
        st1 = [sb(f"st1_{i}", [128, 4], F32) for i in range(2)]
        hT = sb("hT", [128, 8, 512], BF16)
        sq = sb("sq", [128, 512], BF16)
        rs = sb("rs", [128, 512], F32)
        xrb = sb("xrb", [128, 515], F32)
        xgb = sb("xgb", [128, 512], F32)
        cv = sb("cv", [128, 512], F32)
        cvb = sb("cvb", [128, 512], BF16)
        lr = sb("lr", [128, 512], F32)
        li = sb("li", [128, 512], F32)
        la = sb("la", [128, 512], F32)
        la2 = sb("la2", [128, 512], F32)
        lu = sb("lu", [128, 512], F32)
        lh = sb("lh", [128, 512], F32)
        hprev = sb("hprev", [128, 1], F32)
        gt = sb("gt", [128, 512], F32)
        ybs = sb("ybs", [128, 512], BF16)
        ub = sb("ub", [128, 2, 542], BF16)
        sg = sb("sg", [128, 512], F32)
        cvo = sb("cvo", [128, 2, 512], F32)
        sqo = sb("sqo", [128, 2, 512], F32)
        means = sb("means", [128, 512], F32)
        m2 = sb("m2", [128, 512], F32)
        crs = sb("crs", [128, 512], F32)
        cn = sb("cn", [128, 2, 512], F32)
        csb = sb("csb", [128, 2, 512], BF16)
        ycs = sb("ycs", [128, 512], BF16)
        LA = 2
        NRB = LA + 2
        Eb = [sb(f"Eb{i}", [128, 512], BF16) for i in range(NRB)]
        Lb = [sb(f"Lb{i}", [128, 512], BF16) for i in range(NRB)]
        NWB = 3
        Wt = [sb(f"Wt{i}", [128, 512], BF16) for i in range(NWB)]
        LaccP = [sb(f"Lacc{i}", [128, 512], F32) for i in range(2)]
        xC = [sb(f"xC{i}", [128, 512], BF16) for i in range(2)]
        Laccb = [sb(f"Laccb{i}", [128, 512], BF16) for i in range(NRB)]
        yst = [sb(f"yst{i}", [64, 512], BF16) for i in range(2)]
        pA = [ps(f"pA{i}", [128, 512], F32) for i in range(2)]
        pT = ps("pT", [128, 1024], BF16)
        pZ = [ps(f"pZ{i}", [128, 512], F32) for i in range(2)]
        pC = [ps(f"pC{i}", [128, 512], F32) for i in range(2)]
        pO = ps("pO", [128, 512], F32)

        C.dma('sp', vecs[:], vec, [], ['vecs'], 'c0')
        C.dma('sp', g1s[:], g1, [], ['g1s'], 'c1')
        C.dma('sp', wabf[:], wab, [], ['wabf'], 'c2')
        C.dma('sp', pwf[:], pww, [], ['pwf'], 'c3')
        C.cp('dve', wabb[:], wabf[:], ['wabf'], ['wabb'])
        C.cp('dve', pwb[:], pwf[:], ['pwf'], ['pwb'])
        wv = wall.rearrange("(c p) n -> p c n", p=128)
        k = 0
        for c in range(8):
            for hf in range(4):
                b = k % 2
                C.dma('sp', wst[b][:], wv[:, c, hf * 384:(hf + 1) * 384], [], [f'wst{b}'], f'wst{b}')
                C.ts('dve' if k % 2 == 0 else 'pool', Wb[:, c, hf * 384:(hf + 1) * 384], wst[b][:], g1s[:, c:c + 1], None,
                     ALU.mult, None, [f'wst{b}', 'g1s'], [('Wb', c, hf)])
                k += 1
        WbK = [('Wb', c, hf) for c in range(8) for hf in range(4)]
        C.memset('pool', identb[:], 0.0, ['identb'])
        C.aselect(identb[:], identb[:], [[-1, 128]], ALU.not_equal, 1.0, 0, 1, ['identb'], ['identb'])
        C.memset('pool', blk1[:], 0.0, ['blk1'])
        C.memset('pool', blk1[0:64, 0:64], 1.0, ['blk1'])
        C.memset('pool', blk1[64:128, 64:128], 1.0, ['blk1'])
        C.memset('pool', onesm[:], 1.0 / 256.0, ['onesm'])
        C.memset('pool', negones[:], -1.0, ['negones'])
        C.memset('pool', uincl[:], -1.0, ['uincl'])
        C.aselect(uincl[:], uincl[:], [[-1, 128]], ALU.is_ge, 0.0, 0, 1, ['uincl'], ['uincl'])
        C.memset('pool', tri[:], 1.0, ['tri'])
        C.aselect(tri[:], tri[:], [[1, 128]], ALU.is_ge, 0.0, -1, -1, ['tri'], ['tri'])
        for i in range(62):
            C.ts('pool' if i % 2 else 'dve', dg[:, i, :], identb[:], vecs[:, V_DWW + i:V_DWW + i + 1], None, ALU.mult, None,
                 ['identb', 'vecs'], ['dg'])
        C.act(cl[:, 0:1], vecs[:, V_LAM:V_LAM + 1], AF.Exp, ['vecs'], ['cl0'], scale=-1.0)
        C.act(cl[:, 1:2], cl[:, 0:1], AF.Ln, ['cl0'], ['cl1'], bias=1.0)
        C.ts('dve', cl[:, 2:3], cl[:, 1:2], -8.0, None, ALU.mult, None, ['cl1'], ['cl2'])
        C.ts('dve', cl[:, 3:4], cl[:, 1:2], -16.0, None, ALU.mult, None, ['cl1'], ['cl3'])
        C.memset('pool', hprev[:], 0.0, ['hprev'])
        C.memset('pool', xrb[:, 0:3], 0.0, ['xrb_h'])
        C.memset('pool', ub[:, :, 0:30], 0.0, ['ub_h'])

        xv = x.rearrange("(n p) d -> n p d", p=128)
        wcnt = [0]
        ocnt = [0]
        acnt = [0]
        bcnt = [0]
        lcnt = [0]
        pcnt = [0]
        for g in range(NG):
            t0 = g * 512
            for tt in range(4):
                b = tt % 2
                C.dma('sp', xt[b][:], xv[g * 4 + tt] if xtile is None else xtile(g * 4 + tt), [], [f'xt{b}'], f'x{b}')
                C.act(xn[b][:], xt[b][:], AF.Square, [f'xt{b}'], [f'ss{b}', f'xn{b}'], accum=st1[b][:, 0:1])
                C.act(st1[b][:, 1:2], st1[b][:, 0:1], AF.Sqrt, [f'ss{b}'], [f'rt{b}'], scale=1.0 / 1024.0, bias=EPS)
                C.recip(st1[b][:, 2:3], st1[b][:, 1:2], [f'rt{b}'], [f'rstd{b}'])
                C.ts('dve', xn[b][:], xt[b][:], st1[b][:, 2:3], None, ALU.mult, None, [f'xt{b}', f'rstd{b}'], [f'xn{b}'])
                for c in range(8):
                    C.tr(pT[:, c * 128:(c + 1) * 128], xn[b][:, c * 128:(c + 1) * 128], identb[:], [f'xn{b}', 'identb'], ['pT'])
                C.cp('act' if tt % 2 else 'dve', hT[:, :, tt * 128:(tt + 1) * 128], pT[:].rearrange("p (c t) -> p c t", c=8),
                     ['pT'], [('hT', tt)])
            hTK = [('hT', tt) for tt in range(4)]
            pidx = [0]

            def proj(ct):
                bank = pidx[0] % 2
                pidx[0] += 1
                for c in range(8):
                    C.mm(pA[bank][:], Wb[:, c, ct * 128:(ct + 1) * 128], hT[:, c, :], c == 0, c == 7, WbK + hTK, [f'pA{bank}'])
                return bank

            for ct in range(4):
                bk = proj(ct)
                C.act(sq[:], pA[bk][:], AF.Square, [f'pA{bk}'], ['sq'])
                C.mm(pA[1 - bk][:], blk1[:], sq[:], True, True, ['blk1', 'sq'], [f'pA{1 - bk}'])
                pidx[0] += 1
                if ct < 2:
                    C.act(rs[:], pA[1 - bk][:], AF.Sqrt, [f'pA{1 - bk}'], ['rs'], scale=1.0, bias=64.0 * EPS)
                else:
                    C.act(rs[:], pA[1 - bk][:], AF.Sqrt, [f'pA{1 - bk}'], ['rs'], scale=1.0 / 64.0, bias=EPS)
                C.recip(rs[:], rs[:], ['rs'], ['rs'])
                if ct < 2:
                    C.stt(QT[:, ct, :], pA[bk][:], vecs[:, V_GQ:V_GQ + 1], rs[:], ALU.mult, ALU.mult,
                          [f'pA{bk}', 'vecs', 'rs'], [('QT', ct)])
                else:
                    C.stt(KT[:, ct - 2, t0:t0 + 512], pA[bk][:], vecs[:, V_GK:V_GK + 1], rs[:], ALU.mult, ALU.mult,
                          [f'pA{bk}', 'vecs', 'rs'], [('KT', ct - 2, g)])
            bk = proj(4)
            C.cp('act', xrb[:, 3:515], pA[bk][:], [f'pA{bk}'], ['xrb'])
            bk = proj(5)
            C.cp('dve', xgb[:], pA[bk][:], [f'pA{bk}'], ['xgb'])
            for t in range(2):
                bv = proj(6 + t)
                bg = proj(8 + t)
                C.act(sg[:], pA[bg][:], AF.Sigmoid, [f'pA{bg}'], ['sg'])
                C.tt('dve', ub[:, t, 30:542], pA[bv][:], sg[:], ALU.mult, [f'pA{bv}', 'sg'], [('ub', t)])
            for tt in range(4):
                bank = pidx[0] % 2
                pidx[0] += 1
                for c in range(8):
                    C.mm(pA[bank][:, 0:256], hT[:, c, tt * 128:(tt + 1) * 128], Wb[:, c, 1280:1536], c == 0, c == 7,
                         WbK + hTK, [f'pA{bank}'])
                C.cp('act' if tt % 2 else 'dve', Vt[:, g * 4 + tt, :], pA[bank][:, 0:256], [f'pA{bank}'], [('V', g * 4 + tt)])

            vc = lambda i: vecs[:, i:i + 1]
            C.ts('dve', cv[:], xrb[:, 3:515], vc(V_LCW + 3), vc(V_LCB), ALU.mult, ALU.add, ['xrb', 'xrb_h', 'vecs'], ['cv'])
            for kk in range(3):
                C.stt(cv[:], xrb[:, kk:kk + 512], vc(V_LCW + kk), cv[:], ALU.mult, ALU.add, ['xrb', 'xrb_h', 'vecs', 'cv'], ['cv'])
            C.cp('pool', xrb[:, 0:3], xrb[:, 512:515], ['xrb'], ['xrb_h'])
            C.cp('pool', cvb[:], cv[:], ['cv'], ['cvb'])
            b0 = pidx[0] % 2
            pidx[0] += 2
            C.mm(pA[b0][:], wabb[:, 0, :], cvb[:], True, True, ['wabb', 'cvb'], [f'pA{b0}'])
            C.mm(pA[1 - b0][:], wabb[:, 1, :], cvb[:], True, True, ['wabb', 'cvb'], [f'pA{1 - b0}'])
            C.act(lr[:], pA[b0][:], AF.Sigmoid, [f'pA{b0}', 'vecs'], ['lr'], bias=vc(V_BA))
            C.act(li[:], pA[1 - b0][:], AF.Sigmoid, [f'pA{1 - b0}', 'vecs'], ['li'], bias=vc(V_BX))
            C.act(la[:], lr[:], AF.Exp, ['lr', 'cl2'], ['la'], scale=cl[:, 2:3])
            C.act(la2[:], lr[:], AF.Exp, ['lr', 'cl3'], ['la2'], scale=cl[:, 3:4])
            C.act(la2[:], la2[:], AF.Sqrt, ['la2'], ['la2'], scale=-1.0, bias=1.0)
            C.tt('pool', lu[:], li[:], cv[:], ALU.mult, ['li', 'cv'], ['lu'])
            C.tt('pool', lu[:], lu[:], la2[:], ALU.mult, ['lu', 'la2'], ['lu'])
            S.add('dve', lambda e: e.tensor_tensor_scan(out=lh[:], data0=la[:], data1=lu[:], initial=hprev[:, 0:1],
                                                        op0=ALU.mult, op1=ALU.add), r=['la', 'lu', 'hprev'], w=['lh'])
            C.cp('pool', hprev[:], lh[:, 511:512], ['lh'], ['hprev'])
            C.tt('pool', gt[:], xgb[:], xgb[:], ALU.mult, ['xgb'], ['gt'])
            C.ts('pool', gt[:], gt[:], 0.044715, 1.0, ALU.mult, ALU.add, ['gt'], ['gt'])
            C.tt('pool', gt[:], gt[:], xgb[:], ALU.mult, ['gt', 'xgb'], ['gt'])
            C.act(gt[:], gt[:], AF.Sigmoid, ['gt'], ['gt'], scale=1.5957691216)
            C.tt('pool', gt[:], gt[:], xgb[:], ALU.mult, ['gt', 'xgb'], ['gt'])
            C.tt('dve', ybs[:], gt[:], lh[:], ALU.mult, ['gt', 'lh'], ['ybs'])
            C.dma('pool', yT[256:384, t0:t0 + 512], ybs[:], ['ybs'], [], 'yb', cw=['yT'])

            for t in range(2):
                bank = pidx[0] % 2
                pidx[0] += 1
                for kk in range(31):
                    C.mm(pA[bank][:], dg[:, t * 31 + kk, :], ub[:, t, kk:kk + 512], kk == 0, kk == 30,
                         ['dg', ('ub', t), 'ub_h'], [f'pA{bank}'])
                C.act(cvo[:, t, :], pA[bank][:], AF.Identity, [f'pA{bank}', 'vecs'], [('cvo', t)], bias=vc(V_DWB + t))
                C.act(sqo[:, t, :], pA[bank][:], AF.Square, [f'pA{bank}', 'vecs'], [('sqo', t)], bias=vc(V_DWB + t))
            C.cp('pool', ub[:, :, 0:30], ub[:, :, 512:542], [('ub', 0), ('ub', 1)], ['ub_h'])
            bm = pidx[0] % 2
            pidx[0] += 2
            for t in range(2):
                C.mm(pA[bm][:], onesm[:], cvo[:, t, :], t == 0, t == 1, ['onesm', ('cvo', t)], [f'pA{bm}'])
            for t in range(2):
                C.mm(pA[1 - bm][:], onesm[:], sqo[:, t, :], t == 0, t == 1, ['onesm', ('sqo', t)], [f'pA{1 - bm}'])
            C.cp('act', means[:], pA[bm][:], [f'pA{bm}'], ['means'])
            C.tt('pool', m2[:], means[:], means[:], ALU.mult, ['means'], ['m2'])
            C.tt('dve', m2[:], pA[1 - bm][:], m2[:], ALU.subtract, [f'pA{1 - bm}', 'm2'], ['m2'])
            C.act(crs[:], m2[:], AF.Sqrt, ['m2'], ['crs'], scale=1.0, bias=EPS)
            C.recip(crs[:], crs[:], ['crs'], ['crs'])
            for t in range(2):
                C.tt('pool', cn[:, t, :], cvo[:, t, :], means[:], ALU.subtract, [('cvo', t), 'means'], [('cn', t)])
                C.tt('dve' if t else 'pool', cn[:, t, :], cn[:, t, :], crs[:], ALU.mult, [('cn', t), 'crs'], [('cn', t)])
                C.act(csb[:, t, :], cn[:, t, :], AF.Silu, [('cn', t), 'vecs'], [('csb', t)], scale=vc(V_LNG + t), bias=vc(V_LNB + t))
            bank = pidx[0] % 2
            pidx[0] += 1
            for t in range(2):
                C.mm(pA[bank][:], pwb[:, t, :], csb[:, t, :], t == 0, t == 1, ['pwb', ('csb', t)], [f'pA{bank}'])
            C.act(ycs[:], pA[bank][:], AF.Identity, [f'pA{bank}', 'vecs'], ['ycs'], bias=vc(V_PWB))
            C.dma('pool', yT[384:512, t0:t0 + 512], ycs[:], ['ycs'], [], 'yc', cw=['yT'])

            items = []
            for hd in range(4):
                nblk = 4 * g + 4
                for bi, kb in enumerate(range(4 * g + 3, -1, -1)):
                    items.append(dict(hd=hd, bi=bi, kb=kb, nblk=nblk))

            def stageA(it):
                hd, bi, kb = it['hd'], it['bi'], it['kb']
                ct = hd // 2
                pb = 64 * (hd % 2)
                Qh = QT[pb:pb + 64, ct, :]
                j = kb - 4 * g
                c0 = 128 * j if j > 0 else 0
                Kh = KT[pb:pb + 64, ct, kb * 128:(kb + 1) * 128]
                kkey = ('KT', ct, kb // 4)
                zb = acnt[0] % 2
                lb = acnt[0] % NRB
                eb = acnt[0] % NRB
                acnt[0] += 1
                it.update(c0=c0, j=j, Kh=Kh, kkey=kkey, Qh=Qh, ct=ct, lb=lb, eb=eb)
                if bi == 0:
                    C.memset('pool', LaccP[0][:], 0.0, ['Lacc0'])
                    C.memset('pool', LaccP[1][:], 0.0, ['Lacc1'])
                    pcnt[0] = 0
                C.mm(pZ[zb][:, c0:], Kh, Qh[:, c0:], True, True, [kkey, ('QT', ct)], [f'pZ{zb}'])
                C.act(Eb[eb][:, c0:], pZ[zb][:, c0:], AF.Exp, [f'pZ{zb}'], [f'Eb{eb}'])
                C.act(Lb[lb][:, c0:], Eb[eb][:, c0:], AF.Ln, [f'Eb{eb}'], [f'Lb{lb}'], bias=1.0)
                if j >= 0:
                    C.tt('dve', Lb[lb][:, c0:c0 + 128], Lb[lb][:, c0:c0 + 128], tri[:], ALU.mult, [f'Lb{lb}', 'tri'], [f'Lb{lb}'])
                if kb > 0:
                    la_ = lcnt[0] % NRB
                    lcnt[0] += 1
                    pp = pcnt[0] % 2
                    pcnt[0] += 1
                    Lo, Ln_ = LaccP[pp], LaccP[1 - pp]
                    ko, kn = f'Lacc{pp}', f'Lacc{1 - pp}'
                    C.tt('pool', Ln_[:, c0:], Lo[:, c0:], Lb[lb][:, c0:], ALU.add, [ko, f'Lb{lb}'], [kn])
                    C.cp('dve', Laccb[la_][:], Ln_[:], [kn], [f'Laccb{la_}'])
                    it['la_out'] = la_

            def stageB(it, prev):
                hd, bi, kb, c0, j = it['hd'], it['bi'], it['kb'], it['c0'], it['j']
                Kh, kkey, Qh, ct, lb = it['Kh'], it['kkey'], it['Qh'], it['ct'], it['lb']
                cb = bcnt[0] % 2
                bcnt[0] += 1
                C.mm(pC[cb][:, c0:], uincl[:], Lb[lb][:, c0:], True, bi == 0, ['uincl', f'Lb{lb}'], [f'pC{cb}'])
                if bi > 0:
                    la_ = prev['la_out']
                    C.mm(pC[cb][:, c0:], negones[:], Laccb[la_][:, c0:], False, True, ['negones', f'Laccb{la_}'], [f'pC{cb}'])
                wi = wcnt[0] % NWB
                wcnt[0] += 1
                eb = it['eb']
                C.act(xC[cb][:, c0:], pC[cb][:, c0:], AF.Exp, [f'pC{cb}'], [f'xC{cb}'])
                C.tt('dve', Wt[wi][:, c0:], xC[cb][:, c0:], Eb[eb][:, c0:], ALU.mult, [f'xC{cb}', f'Eb{eb}'], [f'Wt{wi}'])
                if j >= 0:
                    C.tt('dve', Wt[wi][:, c0:c0 + 128], Wt[wi][:, c0:c0 + 128], tri[:], ALU.mult, [f'Wt{wi}', 'tri'], [f'Wt{wi}'])
                it['wi'] = wi

            def stagePV(it):
                hd, bi, kb, c0 = it['hd'], it['bi'], it['kb'], it['c0']
                wi = it['wi']
                C.mm(pO[0:64, c0:], Vt[:, kb, hd * 64:(hd + 1) * 64], Wt[wi][:, c0:], bi == 0, bi == it['nblk'] - 1,
                     [('V', kb), f'Wt{wi}'], ['pO'])
                if bi == it['nblk'] - 1:
                    ob = ocnt[0] % 2
                    ocnt[0] += 1
                    C.cp('dve', yst[ob][:], pO[0:64, :], ['pO'], [f'yst{ob}'])
                    C.dma('sp', yT[hd * 64:(hd + 1) * 64, t0:t0 + 512], yst[ob][:], [f'yst{ob}'], [], f'ya{ob}', cw=['yT'])

            for n_ in range(min(LA, len(items))):
                stageA(items[n_])
            for n_ in range(len(items)):
                if n_ > 0:
                    stagePV(items[n_ - 1])
                stageB(items[n_], items[n_ - 1] if n_ > 0 else None)
                if n_ + LA < len(items):
                    stageA(items[n_ + LA])
            stagePV(items[-1])
        if post is not None:
            post(C)
        info = S.emit()
    return info


def prep_mixer_inputs(inp, l, b, hg, SL=None):
    f = np.float32
    w_in = np.asarray(inp['w_in'][l])
    hs = slice(hg * 256, (hg + 1) * 256)
    q = w_in[:, 0:512][:, hs]
    k = w_in[:, 512:1024][:, hs]
    v = w_in[:, 1024:1536][:, hs]
    cs = slice(hg * 128, (hg + 1) * 128)
    xr = w_in[:, 1536:1792][:, cs]
    xg = w_in[:, 1792:2048][:, cs]
    cval = w_in[:, 2048:2304]
    cgate = w_in[:, 2304:2560]
    wall = np.ascontiguousarray(np.concatenate([q, k, xr, xg, cval, cgate, v], axis=1), dtype=f)
    vec = np.zeros((128, NV), f)
    vec[:, V_GQ] = np.tile(inp['q_norm_g'][l], 2)
    vec[:, V_GK] = np.tile(inp['k_norm_g'][l], 2)
    for kk in range(4):
        vec[:, V_LCW + kk] = inp['lru_conv_w'][l][kk, cs]
    vec[:, V_LCB] = inp['lru_conv_b'][l][cs]
    vec[:, V_BA] = inp['lru_ba'][l][cs]
    vec[:, V_BX] = inp['lru_bx'][l][cs]
    vec[:, V_LAM] = inp['lru_lambda'][l][cs]
    for t in range(2):
        ts_ = slice(t * 128, (t + 1) * 128)
        vec[:, V_DWB + t] = inp['conf_dw_b'][l][ts_]
        vec[:, V_LNG + t] = inp['conf_ln_g'][l][ts_]
        vec[:, V_LNB + t] = inp['conf_ln_b'][l][ts_]
        for kk in range(31):
            vec[:, V_DWW + t * 31 + kk] = inp['conf_dw_w'][l][kk, ts_]
    vec[:, V_PWB] = inp['conf_pw_b'][l][cs]
    g1 = np.ascontiguousarray(np.asarray(inp['norm1_g'][l]).reshape(8, 128).T, dtype=f)
    wab = np.zeros((128, 2, 128), f)
    for i in range(2):
        blk = hg * 2 + i
        wab[i * 64:(i + 1) * 64, 0, i * 64:(i + 1) * 64] = inp['lru_wa'][l][blk]
        wab[i * 64:(i + 1) * 64, 1, i * 64:(i + 1) * 64] = inp['lru_wx'][l][blk]
    pw = np.asarray(inp['conf_pw_w'][l])[:, cs]
    pww = np.ascontiguousarray(pw.reshape(2, 128, 128).transpose(1, 0, 2), dtype=f)
    return dict(wall=wall, vec=vec, g1=g1, wab=wab, pww=pww)


CAP = 640
NSLOT = 32 * CAP
NROWS = NSLOT + 128
TRASH = float(NSLOT)


def build_ffn(nc, gs, NT, xin, yG, msk, wout, gout, g2bd, wr, wg, wu, wd, xout, Xs, Ys, xmid, post=None):
    NTT = NT // 128
    stage = 4
    dbg = False
    with ExitStack() as st:
        C = Ctx(nc, st, gs)
        S = C.S
        sb, ps = C.sb, C.ps
        mks = sb("mks", [128, 2], F32)
        ysc = [[sb(f"ysc{i}_{k}", [128, 8, 128], BF16) for k in range(2)] for i in range(2)]
        Woutb = sb("Woutb", [128, 8, 1024], BF16)
        wstg = [sb(f"wstg{i}", [128, 1024], F32) for i in range(2)]
        gos = sb("gos", [128, 8], F32)
        g2b = sb("g2b", [128, 1024], F32)
        wrs = sb("wrs", [128, 8, 36], F32)
        identf = sb("identf", [128, 128], F32)
        identb = sb("identb", [128, 128], BF16)
        onec = sb("onec", [128, 2], BF16)
        ustr = sb("ustr", [128, 128], BF16)
        ones128 = sb("ones128", [128, 128], BF16)
        basef = sb("basef", [128, 32], F32)
        zt = sb("zt", [128, 4, 1024], BF16)
        gates = sb("gates", [128, NTT, 2], F32)
        dsti = sb("dsti", [128, NTT * 2], I32)
        Macc = sb("Macc", [128, 32], F32)
        Maccb = sb("Maccb", [128, 32], BF16)
        xt = [sb(f"xt{i}", [128, 1024], F32) for i in range(2)]
        ys = [sb(f"ys{i}", [128, 8, 128], BF16) for i in range(2)]
        ysq = sb("ysq", [128, 8, 128], BF16)
        x1 = [sb(f"x1_{i}", [128, 1024], F32) for i in range(2)]
        junk = sb("junk", [128, 1024], BF16)
        h2f = sb("h2f", [128, 1024], F32)
        h2b = [sb(f"h2b{i}", [128, 1024], BF16) for i in range(2)]
        h2T = sb("h2T", [128, 8, 128], F32)
        sm = [sb(f"sm{i}", [128, 16], F32) for i in range(2)]
        lg = sb("lg", [128, 36], F32)
        r1 = sb("r1", [128, 224], F32)
        Mb = sb("Mb", [128, 32], BF16)
        dstf = sb("dstf", [128, 2], F32)
        wgb = [sb(f"wgb{i}", [128, 8, 512], BF16) for i in range(2)]
        wub = [sb(f"wub{i}", [128, 8, 512], BF16) for i in range(2)]
        wdb = [sb(f"wdb{i}", [128, 4, 1024], BF16) for i in range(2)]
        NST = CAP // 128
        xe = [sb(f"xe{i}", [128, NST, 1024], BF16) for i in range(2)]
        xeT = sb("xeT", [128, 8, CAP], BF16)
        sgl = [sb(f"sgl{i}", [128, CAP], F32) for i in range(2)]
        aT = sb("aT", [128, 4, CAP], BF16)
        yo = [sb(f"yo{i}", [128, NST, 1024], BF16) for i in range(2)]
        yg = [[sb(f"yg{i}_{k}", [128, 1024], BF16) for k in range(2)] for i in range(2)]
        pM = ps("pM", [128, 512], F32)
        pP = [ps(f"pP{i}", [128, 512], F32) for i in range(2)]
        pTf = ps("pTf", [128, 1024], F32)
        pUU = ps("pUU", [128, 1024], F32)
        pTb = ps("pTb", [128, 1024], BF16)

        C.dma('sp', gos[:], gout, [], ['gos'], 'c0')
        C.dma('sp', mks[:], msk, [], ['mks'], 'c3')
        C.dma('sp', g2b[:], g2bd, [], ['g2b'], 'c1')
        C.dma('sp', wrs[:], wr.rearrange("(c p) n -> p c n", p=128), [], ['wrs'], 'c2')
        wov = wout.rearrange("(c p) n -> p c n", p=128)
        for c in range(8):
            b = c % 2
            C.dma('sp', wstg[b][:], wov[:, c, :], [], [f'wstg{b}'], f'wstg{b}')
            C.ts('dve' if b else 'pool', Woutb[:, c, :], wstg[b][:], gos[:, c:c + 1], None, ALU.mult, None,
                 [f'wstg{b}', 'gos'], [('Wo', c)])
        WoK = [('Wo', c) for c in range(8)]
        C.memset('pool', identf[:], 0.0, ['identf'])
        C.aselect(identf[:], identf[:], [[-1, 128]], ALU.not_equal, 1.0, 0, 1, ['identf'], ['identf'])
        C.cp('pool', identb[:], identf[:], ['identf'], ['identb'])
        C.memset('pool', onec[:], 1.0, ['onec'])
        C.memset('pool', ones128[:], 1.0, ['ones128'])
        C.memset('pool', ustr[:], 1.0, ['ustr'])
        C.aselect(ustr[:], ustr[:], [[1, 128]], ALU.is_ge, 0.0, -1, -1, ['ustr'], ['ustr'])
        S.add('pool', lambda e: e.iota(basef[:], pattern=[[CAP, 32]], base=0, channel_multiplier=0,
                                       allow_small_or_imprecise_dtypes=True), w=['basef'])
        C.memset('pool', zt[:], 0.0, ['zt'])
        C.memset('pool', Macc[:], 0.0, ['Macc'])
        C.memset('pool', Maccb[:], 0.0, ['Maccb'])
        Xv = Xs.rearrange("(n p) d -> p n d", p=128)
        nrt = NROWS // 128
        zi = 0
        for n0 in range(0, nrt, 4):
            n1 = min(nrt, n0 + 4)
            C.dma('sp', Xv[:, n0:n1, :], zt[:, 0:n1 - n0, :], ['zt'], [], f'z{zi % 2}', cw=['Xs'])
            zi += 1
        C.dma('sp', Ys[NSLOT:NROWS, :], zt[:, 0, :], ['zt'], [], 'zy', cw=['Ys'])
        S.add('sp', lambda e: e.nop(), r=[], w=['Xs'])

        xinv = xin.rearrange("(n p) d -> n p d", p=128)
        xmv = xmid.rearrange("(n p) d -> n p d", p=128)
        xov = xout.rearrange("(n p) d -> n p d", p=128)
        yv = yG.rearrange("(c p) t -> p c t", p=128)

        def loads(i):
            b = i % 2
            C.dma('sp', xt[b][:], xinv[i], [], [f'xt{b}'], f'lx{b}')
            for hh in range(2):
                C.dma('sp', ysc[b][hh][:], yv[:, :, hh * NT + i * 128:hh * NT + (i + 1) * 128], [], [f'ysc{b}{hh}'], f'ly{b}{hh}')
            C.ts('dve', ys[b][:], ysc[b][0][:], mks[:, 0:1], None, ALU.mult, None, [f'ysc{b}0', 'mks'], [f'ys{b}'])
            C.stt(ys[b][:], ysc[b][1][:], mks[:, 1:2], ys[b][:], ALU.mult, ALU.add, [f'ysc{b}1', 'mks', f'ys{b}'], [f'ys{b}'])

        loads(0)
        GRP = [([0, 1, 2, 3], 512.0), ([4, 5], 256.0), ([6, 7], 256.0)]
        for i in range(NTT):
            b = i % 2
            if i + 1 < NTT:
                loads(i + 1)
            s = sm[b]
            C.act(ysq[:], ys[b][:], AF.Square, [f'ys{b}'], ['ysq'])
            for gi, (cl_, n) in enumerate(GRP):
                for c in cl_:
                    C.mm(pM[:, gi:gi + 1], ysq[:, c, :], onec[:, 0:1], c == cl_[0], c == cl_[-1], ['ysq', 'onec'], ['pM'])
            C.act(s[:, 0:1], pM[:, 0:1], AF.Sqrt, ['pM'], [f's0{b}'], scale=1.0 / 512.0, bias=EPS)
            C.act(s[:, 1:3], pM[:, 1:3], AF.Sqrt, ['pM'], [f's1{b}'], scale=1.0 / 256.0, bias=EPS)
            C.recip(s[:, 3:6], s[:, 0:3], [f's0{b}', f's1{b}'], [f'rg{b}'])
            k = 0
            for half in range(2):
                for gi, (cl_, n) in enumerate(GRP):
                    bk = k % 2
                    k += 1
                    for c in cl_:
                        C.mm(pP[bk][:], ys[b][:, c, :], Woutb[:, c, half * 512:(half + 1) * 512], c == cl_[0], c == cl_[-1],
                             [f'ys{b}'] + WoK, [f'pP{bk}'])
                    src = xt[b] if gi == 0 else x1[b]
                    C.stt(x1[b][:, half * 512:(half + 1) * 512], pP[bk][:], s[:, 3 + gi:4 + gi], src[:, half * 512:(half + 1) * 512],
                          ALU.mult, ALU.add, [f'pP{bk}', f'rg{b}', f'xt{b}', ('x1', b, half)], [('x1', b, half)])
            x1k = [('x1', b, 0), ('x1', b, 1)]
            C.dma('sp', xmv[i], x1[b][:], x1k, [], f'sx{b}', cw=['xmid'])
            C.act(junk[:], x1[b][:], AF.Square, x1k, [f'ss{b}'], accum=s[:, 6:7])
            C.act(s[:, 7:8], s[:, 6:7], AF.Sqrt, [f'ss{b}'], [f'rt{b}'], scale=1.0 / 1024.0, bias=EPS)
            C.recip(s[:, 8:9], s[:, 7:8], [f'rt{b}'], [f'r2{b}'])
            C.stt(h2f[:], x1[b][:], s[:, 8:9], g2b[:], ALU.mult, ALU.mult, x1k + [f'r2{b}', 'g2b'], ['h2f'])
            C.cp('act', h2b[b][:], h2f[:], ['h2f'], [f'h2b{b}'])
            for c in range(8):
                C.tr(pTf[:, c * 128:(c + 1) * 128], h2f[:, c * 128:(c + 1) * 128], identf[:], ['h2f', 'identf'], ['pTf'])
            C.cp('dve', h2T[:].rearrange("p c t -> p (c t)"), pTf[:], ['pTf'], ['h2T'])
            for c in range(8):
                C.mm(pM[:, 8:44], h2T[:, c, :], wrs[:, c, :], c == 0, c == 7, ['h2T', 'wrs'], ['pM'])
            C.cp('act', lg[:], pM[:, 8:44], ['pM'], ['lg'])
            R = 'r1'
            S.add('dve', lambda e, s=s: e.tensor_reduce(out=s[:, 9:10], in_=lg[:, 0:4], axis=AX.X, op=ALU.max), r=['lg'], w=[f'mx{b}'])
            C.ts('dve', r1[:, 0:4], lg[:, 0:4], s[:, 9:10], None, ALU.is_equal, None, ['lg', f'mx{b}'], ['ohg'])
            C.ts('dve', s[:, 10:11], s[:, 9:10], -1.0, None, ALU.mult, None, [f'mx{b}'], [f'nmx{b}'])
            C.act(r1[:, 4:8], lg[:, 0:4], AF.Exp, ['lg', f'nmx{b}'], ['ec', f'sc{b}'], bias=s[:, 10:11], accum=s[:, 11:12])
            C.recip(s[:, 12:13], s[:, 11:12], [f'sc{b}'], [f'wgrp{b}'])
            C.ts('dve', r1[:, 8:12], r1[:, 0:4], -1.0, 1e30, ALU.add, ALU.mult, ['ohg'], ['pen'])
            for gq in range(4):
                C.ts('dve', r1[:, 16 + gq * 8:24 + gq * 8], lg[:, 4 + gq * 8:12 + gq * 8], r1[:, 8 + gq:9 + gq], None, ALU.add, None,
                     ['lg', 'pen'], [('msk', gq)])
            mk = [('msk', gq) for gq in range(4)]
            S.add('dve', lambda e: e.max(out=r1[:, 48:56], in_=r1[:, 16:48]), r=mk, w=['top8'])
            C.ts('dve', r1[:, 56:88], r1[:, 16:48], r1[:, 48:49], None, ALU.is_equal, None, mk + ['top8'], ['oh1'])
            C.ts('dve', r1[:, 88:120], r1[:, 16:48], r1[:, 49:50], None, ALU.is_equal, None, mk + ['top8'], ['oh2'])
            C.tt('dve', s[:, 13:14], r1[:, 49:50], r1[:, 48:49], ALU.subtract, ['top8'], [f'dd{b}'])
            C.act(s[:, 14:15], s[:, 13:14], AF.Exp, [f'dd{b}'], [f'ee{b}'])
            C.ts('dve', s[:, 15:16], s[:, 14:15], 1.0, None, ALU.add, None, [f'ee{b}'], [f'den{b}'])
            C.recip(s[:, 15:16], s[:, 15:16], [f'den{b}'], [f'den{b}'])
            C.tt('dve', gates[:, i, 0:1], s[:, 15:16], s[:, 12:13], ALU.mult, [f'den{b}', f'wgrp{b}'], [('g1', i)])
            C.tt('dve', gates[:, i, 1:2], gates[:, i, 0:1], s[:, 14:15], ALU.mult, [('g1', i), f'ee{b}'], [('g2', i)])
            C.tt('dve', r1[:, 120:152], r1[:, 56:88], r1[:, 88:120], ALU.add, ['oh1', 'oh2'], ['Mf'])
            C.cp('dve', Mb[:], r1[:, 120:152], ['Mf'], ['Mb'])
            C.mm(pM[:, 64:96], ustr[:], Mb[:], True, i == 0, ['ustr', 'Mb'], ['pM'])
            if i > 0:
                C.mm(pM[:, 64:96], ones128[:], Maccb[:], False, True, ['ones128', 'Maccb'], ['pM'])
            C.tt('pool', Macc[:], Macc[:], r1[:, 120:152], ALU.add, ['Macc', 'Mf'], ['Macc'])
            C.cp('pool', Maccb[:], Macc[:], ['Macc'], ['Maccb'])
            SLT = r1[:, 152:184]
            OKM = r1[:, 184:216]
            C.tt('dve', SLT, pM[:, 64:96], basef[:], ALU.add, ['pM', 'basef'], ['slot'])
            C.ts('dve', OKM, pM[:, 64:96], float(CAP), None, ALU.is_lt, None, ['pM'], ['okm'])
            C.ts('dve', SLT, SLT, -TRASH, None, ALU.add, None, ['slot'], ['slot'])
            C.tt('dve', SLT, SLT, OKM, ALU.mult, ['slot', 'okm'], ['slot'])
            C.ts('dve', SLT, SLT, TRASH, None, ALU.add, None, ['slot'], ['slot'])
            C.tt('dve', r1[:, 56:88], r1[:, 56:88], SLT, ALU.mult, ['oh1', 'slot'], ['oh1'])
            C.tt('dve', r1[:, 88:120], r1[:, 88:120], SLT, ALU.mult, ['oh2', 'slot'], ['oh2'])
            S.add('dve', lambda e: e.reduce_sum(out=dstf[:, 0:1], in_=r1[:, 56:88], axis=AX.X), r=['oh1'], w=['dstf0'])
            S.add('dve', lambda e: e.reduce_sum(out=dstf[:, 1:2], in_=r1[:, 88:120], axis=AX.X), r=['oh2'], w=['dstf1'])
            C.cp('dve', dsti[:, 2 * i:2 * i + 2], dstf[:], ['dstf0', 'dstf1'], [('dsti', i)])
            for k2 in range(2 if stage >= 2 else 0):
                S.add('pool', lambda e, i=i, k2=k2, b=b: e.indirect_dma_start(
                    out=Xs[:, :], out_offset=bass.IndirectOffsetOnAxis(ap=dsti[:, 2 * i + k2:2 * i + k2 + 1], axis=0),
                    in_=h2b[b][:], in_offset=None, oob_is_err=False),
                    r=[f'h2b{b}', ('dsti', i)], cw=['Xs'], tag=f'sc{b}{k2}')

        S.add('dve', lambda e: e.memset(junk[:, 0:8], 0.0), r=['pTf'], w=['pTfg', 'pTfu'])
        def wloads(e_):
            b = e_ % 2
            C.dma('pool', wgb[b][:], wg[e_].rearrange("(c p) f -> p c f", p=128), [], [f'wgb{b}'], f'wg{b}')
            C.dma('pool', wub[b][:], wu[e_].rearrange("(c p) f -> p c f", p=128), [], [f'wub{b}'], f'wu{b}')
            C.dma('pool', wdb[b][:], wd[e_].rearrange("(c p) f -> p c f", p=128), [], [f'wdb{b}'], f'wd{b}')

        def xloads(e_):
            b = e_ % 2
            C.dma('sp', xe[b][:], Xs[e_ * CAP:(e_ + 1) * CAP, :].rearrange("(t p) d -> p t d", p=128), ['Xs'], [f'xe{b}'], f'xe{b}')

        if stage >= 3:
            wloads(0)
            xloads(0)
        dk = 0
        for e_ in range(32 if stage >= 3 else 0):
            b = e_ % 2
            if e_ + 1 < 32:
                wloads(e_ + 1)
                xloads(e_ + 1)
            for stt_ in range(NST):
                for c in range(8):
                    C.tr(pTb[:, c * 128:(c + 1) * 128], xe[b][:, stt_, c * 128:(c + 1) * 128], identb[:], [f'xe{b}', 'identb'], ['pTb'])
                C.cp('act' if stt_ % 2 else 'dve', xeT[:, :, stt_ * 128:(stt_ + 1) * 128], pTb[:].rearrange("p (c t) -> p c t", c=8),
                     ['pTb'], [('xeT', stt_)])
            xk = [('xeT', t_) for t_ in range(NST)]
            for fc in range(4):
                for (a0, a1) in ((0, 512), (512, CAP)):
                    for c in range(8):
                        C.mm(pTf[:, a0:a1], wgb[b][:, c, fc * 128:(fc + 1) * 128], xeT[:, c, a0:a1], c == 0, c == 7, [f'wgb{b}'] + xk, ['pTfg'])
                for (a0, a1) in ((0, 512), (512, CAP)):
                    for c in range(8):
                        C.mm(pUU[:, a0:a1], wub[b][:, c, fc * 128:(fc + 1) * 128], xeT[:, c, a0:a1], c == 0, c == 7, [f'wub{b}'] + xk, ['pTfu'])
                C.act(sgl[fc % 2][:], pTf[:, 0:CAP], AF.Silu, ['pTfg'], [f'sgl{fc % 2}'])
                C.tt('dve', aT[:, fc, :], pUU[:, 0:CAP], sgl[fc % 2][:], ALU.mult, ['pTfu', f'sgl{fc % 2}'], [('aT', fc)])
            ak = [('aT', fc) for fc in range(4)]
            for stt_ in range(NST):
                for half in range(2):
                    bk = dk % 2
                    dk += 1
                    for fc in range(4):
                        C.mm(pP[bk][:], aT[:, fc, stt_ * 128:(stt_ + 1) * 128], wdb[b][:, fc, half * 512:(half + 1) * 512],
                             fc == 0, fc == 3, ak + [f'wdb{b}'], [f'pP{bk}'])
                    C.cp('act' if dk % 2 else 'dve', yo[b][:, stt_, half * 512:(half + 1) * 512], pP[bk][:], [f'pP{bk}'],
                         [('yo', b, stt_, half)])
            yk = [('yo', b, t_, h_) for t_ in range(NST) for h_ in range(2)]
            C.dma('sp', Ys[e_ * CAP:(e_ + 1) * CAP, :].rearrange("(t p) d -> p t d", p=128), yo[b][:], yk, [], f'yo{b}', cw=['Ys'])

        def gloads(i):
            b = i % 2
            C.dma('sp', xt[b][:], xmv[i], ['xmid'], [f'xt{b}'], f'lx{b}')
            for k2 in range(2 if stage >= 4 else 0):
                S.add('pool', lambda e, i=i, k2=k2, b=b: e.indirect_dma_start(
                    out=yg[b][k2][:], out_offset=None, in_=Ys[:, :],
                    in_offset=bass.IndirectOffsetOnAxis(ap=dsti[:, 2 * i + k2:2 * i + k2 + 1], axis=0),
                    oob_is_err=False),
                    r=['Ys', ('dsti', i)], w=[f'yg{b}{k2}'], tag=f'gy{b}{k2}')

        gloads(0)
        for i in range(NTT):
            b = i % 2
            if i + 1 < NTT:
                gloads(i + 1)
            if stage < 4:
                C.dma('sp', xov[i], xt[b][:], [f'xt{b}'], [], f'so{b}')
                continue
            C.stt(x1[b][:], yg[b][0][:], gates[:, i, 0:1], xt[b][:], ALU.mult, ALU.add,
                  [f'yg{b}0', ('g1', i), f'xt{b}'], [('x1', b, 0), ('x1', b, 1)])
            C.stt(x1[b][:], yg[b][1][:], gates[:, i, 1:2], x1[b][:], ALU.mult, ALU.add,
                  [f'yg{b}1', ('g2', i), ('x1', b, 0), ('x1', b, 1)], [('x1', b, 0), ('x1', b, 1)])
            C.dma('sp', xov[i], x1[b][:], [('x1', b, 0), ('x1', b, 1)], [], f'so{b}', cw=['xout'])
        if post is not None:
            post(C)
        info = S.emit()
    return info


STD_OF_YG = [0, 2, 1, 3, 4, 5, 6, 7]


def prep_ffn_inputs(inp, l):
    f = np.float32
    wr = np.ascontiguousarray(np.concatenate([inp['router_coarse'][l], inp['router_fine'][l]], axis=1), dtype=f)
    wout = np.asarray(inp['w_out'][l], dtype=f).reshape(8, 128, 1024)[STD_OF_YG].reshape(1024, 1024)
    gout = np.asarray(inp['out_norm_g'][l], dtype=f).reshape(8, 128)[STD_OF_YG].T
    return dict(
        wout=np.ascontiguousarray(wout),
        gout=np.ascontiguousarray(gout),
        g2bd=np.ascontiguousarray(np.broadcast_to(np.asarray(inp['norm2_g'][l])[None, :], (128, 1024)), dtype=f),
        wr=wr,
        wg=np.ascontiguousarray(inp['exp_w_gate'][l], dtype=f),
        wu=np.ascontiguousarray(inp['exp_w_up'][l], dtype=f),
        wd=np.ascontiguousarray(inp['exp_w_down'][l], dtype=f),
    )


MIX_KEYS = dict(wall=[1024, NW], vec=[128, NV], g1=[128, 8], wab=[128, 2, 128], pww=[128, 2, 128])
FFN_KEYS = dict(wout=[1024, 1024], gout=[128, 8], g2bd=[128, 1024], wr=[1024, 36], wg=[32, 1024, 512],
                wu=[32, 1024, 512], wd=[32, 512, 1024])
RG_PAIRS = [[0, 1], [2, 3], [4, 5], [6, 7]]


def build_fused(SL, depth=2):
    NT = SL // 2
    nc = bass.Bass("TRN2", target_bir_lowering=False)

    def din(name, shape, dt=F32):
        return nc.dram_tensor(name, shape, dt, kind="ExternalInput").ap()

    x_full = din("x_full", [SL, 1024])
    xin0 = din("xin0", [NT, 1024])
    msk = din("msk", [128, 2])
    W = []
    for l in range(depth):
        d = {k: din(f"{k}{l}", shp) for k, shp in MIX_KEYS.items()}
        d.update({k: din(f"{k}{l}", shp) for k, shp in FFN_KEYS.items()})
        W.append(d)
    xout = nc.dram_tensor("xout", [NT, 1024], F32, kind="ExternalOutput").ap()
    yT = nc.dram_tensor("yT_i", [512, SL], BF16, kind="Internal").ap()
    yG = nc.dram_tensor("yG_i", [1024, SL], BF16, kind="Internal").ap()
    Xs = nc.dram_tensor("Xs_i", [NROWS, 1024], BF16, kind="Internal").ap()
    Ys = nc.dram_tensor("Ys_i", [NROWS, 1024], BF16, kind="Internal").ap()
    xmid = nc.dram_tensor("xmid_i", [NT, 1024], F32, kind="Internal").ap()
    xo = nc.dram_tensor("xo_i", [NT, 1024], F32, kind="Internal").ap()
    xG = nc.dram_tensor("xG_i", [SL, 1024], F32, kind="Internal").ap()
    with ExitStack() as outer:
        gs = GSync(nc, outer)
        for l in range(depth):
            last = l == depth - 1
            w = W[l]

            def post_m(C):
                for k in range(4):
                    C.S.add('pool', lambda e, k=k: e.collective_compute(
                        "AllGather", ALU.bypass, replica_groups=RG_PAIRS,
                        ins=[yT[k * 128:(k + 1) * 128, :]], outs=[yG[k * 256:(k + 1) * 256, :]]),
                        r=['yT'], cw=['yG'], tag='cc', inc=1)

            def xtile(n):
                p = n * 128
                r_, q = p // NT, p % NT
                row = (q // 512) * 1024 + r_ * 512 + q % 512
                return xG[row:row + 128, :]

            build_mixer(nc, gs, SL, x_full if l == 0 else xG, w['wall'], w['vec'], w['g1'], w['wab'], w['pww'], yT, post=post_m,
                        xtile=None if l == 0 else xtile)
            nc.all_engine_barrier()

            def post_f(C, last=last):
                if not last:
                    for k in range(NT // 512):
                        C.S.add('pool', lambda e, k=k: e.collective_compute(
                            "AllGather", ALU.bypass, replica_groups=RG_PAIRS,
                            ins=[xo[k * 512:(k + 1) * 512, :]], outs=[xG[k * 1024:(k + 1) * 1024, :]]),
                            r=['xout'], cw=['xG'], tag='cc', inc=1)

            build_ffn(nc, gs, NT, xin0 if l == 0 else xo, yG, msk, w['wout'], w['gout'], w['g2bd'], w['wr'], w['wg'], w['wu'],
                      w['wd'], xout if last else xo, Xs, Ys, xmid, post=post_f)
            if not last:
                nc.all_engine_barrier()
    return nc


_NC_CACHE = {}


def run_fused(inp, X):
    B, SL, D = X.shape
    NT = SL // 2
    depth = np.asarray(inp['w_in']).shape[0]
    key = (SL, depth)
    if key not in _NC_CACHE:
        _NC_CACHE[key] = build_fused(SL, depth)
    nc = _NC_CACHE[key]
    fw = [prep_ffn_inputs(inp, l) for l in range(depth)]
    maps = []
    for c in range(8):
        b, h = c // 2, c % 2
        m = {'x_full': np.ascontiguousarray(X[b]), 'xin0': np.ascontiguousarray(X[b, h * NT:(h + 1) * NT])}
        mk = np.zeros((128, 2), np.float32)
        mk[:, h] = 1.0
        m['msk'] = mk
        for l in range(depth):
            for k, v in prep_mixer_inputs(inp, l, b, h).items():
                m[f'{k}{l}'] = v
            for k, v in fw[l].items():
                m[f'{k}{l}'] = v
        maps.append(m)
    res = run_bass_kernel_spmd(nc, maps, core_ids=list(range(8)))
    out = np.empty_like(X)
    for c in range(8):
        b, h = c // 2, c % 2
        out[b, h * NT:(h + 1) * NT] = np.asarray(res.results[c]['xout'])
    return out


def kernel(**inp):
    X = np.ascontiguousarray(np.asarray(inp['x'], dtype=np.float32))
    return run_fused(inp, X)
```

```python
import numpy as np
import ml_dtypes
from contextlib import ExitStack
import concourse.bass as bass
import concourse.mybir as mybir
from concourse.bass_utils import run_bass_kernel_spmd

F32 = mybir.dt.float32
BF16 = mybir.dt.bfloat16
I32 = mybir.dt.int32
AF = mybir.ActivationFunctionType
ALU = mybir.AluOpType
AX = mybir.AxisListType

ENG_ATTR = {'pe': 'tensor', 'act': 'scalar', 'dve': 'vector', 'pool': 'gpsimd', 'sp': 'sync'}
EPS = 1e-6


class GSync:
    def __init__(self, nc, stack):
        self.nc = nc
        self.stack = stack
        self.sems = {}
        self.cnts = {}
        self.tagmap = {}

    def sem(self, name):
        if name not in self.sems:
            self.sems[name] = self.stack.enter_context(self.nc.semaphore(name))
        return self.sems[name]

    def tagsem(self, tag):
        if tag not in self.tagmap:
            self.tagmap[tag] = 't_' + tag
        return self.tagmap[tag]


class Sched:
    def __init__(self, nc, stack, gs=None):
        self.nc = nc
        self.stack = stack
        self.gs = gs if gs is not None else GSync(nc, stack)
        self.ops = []
        self.wx = {}
        self.wc = {}
        self.rd = {}
        self.tag_last = {}
        self.sems = {}

    @staticmethod
    def _merge(dst, src):
        for s, i in src.items():
            if dst.get(s, -1) < i:
                dst[s] = i

    def add(self, eng, fn, r=(), w=(), cw=(), tag=None, inc=16):
        deps = {}
        for k in r:
            self._merge(deps, self.wx.get(k, {}))
            self._merge(deps, self.wc.get(k, {}))
        for k in w:
            self._merge(deps, self.wx.get(k, {}))
            self._merge(deps, self.wc.get(k, {}))
            self._merge(deps, self.rd.get(k, {}))
        for k in cw:
            self._merge(deps, self.wx.get(k, {}))
            self._merge(deps, self.rd.get(k, {}))
        if tag is not None and tag in self.tag_last:
            self._merge(deps, {('t', tag): self.tag_last[tag]})
        idx = len(self.ops)
        if eng == 'pe':
            deps.pop(('e', 'pe'), None)
        self.ops.append(dict(eng=eng, fn=fn, deps=deps, tag=tag, sem=None, cnt=0, inc=inc))
        sig = ('t', tag) if tag else ('e', eng)
        for k in r:
            self.rd.setdefault(k, {})[sig] = idx
        for k in w:
            self.wx[k] = {sig: idx}
            self.wc[k] = {}
            self.rd[k] = {}
        for k in cw:
            self.wc.setdefault(k, {})[sig] = idx
        if tag is not None:
            self.tag_last[tag] = idx
        return idx

    def _sem(self, name):
        return self.gs.sem(name)

    def emit(self):
        ops = self.ops
        need = set()
        for o in ops:
            for i in o['deps'].values():
                need.add(i)
        cnts = self.gs.cnts
        for i, o in enumerate(ops):
            if o['tag']:
                o['sem'] = self.gs.tagsem(o['tag'])
                cnts[o['sem']] = cnts.get(o['sem'], 0) + o['inc']
                o['cnt'] = cnts[o['sem']]
            elif i in need:
                o['sem'] = 'e_' + o['eng']
                cnts[o['sem']] = cnts.get(o['sem'], 0) + 1
                o['cnt'] = cnts[o['sem']]
        final = {}
        for o in ops:
            if o['sem']:
                final[o['sem']] = max(final.get(o['sem'], 0), o['cnt'])
        for s in final:
            self._sem(s)
        with self.nc.Block() as blk:
            for eng, attr in ENG_ATTR.items():
                mine = [o for o in ops if o['eng'] == eng]

                def body(e, mine=mine, eng=eng):
                    waited = {}
                    for o in mine:
                        for di in sorted(o['deps'].values()):
                            d = ops[di]
                            if waited.get(d['sem'], 0) < d['cnt']:
                                e.wait_ge(self._sem(d['sem']), d['cnt'])
                                waited[d['sem']] = d['cnt']
                        ins = o['fn'](e)
                        if o['sem']:
                            ins.then_inc(self._sem(o['sem']), o['inc'] if o['tag'] else 1)
                    if eng == 'sp':
                        for s, c in final.items():
                            if waited.get(s, 0) < c:
                                e.wait_ge(self._sem(s), c)

                getattr(blk, attr)(body)
        return dict(nops=len(ops), final=final)


class Ctx:
    def __init__(self, nc, st, gs=None):
        self.nc = nc
        self.st = st
        self.S = Sched(nc, st, gs)
        g = self.S.gs
        g.phase = getattr(g, 'phase', 0) + 1
        self.pfx = f"p{g.phase}_"

    def sb(self, name, shape, dt):
        return self.st.enter_context(self.nc.sbuf_tensor(self.pfx + name, shape, dt))

    def ps(self, name, shape, dt):
        return self.st.enter_context(self.nc.psum_tensor(self.pfx + name, shape, dt))

    def dma(self, q, out, in_, r, w, tag, cw=()):
        self.S.add(q, lambda e: e.dma_start(out=out, in_=in_), r=r, w=w, cw=cw, tag=tag)

    def mm(self, out, lhsT, rhs, start, stop, r, w):
        self.S.add('pe', lambda e: e.matmul(out, lhsT=lhsT, rhs=rhs, start=start, stop=stop,
                                            skip_group_check=True), r=r, w=w)

    def tr(self, out, in_, ident, r, w):
        self.S.add('pe', lambda e: e.transpose(out, in_, ident), r=r, w=w)

    def act(self, out, in_, func, r, w, bias=None, scale=None, accum=None):
        kw = {}
        if bias is not None:
            kw['bias'] = bias
        if scale is not None:
            kw['scale'] = scale
        if accum is not None:
            kw['accum_out'] = accum
        self.S.add('act', lambda e: e.activation(out=out, in_=in_, func=func, **kw), r=r, w=w)

    def ts(self, eng, out, in0, s1, s2, op0, op1, r, w):
        if op1 is None:
            self.S.add(eng, lambda e: e.tensor_scalar(out=out, in0=in0, scalar1=s1, scalar2=None, op0=op0), r=r, w=w)
        else:
            self.S.add(eng, lambda e: e.tensor_scalar(out=out, in0=in0, scalar1=s1, scalar2=s2, op0=op0, op1=op1), r=r, w=w)

    def tt(self, eng, out, in0, in1, op, r, w):
        self.S.add(eng, lambda e: e.tensor_tensor(out=out, in0=in0, in1=in1, op=op), r=r, w=w)

    def stt(self, out, in0, scalar, in1, op0, op1, r, w):
        self.S.add('dve', lambda e: e.scalar_tensor_tensor(out=out, in0=in0, scalar=scalar, in1=in1, op0=op0, op1=op1), r=r, w=w)

    def cp(self, eng, out, in_, r, w):
        if eng == 'act':
            self.S.add('act', lambda e: e.copy(out=out, in_=in_), r=r, w=w)
        else:
            self.S.add(eng, lambda e: e.tensor_copy(out=out, in_=in_), r=r, w=w)

    def memset(self, eng, ap, val, w):
        self.S.add(eng, lambda e: e.memset(ap, val), w=w)

    def recip(self, out, in_, r, w):
        self.S.add('dve', lambda e: e.reciprocal(out=out, in_=in_), r=r, w=w)

    def aselect(self, out, in_, pattern, op, fill, base, cm, r, w):
        self.S.add('pool', lambda e: e.affine_select(out=out, in_=in_, pattern=pattern, compare_op=op, fill=fill,
                                                     base=base, channel_multiplier=cm), r=r, w=w)


V_GQ, V_GK, V_LCW, V_LCB, V_BA, V_BX, V_LAM = 0, 1, 2, 6, 7, 8, 9
V_DWB, V_LNG, V_LNB, V_PWB, V_DWW = 10, 12, 14, 16, 17
NV = 17 + 62
NW = 1536


def build_mixer(nc, gs, SL, x, wall, vec, g1, wab, pww, yT, post=None, xtile=None):
    NG = SL // 512
    with ExitStack() as st:
        C = Ctx(nc, st, gs)
        S = C.S
        sb, ps = C.sb, C.ps
        Wb = sb("Wb", [128, 8, NW], BF16)
        wst = [sb(f"wst{i}", [128, 384], F32) for i in range(2)]
        vecs = sb("vecs", [128, NV], F32)
        g1s = sb("g1s", [128, 8], F32)
        wabf = sb("wabf", [128, 2, 128], F32)
        wabb = sb("wabb", [128, 2, 128], BF16)
        pwf = sb("pwf", [128, 2, 128], F32)
        pwb = sb("pwb", [128, 2, 128], BF16)
        identb = sb("identb", [128, 128], BF16)
        blk1 = sb("blk1", [128, 128], BF16)
        onesm = sb("onesm", [128, 128], F32)
        uincl = sb("uincl", [128, 128], BF16)
        negones = sb("negones", [128, 128], BF16)
        tri = sb("tri", [128, 128], BF16)
        dg = sb("dg", [128, 62, 128], BF16)
        cl = sb("cl", [128, 4], F32)
        KT = sb("KT", [128, 2, SL], BF16)
        Vt = sb("Vt", [128, SL // 128, 256], BF16)
        QT = sb("QT", [128, 2, 512], BF16)
        xt = [sb(f"xt{i}", [128, 1024], F32) for i in range(2)]
        xn = [sb(f"xn{i}", [128, 1024], BF16) for i in range(2)]
        st1 = [sb(f"st1_{i}", [128, 4], F32) for i in range(2)]
        hT = sb("hT", [128, 8, 512], BF16)
        sq = sb("sq", [128, 512], BF16)
        rs = sb("rs", [128, 512], F32)
        xrb = sb("xrb", [128, 515], F32)
        xgb = sb("xgb", [128, 512], F32)
        cv = sb("cv", [128, 512], F32)
        cvb = sb("cvb", [128, 512], BF16)
        lr = sb("lr", [128, 512], F32)
        li = sb("li", [128, 512], F32)
        la = sb("la", [128, 512], F32)
        la2 = sb("la2", [128, 512], F32)
        lu = sb("lu", [128, 512], F32)
        lh = sb("lh", [128, 512], F32)
        hprev = sb("hprev", [128, 1], F32)
        gt = sb("gt", [128, 512], F32)
        ybs = sb("ybs", [128, 512], BF16)
        ub = sb("ub", [128, 2, 542], BF16)
        sg = sb("sg", [128, 512], F32)
        cvo = sb("cvo", [128, 2, 512], F32)
        sqo = sb("sqo", [128, 2, 512], F32)
        means = sb("means", [128, 512], F32)
        m2 = sb("m2", [128, 512], F32)
        crs = sb("crs", [128, 512], F32)
        cn = sb("cn", [128, 2, 512], F32)
        csb = sb("csb", [128, 2, 512], BF16)
        ycs = sb("ycs", [128, 512], BF16)
        LA = 2
        NRB = LA + 2
        Eb = [sb(f"Eb{i}", [128, 512], BF16) for i in range(NRB)]
        Lb = [sb(f"Lb{i}", [128, 512], BF16) for i in range(NRB)]
        NWB = 3
        Wt = [sb(f"Wt{i}", [128, 512], BF16) for i in range(NWB)]
        LaccP = [sb(f"Lacc{i}", [128, 512], F32) for i in range(2)]
        xC = [sb(f"xC{i}", [128, 512], BF16) for i in range(2)]
        Laccb = [sb(f"Laccb{i}", [128, 512], BF16) for i in range(NRB)]
        yst = [sb(f"yst{i}", [64, 512], BF16) for i in range(2)]
        pA = [ps(f"pA{i}", [128, 512], F32) for i in range(2)]
        pT = ps("pT", [128, 1024], BF16)
        pZ = [ps(f"pZ{i}", [128, 512], F32) for i in range(2)]
        pC = [ps(f"pC{i}", [128, 512], F32) for i in range(2)]
        pO = ps("pO", [128, 512], F32)

        C.dma('sp', vecs[:], vec, [], ['vecs'], 'c0')
        C.dma('sp', g1s[:], g1, [], ['g1s'], 'c1')
        C.dma('sp', wabf[:], wab, [], ['wabf'], 'c2')
        C.dma('sp', pwf[:], pww, [], ['pwf'], 'c3')
        C.cp('dve', wabb[:], wabf[:], ['wabf'], ['wabb'])
        C.cp('dve', pwb[:], pwf[:], ['pwf'], ['pwb'])
        wv = wall.rearrange("(c p) n -> p c n", p=128)
        k = 0
        for c in range(8):
            for hf in range(4):
                b = k % 2
                C.dma('sp', wst[b][:], wv[:, c, hf * 384:(hf + 1) * 384], [], [f'wst{b}'], f'wst{b}')
                C.ts('dve' if k % 2 == 0 else 'pool', Wb[:, c, hf * 384:(hf + 1) * 384], wst[b][:], g1s[:, c:c + 1], None,
                     ALU.mult, None, [f'wst{b}', 'g1s'], [('Wb', c, hf)])
                k += 1
        WbK = [('Wb', c, hf) for c in range(8) for hf in range(4)]
        C.memset('pool', identb[:], 0.0, ['identb'])
        C.aselect(identb[:], identb[:], [[-1, 128]], ALU.not_equal, 1.0, 0, 1, ['identb'], ['identb'])
        C.memset('pool', blk1[:], 0.0, ['blk1'])
        C.memset('pool', blk1[0:64, 0:64], 1.0, ['blk1'])
        C.memset('pool', blk1[64:128, 64:128], 1.0, ['blk1'])
        C.memset('pool', onesm[:], 1.0 / 256.0, ['onesm'])
        C.memset('pool', negones[:], -1.0, ['negones'])
        C.memset('pool', uincl[:], -1.0, ['uincl'])
        C.aselect(uincl[:], uincl[:], [[-1, 128]], ALU.is_ge, 0.0, 0, 1, ['uincl'], ['uincl'])
        C.memset('pool', tri[:], 1.0, ['tri'])
        C.aselect(tri[:], tri[:], [[1, 128]], ALU.is_ge, 0.0, -1, -1, ['tri'], ['tri'])
        for i in range(62):
            C.ts('pool' if i % 2 else 'dve', dg[:, i, :], identb[:], vecs[:, V_DWW + i:V_DWW + i + 1], None, ALU.mult, None,
                 ['identb', 'vecs'], ['dg'])
        C.act(cl[:, 0:1], vecs[:, V_LAM:V_LAM + 1], AF.Exp, ['vecs'], ['cl0'], scale=-1.0)
        C.act(cl[:, 1:2], cl[:, 0:1], AF.Ln, ['cl0'], ['cl1'], bias=1.0)
        C.ts('dve', cl[:, 2:3], cl[:, 1:2], -8.0, None, ALU.mult, None, ['cl1'], ['cl2'])
        C.ts('dve', cl[:, 3:4], cl[:, 1:2], -16.0, None, ALU.mult, None, ['cl1'], ['cl3'])
        C.memset('pool', hprev[:], 0.0, ['hprev'])
        C.memset('pool', xrb[:, 0:3], 0.0, ['xrb_h'])
        C.memset('pool', ub[:, :, 0:30], 0.0, ['ub_h'])

        xv = x.rearrange("(n p) d -> n p d", p=128)
        wcnt = [0]
        ocnt = [0]
        acnt = [0]
        bcnt = [0]
        lcnt = [0]
        pcnt = [0]
        for g in range(NG):
            t0 = g * 512
            for tt in range(4):
                b = tt % 2
                C.dma('sp', xt[b][:], xv[g * 4 + tt] if xtile is None else xtile(g * 4 + tt), [], [f'xt{b}'], f'x{b}')
                C.act(xn[b][:], xt[b][:], AF.Square, [f'xt{b}'], [f'ss{b}', f'xn{b}'], accum=st1[b][:, 0:1])
                C.act(st1[b][:, 1:2], st1[b][:, 0:1], AF.Sqrt, [f'ss{b}'], [f'rt{b}'], scale=1.0 / 1024.0, bias=EPS)
                C.recip(st1[b][:, 2:3], st1[b][:, 1:2], [f'rt{b}'], [f'rstd{b}'])
                C.ts('dve', xn[b][:], xt[b][:], st1[b][:, 2:3], None, ALU.mult, None, [f'xt{b}', f'rstd{b}'], [f'xn{b}'])
                for c in range(8):
                    C.tr(pT[:, c * 128:(c + 1) * 128], xn[b][:, c * 128:(c + 1) * 128], identb[:], [f'xn{b}', 'identb'], ['pT'])
                C.cp('act' if tt % 2 else 'dve', hT[:, :, tt * 128:(tt + 1) * 128], pT[:].rearrange("p (c t) -> p c t", c=8),
                     ['pT'], [('hT', tt)])
            hTK = [('hT', tt) for tt in range(4)]
            pidx = [0]

            def proj(ct):
                bank = pidx[0] % 2
                pidx[0] += 1
                for c in range(8):
                    C.mm(pA[bank][:], Wb[:, c, ct * 128:(ct + 1) * 128], hT[:, c, :], c == 0, c == 7, WbK + hTK, [f'pA{bank}'])
                return bank

            for ct in range(4):
                bk = proj(ct)
                C.act(sq[:], pA[bk][:], AF.Square, [f'pA{bk}'], ['sq'])
                C.mm(pA[1 - bk][:], blk1[:], sq[:], True, True, ['blk1', 'sq'], [f'pA{1 - bk}'])
                pidx[0] += 1
                if ct < 2:
                    C.act(rs[:], pA[1 - bk][:], AF.Sqrt, [f'pA{1 - bk}'], ['rs'], scale=1.0, bias=64.0 * EPS)
                else:
                    C.act(rs[:], pA[1 - bk][:], AF.Sqrt, [f'pA{1 - bk}'], ['rs'], scale=1.0 / 64.0, bias=EPS)
                C.recip(rs[:], rs[:], ['rs'], ['rs'])
                if ct < 2:
                    C.stt(QT[:, ct, :], pA[bk][:], vecs[:, V_GQ:V_GQ + 1], rs[:], ALU.mult, ALU.mult,
                          [f'pA{bk}', 'vecs', 'rs'], [('QT', ct)])
                else:
                    C.stt(KT[:, ct - 2, t0:t0 + 512], pA[bk][:], vecs[:, V_GK:V_GK + 1], rs[:], ALU.mult, ALU.mult,
                          [f'pA{bk}', 'vecs', 'rs'], [('KT', ct - 2, g)])
            bk = proj(4)
            C.cp('act', xrb[:, 3:515], pA[bk][:], [f'pA{bk}'], ['xrb'])
            bk = proj(5)
            C.cp('dve', xgb[:], pA[bk][:], [f'pA{bk}'], ['xgb'])
            for t in range(2):
                bv = proj(6 + t)
                bg = proj(8 + t)
                C.act(sg[:], pA[bg][:], AF.Sigmoid, [f'pA{bg}'], ['sg'])
                C.tt('dve', ub[:, t, 30:542], pA[bv][:], sg[:], ALU.mult, [f'pA{bv}', 'sg'], [('ub', t)])
            for tt in range(4):
                bank = pidx[0] % 2
                pidx[0] += 1
                for c in range(8):
                    C.mm(pA[bank][:, 0:256], hT[:, c, tt * 128:(tt + 1) * 128], Wb[:, c, 1280:1536], c == 0, c == 7,
                         WbK + hTK, [f'pA{bank}'])
                C.cp('act' if tt % 2 else 'dve', Vt[:, g * 4 + tt, :], pA[bank][:, 0:256], [f'pA{bank}'], [('V', g * 4 + tt)])

            vc = lambda i: vecs[:, i:i + 1]
            C.ts('dve', cv[:], xrb[:, 3:515], vc(V_LCW + 3), vc(V_LCB), ALU.mult, ALU.add, ['xrb', 'xrb_h', 'vecs'], ['cv'])
            for kk in range(3):
                C.stt(cv[:], xrb[:, kk:kk + 512], vc(V_LCW + kk), cv[:], ALU.mult, ALU.add, ['xrb', 'xrb_h', 'vecs', 'cv'], ['cv'])
            C.cp('pool', xrb[:, 0:3], xrb[:, 512:515], ['xrb'], ['xrb_h'])
            C.cp('pool', cvb[:], cv[:], ['cv'], ['cvb'])
            b0 = pidx[0] % 2
            pidx[0] += 2
            C.mm(pA[b0][:], wabb[:, 0, :], cvb[:], True, True, ['wabb', 'cvb'], [f'pA{b0}'])
            C.mm(pA[1 - b0][:], wabb[:, 1, :], cvb[:], True, True, ['wabb', 'cvb'], [f'pA{1 - b0}'])
            C.act(lr[:], pA[b0][:], AF.Sigmoid, [f'pA{b0}', 'vecs'], ['lr'], bias=vc(V_BA))
            C.act(li[:], pA[1 - b0][:], AF.Sigmoid, [f'pA{1 - b0}', 'vecs'], ['li'], bias=vc(V_BX))
            C.act(la[:], lr[:], AF.Exp, ['lr', 'cl2'], ['la'], scale=cl[:, 2:3])
            C.act(la2[:], lr[:], AF.Exp, ['lr', 'cl3'], ['la2'], scale=cl[:, 3:4])
            C.act(la2[:], la2[:], AF.Sqrt, ['la2'], ['la2'], scale=-1.0, bias=1.0)
            C.tt('pool', lu[:], li[:], cv[:], ALU.mult, ['li', 'cv'], ['lu'])
            C.tt('pool', lu[:], lu[:], la2[:], ALU.mult, ['lu', 'la2'], ['lu'])
            S.add('dve', lambda e: e.tensor_tensor_scan(out=lh[:], data0=la[:], data1=lu[:], initial=hprev[:, 0:1],
                                                        op0=ALU.mult, op1=ALU.add), r=['la', 'lu', 'hprev'], w=['lh'])
            C.cp('pool', hprev[:], lh[:, 511:512], ['lh'], ['hprev'])
            C.tt('pool', gt[:], xgb[:], xgb[:], ALU.mult, ['xgb'], ['gt'])
            C.ts('pool', gt[:], gt[:], 0.044715, 1.0, ALU.mult, ALU.add, ['gt'], ['gt'])
            C.tt('pool', gt[:], gt[:], xgb[:], ALU.mult, ['gt', 'xgb'], ['gt'])
            C.act(gt[:], gt[:], AF.Sigmoid, ['gt'], ['gt'], scale=1.5957691216)
            C.tt('pool', gt[:], gt[:], xgb[:], ALU.mult, ['gt', 'xgb'], ['gt'])
            C.tt('dve', ybs[:], gt[:], lh[:], ALU.mult, ['gt', 'lh'], ['ybs'])
            C.dma('pool', yT[256:384, t0:t0 + 512], ybs[:], ['ybs'], [], 'yb', cw=['yT'])

            for t in range(2):
                bank = pidx[0] % 2
                pidx[0] += 1
                for kk in range(31):
                    C.mm(pA[bank][:], dg[:, t * 31 + kk, :], ub[:, t, kk:kk + 512], kk == 0, kk == 30,
                         ['dg', ('ub', t), 'ub_h'], [f'pA{bank}'])
                C.act(cvo[:, t, :], pA[bank][:], AF.Identity, [f'pA{bank}', 'vecs'], [('cvo', t)], bias=vc(V_DWB + t))
                C.act(sqo[:, t, :], pA[bank][:], AF.Square, [f'pA{bank}', 'vecs'], [('sqo', t)], bias=vc(V_DWB + t))
            C.cp('pool', ub[:, :, 0:30], ub[:, :, 512:542], [('ub', 0), ('ub', 1)], ['ub_h'])
            bm = pidx[0] % 2
            pidx[0] += 2
            for t in range(2):
                C.mm(pA[bm][:], onesm[:], cvo[:, t, :], t == 0, t == 1, ['onesm', ('cvo', t)], [f'pA{bm}'])
            for t in range(2):
                C.mm(pA[1 - bm][:], onesm[:], sqo[:, t, :], t == 0, t == 1, ['onesm', ('sqo', t)], [f'pA{1 - bm}'])
            C.cp('act', means[:], pA[bm][:], [f'pA{bm}'], ['means'])
            C.tt('pool', m2[:], means[:], means[:], ALU.mult, ['means'], ['m2'])
            C.tt('dve', m2[:], pA[1 - bm][:], m2[:], ALU.subtract, [f'pA{1 - bm}', 'm2'], ['m2'])
            C.act(crs[:], m2[:], AF.Sqrt, ['m2'], ['crs'], scale=1.0, bias=EPS)
            C.recip(crs[:], crs[:], ['crs'], ['crs'])
            for t in range(2):
                C.tt('pool', cn[:, t, :], cvo[:, t, :], means[:], ALU.subtract, [('cvo', t), 'means'], [('cn', t)])
                C.tt('dve' if t else 'pool', cn[:, t, :], cn[:, t, :], crs[:], ALU.mult, [('cn', t), 'crs'], [('cn', t)])
                C.act(csb[:, t, :], cn[:, t, :], AF.Silu, [('cn', t), 'vecs'], [('csb', t)], scale=vc(V_LNG + t), bias=vc(V_LNB + t))
            bank = pidx[0] % 2
            pidx[0] += 1
            for t in range(2):
                C.mm(pA[bank][:], pwb[:, t, :], csb[:, t, :], t == 0, t == 1, ['pwb', ('csb', t)], [f'pA{bank}'])
            C.act(ycs[:], pA[bank][:], AF.Identity, [f'pA{bank}', 'vecs'], ['ycs'], bias=vc(V_PWB))
            C.dma('pool', yT[384:512, t0:t0 + 512], ycs[:], ['ycs'], [], 'yc', cw=['yT'])

            items = []
            for hd in range(4):
                nblk = 4 * g + 4
                for bi, kb in enumerate(range(4 * g + 3, -1, -1)):
                    items.append(dict(hd=hd, bi=bi, kb=kb, nblk=nblk))

            def stageA(it):
                hd, bi, kb = it['hd'], it['bi'], it['kb']
                ct = hd // 2
                pb = 64 * (hd % 2)
                Qh = QT[pb:pb + 64, ct, :]
                j = kb - 4 * g
                c0 = 128 * j if j > 0 else 0
                Kh = KT[pb:pb + 64, ct, kb * 128:(kb + 1) * 128]
                kkey = ('KT', ct, kb // 4)
                zb = acnt[0] % 2
                lb = acnt[0] % NRB
                eb = acnt[0] % NRB
                acnt[0] += 1
                it.update(c0=c0, j=j, Kh=Kh, kkey=kkey, Qh=Qh, ct=ct, lb=lb, eb=eb)
                if bi == 0:
                    C.memset('pool', LaccP[0][:], 0.0, ['Lacc0'])
                    C.memset('pool', LaccP[1][:], 0.0, ['Lacc1'])
                    pcnt[0] = 0
                C.mm(pZ[zb][:, c0:], Kh, Qh[:, c0:], True, True, [kkey, ('QT', ct)], [f'pZ{zb}'])
                C.act(Eb[eb][:, c0:], pZ[zb][:, c0:], AF.Exp, [f'pZ{zb}'], [f'Eb{eb}'])
                C.act(Lb[lb][:, c0:], Eb[eb][:, c0:], AF.Ln, [f'Eb{eb}'], [f'Lb{lb}'], bias=1.0)
                if j >= 0:
                    C.tt('dve', Lb[lb][:, c0:c0 + 128], Lb[lb][:, c0:c0 + 128], tri[:], ALU.mult, [f'Lb{lb}', 'tri'], [f'Lb{lb}'])
                if kb > 0:
                    la_ = lcnt[0] % NRB
                    lcnt[0] += 1
                    pp = pcnt[0] % 2
                    pcnt[0] += 1
                    Lo, Ln_ = LaccP[pp], LaccP[1 - pp]
                    ko, kn = f'Lacc{pp}', f'Lacc{1 - pp}'
                    C.tt('pool', Ln_[:, c0:], Lo[:, c0:], Lb[lb][:, c0:], ALU.add, [ko, f'Lb{lb}'], [kn])
                    C.cp('dve', Laccb[la_][:], Ln_[:], [kn], [f'Laccb{la_}'])
                    it['la_out'] = la_

            def stageB(it, prev):
                hd, bi, kb, c0, j = it['hd'], it['bi'], it['kb'], it['c0'], it['j']
                Kh, kkey, Qh, ct, lb = it['Kh'], it['kkey'], it['Qh'], it['ct'], it['lb']
                cb = bcnt[0] % 2
                bcnt[0] += 1
                C.mm(pC[cb][:, c0:], uincl[:], Lb[lb][:, c0:], True, bi == 0, ['uincl', f'Lb{lb}'], [f'pC{cb}'])
                if bi > 0:
                    la_ = prev['la_out']
                    C.mm(pC[cb][:, c0:], negones[:], Laccb[la_][:, c0:], False, True, ['negones', f'Laccb{la_}'], [f'pC{cb}'])
                wi = wcnt[0] % NWB
                wcnt[0] += 1
                eb = it['eb']
                C.act(xC[cb][:, c0:], pC[cb][:, c0:], AF.Exp, [f'pC{cb}'], [f'xC{cb}'])
                C.tt('dve', Wt[wi][:, c0:], xC[cb][:, c0:], Eb[eb][:, c0:], ALU.mult, [f'xC{cb}', f'Eb{eb}'], [f'Wt{wi}'])
                if j >= 0:
                    C.tt('dve', Wt[wi][:, c0:c0 + 128], Wt[wi][:, c0:c0 + 128], tri[:], ALU.mult, [f'Wt{wi}', 'tri'], [f'Wt{wi}'])
                it['wi'] = wi

            def stagePV(it):
                hd, bi, kb, c0 = it['hd'], it['bi'], it['kb'], it['c0']
                wi = it['wi']
                C.mm(pO[0:64, c0:], Vt[:, kb, hd * 64:(hd + 1) * 64], Wt[wi][:, c0:], bi == 0, bi == it['nblk'] - 1,
                     [('V', kb), f'Wt{wi}'], ['pO'])
                if bi == it['nblk'] - 1:
                    ob = ocnt[0] % 2
                    ocnt[0] += 1
                    C.cp('dve', yst[ob][:], pO[0:64, :], ['pO'], [f'yst{ob}'])
                    C.dma('sp', yT[hd * 64:(hd + 1) * 64, t0:t0 + 512], yst[ob][:], [f'yst{ob}'], [], f'ya{ob}', cw=['yT'])

            for n_ in range(min(LA, len(items))):
                stageA(items[n_])
            for n_ in range(len(items)):
                if n_ > 0:
                    stagePV(items[n_ - 1])
                stageB(items[n_], items[n_ - 1] if n_ > 0 else None)
                if n_ + LA < len(items):
                    stageA(items[n_ + LA])
            stagePV(items[-1])
        if post is not None:
            post(C)
        info = S.emit()
    return info


def prep_mixer_inputs(inp, l, b, hg, SL=None):
    f = np.float32
    w_in = np.asarray(inp['w_in'][l])
    hs = slice(hg * 256, (hg + 1) * 256)
    q = w_in[:, 0:512][:, hs]
    k = w_in[:, 512:1024][:, hs]
    v = w_in[:, 1024:1536][:, hs]
    cs = slice(hg * 128, (hg + 1) * 128)
    xr = w_in[:, 1536:1792][:, cs]
    xg = w_in[:, 1792:2048][:, cs]
    cval = w_in[:, 2048:2304]
    cgate = w_in[:, 2304:2560]
    wall = np.ascontiguousarray(np.concatenate([q, k, xr, xg, cval, cgate, v], axis=1), dtype=f)
    vec = np.zeros((128, NV), f)
    vec[:, V_GQ] = np.tile(inp['q_norm_g'][l], 2)
    vec[:, V_GK] = np.tile(inp['k_norm_g'][l], 2)
    for kk in range(4):
        vec[:, V_LCW + kk] = inp['lru_conv_w'][l][kk, cs]
    vec[:, V_LCB] = inp['lru_conv_b'][l][cs]
    vec[:, V_BA] = inp['lru_ba'][l][cs]
    vec[:, V_BX] = inp['lru_bx'][l][cs]
    vec[:, V_LAM] = inp['lru_lambda'][l][cs]
    for t in range(2):
        ts_ = slice(t * 128, (t + 1) * 128)
        vec[:, V_DWB + t] = inp['conf_dw_b'][l][ts_]
        vec[:, V_LNG + t] = inp['conf_ln_g'][l][ts_]
        vec[:, V_LNB + t] = inp['conf_ln_b'][l][ts_]
        for kk in range(31):
            vec[:, V_DWW + t * 31 + kk] = inp['conf_dw_w'][l][kk, ts_]
    vec[:, V_PWB] = inp['conf_pw_b'][l][cs]
    g1 = np.ascontiguousarray(np.asarray(inp['norm1_g'][l]).reshape(8, 128).T, dtype=f)
    wab = np.zeros((128, 2, 128), f)
    for i in range(2):
        blk = hg * 2 + i
        wab[i * 64:(i + 1) * 64, 0, i * 64:(i + 1) * 64] = inp['lru_wa'][l][blk]
        wab[i * 64:(i + 1) * 64, 1, i * 64:(i + 1) * 64] = inp['lru_wx'][l][blk]
    pw = np.asarray(inp['conf_pw_w'][l])[:, cs]
    pww = np.ascontiguousarray(pw.reshape(2, 128, 128).transpose(1, 0, 2), dtype=f)
    return dict(wall=wall, vec=vec, g1=g1, wab=wab, pww=pww)


CAP = 640
NSLOT = 32 * CAP
NROWS = NSLOT + 128
TRASH = float(NSLOT)


def build_ffn(nc, gs, NT, xin, yG, msk, wout, gout, g2bd, wr, wg, wu, wd, xout, Xs, Ys, xmid, post=None):
    NTT = NT // 128
    stage = 4
    dbg = False
    with ExitStack() as st:
        C = Ctx(nc, st, gs)
        S = C.S
        sb, ps = C.sb, C.ps
        mks = sb("mks", [128, 2], F32)
        ysc = [[sb(f"ysc{i}_{k}", [128, 8, 128], BF16) for k in range(2)] for i in range(2)]
        Woutb = sb("Woutb", [128, 8, 1024], BF16)
        wstg = [sb(f"wstg{i}", [128, 1024], F32) for i in range(2)]
        gos = sb("gos", [128, 8], F32)
        g2b = sb("g2b", [128, 1024], F32)
        wrs = sb("wrs", [128, 8, 36], F32)
        identf = sb("identf", [128, 128], F32)
        identb = sb("identb", [128, 128], BF16)
        onec = sb("onec", [128, 2], BF16)
        ustr = sb("ustr", [128, 128], BF16)
        ones128 = sb("ones128", [128, 128], BF16)
        basef = sb("basef", [128, 32], F32)
        zt = sb("zt", [128, 4, 1024], BF16)
        gates = sb("gates", [128, NTT, 2], F32)
        dsti = sb("dsti", [128, NTT * 2], I32)
        Macc = sb("Macc", [128, 32], F32)
        Maccb = sb("Maccb", [128, 32], BF16)
        xt = [sb(f"xt{i}", [128, 1024], F32) for i in range(2)]
        ys = [sb(f"ys{i}", [128, 8, 128], BF16) for i in range(2)]
        ysq = sb("ysq", [128, 8, 128], BF16)
        x1 = [sb(f"x1_{i}", [128, 1024], F32) for i in range(2)]
        junk = sb("junk", [128, 1024], BF16)
        h2f = sb("h2f", [128, 1024], F32)
        h2b = [sb(f"h2b{i}", [128, 1024], BF16) for i in range(2)]
        h2T = sb("h2T", [128, 8, 128], F32)
        sm = [sb(f"sm{i}", [128, 16], F32) for i in range(2)]
        lg = sb("lg", [128, 36], F32)
        r1 = sb("r1", [128, 224], F32)
        Mb = sb("Mb", [128, 32], BF16)
        dstf = sb("dstf", [128, 2], F32)
        wgb = [sb(f"wgb{i}", [128, 8, 512], BF16) for i in range(2)]
        wub = [sb(f"wub{i}", [128, 8, 512], BF16) for i in range(2)]
        wdb = [sb(f"wdb{i}", [128, 4, 1024], BF16) for i in range(2)]
        NST = CAP // 128
        xe = [sb(f"xe{i}", [128, NST, 1024], BF16) for i in range(2)]
        xeT = sb("xeT", [128, 8, CAP], BF16)
        sgl = [sb(f"sgl{i}", [128, CAP], F32) for i in range(2)]
        aT = sb("aT", [128, 4, CAP], BF16)
        yo = [sb(f"yo{i}", [128, NST, 1024], BF16) for i in range(2)]
        yg = [[sb(f"yg{i}_{k}", [128, 1024], BF16) for k in range(2)] for i in range(2)]
        pM = ps("pM", [128, 512], F32)
        pP = [ps(f"pP{i}", [128, 512], F32) for i in range(2)]
        pTf = ps("pTf", [128, 1024], F32)
        pUU = ps("pUU", [128, 1024], F32)
        pTb = ps("pTb", [128, 1024], BF16)

        C.dma('sp', gos[:], gout, [], ['gos'], 'c0')
        C.dma('sp', mks[:], msk, [], ['mks'], 'c3')
        C.dma('sp', g2b[:], g2bd, [], ['g2b'], 'c1')
        C.dma('sp', wrs[:], wr.rearrange("(c p) n -> p c n", p=128), [], ['wrs'], 'c2')
        wov = wout.rearrange("(c p) n -> p c n", p=128)
        for c in range(8):
            b = c % 2
            C.dma('sp', wstg[b][:], wov[:, c, :], [], [f'wstg{b}'], f'wstg{b}')
            C.ts('dve' if b else 'pool', Woutb[:, c, :], wstg[b][:], gos[:, c:c + 1], None, ALU.mult, None,
                 [f'wstg{b}', 'gos'], [('Wo', c)])
        WoK = [('Wo', c) for c in range(8)]
        C.memset('pool', identf[:], 0.0, ['identf'])
        C.aselect(identf[:], identf[:], [[-1, 128]], ALU.not_equal, 1.0, 0, 1, ['identf'], ['identf'])
        C.cp('pool', identb[:], identf[:], ['identf'], ['identb'])
        C.memset('pool', onec[:], 1.0, ['onec'])
        C.memset('pool', ones128[:], 1.0, ['ones128'])
        C.memset('pool', ustr[:], 1.0, ['ustr'])
        C.aselect(ustr[:], ustr[:], [[1, 128]], ALU.is_ge, 0.0, -1, -1, ['ustr'], ['ustr'])
        S.add('pool', lambda e: e.iota(basef[:], pattern=[[CAP, 32]], base=0, channel_multiplier=0,
                                       allow_small_or_imprecise_dtypes=True), w=['basef'])
        C.memset('pool', zt[:], 0.0, ['zt'])
        C.memset('pool', Macc[:], 0.0, ['Macc'])
        C.memset('pool', Maccb[:], 0.0, ['Maccb'])
        Xv = Xs.rearrange("(n p) d -> p n d", p=128)
        nrt = NROWS // 128
        zi = 0
        for n0 in range(0, nrt, 4):
            n1 = min(nrt, n0 + 4)
            C.dma('sp', Xv[:, n0:n1, :], zt[:, 0:n1 - n0, :], ['zt'], [], f'z{zi % 2}', cw=['Xs'])
            zi += 1
        C.dma('sp', Ys[NSLOT:NROWS, :], zt[:, 0, :], ['zt'], [], 'zy', cw=['Ys'])
        S.add('sp', lambda e: e.nop(), r=[], w=['Xs'])

        xinv = xin.rearrange("(n p) d -> n p d", p=128)
        xmv = xmid.rearrange("(n p) d -> n p d", p=128)
        xov = xout.rearrange("(n p) d -> n p d", p=128)
        yv = yG.rearrange("(c p) t -> p c t", p=128)

        def loads(i):
            b = i % 2
            C.dma('sp', xt[b][:], xinv[i], [], [f'xt{b}'], f'lx{b}')
            for hh in range(2):
                C.dma('sp', ysc[b][hh][:], yv[:, :, hh * NT + i * 128:hh * NT + (i + 1) * 128], [], [f'ysc{b}{hh}'], f'ly{b}{hh}')
            C.ts('dve', ys[b][:], ysc[b][0][:], mks[:, 0:1], None, ALU.mult, None, [f'ysc{b}0', 'mks'], [f'ys{b}'])
            C.stt(ys[b][:], ysc[b][1][:], mks[:, 1:2], ys[b][:], ALU.mult, ALU.add, [f'ysc{b}1', 'mks', f'ys{b}'], [f'ys{b}'])

        loads(0)
        GRP = [([0, 1, 2, 3], 512.0), ([4, 5], 256.0), ([6, 7], 256.0)]
        for i in range(NTT):
            b = i % 2
            if i + 1 < NTT:
                loads(i + 1)
            s = sm[b]
            C.act(ysq[:], ys[b][:], AF.Square, [f'ys{b}'], ['ysq'])
            for gi, (cl_, n) in enumerate(GRP):
                for c in cl_:
                    C.mm(pM[:, gi:gi + 1], ysq[:, c, :], onec[:, 0:1], c == cl_[0], c == cl_[-1], ['ysq', 'onec'], ['pM'])
            C.act(s[:, 0:1], pM[:, 0:1], AF.Sqrt, ['pM'], [f's0{b}'], scale=1.0 / 512.0, bias=EPS)
            C.act(s[:, 1:3], pM[:, 1:3], AF.Sqrt, ['pM'], [f's1{b}'], scale=1.0 / 256.0, bias=EPS)
            C.recip(s[:, 3:6], s[:, 0:3], [f's0{b}', f's1{b}'], [f'rg{b}'])
            k = 0
            for half in range(2):
                for gi, (cl_, n) in enumerate(GRP):
                    bk = k % 2
                    k += 1
                    for c in cl_:
                        C.mm(pP[bk][:], ys[b][:, c, :], Woutb[:, c, half * 512:(half + 1) * 512], c == cl_[0], c == cl_[-1],
                             [f'ys{b}'] + WoK, [f'pP{bk}'])
                    src = xt[b] if gi == 0 else x1[b]
                    C.stt(x1[b][:, half * 512:(half + 1) * 512], pP[bk][:], s[:, 3 + gi:4 + gi], src[:, half * 512:(half + 1) * 512],
                          ALU.mult, ALU.add, [f'pP{bk}', f'rg{b}', f'xt{b}', ('x1', b, half)], [('x1', b, half)])
            x1k = [('x1', b, 0), ('x1', b, 1)]
            C.dma('sp', xmv[i], x1[b][:], x1k, [], f'sx{b}', cw=['xmid'])
            C.act(junk[:], x1[b][:], AF.Square, x1k, [f'ss{b}'], accum=s[:, 6:7])
            C.act(s[:, 7:8], s[:, 6:7], AF.Sqrt, [f'ss{b}'], [f'rt{b}'], scale=1.0 / 1024.0, bias=EPS)
            C.recip(s[:, 8:9], s[:, 7:8], [f'rt{b}'], [f'r2{b}'])
            C.stt(h2f[:], x1[b][:], s[:, 8:9], g2b[:], ALU.mult, ALU.mult, x1k + [f'r2{b}', 'g2b'], ['h2f'])
            C.cp('act', h2b[b][:], h2f[:], ['h2f'], [f'h2b{b}'])
            for c in range(8):
                C.tr(pTf[:, c * 128:(c + 1) * 128], h2f[:, c * 128:(c + 1) * 128], identf[:], ['h2f', 'identf'], ['pTf'])
            C.cp('dve', h2T[:].rearrange("p c t -> p (c t)"), pTf[:], ['pTf'], ['h2T'])
            for c in range(8):
                C.mm(pM[:, 8:44], h2T[:, c, :], wrs[:, c, :], c == 0, c == 7, ['h2T', 'wrs'], ['pM'])
            C.cp('act', lg[:], pM[:, 8:44], ['pM'], ['lg'])
            R = 'r1'
            S.add('dve', lambda e, s=s: e.tensor_reduce(out=s[:, 9:10], in_=lg[:, 0:4], axis=AX.X, op=ALU.max), r=['lg'], w=[f'mx{b}'])
            C.ts('dve', r1[:, 0:4], lg[:, 0:4], s[:, 9:10], None, ALU.is_equal, None, ['lg', f'mx{b}'], ['ohg'])
            C.ts('dve', s[:, 10:11], s[:, 9:10], -1.0, None, ALU.mult, None, [f'mx{b}'], [f'nmx{b}'])
            C.act(r1[:, 4:8], lg[:, 0:4], AF.Exp, ['lg', f'nmx{b}'], ['ec', f'sc{b}'], bias=s[:, 10:11], accum=s[:, 11:12])
            C.recip(s[:, 12:13], s[:, 11:12], [f'sc{b}'], [f'wgrp{b}'])
            C.ts('dve', r1[:, 8:12], r1[:, 0:4], -1.0, 1e30, ALU.add, ALU.mult, ['ohg'], ['pen'])
            for gq in range(4):
                C.ts('dve', r1[:, 16 + gq * 8:24 + gq * 8], lg[:, 4 + gq * 8:12 + gq * 8], r1[:, 8 + gq:9 + gq], None, ALU.add, None,
                     ['lg', 'pen'], [('msk', gq)])
            mk = [('msk', gq) for gq in range(4)]
            S.add('dve', lambda e: e.max(out=r1[:, 48:56], in_=r1[:, 16:48]), r=mk, w=['top8'])
            C.ts('dve', r1[:, 56:88], r1[:, 16:48], r1[:, 48:49], None, ALU.is_equal, None, mk + ['top8'], ['oh1'])
            C.ts('dve', r1[:, 88:120], r1[:, 16:48], r1[:, 49:50], None, ALU.is_equal, None, mk + ['top8'], ['oh2'])
            C.tt('dve', s[:, 13:14], r1[:, 49:50], r1[:, 48:49], ALU.subtract, ['top8'], [f'dd{b}'])
            C.act(s[:, 14:15], s[:, 13:14], AF.Exp, [f'dd{b}'], [f'ee{b}'])
            C.ts('dve', s[:, 15:16], s[:, 14:15], 1.0, None, ALU.add, None, [f'ee{b}'], [f'den{b}'])
            C.recip(s[:, 15:16], s[:, 15:16], [f'den{b}'], [f'den{b}'])
            C.tt('dve', gates[:, i, 0:1], s[:, 15:16], s[:, 12:13], ALU.mult, [f'den{b}', f'wgrp{b}'], [('g1', i)])
            C.tt('dve', gates[:, i, 1:2], gates[:, i, 0:1], s[:, 14:15], ALU.mult, [('g1', i), f'ee{b}'], [('g2', i)])
            C.tt('dve', r1[:, 120:152], r1[:, 56:88], r1[:, 88:120], ALU.add, ['oh1', 'oh2'], ['Mf'])
            C.cp('dve', Mb[:], r1[:, 120:152], ['Mf'], ['Mb'])
            C.mm(pM[:, 64:96], ustr[:], Mb[:], True, i == 0, ['ustr', 'Mb'], ['pM'])
            if i > 0:
                C.mm(pM[:, 64:96], ones128[:], Maccb[:], False, True, ['ones128', 'Maccb'], ['pM'])
            C.tt('pool', Macc[:], Macc[:], r1[:, 120:152], ALU.add, ['Macc', 'Mf'], ['Macc'])
            C.cp('pool', Maccb[:], Macc[:], ['Macc'], ['Maccb'])
            SLT = r1[:, 152:184]
            OKM = r1[:, 184:216]
            C.tt('dve', SLT, pM[:, 64:96], basef[:], ALU.add, ['pM', 'basef'], ['slot'])
            C.ts('dve', OKM, pM[:, 64:96], float(CAP), None, ALU.is_lt, None, ['pM'], ['okm'])
            C.ts('dve', SLT, SLT, -TRASH, None, ALU.add, None, ['slot'], ['slot'])
            C.tt('dve', SLT, SLT, OKM, ALU.mult, ['slot', 'okm'], ['slot'])
            C.ts('dve', SLT, SLT, TRASH, None, ALU.add, None, ['slot'], ['slot'])
            C.tt('dve', r1[:, 56:88], r1[:, 56:88], SLT, ALU.mult, ['oh1', 'slot'], ['oh1'])
            C.tt('dve', r1[:, 88:120], r1[:, 88:120], SLT, ALU.mult, ['oh2', 'slot'], ['oh2'])
            S.add('dve', lambda e: e.reduce_sum(out=dstf[:, 0:1], in_=r1[:, 56:88], axis=AX.X), r=['oh1'], w=['dstf0'])
            S.add('dve', lambda e: e.reduce_sum(out=dstf[:, 1:2], in_=r1[:, 88:120], axis=AX.X), r=['oh2'], w=['dstf1'])
            C.ts('dve', dstf[:], dstf[:], 0.0, float(NROWS - 1), ALU.max, ALU.min, ['dstf0', 'dstf1'], ['dstf0', 'dstf1'])
            C.cp('dve', dsti[:, 2 * i:2 * i + 2], dstf[:], ['dstf0', 'dstf1'], [('dsti', i)])
            for k2 in range(2 if stage >= 2 else 0):
                S.add('pool', lambda e, i=i, k2=k2, b=b: e.indirect_dma_start(
                    out=Xs[:, :], out_offset=bass.IndirectOffsetOnAxis(ap=dsti[:, 2 * i + k2:2 * i + k2 + 1], axis=0),
                    in_=h2b[b][:], in_offset=None, oob_is_err=False),
                    r=[f'h2b{b}', ('dsti', i)], cw=['Xs'], tag=f'sc{b}{k2}')

        S.add('dve', lambda e: e.memset(junk[:, 0:8], 0.0), r=['pTf'], w=['pTfg', 'pTfu'])
        def wloads(e_):
            b = e_ % 2
            C.dma('pool', wgb[b][:], wg[e_].rearrange("(c p) f -> p c f", p=128), [], [f'wgb{b}'], f'wg{b}')
            C.dma('pool', wub[b][:], wu[e_].rearrange("(c p) f -> p c f", p=128), [], [f'wub{b}'], f'wu{b}')
            C.dma('pool', wdb[b][:], wd[e_].rearrange("(c p) f -> p c f", p=128), [], [f'wdb{b}'], f'wd{b}')

        def xloads(e_):
            b = e_ % 2
            C.dma('sp', xe[b][:], Xs[e_ * CAP:(e_ + 1) * CAP, :].rearrange("(t p) d -> p t d", p=128), ['Xs'], [f'xe{b}'], f'xe{b}')

        if stage >= 3:
            wloads(0)
            xloads(0)
        dk = 0
        for e_ in range(32 if stage >= 3 else 0):
            b = e_ % 2
            if e_ + 1 < 32:
                wloads(e_ + 1)
                xloads(e_ + 1)
            for stt_ in range(NST):
                for c in range(8):
                    C.tr(pTb[:, c * 128:(c + 1) * 128], xe[b][:, stt_, c * 128:(c + 1) * 128], identb[:], [f'xe{b}', 'identb'], ['pTb'])
                C.cp('act' if stt_ % 2 else 'dve', xeT[:, :, stt_ * 128:(stt_ + 1) * 128], pTb[:].rearrange("p (c t) -> p c t", c=8),
                     ['pTb'], [('xeT', stt_)])
            xk = [('xeT', t_) for t_ in range(NST)]
            for fc in range(4):
                for (a0, a1) in ((0, 512), (512, CAP)):
                    for c in range(8):
                        C.mm(pTf[:, a0:a1], wgb[b][:, c, fc * 128:(fc + 1) * 128], xeT[:, c, a0:a1], c == 0, c == 7, [f'wgb{b}'] + xk, ['pTfg'])
                for (a0, a1) in ((0, 512), (512, CAP)):
                    for c in range(8):
                        C.mm(pUU[:, a0:a1], wub[b][:, c, fc * 128:(fc + 1) * 128], xeT[:, c, a0:a1], c == 0, c == 7, [f'wub{b}'] + xk, ['pTfu'])
                C.act(sgl[fc % 2][:], pTf[:, 0:CAP], AF.Silu, ['pTfg'], [f'sgl{fc % 2}'])
                C.tt('dve', aT[:, fc, :], pUU[:, 0:CAP], sgl[fc % 2][:], ALU.mult, ['pTfu', f'sgl{fc % 2}'], [('aT', fc)])
            ak = [('aT', fc) for fc in range(4)]
            for stt_ in range(NST):
                for half in range(2):
                    bk = dk % 2
                    dk += 1
                    for fc in range(4):
                        C.mm(pP[bk][:], aT[:, fc, stt_ * 128:(stt_ + 1) * 128], wdb[b][:, fc, half * 512:(half + 1) * 512],
                             fc == 0, fc == 3, ak + [f'wdb{b}'], [f'pP{bk}'])
                    C.cp('act' if dk % 2 else 'dve', yo[b][:, stt_, half * 512:(half + 1) * 512], pP[bk][:], [f'pP{bk}'],
                         [('yo', b, stt_, half)])
            yk = [('yo', b, t_, h_) for t_ in range(NST) for h_ in range(2)]
            C.dma('sp', Ys[e_ * CAP:(e_ + 1) * CAP, :].rearrange("(t p) d -> p t d", p=128), yo[b][:], yk, [], f'yo{b}', cw=['Ys'])

        def gloads(i):
            b = i % 2
            C.dma('sp', xt[b][:], xmv[i], ['xmid'], [f'xt{b}'], f'lx{b}')
            for k2 in range(2 if stage >= 4 else 0):
                S.add('pool', lambda e, i=i, k2=k2, b=b: e.indirect_dma_start(
                    out=yg[b][k2][:], out_offset=None, in_=Ys[:, :],
                    in_offset=bass.IndirectOffsetOnAxis(ap=dsti[:, 2 * i + k2:2 * i + k2 + 1], axis=0),
                    oob_is_err=False),
                    r=['Ys', ('dsti', i)], w=[f'yg{b}{k2}'], tag=f'gy{b}{k2}')

        gloads(0)
        for i in range(NTT):
            b = i % 2
            if i + 1 < NTT:
                gloads(i + 1)
            if stage < 4:
                C.dma('sp', xov[i], xt[b][:], [f'xt{b}'], [], f'so{b}')
                continue
            C.stt(x1[b][:], yg[b][0][:], gates[:, i, 0:1], xt[b][:], ALU.mult, ALU.add,
                  [f'yg{b}0', ('g1', i), f'xt{b}'], [('x1', b, 0), ('x1', b, 1)])
            C.stt(x1[b][:], yg[b][1][:], gates[:, i, 1:2], x1[b][:], ALU.mult, ALU.add,
                  [f'yg{b}1', ('g2', i), ('x1', b, 0), ('x1', b, 1)], [('x1', b, 0), ('x1', b, 1)])
            C.dma('sp', xov[i], x1[b][:], [('x1', b, 0), ('x1', b, 1)], [], f'so{b}', cw=['xout'])
        if post is not None:
            post(C)
        info = S.emit()
    return info


STD_OF_YG = [0, 2, 1, 3, 4, 5, 6, 7]


def prep_ffn_inputs(inp, l):
    f = np.float32
    wr = np.ascontiguousarray(np.concatenate([inp['router_coarse'][l], inp['router_fine'][l]], axis=1), dtype=f)
    wout = np.asarray(inp['w_out'][l], dtype=f).reshape(8, 128, 1024)[STD_OF_YG].reshape(1024, 1024)
    gout = np.asarray(inp['out_norm_g'][l], dtype=f).reshape(8, 128)[STD_OF_YG].T
    return dict(
        wout=np.ascontiguousarray(wout),
        gout=np.ascontiguousarray(gout),
        g2bd=np.ascontiguousarray(np.broadcast_to(np.asarray(inp['norm2_g'][l])[None, :], (128, 1024)), dtype=f),
        wr=wr,
        wg=np.ascontiguousarray(inp['exp_w_gate'][l], dtype=f),
        wu=np.ascontiguousarray(inp['exp_w_up'][l], dtype=f),
        wd=np.ascontiguousarray(inp['exp_w_down'][l], dtype=f),
    )


MIX_KEYS = dict(wall=[1024, NW], vec=[128, NV], g1=[128, 8], wab=[128, 2, 128], pww=[128, 2, 128])
FFN_KEYS = dict(wout=[1024, 1024], gout=[128, 8], g2bd=[128, 1024], wr=[1024, 36], wg=[32, 1024, 512],
                wu=[32, 1024, 512], wd=[32, 512, 1024])
RG_PAIRS = [[0, 1], [2, 3], [4, 5], [6, 7]]


def build_fused(SL, depth=2):
    NT = SL // 2
    nc = bass.Bass("TRN2", target_bir_lowering=False)

    def din(name, shape, dt=F32):
        return nc.dram_tensor(name, shape, dt, kind="ExternalInput").ap()

    x_full = din("x_full", [SL, 1024])
    xin0 = din("xin0", [NT, 1024])
    msk = din("msk", [128, 2])
    W = []
    for l in range(depth):
        d = {k: din(f"{k}{l}", shp) for k, shp in MIX_KEYS.items()}
        d.update({k: din(f"{k}{l}", shp) for k, shp in FFN_KEYS.items()})
        W.append(d)
    xout = nc.dram_tensor("xout", [NT, 1024], F32, kind="ExternalOutput").ap()
    yT = nc.dram_tensor("yT_i", [512, SL], BF16, kind="Internal").ap()
    yG = nc.dram_tensor("yG_i", [1024, SL], BF16, kind="Internal").ap()
    Xs = nc.dram_tensor("Xs_i", [NROWS, 1024], BF16, kind="Internal").ap()
    Ys = nc.dram_tensor("Ys_i", [NROWS, 1024], BF16, kind="Internal").ap()
    xmid = nc.dram_tensor("xmid_i", [NT, 1024], F32, kind="Internal").ap()
    xo = nc.dram_tensor("xo_i", [NT, 1024], F32, kind="Internal").ap()
    xG = nc.dram_tensor("xG_i", [SL, 1024], F32, kind="Internal").ap()
    with ExitStack() as outer:
        gs = GSync(nc, outer)
        for l in range(depth):
            last = l == depth - 1
            w = W[l]

            def post_m(C):
                for k in range(4):
                    C.S.add('pool', lambda e, k=k: e.collective_compute(
                        "AllGather", ALU.bypass, replica_groups=RG_PAIRS,
                        ins=[yT[k * 128:(k + 1) * 128, :]], outs=[yG[k * 256:(k + 1) * 256, :]]),
                        r=['yT'], cw=['yG'], tag='cc', inc=1)

            def xtile(n):
                p = n * 128
                r_, q = p // NT, p % NT
                row = (q // 512) * 1024 + r_ * 512 + q % 512
                return xG[row:row + 128, :]

            build_mixer(nc, gs, SL, x_full if l == 0 else xG, w['wall'], w['vec'], w['g1'], w['wab'], w['pww'], yT, post=post_m,
                        xtile=None if l == 0 else xtile)
            nc.all_engine_barrier()

            def post_f(C, last=last):
                if not last:
                    for k in range(NT // 512):
                        C.S.add('pool', lambda e, k=k: e.collective_compute(
                            "AllGather", ALU.bypass, replica_groups=RG_PAIRS,
                            ins=[xo[k * 512:(k + 1) * 512, :]], outs=[xG[k * 1024:(k + 1) * 1024, :]]),
                            r=['xout'], cw=['xG'], tag='cc', inc=1)

            build_ffn(nc, gs, NT, xin0 if l == 0 else xo, yG, msk, w['wout'], w['gout'], w['g2bd'], w['wr'], w['wg'], w['wu'],
                      w['wd'], xout if last else xo, Xs, Ys, xmid, post=post_f)
            if not last:
                nc.all_engine_barrier()
    return nc


_NC_CACHE = {}


def run_fused(inp, X):
    B, SL, D = X.shape
    NT = SL // 2
    depth = np.asarray(inp['w_in']).shape[0]
    key = (SL, depth)
    if key not in _NC_CACHE:
        _NC_CACHE[key] = build_fused(SL, depth)
    nc = _NC_CACHE[key]
    fw = [prep_ffn_inputs(inp, l) for l in range(depth)]
    maps = []
    for c in range(8):
        b, h = c // 2, c % 2
        m = {'x_full': np.ascontiguousarray(X[b]), 'xin0': np.ascontiguousarray(X[b, h * NT:(h + 1) * NT])}
        mk = np.zeros((128, 2), np.float32)
        mk[:, h] = 1.0
        m['msk'] = mk
        for l in range(depth):
            for k, v in prep_mixer_inputs(inp, l, b, h).items():
                m[f'{k}{l}'] = v
            for k, v in fw[l].items():
                m[f'{k}{l}'] = v
        maps.append(m)
    res = run_bass_kernel_spmd(nc, maps, core_ids=list(range(8)))
    out = np.empty_like(X)
    for c in range(8):
        b, h = c // 2, c % 2
        out[b, h * NT:(h + 1) * NT] = np.asarray(res.results[c]['xout'])
    return out


def kernel(**inp):
    X = np.ascontiguousarray(np.asarray(inp['x'], dtype=np.float32))
    return run_fused(inp, X)
```

```python
import numpy as np
import ml_dtypes
from contextlib import ExitStack
import concourse.bass as bass
import concourse.mybir as mybir
from concourse.bass_utils import run_bass_kernel_spmd

F32 = mybir.dt.float32
BF16 = mybir.dt.bfloat16
I32 = mybir.dt.int32
AF = mybir.ActivationFunctionType
ALU = mybir.AluOpType
AX = mybir.AxisListType

ENG_ATTR = {'pe': 'tensor', 'act': 'scalar', 'dve': 'vector', 'pool': 'gpsimd', 'sp': 'sync'}
EPS = 1e-6


class GSync:
    def __init__(self, nc, stack):
        self.nc = nc
        self.stack = stack
        self.sems = {}
        self.cnts = {}
        self.tagmap = {}

    def sem(self, name):
        if name not in self.sems:
            self.sems[name] = self.stack.enter_context(self.nc.semaphore(name))
        return self.sems[name]

    def tagsem(self, tag):
        if tag not in self.tagmap:
            self.tagmap[tag] = 't_' + tag
        return self.tagmap[tag]


class Sched:
    def __init__(self, nc, stack, gs=None):
        self.nc = nc
        self.stack = stack
        self.gs = gs if gs is not None else GSync(nc, stack)
        self.ops = []
        self.wx = {}
        self.wc = {}
        self.rd = {}
        self.tag_last = {}
        self.sems = {}

    @staticmethod
    def _merge(dst, src):
        for s, i in src.items():
            if dst.get(s, -1) < i:
                dst[s] = i

    def add(self, eng, fn, r=(), w=(), cw=(), tag=None, inc=16):
        deps = {}
        for k in r:
            self._merge(deps, self.wx.get(k, {}))
            self._merge(deps, self.wc.get(k, {}))
        for k in w:
            self._merge(deps, self.wx.get(k, {}))
            self._merge(deps, self.wc.get(k, {}))
            self._merge(deps, self.rd.get(k, {}))
        for k in cw:
            self._merge(deps, self.wx.get(k, {}))
            self._merge(deps, self.rd.get(k, {}))
        if tag is not None and tag in self.tag_last:
            self._merge(deps, {('t', tag): self.tag_last[tag]})
        idx = len(self.ops)
        if eng == 'pe':
            deps.pop(('e', 'pe'), None)
        self.ops.append(dict(eng=eng, fn=fn, deps=deps, tag=tag, sem=None, cnt=0, inc=inc))
        sig = ('t', tag) if tag else ('e', eng)
        for k in r:
            self.rd.setdefault(k, {})[sig] = idx
        for k in w:
            self.wx[k] = {sig: idx}
            self.wc[k] = {}
            self.rd[k] = {}
        for k in cw:
            self.wc.setdefault(k, {})[sig] = idx
        if tag is not None:
            self.tag_last[tag] = idx
        return idx

    def _sem(self, name):
        return self.gs.sem(name)

    def emit(self):
        ops = self.ops
        need = set()
        for o in ops:
            for i in o['deps'].values():
                need.add(i)
        cnts = self.gs.cnts
        for i, o in enumerate(ops):
            if o['tag']:
                o['sem'] = self.gs.tagsem(o['tag'])
                cnts[o['sem']] = cnts.get(o['sem'], 0) + o['inc']
                o['cnt'] = cnts[o['sem']]
            elif i in need:
                o['sem'] = 'e_' + o['eng']
                cnts[o['sem']] = cnts.get(o['sem'], 0) + 1
                o['cnt'] = cnts[o['sem']]
        final = {}
        for o in ops:
            if o['sem']:
                final[o['sem']] = max(final.get(o['sem'], 0), o['cnt'])
        for s in final:
            self._sem(s)
        with self.nc.Block() as blk:
            for eng, attr in ENG_ATTR.items():
                mine = [o for o in ops if o['eng'] == eng]

                def body(e, mine=mine, eng=eng):
                    waited = {}
                    for o in mine:
                        for di in sorted(o['deps'].values()):
                            d = ops[di]
                            if waited.get(d['sem'], 0) < d['cnt']:
                                e.wait_ge(self._sem(d['sem']), d['cnt'])
                                waited[d['sem']] = d['cnt']
                        ins = o['fn'](e)
                        if o['sem']:
                            ins.then_inc(self._sem(o['sem']), o['inc'] if o['tag'] else 1)
                    if eng == 'sp':
                        for s, c in final.items():
                            if waited.get(s, 0) < c:
                                e.wait_ge(self._sem(s), c)

                getattr(blk, attr)(body)
        return dict(nops=len(ops), final=final)


class Ctx:
    def __init__(self, nc, st, gs=None):
        self.nc = nc
        self.st = st
        self.S = Sched(nc, st, gs)
        g = self.S.gs
        g.phase = getattr(g, 'phase', 0) + 1
        self.pfx = f"p{g.phase}_"

    def sb(self, name, shape, dt):
        return self.st.enter_context(self.nc.sbuf_tensor(self.pfx + name, shape, dt))

    def ps(self, name, shape, dt):
        return self.st.enter_context(self.nc.psum_tensor(self.pfx + name, shape, dt))

    def dma(self, q, out, in_, r, w, tag, cw=()):
        self.S.add(q, lambda e: e.dma_start(out=out, in_=in_), r=r, w=w, cw=cw, tag=tag)

    def mm(self, out, lhsT, rhs, start, stop, r, w):
        self.S.add('pe', lambda e: e.matmul(out, lhsT=lhsT, rhs=rhs, start=start, stop=stop,
                                            skip_group_check=True), r=r, w=w)

    def tr(self, out, in_, ident, r, w):
        self.S.add('pe', lambda e: e.transpose(out, in_, ident), r=r, w=w)

    def act(self, out, in_, func, r, w, bias=None, scale=None, accum=None):
        kw = {}
        if bias is not None:
            kw['bias'] = bias
        if scale is not None:
            kw['scale'] = scale
        if accum is not None:
            kw['accum_out'] = accum
        self.S.add('act', lambda e: e.activation(out=out, in_=in_, func=func, **kw), r=r, w=w)

    def ts(self, eng, out, in0, s1, s2, op0, op1, r, w):
        if op1 is None:
            self.S.add(eng, lambda e: e.tensor_scalar(out=out, in0=in0, scalar1=s1, scalar2=None, op0=op0), r=r, w=w)
        else:
            self.S.add(eng, lambda e: e.tensor_scalar(out=out, in0=in0, scalar1=s1, scalar2=s2, op0=op0, op1=op1), r=r, w=w)

    def tt(self, eng, out, in0, in1, op, r, w):
        self.S.add(eng, lambda e: e.tensor_tensor(out=out, in0=in0, in1=in1, op=op), r=r, w=w)

    def stt(self, out, in0, scalar, in1, op0, op1, r, w):
        self.S.add('dve', lambda e: e.scalar_tensor_tensor(out=out, in0=in0, scalar=scalar, in1=in1, op0=op0, op1=op1), r=r, w=w)

    def cp(self, eng, out, in_, r, w):
        if eng == 'act':
            self.S.add('act', lambda e: e.copy(out=out, in_=in_), r=r, w=w)
        else:
            self.S.add(eng, lambda e: e.tensor_copy(out=out, in_=in_), r=r, w=w)

    def memset(self, eng, ap, val, w):
        self.S.add(eng, lambda e: e.memset(ap, val), w=w)

    def recip(self, out, in_, r, w):
        self.S.add('dve', lambda e: e.reciprocal(out=out, in_=in_), r=r, w=w)

    def aselect(self, out, in_, pattern, op, fill, base, cm, r, w):
        self.S.add('pool', lambda e: e.affine_select(out=out, in_=in_, pattern=pattern, compare_op=op, fill=fill,
                                                     base=base, channel_multiplier=cm), r=r, w=w)


V_GQ, V_GK, V_LCW, V_LCB, V_BA, V_BX, V_LAM = 0, 1, 2, 6, 7, 8, 9
V_DWB, V_LNG, V_LNB, V_PWB, V_DWW = 10, 12, 14, 16, 17
NV = 17 + 62
NW = 1536


def build_mixer(nc, gs, SL, x, wall, vec, g1, wab, pww, yT, post=None, xtile=None):
    NG = SL // 512
    with ExitStack() as st:
        C = Ctx(nc, st, gs)
        S = C.S
        sb, ps = C.sb, C.ps
        Wb = sb("Wb", [128, 8, NW], BF16)
        wst = [sb(f"wst{i}", [128, 192], F32) for i in range(2)]
        vecs = sb("vecs", [128, NV], F32)
        g1s = sb("g1s", [128, 8], F32)
        wabf = sb("wabf", [128, 2, 128], F32)
        wabb = sb("wabb", [128, 2, 128], BF16)
        pwf = sb("pwf", [128, 2, 128], F32)
        pwb = sb("pwb", [128, 2, 128], BF16)
        identb = sb("identb", [128, 128], BF16)
        blk1 = sb("blk1", [128, 128], BF16)
        onesm = sb("onesm", [128, 128], F32)
        uincl = sb("uincl", [128, 128], BF16)
        negones = sb("negones", [128, 128], BF16)
        tri = sb("tri", [128, 128], BF16)
        dg = sb("dg", [128, 62, 128], BF16)
        cl = sb("cl", [128, 4], F32)
        KT = sb("KT", [128, 2, SL], BF16)
        Vt = sb("Vt", [128, SL // 128, 256], BF16)
        QT = sb("QT", [128, 2, 512], BF16)
        xt = [sb(f"xt{i}", [128, 1024], F32) for i in range(2)]
        xn = [sb(f"xn{i}", [128, 1024], BF16) for i in range(2)]
        st1 = [sb(f"st1_{i}", [128, 4], F32) for i in range(2)]
        hT = sb("hT", [128, 8, 512], BF16)
        sq = sb("sq", [128, 512], BF16)
        rs = sb("rs", [128, 512], F32)
        xrb = sb("xrb", [128, 515], F32)
        xgb = sb("xgb", [128, 512], F32)
        cv = sb("cv", [128, 512], F32)
        cvb = sb("cvb", [128, 512], BF16)
        lr = sb("lr", [128, 512], F32)
        li = sb("li", [128, 512], F32)
        la = sb("la", [128, 512], F32)
        la2 = sb("la2", [128, 512], F32)
        lu = sb("lu", [128, 512], F32)
        lh = sb("lh", [128, 512], F32)
        hprev = sb("hprev", [128, 1], F32)
        gt = sb("gt", [128, 512], F32)
        ybs = sb("ybs", [128, 512], BF16)
        ub = sb("ub", [128, 2, 542], BF16)
        sg = sb("sg", [128, 512], F32)
        cvo = sb("cvo", [128, 2, 512], F32)
        sqo = sb("sqo", [128, 2, 512], F32)
        means = sb("means", [128, 512], F32)
        m2 = sb("m2", [128, 512], F32)
        crs = sb("crs", [128, 512], F32)
        cn = sb("cn", [128, 2, 512], F32)
        csb = sb("csb", [128, 2, 512], BF16)
        ycs = sb("ycs", [128, 512], BF16)
        LA = 2
        NRB = LA + 2
        Eb = [sb(f"Eb{i}", [128, 512], BF16) for i in range(NRB)]
        Lb = [sb(f"Lb{i}", [128, 512], BF16) for i in range(NRB)]
        NWB = 4
        Wt = [sb(f"Wt{i}", [128, 512], BF16) for i in range(NWB)]
        LaccP = [sb(f"Lacc{i}", [128, 512], F32) for i in range(2)]
        xC = [sb(f"xC{i}", [128, 512], BF16) for i in range(2)]
        Laccb = [sb(f"Laccb{i}", [128, 512], BF16) for i in range(NRB)]
        yst = [sb(f"yst{i}", [64, 512], BF16) for i in range(2)]
        pA = [ps(f"pA{i}", [128, 512], F32) for i in range(2)]
        pT = ps("pT", [128, 1024], BF16)
        pZ = [ps(f"pZ{i}", [128, 512], F32) for i in range(2)]
        pC = [ps(f"pC{i}", [128, 512], F32) for i in range(2)]
        pO = ps("pO", [128, 512], F32)

        C.dma('sp', vecs[:], vec, [], ['vecs'], 'c0')
        C.dma('sp', g1s[:], g1, [], ['g1s'], 'c1')
        C.dma('sp', wabf[:], wab, [], ['wabf'], 'c2')
        C.dma('sp', pwf[:], pww, [], ['pwf'], 'c3')
        C.cp('dve', wabb[:], wabf[:], ['wabf'], ['wabb'])
        C.cp('dve', pwb[:], pwf[:], ['pwf'], ['pwb'])
        wv = wall.rearrange("(c p) n -> p c n", p=128)
        k = 0
        for c in range(8):
            for hf in range(8):
                b = k % 2
                C.dma('sp', wst[b][:], wv[:, c, hf * 192:(hf + 1) * 192], [], [f'wst{b}'], f'wst{b}')
                C.ts('dve' if k % 2 == 0 else 'pool', Wb[:, c, hf * 192:(hf + 1) * 192], wst[b][:], g1s[:, c:c + 1], None,
                     ALU.mult, None, [f'wst{b}', 'g1s'], [('Wb', c, hf)])
                k += 1
        WbK = [('Wb', c, hf) for c in range(8) for hf in range(8)]
        C.memset('pool', identb[:], 0.0, ['identb'])
        C.aselect(identb[:], identb[:], [[-1, 128]], ALU.not_equal, 1.0, 0, 1, ['identb'], ['identb'])
        C.memset('pool', blk1[:], 0.0, ['blk1'])
        C.memset('pool', blk1[0:64, 0:64], 1.0, ['blk1'])
        C.memset('pool', blk1[64:128, 64:128], 1.0, ['blk1'])
        C.memset('pool', onesm[:], 1.0 / 256.0, ['onesm'])
        C.memset('pool', negones[:], -1.0, ['negones'])
        C.memset('pool', uincl[:], -1.0, ['uincl'])
        C.aselect(uincl[:], uincl[:], [[-1, 128]], ALU.is_ge, 0.0, 0, 1, ['uincl'], ['uincl'])
        C.memset('pool', tri[:], 1.0, ['tri'])
        C.aselect(tri[:], tri[:], [[1, 128]], ALU.is_ge, 0.0, -1, -1, ['tri'], ['tri'])
        for i in range(62):
            C.ts('pool' if i % 2 else 'dve', dg[:, i, :], identb[:], vecs[:, V_DWW + i:V_DWW + i + 1], None, ALU.mult, None,
                 ['identb', 'vecs'], ['dg'])
        C.act(cl[:, 0:1], vecs[:, V_LAM:V_LAM + 1], AF.Exp, ['vecs'], ['cl0'], scale=-1.0)
        C.act(cl[:, 1:2], cl[:, 0:1], AF.Ln, ['cl0'], ['cl1'], bias=1.0)
        C.ts('dve', cl[:, 2:3], cl[:, 1:2], -8.0, None, ALU.mult, None, ['cl1'], ['cl2'])
        C.ts('dve', cl[:, 3:4], cl[:, 1:2], -16.0, None, ALU.mult, None, ['cl1'], ['cl3'])
        C.memset('pool', hprev[:], 0.0, ['hprev'])
        C.memset('pool', xrb[:, 0:3], 0.0, ['xrb_h'])
        C.memset('pool', ub[:, :, 0:30], 0.0, ['ub_h'])

        xv = x.rearrange("(n p) d -> n p d", p=128)
        wcnt = [0]
        ocnt = [0]
        acnt = [0]
        bcnt = [0]
        lcnt = [0]
        pcnt = [0]
        for g in range(NG):
            t0 = g * 512
            for tt in range(4):
                b = tt % 2
                C.dma('sp', xt[b][:], xv[g * 4 + tt] if xtile is None else xtile(g * 4 + tt), [], [f'xt{b}'], f'x{b}')
                C.act(xn[b][:], xt[b][:], AF.Square, [f'xt{b}'], [f'ss{b}', f'xn{b}'], accum=st1[b][:, 0:1])
                C.act(st1[b][:, 1:2], st1[b][:, 0:1], AF.Sqrt, [f'ss{b}'], [f'rt{b}'], scale=1.0 / 1024.0, bias=EPS)
                C.recip(st1[b][:, 2:3], st1[b][:, 1:2], [f'rt{b}'], [f'rstd{b}'])
                C.ts('dve', xn[b][:], xt[b][:], st1[b][:, 2:3], None, ALU.mult, None, [f'xt{b}', f'rstd{b}'], [f'xn{b}'])
                for c in range(8):
                    C.tr(pT[:, c * 128:(c + 1) * 128], xn[b][:, c * 128:(c + 1) * 128], identb[:], [f'xn{b}', 'identb'], ['pT'])
                C.cp('act' if tt % 2 else 'dve', hT[:, :, tt * 128:(tt + 1) * 128], pT[:].rearrange("p (c t) -> p c t", c=8),
                     ['pT'], [('hT', tt)])
            hTK = [('hT', tt) for tt in range(4)]
            pidx = [0]

            def proj(ct):
                bank = pidx[0] % 2
                pidx[0] += 1
                for c in range(8):
                    C.mm(pA[bank][:], Wb[:, c, ct * 128:(ct + 1) * 128], hT[:, c, :], c == 0, c == 7, WbK + hTK, [f'pA{bank}'])
                return bank

            for ct in range(4):
                bk = proj(ct)
                C.act(sq[:], pA[bk][:], AF.Square, [f'pA{bk}'], ['sq'])
                C.mm(pA[1 - bk][:], blk1[:], sq[:], True, True, ['blk1', 'sq'], [f'pA{1 - bk}'])
                pidx[0] += 1
                if ct < 2:
                    C.act(rs[:], pA[1 - bk][:], AF.Sqrt, [f'pA{1 - bk}'], ['rs'], scale=1.0, bias=64.0 * EPS)
                else:
                    C.act(rs[:], pA[1 - bk][:], AF.Sqrt, [f'pA{1 - bk}'], ['rs'], scale=1.0 / 64.0, bias=EPS)
                C.recip(rs[:], rs[:], ['rs'], ['rs'])
                if ct < 2:
                    C.stt(QT[:, ct, :], pA[bk][:], vecs[:, V_GQ:V_GQ + 1], rs[:], ALU.mult, ALU.mult,
                          [f'pA{bk}', 'vecs', 'rs'], [('QT', ct)])
                else:
                    C.stt(KT[:, ct - 2, t0:t0 + 512], pA[bk][:], vecs[:, V_GK:V_GK + 1], rs[:], ALU.mult, ALU.mult,
                          [f'pA{bk}', 'vecs', 'rs'], [('KT', ct - 2, g)])
            bk = proj(4)
            C.cp('act', xrb[:, 3:515], pA[bk][:], [f'pA{bk}'], ['xrb'])
            bk = proj(5)
            C.cp('dve', xgb[:], pA[bk][:], [f'pA{bk}'], ['xgb'])
            for t in range(2):
                bv = proj(6 + t)
                bg = proj(8 + t)
                C.act(sg[:], pA[bg][:], AF.Sigmoid, [f'pA{bg}'], ['sg'])
                C.tt('dve', ub[:, t, 30:542], pA[bv][:], sg[:], ALU.mult, [f'pA{bv}', 'sg'], [('ub', t)])
            for tt in range(4):
                bank = pidx[0] % 2
                pidx[0] += 1
                for c in range(8):
                    C.mm(pA[bank][:, 0:256], hT[:, c, tt * 128:(tt + 1) * 128], Wb[:, c, 1280:1536], c == 0, c == 7,
                         WbK + hTK, [f'pA{bank}'])
                C.cp('act' if tt % 2 else 'dve', Vt[:, g * 4 + tt, :], pA[bank][:, 0:256], [f'pA{bank}'], [('V', g * 4 + tt)])

            vc = lambda i: vecs[:, i:i + 1]
            C.ts('dve', cv[:], xrb[:, 3:515], vc(V_LCW + 3), vc(V_LCB), ALU.mult, ALU.add, ['xrb', 'xrb_h', 'vecs'], ['cv'])
            for kk in range(3):
                C.stt(cv[:], xrb[:, kk:kk + 512], vc(V_LCW + kk), cv[:], ALU.mult, ALU.add, ['xrb', 'xrb_h', 'vecs', 'cv'], ['cv'])
            C.cp('pool', xrb[:, 0:3], xrb[:, 512:515], ['xrb'], ['xrb_h'])
            C.cp('pool', cvb[:], cv[:], ['cv'], ['cvb'])
            b0 = pidx[0] % 2
            pidx[0] += 2
            C.mm(pA[b0][:], wabb[:, 0, :], cvb[:], True, True, ['wabb', 'cvb'], [f'pA{b0}'])
            C.mm(pA[1 - b0][:], wabb[:, 1, :], cvb[:], True, True, ['wabb', 'cvb'], [f'pA{1 - b0}'])
            C.act(lr[:], pA[b0][:], AF.Sigmoid, [f'pA{b0}', 'vecs'], ['lr'], bias=vc(V_BA))
            C.act(li[:], pA[1 - b0][:], AF.Sigmoid, [f'pA{1 - b0}', 'vecs'], ['li'], bias=vc(V_BX))
            C.act(la[:], lr[:], AF.Exp, ['lr', 'cl2'], ['la'], scale=cl[:, 2:3])
            C.act(la2[:], lr[:], AF.Exp, ['lr', 'cl3'], ['la2'], scale=cl[:, 3:4])
            C.act(la2[:], la2[:], AF.Sqrt, ['la2'], ['la2'], scale=-1.0, bias=1.0)
            C.tt('pool', lu[:], li[:], cv[:], ALU.mult, ['li', 'cv'], ['lu'])
            C.tt('pool', lu[:], lu[:], la2[:], ALU.mult, ['lu', 'la2'], ['lu'])
            S.add('dve', lambda e: e.tensor_tensor_scan(out=lh[:], data0=la[:], data1=lu[:], initial=hprev[:, 0:1],
                                                        op0=ALU.mult, op1=ALU.add), r=['la', 'lu', 'hprev'], w=['lh'])
            C.cp('pool', hprev[:], lh[:, 511:512], ['lh'], ['hprev'])
            C.tt('pool', gt[:], xgb[:], xgb[:], ALU.mult, ['xgb'], ['gt'])
            C.ts('pool', gt[:], gt[:], 0.044715, 1.0, ALU.mult, ALU.add, ['gt'], ['gt'])
            C.tt('pool', gt[:], gt[:], xgb[:], ALU.mult, ['gt', 'xgb'], ['gt'])
            C.act(gt[:], gt[:], AF.Sigmoid, ['gt'], ['gt'], scale=1.5957691216)
            C.tt('pool', gt[:], gt[:], xgb[:], ALU.mult, ['gt', 'xgb'], ['gt'])
            C.tt('dve', ybs[:], gt[:], lh[:], ALU.mult, ['gt', 'lh'], ['ybs'])
            C.dma('pool', yT[256:384, t0:t0 + 512], ybs[:], ['ybs'], [], 'yb', cw=['yT'])

            for t in range(2):
                bank = pidx[0] % 2
                pidx[0] += 1
                for kk in range(31):
                    C.mm(pA[bank][:], dg[:, t * 31 + kk, :], ub[:, t, kk:kk + 512], kk == 0, kk == 30,
                         ['dg', ('ub', t), 'ub_h'], [f'pA{bank}'])
                C.act(cvo[:, t, :], pA[bank][:], AF.Identity, [f'pA{bank}', 'vecs'], [('cvo', t)], bias=vc(V_DWB + t))
                C.act(sqo[:, t, :], pA[bank][:], AF.Square, [f'pA{bank}', 'vecs'], [('sqo', t)], bias=vc(V_DWB + t))
            C.cp('pool', ub[:, :, 0:30], ub[:, :, 512:542], [('ub', 0), ('ub', 1)], ['ub_h'])
            bm = pidx[0] % 2
            pidx[0] += 2
            for t in range(2):
                C.mm(pA[bm][:], onesm[:], cvo[:, t, :], t == 0, t == 1, ['onesm', ('cvo', t)], [f'pA{bm}'])
            for t in range(2):
                C.mm(pA[1 - bm][:], onesm[:], sqo[:, t, :], t == 0, t == 1, ['onesm', ('sqo', t)], [f'pA{1 - bm}'])
            C.cp('act', means[:], pA[bm][:], [f'pA{bm}'], ['means'])
            C.tt('pool', m2[:], means[:], means[:], ALU.mult, ['means'], ['m2'])
            C.tt('dve', m2[:], pA[1 - bm][:], m2[:], ALU.subtract, [f'pA{1 - bm}', 'm2'], ['m2'])
            C.act(crs[:], m2[:], AF.Sqrt, ['m2'], ['crs'], scale=1.0, bias=EPS)
            C.recip(crs[:], crs[:], ['crs'], ['crs'])
            for t in range(2):
                C.tt('pool', cn[:, t, :], cvo[:, t, :], means[:], ALU.subtract, [('cvo', t), 'means'], [('cn', t)])
                C.tt('dve' if t else 'pool', cn[:, t, :], cn[:, t, :], crs[:], ALU.mult, [('cn', t), 'crs'], [('cn', t)])
                C.act(csb[:, t, :], cn[:, t, :], AF.Silu, [('cn', t), 'vecs'], [('csb', t)], scale=vc(V_LNG + t), bias=vc(V_LNB + t))
            bank = pidx[0] % 2
            pidx[0] += 1
            for t in range(2):
                C.mm(pA[bank][:], pwb[:, t, :], csb[:, t, :], t == 0, t == 1, ['pwb', ('csb', t)], [f'pA{bank}'])
            C.act(ycs[:], pA[bank][:], AF.Identity, [f'pA{bank}', 'vecs'], ['ycs'], bias=vc(V_PWB))
            C.dma('pool', yT[384:512, t0:t0 + 512], ycs[:], ['ycs'], [], 'yc', cw=['yT'])

            items = []
            for hd in range(4):
                nblk = 4 * g + 4
                for bi, kb in enumerate(range(4 * g + 3, -1, -1)):
                    items.append(dict(hd=hd, bi=bi, kb=kb, nblk=nblk))

            def stageA(it):
                hd, bi, kb = it['hd'], it['bi'], it['kb']
                ct = hd // 2
                pb = 64 * (hd % 2)
                Qh = QT[pb:pb + 64, ct, :]
                j = kb - 4 * g
                c0 = 128 * j if j > 0 else 0
                Kh = KT[pb:pb + 64, ct, kb * 128:(kb + 1) * 128]
                kkey = ('KT', ct, kb // 4)
                zb = acnt[0] % 2
                lb = acnt[0] % NRB
                eb = acnt[0] % NRB
                acnt[0] += 1
                it.update(c0=c0, j=j, Kh=Kh, kkey=kkey, Qh=Qh, ct=ct, lb=lb, eb=eb)
                if bi == 0:
                    C.memset('pool', LaccP[0][:], 0.0, ['Lacc0'])
                    C.memset('pool', LaccP[1][:], 0.0, ['Lacc1'])
                    pcnt[0] = 0
                C.mm(pZ[zb][:, c0:], Kh, Qh[:, c0:], True, True, [kkey, ('QT', ct)], [f'pZ{zb}'])
                C.act(Eb[eb][:, c0:], pZ[zb][:, c0:], AF.Exp, [f'pZ{zb}'], [f'Eb{eb}'])
                C.act(Lb[lb][:, c0:], Eb[eb][:, c0:], AF.Ln, [f'Eb{eb}'], [f'Lb{lb}'], bias=1.0)
                if j >= 0:
                    C.tt('dve', Lb[lb][:, c0:c0 + 128], Lb[lb][:, c0:c0 + 128], tri[:], ALU.mult, [f'Lb{lb}', 'tri'], [f'Lb{lb}'])
                if kb > 0:
                    la_ = lcnt[0] % NRB
                    lcnt[0] += 1
                    pp = pcnt[0] % 2
                    pcnt[0] += 1
                    Lo, Ln_ = LaccP[pp], LaccP[1 - pp]
                    ko, kn = f'Lacc{pp}', f'Lacc{1 - pp}'
                    C.tt('pool', Ln_[:, c0:], Lo[:, c0:], Lb[lb][:, c0:], ALU.add, [ko, f'Lb{lb}'], [kn])
                    C.cp('dve', Laccb[la_][:], Ln_[:], [kn], [f'Laccb{la_}'])
                    it['la_out'] = la_

            def stageB(it, prev):
                hd, bi, kb, c0, j = it['hd'], it['bi'], it['kb'], it['c0'], it['j']
                Kh, kkey, Qh, ct, lb = it['Kh'], it['kkey'], it['Qh'], it['ct'], it['lb']
                cb = bcnt[0] % 2
                bcnt[0] += 1
                C.mm(pC[cb][:, c0:], uincl[:], Lb[lb][:, c0:], True, bi == 0, ['uincl', f'Lb{lb}'], [f'pC{cb}'])
                if bi > 0:
                    la_ = prev['la_out']
                    C.mm(pC[cb][:, c0:], negones[:], Laccb[la_][:, c0:], False, True, ['negones', f'Laccb{la_}'], [f'pC{cb}'])
                wi = wcnt[0] % NWB
                wcnt[0] += 1
                eb = it['eb']
                C.act(xC[cb][:, c0:], pC[cb][:, c0:], AF.Exp, [f'pC{cb}'], [f'xC{cb}'])
                C.tt('dve', Wt[wi][:, c0:], xC[cb][:, c0:], Eb[eb][:, c0:], ALU.mult, [f'xC{cb}', f'Eb{eb}'], [f'Wt{wi}'])
                if j >= 0:
                    C.tt('dve', Wt[wi][:, c0:c0 + 128], Wt[wi][:, c0:c0 + 128], tri[:], ALU.mult, [f'Wt{wi}', 'tri'], [f'Wt{wi}'])
                it['wi'] = wi

            def stagePV(it):
                hd, bi, kb, c0 = it['hd'], it['bi'], it['kb'], it['c0']
                wi = it['wi']
                C.mm(pO[0:64, c0:], Vt[:, kb, hd * 64:(hd + 1) * 64], Wt[wi][:, c0:], bi == 0, bi == it['nblk'] - 1,
                     [('V', kb), f'Wt{wi}'], ['pO'])
                if bi == it['nblk'] - 1:
                    ob = ocnt[0] % 2
                    ocnt[0] += 1
                    C.cp('dve', yst[ob][:], pO[0:64, :], ['pO'], [f'yst{ob}'])
                    C.dma('sp', yT[hd * 64:(hd + 1) * 64, t0:t0 + 512], yst[ob][:], [f'yst{ob}'], [], f'ya{ob}', cw=['yT'])

            for n_ in range(min(LA, len(items))):
                stageA(items[n_])
            PVL = 2
            for n_ in range(len(items)):
                if n_ >= PVL:
                    stagePV(items[n_ - PVL])
                stageB(items[n_], items[n_ - 1] if n_ > 0 else None)
                if n_ + LA < len(items):
                    stageA(items[n_ + LA])
            for n_ in range(max(0, len(items) - PVL), len(items)):
                stagePV(items[n_])
        if post is not None:
            post(C)
        info = S.emit()
    return info


def prep_mixer_inputs(inp, l, b, hg, SL=None):
    f = np.float32
    w_in = np.asarray(inp['w_in'][l])
    hs = slice(hg * 256, (hg + 1) * 256)
    q = w_in[:, 0:512][:, hs]
    k = w_in[:, 512:1024][:, hs]
    v = w_in[:, 1024:1536][:, hs]
    cs = slice(hg * 128, (hg + 1) * 128)
    xr = w_in[:, 1536:1792][:, cs]
    xg = w_in[:, 1792:2048][:, cs]
    cval = w_in[:, 2048:2304]
    cgate = w_in[:, 2304:2560]
    wall = np.ascontiguousarray(np.concatenate([q, k, xr, xg, cval, cgate, v], axis=1), dtype=f)
    vec = np.zeros((128, NV), f)
    vec[:, V_GQ] = np.tile(inp['q_norm_g'][l], 2)
    vec[:, V_GK] = np.tile(inp['k_norm_g'][l], 2)
    for kk in range(4):
        vec[:, V_LCW + kk] = inp['lru_conv_w'][l][kk, cs]
    vec[:, V_LCB] = inp['lru_conv_b'][l][cs]
    vec[:, V_BA] = inp['lru_ba'][l][cs]
    vec[:, V_BX] = inp['lru_bx'][l][cs]
    vec[:, V_LAM] = inp['lru_lambda'][l][cs]
    for t in range(2):
        ts_ = slice(t * 128, (t + 1) * 128)
        vec[:, V_DWB + t] = inp['conf_dw_b'][l][ts_]
        vec[:, V_LNG + t] = inp['conf_ln_g'][l][ts_]
        vec[:, V_LNB + t] = inp['conf_ln_b'][l][ts_]
        for kk in range(31):
            vec[:, V_DWW + t * 31 + kk] = inp['conf_dw_w'][l][kk, ts_]
    vec[:, V_PWB] = inp['conf_pw_b'][l][cs]
    g1 = np.ascontiguousarray(np.asarray(inp['norm1_g'][l]).reshape(8, 128).T, dtype=f)
    wab = np.zeros((128, 2, 128), f)
    for i in range(2):
        blk = hg * 2 + i
        wab[i * 64:(i + 1) * 64, 0, i * 64:(i + 1) * 64] = inp['lru_wa'][l][blk]
        wab[i * 64:(i + 1) * 64, 1, i * 64:(i + 1) * 64] = inp['lru_wx'][l][blk]
    pw = np.asarray(inp['conf_pw_w'][l])[:, cs]
    pww = np.ascontiguousarray(pw.reshape(2, 128, 128).transpose(1, 0, 2), dtype=f)
    return dict(wall=wall, vec=vec, g1=g1, wab=wab, pww=pww)


CAP = 640
NSLOT = 32 * CAP
NROWS = NSLOT + 128
TRASH = float(NSLOT)


def build_ffn(nc, gs, NT, xin, yG, msk, wout, gout, g2bd, wr, wg, wu, wd, xout, Xs, Ys, xmid, post=None):
    NTT = NT // 128
    stage = 4
    dbg = False
    with ExitStack() as st:
        C = Ctx(nc, st, gs)
        S = C.S
        sb, ps = C.sb, C.ps
        mks = sb("mks", [128, 2], F32)
        ysc = [[sb(f"ysc{i}_{k}", [128, 8, 128], BF16) for k in range(2)] for i in range(2)]
        Woutb = sb("Woutb", [128, 8, 1024], BF16)
        wstg = [sb(f"wstg{i}", [128, 1024], F32) for i in range(2)]
        gos = sb("gos", [128, 8], F32)
        g2b = sb("g2b", [128, 1024], F32)
        wrs = sb("wrs", [128, 8, 36], F32)
        identf = sb("identf", [128, 128], F32)
        identb = sb("identb", [128, 128], BF16)
        onec = sb("onec", [128, 2], BF16)
        ustr = sb("ustr", [128, 128], BF16)
        ones128 = sb("ones128", [128, 128], BF16)
        basef = sb("basef", [128, 32], F32)
        zt = sb("zt", [128, 4, 1024], BF16)
        gates = sb("gates", [128, NTT, 2], F32)
        dsti = sb("dsti", [128, NTT * 2], I32)
        Macc = sb("Macc", [128, 32], F32)
        Maccb = sb("Maccb", [128, 32], BF16)
        xt = [sb(f"xt{i}", [128, 1024], F32) for i in range(2)]
        ys = [sb(f"ys{i}", [128, 8, 128], BF16) for i in range(2)]
        ysq = sb("ysq", [128, 8, 128], BF16)
        x1 = [sb(f"x1_{i}", [128, 1024], F32) for i in range(2)]
        junk = sb("junk", [128, 1024], BF16)
        h2f = sb("h2f", [128, 1024], F32)
        h2b = [sb(f"h2b{i}", [128, 1024], BF16) for i in range(2)]
        h2T = sb("h2T", [128, 8, 128], F32)
        sm = [sb(f"sm{i}", [128, 16], F32) for i in range(2)]
        lg = sb("lg", [128, 36], F32)
        r1 = sb("r1", [128, 224], F32)
        Mb = sb("Mb", [128, 32], BF16)
        dstf = sb("dstf", [128, 2], F32)
        wgb = [sb(f"wgb{i}", [128, 8, 512], BF16) for i in range(2)]
        wub = [sb(f"wub{i}", [128, 8, 512], BF16) for i in range(2)]
        wdb = [sb(f"wdb{i}", [128, 4, 1024], BF16) for i in range(2)]
        NST = CAP // 128
        xe = [sb(f"xe{i}", [128, NST, 1024], BF16) for i in range(2)]
        xeT = sb("xeT", [128, 8, CAP], BF16)
        sgl = [sb(f"sgl{i}", [128, CAP], F32) for i in range(2)]
        aT = sb("aT", [128, 4, CAP], BF16)
        yo = [sb(f"yo{i}", [128, NST, 1024], BF16) for i in range(2)]
        yg = [[sb(f"yg{i}_{k}", [128, 1024], BF16) for k in range(2)] for i in range(2)]
        pM = ps("pM", [128, 512], F32)
        pP = [ps(f"pP{i}", [128, 512], F32) for i in range(2)]
        pTf = ps("pTf", [128, 1024], F32)
        pUU = ps("pUU", [128, 1024], F32)
        pTb = ps("pTb", [128, 1024], BF16)

        C.dma('sp', gos[:], gout, [], ['gos'], 'c0')
        C.dma('sp', mks[:], msk, [], ['mks'], 'c3')
        C.dma('sp', g2b[:], g2bd, [], ['g2b'], 'c1')
        C.dma('sp', wrs[:], wr.rearrange("(c p) n -> p c n", p=128), [], ['wrs'], 'c2')
        wov = wout.rearrange("(c p) n -> p c n", p=128)
        for c in range(8):
            b = c % 2
            C.dma('sp', wstg[b][:], wov[:, c, :], [], [f'wstg{b}'], f'wstg{b}')
            C.ts('dve' if b else 'pool', Woutb[:, c, :], wstg[b][:], gos[:, c:c + 1], None, ALU.mult, None,
                 [f'wstg{b}', 'gos'], [('Wo', c)])
        WoK = [('Wo', c) for c in range(8)]
        C.memset('pool', identf[:], 0.0, ['identf'])
        C.aselect(identf[:], identf[:], [[-1, 128]], ALU.not_equal, 1.0, 0, 1, ['identf'], ['identf'])
        C.cp('pool', identb[:], identf[:], ['identf'], ['identb'])
        C.memset('pool', onec[:], 1.0, ['onec'])
        C.memset('pool', ones128[:], 1.0, ['ones128'])
        C.memset('pool', ustr[:], 1.0, ['ustr'])
        C.aselect(ustr[:], ustr[:], [[1, 128]], ALU.is_ge, 0.0, -1, -1, ['ustr'], ['ustr'])
        S.add('pool', lambda e: e.iota(basef[:], pattern=[[CAP, 32]], base=0, channel_multiplier=0,
                                       allow_small_or_imprecise_dtypes=True), w=['basef'])
        C.memset('pool', zt[:], 0.0, ['zt'])
        C.memset('pool', Macc[:], 0.0, ['Macc'])
        C.memset('pool', Maccb[:], 0.0, ['Maccb'])
        Xv = Xs.rearrange("(n p) d -> p n d", p=128)
        nrt = NROWS // 128
        zi = 0
        for n0 in range(0, nrt, 4):
            n1 = min(nrt, n0 + 4)
            C.dma('sp', Xv[:, n0:n1, :], zt[:, 0:n1 - n0, :], ['zt'], [], f'z{zi % 2}', cw=['Xs'])
            zi += 1
        C.dma('sp', Ys[NSLOT:NROWS, :], zt[:, 0, :], ['zt'], [], 'zy', cw=['Ys'])
        S.add('sp', lambda e: e.nop(), r=[], w=['Xs'])

        xinv = xin.rearrange("(n p) d -> n p d", p=128)
        xmv = xmid.rearrange("(n p) d -> n p d", p=128)
        xov = xout.rearrange("(n p) d -> n p d", p=128)
        yv = yG.rearrange("(c p) t -> p c t", p=128)

        def loads(i):
            b = i % 2
            C.dma('sp', xt[b][:], xinv[i], [], [f'xt{b}'], f'lx{b}')
            for hh in range(2):
                C.dma('sp', ysc[b][hh][:], yv[:, :, hh * NT + i * 128:hh * NT + (i + 1) * 128], [], [f'ysc{b}{hh}'], f'ly{b}{hh}')
            C.ts('dve', ys[b][:], ysc[b][0][:], mks[:, 0:1], None, ALU.mult, None, [f'ysc{b}0', 'mks'], [f'ys{b}'])
            C.stt(ys[b][:], ysc[b][1][:], mks[:, 1:2], ys[b][:], ALU.mult, ALU.add, [f'ysc{b}1', 'mks', f'ys{b}'], [f'ys{b}'])

        loads(0)
        GRP = [([0, 1, 2, 3], 512.0), ([4, 5], 256.0), ([6, 7], 256.0)]
        for i in range(NTT):
            b = i % 2
            if i + 1 < NTT:
                loads(i + 1)
            s = sm[b]
            C.act(ysq[:], ys[b][:], AF.Square, [f'ys{b}'], ['ysq'])
            for gi, (cl_, n) in enumerate(GRP):
                for c in cl_:
                    C.mm(pM[:, gi:gi + 1], ysq[:, c, :], onec[:, 0:1], c == cl_[0], c == cl_[-1], ['ysq', 'onec'], ['pM'])
            C.act(s[:, 0:1], pM[:, 0:1], AF.Sqrt, ['pM'], [f's0{b}'], scale=1.0 / 512.0, bias=EPS)
            C.act(s[:, 1:3], pM[:, 1:3], AF.Sqrt, ['pM'], [f's1{b}'], scale=1.0 / 256.0, bias=EPS)
            C.recip(s[:, 3:6], s[:, 0:3], [f's0{b}', f's1{b}'], [f'rg{b}'])
            k = 0
            for half in range(2):
                for gi, (cl_, n) in enumerate(GRP):
                    bk = k % 2
                    k += 1
                    for c in cl_:
                        C.mm(pP[bk][:], ys[b][:, c, :], Woutb[:, c, half * 512:(half + 1) * 512], c == cl_[0], c == cl_[-1],
                             [f'ys{b}'] + WoK, [f'pP{bk}'])
                    src = xt[b] if gi == 0 else x1[b]
                    C.stt(x1[b][:, half * 512:(half + 1) * 512], pP[bk][:], s[:, 3 + gi:4 + gi], src[:, half * 512:(half + 1) * 512],
                          ALU.mult, ALU.add, [f'pP{bk}', f'rg{b}', f'xt{b}', ('x1', b, half)], [('x1', b, half)])
            x1k = [('x1', b, 0), ('x1', b, 1)]
            C.dma('sp', xmv[i], x1[b][:], x1k, [], f'sx{b}', cw=['xmid'])
            C.act(junk[:], x1[b][:], AF.Square, x1k, [f'ss{b}'], accum=s[:, 6:7])
            C.act(s[:, 7:8], s[:, 6:7], AF.Sqrt, [f'ss{b}'], [f'rt{b}'], scale=1.0 / 1024.0, bias=EPS)
            C.recip(s[:, 8:9], s[:, 7:8], [f'rt{b}'], [f'r2{b}'])
            C.stt(h2f[:], x1[b][:], s[:, 8:9], g2b[:], ALU.mult, ALU.mult, x1k + [f'r2{b}', 'g2b'], ['h2f'])
            C.cp('act', h2b[b][:], h2f[:], ['h2f'], [f'h2b{b}'])
            for c in range(8):
                C.tr(pTf[:, c * 128:(c + 1) * 128], h2f[:, c * 128:(c + 1) * 128], identf[:], ['h2f', 'identf'], ['pTf'])
            C.cp('dve', h2T[:].rearrange("p c t -> p (c t)"), pTf[:], ['pTf'], ['h2T'])
            for c in range(8):
                C.mm(pM[:, 8:44], h2T[:, c, :], wrs[:, c, :], c == 0, c == 7, ['h2T', 'wrs'], ['pM'])
            C.cp('act', lg[:], pM[:, 8:44], ['pM'], ['lg'])
            R = 'r1'
            S.add('dve', lambda e, s=s: e.tensor_reduce(out=s[:, 9:10], in_=lg[:, 0:4], axis=AX.X, op=ALU.max), r=['lg'], w=[f'mx{b}'])
            C.ts('dve', r1[:, 0:4], lg[:, 0:4], s[:, 9:10], None, ALU.is_equal, None, ['lg', f'mx{b}'], ['ohg'])
            C.ts('dve', s[:, 10:11], s[:, 9:10], -1.0, None, ALU.mult, None, [f'mx{b}'], [f'nmx{b}'])
            C.act(r1[:, 4:8], lg[:, 0:4], AF.Exp, ['lg', f'nmx{b}'], ['ec', f'sc{b}'], bias=s[:, 10:11], accum=s[:, 11:12])
            C.recip(s[:, 12:13], s[:, 11:12], [f'sc{b}'], [f'wgrp{b}'])
            C.ts('dve', r1[:, 8:12], r1[:, 0:4], -1.0, 1e30, ALU.add, ALU.mult, ['ohg'], ['pen'])
            for gq in range(4):
                C.ts('dve', r1[:, 16 + gq * 8:24 + gq * 8], lg[:, 4 + gq * 8:12 + gq * 8], r1[:, 8 + gq:9 + gq], None, ALU.add, None,
                     ['lg', 'pen'], [('msk', gq)])
            mk = [('msk', gq) for gq in range(4)]
            S.add('dve', lambda e: e.max(out=r1[:, 48:56], in_=r1[:, 16:48]), r=mk, w=['top8'])
            C.ts('dve', r1[:, 56:88], r1[:, 16:48], r1[:, 48:49], None, ALU.is_equal, None, mk + ['top8'], ['oh1'])
            C.ts('dve', r1[:, 88:120], r1[:, 16:48], r1[:, 49:50], None, ALU.is_equal, None, mk + ['top8'], ['oh2'])
            C.tt('dve', s[:, 13:14], r1[:, 49:50], r1[:, 48:49], ALU.subtract, ['top8'], [f'dd{b}'])
            C.act(s[:, 14:15], s[:, 13:14], AF.Exp, [f'dd{b}'], [f'ee{b}'])
            C.ts('dve', s[:, 15:16], s[:, 14:15], 1.0, None, ALU.add, None, [f'ee{b}'], [f'den{b}'])
            C.recip(s[:, 15:16], s[:, 15:16], [f'den{b}'], [f'den{b}'])
            C.tt('dve', gates[:, i, 0:1], s[:, 15:16], s[:, 12:13], ALU.mult, [f'den{b}', f'wgrp{b}'], [('g1', i)])
            C.tt('dve', gates[:, i, 1:2], gates[:, i, 0:1], s[:, 14:15], ALU.mult, [('g1', i), f'ee{b}'], [('g2', i)])
            C.tt('dve', r1[:, 120:152], r1[:, 56:88], r1[:, 88:120], ALU.add, ['oh1', 'oh2'], ['Mf'])
            C.cp('dve', Mb[:], r1[:, 120:152], ['Mf'], ['Mb'])
            C.mm(pM[:, 64:96], ustr[:], Mb[:], True, i == 0, ['ustr', 'Mb'], ['pM'])
            if i > 0:
                C.mm(pM[:, 64:96], ones128[:], Maccb[:], False, True, ['ones128', 'Maccb'], ['pM'])
            C.tt('pool', Macc[:], Macc[:], r1[:, 120:152], ALU.add, ['Macc', 'Mf'], ['Macc'])
            C.cp('pool', Maccb[:], Macc[:], ['Macc'], ['Maccb'])
            SLT = r1[:, 152:184]
            OKM = r1[:, 184:216]
            C.tt('dve', SLT, pM[:, 64:96], basef[:], ALU.add, ['pM', 'basef'], ['slot'])
            C.ts('dve', OKM, pM[:, 64:96], float(CAP), None, ALU.is_lt, None, ['pM'], ['okm'])
            C.ts('dve', SLT, SLT, -TRASH, None, ALU.add, None, ['slot'], ['slot'])
            C.tt('dve', SLT, SLT, OKM, ALU.mult, ['slot', 'okm'], ['slot'])
            C.ts('dve', SLT, SLT, TRASH, None, ALU.add, None, ['slot'], ['slot'])
            C.tt('dve', r1[:, 56:88], r1[:, 56:88], SLT, ALU.mult, ['oh1', 'slot'], ['oh1'])
            C.tt('dve', r1[:, 88:120], r1[:, 88:120], SLT, ALU.mult, ['oh2', 'slot'], ['oh2'])
            S.add('dve', lambda e: e.reduce_sum(out=dstf[:, 0:1], in_=r1[:, 56:88], axis=AX.X), r=['oh1'], w=['dstf0'])
            S.add('dve', lambda e: e.reduce_sum(out=dstf[:, 1:2], in_=r1[:, 88:120], axis=AX.X), r=['oh2'], w=['dstf1'])
            C.ts('dve', dstf[:], dstf[:], 0.0, float(NROWS - 1), ALU.max, ALU.min, ['dstf0', 'dstf1'], ['dstf0', 'dstf1'])
            C.cp('dve', dsti[:, 2 * i:2 * i + 2], dstf[:], ['dstf0', 'dstf1'], [('dsti', i)])
            for k2 in range(2 if stage >= 2 else 0):
                S.add('pool', lambda e, i=i, k2=k2, b=b: e.indirect_dma_start(
                    out=Xs[:, :], out_offset=bass.IndirectOffsetOnAxis(ap=dsti[:, 2 * i + k2:2 * i + k2 + 1], axis=0),
                    in_=h2b[b][:], in_offset=None, oob_is_err=False),
                    r=[f'h2b{b}', ('dsti', i)], cw=['Xs'], tag=f'sc{b}{k2}')

        S.add('dve', lambda e: e.memset(junk[:, 0:8], 0.0), r=['pTf'], w=['pTfg', 'pTfu'])
        def wloads(e_):
            b = e_ % 2
            C.dma('pool', wgb[b][:], wg[e_].rearrange("(c p) f -> p c f", p=128), [], [f'wgb{b}'], f'wg{b}')
            C.dma('pool', wub[b][:], wu[e_].rearrange("(c p) f -> p c f", p=128), [], [f'wub{b}'], f'wu{b}')
            C.dma('pool', wdb[b][:], wd[e_].rearrange("(c p) f -> p c f", p=128), [], [f'wdb{b}'], f'wd{b}')

        def xloads(e_):
            b = e_ % 2
            C.dma('sp', xe[b][:], Xs[e_ * CAP:(e_ + 1) * CAP, :].rearrange("(t p) d -> p t d", p=128), ['Xs'], [f'xe{b}'], f'xe{b}')

        if stage >= 3:
            wloads(0)
            xloads(0)
        dk = 0
        for e_ in range(32 if stage >= 3 else 0):
            b = e_ % 2
            if e_ + 1 < 32:
                wloads(e_ + 1)
                xloads(e_ + 1)
            for stt_ in range(NST):
                for c in range(8):
                    C.tr(pTb[:, c * 128:(c + 1) * 128], xe[b][:, stt_, c * 128:(c + 1) * 128], identb[:], [f'xe{b}', 'identb'], ['pTb'])
                C.cp('act' if stt_ % 2 else 'dve', xeT[:, :, stt_ * 128:(stt_ + 1) * 128], pTb[:].rearrange("p (c t) -> p c t", c=8),
                     ['pTb'], [('xeT', stt_)])
            xk = [('xeT', t_) for t_ in range(NST)]
            for fc in range(4):
                for (a0, a1) in ((0, 512), (512, CAP)):
                    for c in range(8):
                        C.mm(pTf[:, a0:a1], wgb[b][:, c, fc * 128:(fc + 1) * 128], xeT[:, c, a0:a1], c == 0, c == 7, [f'wgb{b}'] + xk, ['pTfg'])
                for (a0, a1) in ((0, 512), (512, CAP)):
                    for c in range(8):
                        C.mm(pUU[:, a0:a1], wub[b][:, c, fc * 128:(fc + 1) * 128], xeT[:, c, a0:a1], c == 0, c == 7, [f'wub{b}'] + xk, ['pTfu'])
                C.act(sgl[fc % 2][:], pTf[:, 0:CAP], AF.Silu, ['pTfg'], [f'sgl{fc % 2}'])
                C.tt('dve', aT[:, fc, :], pUU[:, 0:CAP], sgl[fc % 2][:], ALU.mult, ['pTfu', f'sgl{fc % 2}'], [('aT', fc)])
            ak = [('aT', fc) for fc in range(4)]
            for stt_ in range(NST):
                for half in range(2):
                    bk = dk % 2
                    dk += 1
                    for fc in range(4):
                        C.mm(pP[bk][:], aT[:, fc, stt_ * 128:(stt_ + 1) * 128], wdb[b][:, fc, half * 512:(half + 1) * 512],
                             fc == 0, fc == 3, ak + [f'wdb{b}'], [f'pP{bk}'])
                    C.cp('act' if dk % 2 else 'dve', yo[b][:, stt_, half * 512:(half + 1) * 512], pP[bk][:], [f'pP{bk}'],
                         [('yo', b, stt_, half)])
            yk = [('yo', b, t_, h_) for t_ in range(NST) for h_ in range(2)]
            C.dma('sp', Ys[e_ * CAP:(e_ + 1) * CAP, :].rearrange("(t p) d -> p t d", p=128), yo[b][:], yk, [], f'yo{b}', cw=['Ys'])

        def gloads(i):
            b = i % 2
            C.dma('sp', xt[b][:], xmv[i], ['xmid'], [f'xt{b}'], f'lx{b}')
            for k2 in range(2 if stage >= 4 else 0):
                S.add('pool', lambda e, i=i, k2=k2, b=b: e.indirect_dma_start(
                    out=yg[b][k2][:], out_offset=None, in_=Ys[:, :],
                    in_offset=bass.IndirectOffsetOnAxis(ap=dsti[:, 2 * i + k2:2 * i + k2 + 1], axis=0),
                    oob_is_err=False),
                    r=['Ys', ('dsti', i)], w=[f'yg{b}{k2}'], tag=f'gy{b}{k2}')

        gloads(0)
        for i in range(NTT):
            b = i % 2
            if i + 1 < NTT:
                gloads(i + 1)
            if stage < 4:
                C.dma('sp', xov[i], xt[b][:], [f'xt{b}'], [], f'so{b}')
                continue
            C.stt(x1[b][:], yg[b][0][:], gates[:, i, 0:1], xt[b][:], ALU.mult, ALU.add,
                  [f'yg{b}0', ('g1', i), f'xt{b}'], [('x1', b, 0), ('x1', b, 1)])
            C.stt(x1[b][:], yg[b][1][:], gates[:, i, 1:2], x1[b][:], ALU.mult, ALU.add,
                  [f'yg{b}1', ('g2', i), ('x1', b, 0), ('x1', b, 1)], [('x1', b, 0), ('x1', b, 1)])
            C.dma('sp', xov[i], x1[b][:], [('x1', b, 0), ('x1', b, 1)], [], f'so{b}', cw=['xout'])
        if post is not None:
            post(C)
        info = S.emit()
    return info


STD_OF_YG = [0, 2, 1, 3, 4, 5, 6, 7]


def prep_ffn_inputs(inp, l):
    f = np.float32
    wr = np.ascontiguousarray(np.concatenate([inp['router_coarse'][l], inp['router_fine'][l]], axis=1), dtype=f)
    wout = np.asarray(inp['w_out'][l], dtype=f).reshape(8, 128, 1024)[STD_OF_YG].reshape(1024, 1024)
    gout = np.asarray(inp['out_norm_g'][l], dtype=f).reshape(8, 128)[STD_OF_YG].T
    return dict(
        wout=np.ascontiguousarray(wout),
        gout=np.ascontiguousarray(gout),
        g2bd=np.ascontiguousarray(np.broadcast_to(np.asarray(inp['norm2_g'][l])[None, :], (128, 1024)), dtype=f),
        wr=wr,
        wg=np.ascontiguousarray(inp['exp_w_gate'][l], dtype=f),
        wu=np.ascontiguousarray(inp['exp_w_up'][l], dtype=f),
        wd=np.ascontiguousarray(inp['exp_w_down'][l], dtype=f),
    )


MIX_KEYS = dict(wall=[1024, NW], vec=[128, NV], g1=[128, 8], wab=[128, 2, 128], pww=[128, 2, 128])
FFN_KEYS = dict(wout=[1024, 1024], gout=[128, 8], g2bd=[128, 1024], wr=[1024, 36], wg=[32, 1024, 512],
                wu=[32, 1024, 512], wd=[32, 512, 1024])
RG_PAIRS = [[0, 1], [2, 3], [4, 5], [6, 7]]


def build_fused(SL, depth=2):
    NT = SL // 2
    nc = bass.Bass("TRN2", target_bir_lowering=False)

    def din(name, shape, dt=F32):
        return nc.dram_tensor(name, shape, dt, kind="ExternalInput").ap()

    x_full = din("x_full", [SL, 1024])
    xin0 = din("xin0", [NT, 1024])
    msk = din("msk", [128, 2])
    W = []
    for l in range(depth):
        d = {k: din(f"{k}{l}", shp) for k, shp in MIX_KEYS.items()}
        d.update({k: din(f"{k}{l}", shp) for k, shp in FFN_KEYS.items()})
        W.append(d)
    xout = nc.dram_tensor("xout", [NT, 1024], F32, kind="ExternalOutput").ap()
    yT = nc.dram_tensor("yT_i", [512, SL], BF16, kind="Internal").ap()
    yG = nc.dram_tensor("yG_i", [1024, SL], BF16, kind="Internal").ap()
    Xs = nc.dram_tensor("Xs_i", [NROWS, 1024], BF16, kind="Internal").ap()
    Ys = nc.dram_tensor("Ys_i", [NROWS, 1024], BF16, kind="Internal").ap()
    xmid = nc.dram_tensor("xmid_i", [NT, 1024], F32, kind="Internal").ap()
    xo = nc.dram_tensor("xo_i", [NT, 1024], F32, kind="Internal").ap()
    xG = nc.dram_tensor("xG_i", [SL, 1024], F32, kind="Internal").ap()
    with ExitStack() as outer:
        gs = GSync(nc, outer)
        for l in range(depth):
            last = l == depth - 1
            w = W[l]

            def post_m(C):
                for k in range(4):
                    C.S.add('pool', lambda e, k=k: e.collective_compute(
                        "AllGather", ALU.bypass, replica_groups=RG_PAIRS,
                        ins=[yT[k * 128:(k + 1) * 128, :]], outs=[yG[k * 256:(k + 1) * 256, :]]),
                        r=['yT'], cw=['yG'], tag='cc', inc=1)

            def xtile(n):
                p = n * 128
                r_, q = p // NT, p % NT
                row = (q // 512) * 1024 + r_ * 512 + q % 512
                return xG[row:row + 128, :]

            build_mixer(nc, gs, SL, x_full if l == 0 else xG, w['wall'], w['vec'], w['g1'], w['wab'], w['pww'], yT, post=post_m,
                        xtile=None if l == 0 else xtile)
            nc.all_engine_barrier()

            def post_f(C, last=last):
                if not last:
                    for k in range(NT // 512):
                        C.S.add('pool', lambda e, k=k: e.collective_compute(
                            "AllGather", ALU.bypass, replica_groups=RG_PAIRS,
                            ins=[xo[k * 512:(k + 1) * 512, :]], outs=[xG[k * 1024:(k + 1) * 1024, :]]),
                            r=['xout'], cw=['xG'], tag='cc', inc=1)

            build_ffn(nc, gs, NT, xin0 if l == 0 else xo, yG, msk, w['wout'], w['gout'], w['g2bd'], w['wr'], w['wg'], w['wu'],
                      w['wd'], xout if last else xo, Xs, Ys, xmid, post=post_f)
            if not last:
                nc.all_engine_barrier()
    return nc


_NC_CACHE = {}


def run_fused(inp, X):
    B, SL, D = X.shape
    NT = SL // 2
    depth = np.asarray(inp['w_in']).shape[0]
    key = (SL, depth)
    if key not in _NC_CACHE:
        _NC_CACHE[key] = build_fused(SL, depth)
    nc = _NC_CACHE[key]
    fw = [prep_ffn_inputs(inp, l) for l in range(depth)]
    maps = []
    for c in range(8):
        b, h = c // 2, c % 2
        m = {'x_full': np.ascontiguousarray(X[b]), 'xin0': np.ascontiguousarray(X[b, h * NT:(h + 1) * NT])}
        mk = np.zeros((128, 2), np.float32)
        mk[:, h] = 1.0
        m['msk'] = mk
        for l in range(depth):
            for k, v in prep_mixer_inputs(inp, l, b, h).items():
                m[f'{k}{l}'] = v
            for k, v in fw[l].items():
                m[f'{k}{l}'] = v
        maps.append(m)
    res = run_bass_kernel_spmd(nc, maps, core_ids=list(range(8)))
    out = np.empty_like(X)
    for c in range(8):
        b, h = c // 2, c % 2
        out[b, h * NT:(h + 1) * NT] = np.asarray(res.results[c]['xout'])
    return out


def kernel(**inp):
    X = np.ascontiguousarray(np.asarray(inp['x'], dtype=np.float32))
    return run_fused(inp, X)
```

```python
import numpy as np
import ml_dtypes
from contextlib import ExitStack
import concourse.bass as bass
import concourse.mybir as mybir
from concourse.bass_utils import run_bass_kernel_spmd

F32 = mybir.dt.float32
BF16 = mybir.dt.bfloat16
I32 = mybir.dt.int32
AF = mybir.ActivationFunctionType
ALU = mybir.AluOpType
AX = mybir.AxisListType

ENG_ATTR = {'pe': 'tensor', 'act': 'scalar', 'dve': 'vector', 'pool': 'gpsimd', 'sp': 'sync'}
EPS = 1e-6


class GSync:
    def __init__(self, nc, stack):
        self.nc = nc
        self.stack = stack
        self.sems = {}
        self.cnts = {}
        self.tagmap = {}

    def sem(self, name):
        if name not in self.sems:
            self.sems[name] = self.stack.enter_context(self.nc.semaphore(name))
        return self.sems[name]

    def tagsem(self, tag):
        if tag not in self.tagmap:
            self.tagmap[tag] = 't_' + tag
        return self.tagmap[tag]


class Sched:
    def __init__(self, nc, stack, gs=None):
        self.nc = nc
        self.stack = stack
        self.gs = gs if gs is not None else GSync(nc, stack)
        self.ops = []
        self.wx = {}
        self.wc = {}
        self.rd = {}
        self.tag_last = {}
        self.sems = {}

    @staticmethod
    def _merge(dst, src):
        for s, i in src.items():
            if dst.get(s, -1) < i:
                dst[s] = i

    def add(self, eng, fn, r=(), w=(), cw=(), tag=None, inc=16):
        deps = {}
        for k in r:
            self._merge(deps, self.wx.get(k, {}))
            self._merge(deps, self.wc.get(k, {}))
        for k in w:
            self._merge(deps, self.wx.get(k, {}))
            self._merge(deps, self.wc.get(k, {}))
            self._merge(deps, self.rd.get(k, {}))
        for k in cw:
            self._merge(deps, self.wx.get(k, {}))
            self._merge(deps, self.rd.get(k, {}))
        if tag is not None and tag in self.tag_last:
            self._merge(deps, {('t', tag): self.tag_last[tag]})
        idx = len(self.ops)
        if eng == 'pe':
            deps.pop(('e', 'pe'), None)
        self.ops.append(dict(eng=eng, fn=fn, deps=deps, tag=tag, sem=None, cnt=0, inc=inc))
        sig = ('t', tag) if tag else ('e', eng)
        for k in r:
            self.rd.setdefault(k, {})[sig] = idx
        for k in w:
            self.wx[k] = {sig: idx}
            self.wc[k] = {}
            self.rd[k] = {}
        for k in cw:
            self.wc.setdefault(k, {})[sig] = idx
        if tag is not None:
            self.tag_last[tag] = idx
        return idx

    def _sem(self, name):
        return self.gs.sem(name)

    def emit(self):
        ops = self.ops
        need = set()
        for o in ops:
            for i in o['deps'].values():
                need.add(i)
        cnts = self.gs.cnts
        for i, o in enumerate(ops):
            if o['tag']:
                o['sem'] = self.gs.tagsem(o['tag'])
                cnts[o['sem']] = cnts.get(o['sem'], 0) + o['inc']
                o['cnt'] = cnts[o['sem']]
            elif i in need:
                o['sem'] = 'e_' + o['eng']
                cnts[o['sem']] = cnts.get(o['sem'], 0) + 1
                o['cnt'] = cnts[o['sem']]
        final = {}
        for o in ops:
            if o['sem']:
                final[o['sem']] = max(final.get(o['sem'], 0), o['cnt'])
        for s in final:
            self._sem(s)
        with self.nc.Block() as blk:
            for eng, attr in ENG_ATTR.items():
                mine = [o for o in ops if o['eng'] == eng]

                def body(e, mine=mine, eng=eng):
                    waited = {}
                    for o in mine:
                        for di in sorted(o['deps'].values()):
                            d = ops[di]
                            if waited.get(d['sem'], 0) < d['cnt']:
                                e.wait_ge(self._sem(d['sem']), d['cnt'])
                                waited[d['sem']] = d['cnt']
                        ins = o['fn'](e)
                        if o['sem']:
                            ins.then_inc(self._sem(o['sem']), o['inc'] if o['tag'] else 1)
                    if eng == 'sp':
                        for s, c in final.items():
                            if waited.get(s, 0) < c:
                                e.wait_ge(self._sem(s), c)

                getattr(blk, attr)(body)
        return dict(nops=len(ops), final=final)


class Ctx:
    def __init__(self, nc, st, gs=None):
        self.nc = nc
        self.st = st
        self.S = Sched(nc, st, gs)
        g = self.S.gs
        g.phase = getattr(g, 'phase', 0) + 1
        self.pfx = f"p{g.phase}_"

    def sb(self, name, shape, dt):
        return self.st.enter_context(self.nc.sbuf_tensor(self.pfx + name, shape, dt))

    def ps(self, name, shape, dt):
        return self.st.enter_context(self.nc.psum_tensor(self.pfx + name, shape, dt))

    def dma(self, q, out, in_, r, w, tag, cw=()):
        self.S.add(q, lambda e: e.dma_start(out=out, in_=in_), r=r, w=w, cw=cw, tag=tag)

    def mm(self, out, lhsT, rhs, start, stop, r, w):
        self.S.add('pe', lambda e: e.matmul(out, lhsT=lhsT, rhs=rhs, start=start, stop=stop,
                                            skip_group_check=True), r=r, w=w)

    def tr(self, out, in_, ident, r, w):
        self.S.add('pe', lambda e: e.transpose(out, in_, ident), r=r, w=w)

    def act(self, out, in_, func, r, w, bias=None, scale=None, accum=None):
        kw = {}
        if bias is not None:
            kw['bias'] = bias
        if scale is not None:
            kw['scale'] = scale
        if accum is not None:
            kw['accum_out'] = accum
        self.S.add('act', lambda e: e.activation(out=out, in_=in_, func=func, **kw), r=r, w=w)

    def ts(self, eng, out, in0, s1, s2, op0, op1, r, w):
        if op1 is None:
            self.S.add(eng, lambda e: e.tensor_scalar(out=out, in0=in0, scalar1=s1, scalar2=None, op0=op0), r=r, w=w)
        else:
            self.S.add(eng, lambda e: e.tensor_scalar(out=out, in0=in0, scalar1=s1, scalar2=s2, op0=op0, op1=op1), r=r, w=w)

    def tt(self, eng, out, in0, in1, op, r, w):
        self.S.add(eng, lambda e: e.tensor_tensor(out=out, in0=in0, in1=in1, op=op), r=r, w=w)

    def stt(self, out, in0, scalar, in1, op0, op1, r, w):
        self.S.add('dve', lambda e: e.scalar_tensor_tensor(out=out, in0=in0, scalar=scalar, in1=in1, op0=op0, op1=op1), r=r, w=w)

    def cp(self, eng, out, in_, r, w):
        if eng == 'act':
            self.S.add('act', lambda e: e.copy(out=out, in_=in_), r=r, w=w)
        else:
            self.S.add(eng, lambda e: e.tensor_copy(out=out, in_=in_), r=r, w=w)

    def memset(self, eng, ap, val, w):
        self.S.add(eng, lambda e: e.memset(ap, val), w=w)

    def recip(self, out, in_, r, w):
        self.S.add('dve', lambda e: e.reciprocal(out=out, in_=in_), r=r, w=w)

    def aselect(self, out, in_, pattern, op, fill, base, cm, r, w):
        self.S.add('pool', lambda e: e.affine_select(out=out, in_=in_, pattern=pattern, compare_op=op, fill=fill,
                                                     base=base, channel_multiplier=cm), r=r, w=w)


V_GQ, V_GK, V_LCW, V_LCB, V_BA, V_BX, V_LAM = 0, 1, 2, 6, 7, 8, 9
V_DWB, V_LNG, V_LNB, V_PWB, V_DWW = 10, 12, 14, 16, 17
NV = 17 + 62
NW = 1536


def build_mixer(nc, gs, SL, x, wall, vec, g1, wab, pww, yT, post=None, xtile=None):
    NG = SL // 512
    with ExitStack() as st:
        C = Ctx(nc, st, gs)
        S = C.S
        sb, ps = C.sb, C.ps
        Wb = sb("Wb", [128, 8, NW], BF16)
        wst = [sb(f"wst{i}", [128, 192], F32) for i in range(2)]
        vecs = sb("vecs", [128, NV], F32)
        g1s = sb("g1s", [128, 8], F32)
        wabf = sb("wabf", [128, 2, 128], F32)
        wabb = sb("wabb", [128, 2, 128], BF16)
        pwf = sb("pwf", [128, 2, 128], F32)
        pwb = sb("pwb", [128, 2, 128], BF16)
        identb = sb("identb", [128, 128], BF16)
        blk1 = sb("blk1", [128, 128], BF16)
        onesm = sb("onesm", [128, 128], F32)
        uincl = sb("uincl", [128, 128], BF16)
        negones = sb("negones", [128, 128], BF16)
        tri = sb("tri", [128, 128], BF16)
        dg = sb("dg", [128, 62, 128], BF16)
        cl = sb("cl", [128, 4], F32)
        KT = sb("KT", [128, 2, SL], BF16)
        Vt = sb("Vt", [128, SL // 128, 256], BF16)
        QT = sb("QT", [128, 2, 512], BF16)
        xt = [sb(f"xt{i}", [128, 1024], F32) for i in range(2)]
        xn = [sb(f"xn{i}", [128, 1024], BF16) for i in range(2)]
        st1 = [sb(f"st1_{i}", [128, 4], F32) for i in range(2)]
        hT = sb("hT", [128, 8, 512], BF16)
        sq = sb("sq", [128, 512], BF16)
        rs = sb("rs", [128, 512], F32)
        xrb = sb("xrb", [128, 515], F32)
        xgb = sb("xgb", [128, 512], F32)
        cv = sb("cv", [128, 512], F32)
        cvb = sb("cvb", [128, 512], BF16)
        lr = sb("lr", [128, 512], F32)
        li = sb("li", [128, 512], F32)
        la = sb("la", [128, 512], F32)
        la2 = sb("la2", [128, 512], F32)
        lu = sb("lu", [128, 512], F32)
        lh = sb("lh", [128, 512], F32)
        hprev = sb("hprev", [128, 1], F32)
        gt = sb("gt", [128, 512], F32)
        ybs = sb("ybs", [128, 512], BF16)
        ub = sb("ub", [128, 2, 542], BF16)
        sg = sb("sg", [128, 512], F32)
        cvo = sb("cvo", [128, 2, 512], F32)
        sqo = sb("sqo", [128, 2, 512], F32)
        means = sb("means", [128, 512], F32)
        m2 = sb("m2", [128, 512], F32)
        crs = sb("crs", [128, 512], F32)
        cn = sb("cn", [128, 2, 512], F32)
        csb = sb("csb", [128, 2, 512], BF16)
        ycs = sb("ycs", [128, 512], BF16)
        LA = 2
        NRB = LA + 2
        Eb = [sb(f"Eb{i}", [128, 512], BF16) for i in range(NRB)]
        Lb = [sb(f"Lb{i}", [128, 512], BF16) for i in range(NRB)]
        NWB = 4
        Wt = [sb(f"Wt{i}", [128, 512], BF16) for i in range(NWB)]
        LaccP = [sb(f"Lacc{i}", [128, 512], F32) for i in range(2)]
        xC = [sb(f"xC{i}", [128, 512], BF16) for i in range(2)]
        Laccb = [sb(f"Laccb{i}", [128, 512], BF16) for i in range(NRB)]
        yst = [sb(f"yst{i}", [128, 512], BF16) for i in range(2)]
        pA = [ps(f"pA{i}", [128, 512], F32) for i in range(2)]
        pT = ps("pT", [128, 1024], BF16)
        pZ = [ps(f"pZ{i}", [128, 512], F32) for i in range(2)]
        pC = [ps(f"pC{i}", [128, 512], F32) for i in range(2)]
        pO = ps("pO", [128, 512], F32)

        C.dma('sp', vecs[:], vec, [], ['vecs'], 'c0')
        C.dma('sp', g1s[:], g1, [], ['g1s'], 'c1')
        C.dma('sp', wabf[:], wab, [], ['wabf'], 'c2')
        C.dma('sp', pwf[:], pww, [], ['pwf'], 'c3')
        C.cp('dve', wabb[:], wabf[:], ['wabf'], ['wabb'])
        C.cp('dve', pwb[:], pwf[:], ['pwf'], ['pwb'])
        wv = wall.rearrange("(c p) n -> p c n", p=128)
        k = 0
        for c in range(8):
            for hf in range(8):
                b = k % 2
                C.dma('sp', wst[b][:], wv[:, c, hf * 192:(hf + 1) * 192], [], [f'wst{b}'], f'wst{b}')
                C.ts('dve' if k % 2 == 0 else 'pool', Wb[:, c, hf * 192:(hf + 1) * 192], wst[b][:], g1s[:, c:c + 1], None,
                     ALU.mult, None, [f'wst{b}', 'g1s'], [('Wb', c, hf)])
                k += 1
        WbK = [('Wb', c, hf) for c in range(8) for hf in range(8)]
        C.memset('pool', identb[:], 0.0, ['identb'])
        C.aselect(identb[:], identb[:], [[-1, 128]], ALU.not_equal, 1.0, 0, 1, ['identb'], ['identb'])
        C.memset('pool', blk1[:], 0.0, ['blk1'])
        C.memset('pool', blk1[0:64, 0:64], 1.0, ['blk1'])
        C.memset('pool', blk1[64:128, 64:128], 1.0, ['blk1'])
        C.memset('pool', onesm[:], 1.0 / 256.0, ['onesm'])
        C.memset('pool', negones[:], -1.0, ['negones'])
        C.memset('pool', uincl[:], -1.0, ['uincl'])
        C.aselect(uincl[:], uincl[:], [[-1, 128]], ALU.is_ge, 0.0, 0, 1, ['uincl'], ['uincl'])
        C.memset('pool', tri[:], 1.0, ['tri'])
        C.aselect(tri[:], tri[:], [[1, 128]], ALU.is_ge, 0.0, -1, -1, ['tri'], ['tri'])
        for i in range(62):
            C.ts('pool' if i % 2 else 'dve', dg[:, i, :], identb[:], vecs[:, V_DWW + i:V_DWW + i + 1], None, ALU.mult, None,
                 ['identb', 'vecs'], ['dg'])
        C.act(cl[:, 0:1], vecs[:, V_LAM:V_LAM + 1], AF.Exp, ['vecs'], ['cl0'], scale=-1.0)
        C.act(cl[:, 1:2], cl[:, 0:1], AF.Ln, ['cl0'], ['cl1'], bias=1.0)
        C.ts('dve', cl[:, 2:3], cl[:, 1:2], -8.0, None, ALU.mult, None, ['cl1'], ['cl2'])
        C.ts('dve', cl[:, 3:4], cl[:, 1:2], -16.0, None, ALU.mult, None, ['cl1'], ['cl3'])
        C.memset('pool', hprev[:], 0.0, ['hprev'])
        C.memset('pool', xrb[:, 0:3], 0.0, ['xrb_h'])
        C.memset('pool', ub[:, :, 0:30], 0.0, ['ub_h'])

        xv = x.rearrange("(n p) d -> n p d", p=128)
        wcnt = [0]
        ocnt = [0]
        acnt = [0]
        bcnt = [0]
        lcnt = [0]
        pcnt = [0]
        for g in range(NG):
            t0 = g * 512
            for tt in range(4):
                b = tt % 2
                C.dma('sp', xt[b][:], xv[g * 4 + tt] if xtile is None else xtile(g * 4 + tt), [], [f'xt{b}'], f'x{b}')
                C.act(xn[b][:], xt[b][:], AF.Square, [f'xt{b}'], [f'ss{b}', f'xn{b}'], accum=st1[b][:, 0:1])
                C.act(st1[b][:, 1:2], st1[b][:, 0:1], AF.Sqrt, [f'ss{b}'], [f'rt{b}'], scale=1.0 / 1024.0, bias=EPS)
                C.recip(st1[b][:, 2:3], st1[b][:, 1:2], [f'rt{b}'], [f'rstd{b}'])
                C.ts('dve', xn[b][:], xt[b][:], st1[b][:, 2:3], None, ALU.mult, None, [f'xt{b}', f'rstd{b}'], [f'xn{b}'])
                for c in range(8):
                    C.tr(pT[:, c * 128:(c + 1) * 128], xn[b][:, c * 128:(c + 1) * 128], identb[:], [f'xn{b}', 'identb'], ['pT'])
                C.cp('act' if tt % 2 else 'dve', hT[:, :, tt * 128:(tt + 1) * 128], pT[:].rearrange("p (c t) -> p c t", c=8),
                     ['pT'], [('hT', tt)])
            hTK = [('hT', tt) for tt in range(4)]
            pidx = [0]

            def proj(ct):
                bank = pidx[0] % 2
                pidx[0] += 1
                for c in range(8):
                    C.mm(pA[bank][:], Wb[:, c, ct * 128:(ct + 1) * 128], hT[:, c, :], c == 0, c == 7, WbK + hTK, [f'pA{bank}'])
                return bank

            for ct in range(4):
                bk = proj(ct)
                C.act(sq[:], pA[bk][:], AF.Square, [f'pA{bk}'], ['sq'])
                C.mm(pA[1 - bk][:], blk1[:], sq[:], True, True, ['blk1', 'sq'], [f'pA{1 - bk}'])
                pidx[0] += 1
                if ct < 2:
                    C.act(rs[:], pA[1 - bk][:], AF.Sqrt, [f'pA{1 - bk}'], ['rs'], scale=1.0, bias=64.0 * EPS)
                else:
                    C.act(rs[:], pA[1 - bk][:], AF.Sqrt, [f'pA{1 - bk}'], ['rs'], scale=1.0 / 64.0, bias=EPS)
                C.recip(rs[:], rs[:], ['rs'], ['rs'])
                if ct < 2:
                    C.stt(QT[:, ct, :], pA[bk][:], vecs[:, V_GQ:V_GQ + 1], rs[:], ALU.mult, ALU.mult,
                          [f'pA{bk}', 'vecs', 'rs'], [('QT', ct)])
                else:
                    C.stt(KT[:, ct - 2, t0:t0 + 512], pA[bk][:], vecs[:, V_GK:V_GK + 1], rs[:], ALU.mult, ALU.mult,
                          [f'pA{bk}', 'vecs', 'rs'], [('KT', ct - 2, g)])
            bk = proj(4)
            C.cp('act', xrb[:, 3:515], pA[bk][:], [f'pA{bk}'], ['xrb'])
            bk = proj(5)
            C.cp('dve', xgb[:], pA[bk][:], [f'pA{bk}'], ['xgb'])
            for t in range(2):
                bv = proj(6 + t)
                bg = proj(8 + t)
                C.act(sg[:], pA[bg][:], AF.Sigmoid, [f'pA{bg}'], ['sg'])
                C.tt('dve', ub[:, t, 30:542], pA[bv][:], sg[:], ALU.mult, [f'pA{bv}', 'sg'], [('ub', t)])
            for tt in range(4):
                bank = pidx[0] % 2
                pidx[0] += 1
                for c in range(8):
                    C.mm(pA[bank][:, 0:256], hT[:, c, tt * 128:(tt + 1) * 128], Wb[:, c, 1280:1536], c == 0, c == 7,
                         WbK + hTK, [f'pA{bank}'])
                C.cp('act' if tt % 2 else 'dve', Vt[:, g * 4 + tt, :], pA[bank][:, 0:256], [f'pA{bank}'], [('V', g * 4 + tt)])

            vc = lambda i: vecs[:, i:i + 1]
            C.ts('dve', cv[:], xrb[:, 3:515], vc(V_LCW + 3), vc(V_LCB), ALU.mult, ALU.add, ['xrb', 'xrb_h', 'vecs'], ['cv'])
            for kk in range(3):
                C.stt(cv[:], xrb[:, kk:kk + 512], vc(V_LCW + kk), cv[:], ALU.mult, ALU.add, ['xrb', 'xrb_h', 'vecs', 'cv'], ['cv'])
            C.cp('pool', xrb[:, 0:3], xrb[:, 512:515], ['xrb'], ['xrb_h'])
            C.cp('pool', cvb[:], cv[:], ['cv'], ['cvb'])
            b0 = pidx[0] % 2
            pidx[0] += 2
            C.mm(pA[b0][:], wabb[:, 0, :], cvb[:], True, True, ['wabb', 'cvb'], [f'pA{b0}'])
            C.mm(pA[1 - b0][:], wabb[:, 1, :], cvb[:], True, True, ['wabb', 'cvb'], [f'pA{1 - b0}'])
            C.act(lr[:], pA[b0][:], AF.Sigmoid, [f'pA{b0}', 'vecs'], ['lr'], bias=vc(V_BA))
            C.act(li[:], pA[1 - b0][:], AF.Sigmoid, [f'pA{1 - b0}', 'vecs'], ['li'], bias=vc(V_BX))
            C.act(la[:], lr[:], AF.Exp, ['lr', 'cl2'], ['la'], scale=cl[:, 2:3])
            C.act(la2[:], lr[:], AF.Exp, ['lr', 'cl3'], ['la2'], scale=cl[:, 3:4])
            C.act(la2[:], la2[:], AF.Sqrt, ['la2'], ['la2'], scale=-1.0, bias=1.0)
            C.tt('pool', lu[:], li[:], cv[:], ALU.mult, ['li', 'cv'], ['lu'])
            C.tt('pool', lu[:], lu[:], la2[:], ALU.mult, ['lu', 'la2'], ['lu'])
            S.add('dve', lambda e: e.tensor_tensor_scan(out=lh[:], data0=la[:], data1=lu[:], initial=hprev[:, 0:1],
                                                        op0=ALU.mult, op1=ALU.add), r=['la', 'lu', 'hprev'], w=['lh'])
            C.cp('pool', hprev[:], lh[:, 511:512], ['lh'], ['hprev'])
            C.tt('pool', gt[:], xgb[:], xgb[:], ALU.mult, ['xgb'], ['gt'])
            C.ts('pool', gt[:], gt[:], 0.044715, 1.0, ALU.mult, ALU.add, ['gt'], ['gt'])
            C.tt('pool', gt[:], gt[:], xgb[:], ALU.mult, ['gt', 'xgb'], ['gt'])
            C.act(gt[:], gt[:], AF.Sigmoid, ['gt'], ['gt'], scale=1.5957691216)
            C.tt('pool', gt[:], gt[:], xgb[:], ALU.mult, ['gt', 'xgb'], ['gt'])
            C.tt('dve', ybs[:], gt[:], lh[:], ALU.mult, ['gt', 'lh'], ['ybs'])
            C.dma('pool', yT[256:384, t0:t0 + 512], ybs[:], ['ybs'], [], 'yb', cw=['yT'])

            for t in range(2):
                bank = pidx[0] % 2
                pidx[0] += 1
                for kk in range(31):
                    C.mm(pA[bank][:], dg[:, t * 31 + kk, :], ub[:, t, kk:kk + 512], kk == 0, kk == 30,
                         ['dg', ('ub', t), 'ub_h'], [f'pA{bank}'])
                C.act(cvo[:, t, :], pA[bank][:], AF.Identity, [f'pA{bank}', 'vecs'], [('cvo', t)], bias=vc(V_DWB + t))
                C.act(sqo[:, t, :], pA[bank][:], AF.Square, [f'pA{bank}', 'vecs'], [('sqo', t)], bias=vc(V_DWB + t))
            C.cp('pool', ub[:, :, 0:30], ub[:, :, 512:542], [('ub', 0), ('ub', 1)], ['ub_h'])
            bm = pidx[0] % 2
            pidx[0] += 2
            for t in range(2):
                C.mm(pA[bm][:], onesm[:], cvo[:, t, :], t == 0, t == 1, ['onesm', ('cvo', t)], [f'pA{bm}'])
            for t in range(2):
                C.mm(pA[1 - bm][:], onesm[:], sqo[:, t, :], t == 0, t == 1, ['onesm', ('sqo', t)], [f'pA{1 - bm}'])
            C.cp('act', means[:], pA[bm][:], [f'pA{bm}'], ['means'])
            C.tt('pool', m2[:], means[:], means[:], ALU.mult, ['means'], ['m2'])
            C.tt('dve', m2[:], pA[1 - bm][:], m2[:], ALU.subtract, [f'pA{1 - bm}', 'm2'], ['m2'])
            C.act(crs[:], m2[:], AF.Sqrt, ['m2'], ['crs'], scale=1.0, bias=EPS)
            C.recip(crs[:], crs[:], ['crs'], ['crs'])
            for t in range(2):
                C.tt('pool', cn[:, t, :], cvo[:, t, :], means[:], ALU.subtract, [('cvo', t), 'means'], [('cn', t)])
                C.tt('dve' if t else 'pool', cn[:, t, :], cn[:, t, :], crs[:], ALU.mult, [('cn', t), 'crs'], [('cn', t)])
                C.act(csb[:, t, :], cn[:, t, :], AF.Silu, [('cn', t), 'vecs'], [('csb', t)], scale=vc(V_LNG + t), bias=vc(V_LNB + t))
            bank = pidx[0] % 2
            pidx[0] += 1
            for t in range(2):
                C.mm(pA[bank][:], pwb[:, t, :], csb[:, t, :], t == 0, t == 1, ['pwb', ('csb', t)], [f'pA{bank}'])
            C.act(ycs[:], pA[bank][:], AF.Identity, [f'pA{bank}', 'vecs'], ['ycs'], bias=vc(V_PWB))
            C.dma('pool', yT[384:512, t0:t0 + 512], ycs[:], ['ycs'], [], 'yc', cw=['yT'])

            items = []
            for hd in range(4):
                nblk = 4 * g + 4
                for bi, kb in enumerate(range(4 * g + 3, -1, -1)):
                    items.append(dict(hd=hd, bi=bi, kb=kb, nblk=nblk))

            def stageA(it):
                hd, bi, kb = it['hd'], it['bi'], it['kb']
                ct = hd // 2
                pb = 64 * (hd % 2)
                Qh = QT[pb:pb + 64, ct, :]
                j = kb - 4 * g
                c0 = 128 * j if j > 0 else 0
                Kh = KT[pb:pb + 64, ct, kb * 128:(kb + 1) * 128]
                kkey = ('KT', ct, kb // 4)
                zb = acnt[0] % 2
                lb = acnt[0] % NRB
                eb = acnt[0] % NRB
                acnt[0] += 1
                it.update(c0=c0, j=j, Kh=Kh, kkey=kkey, Qh=Qh, ct=ct, lb=lb, eb=eb)
                if bi == 0:
                    C.memset('pool', LaccP[0][:], 0.0, ['Lacc0'])
                    C.memset('pool', LaccP[1][:], 0.0, ['Lacc1'])
                    pcnt[0] = 0
                C.mm(pZ[zb][:, c0:], Kh, Qh[:, c0:], True, True, [kkey, ('QT', ct)], [f'pZ{zb}'])
                C.act(Eb[eb][:, c0:], pZ[zb][:, c0:], AF.Exp, [f'pZ{zb}'], [f'Eb{eb}'])
                C.act(Lb[lb][:, c0:], Eb[eb][:, c0:], AF.Ln, [f'Eb{eb}'], [f'Lb{lb}'], bias=1.0)
                if j >= 0:
                    C.tt('dve', Lb[lb][:, c0:c0 + 128], Lb[lb][:, c0:c0 + 128], tri[:], ALU.mult, [f'Lb{lb}', 'tri'], [f'Lb{lb}'])
                if kb > 0:
                    la_ = lcnt[0] % NRB
                    lcnt[0] += 1
                    pp = pcnt[0] % 2
                    pcnt[0] += 1
                    Lo, Ln_ = LaccP[pp], LaccP[1 - pp]
                    ko, kn = f'Lacc{pp}', f'Lacc{1 - pp}'
                    C.tt('pool', Ln_[:, c0:], Lo[:, c0:], Lb[lb][:, c0:], ALU.add, [ko, f'Lb{lb}'], [kn])
                    C.cp('dve', Laccb[la_][:], Ln_[:], [kn], [f'Laccb{la_}'])
                    it['la_out'] = la_

            def stageB(it, prev):
                hd, bi, kb, c0, j = it['hd'], it['bi'], it['kb'], it['c0'], it['j']
                Kh, kkey, Qh, ct, lb = it['Kh'], it['kkey'], it['Qh'], it['ct'], it['lb']
                cb = bcnt[0] % 2
                bcnt[0] += 1
                C.mm(pC[cb][:, c0:], uincl[:], Lb[lb][:, c0:], True, bi == 0, ['uincl', f'Lb{lb}'], [f'pC{cb}'])
                if bi > 0:
                    la_ = prev['la_out']
                    C.mm(pC[cb][:, c0:], negones[:], Laccb[la_][:, c0:], False, True, ['negones', f'Laccb{la_}'], [f'pC{cb}'])
                wi = wcnt[0] % NWB
                wcnt[0] += 1
                eb = it['eb']
                C.act(xC[cb][:, c0:], pC[cb][:, c0:], AF.Exp, [f'pC{cb}'], [f'xC{cb}'])
                C.tt('dve', Wt[wi][:, c0:], xC[cb][:, c0:], Eb[eb][:, c0:], ALU.mult, [f'xC{cb}', f'Eb{eb}'], [f'Wt{wi}'])
                if j >= 0:
                    C.tt('dve', Wt[wi][:, c0:c0 + 128], Wt[wi][:, c0:c0 + 128], tri[:], ALU.mult, [f'Wt{wi}', 'tri'], [f'Wt{wi}'])
                it['wi'] = wi

            def stagePV(it):
                hd, bi, kb, c0 = it['hd'], it['bi'], it['kb'], it['c0']
                wi = it['wi']
                hp = hd // 2
                pb = 64 * (hd % 2)
                C.mm(pO[:, c0:], Vt[:, kb, hp * 128:(hp + 1) * 128], Wt[wi][:, c0:], bi == 0, bi == it['nblk'] - 1,
                     [('V', kb), f'Wt{wi}'], ['pO'])
                if bi == it['nblk'] - 1:
                    ob = ocnt[0] % 2
                    ocnt[0] += 1
                    C.cp('dve', yst[ob][pb:pb + 64, :], pO[pb:pb + 64, :], ['pO'], [f'yst{ob}'])
                    C.dma('sp', yT[hd * 64:(hd + 1) * 64, t0:t0 + 512], yst[ob][pb:pb + 64, :], [f'yst{ob}'], [], f'ya{ob}', cw=['yT'])

            for n_ in range(min(LA, len(items))):
                stageA(items[n_])
            PVL = 2
            for n_ in range(len(items)):
                if n_ >= PVL:
                    stagePV(items[n_ - PVL])
                stageB(items[n_], items[n_ - 1] if n_ > 0 else None)
                if n_ + LA < len(items):
                    stageA(items[n_ + LA])
            for n_ in range(max(0, len(items) - PVL), len(items)):
                stagePV(items[n_])
        if post is not None:
            post(C)
        info = S.emit()
    return info


def prep_mixer_inputs(inp, l, b, hg, SL=None):
    f = np.float32
    w_in = np.asarray(inp['w_in'][l])
    hs = slice(hg * 256, (hg + 1) * 256)
    q = w_in[:, 0:512][:, hs]
    k = w_in[:, 512:1024][:, hs]
    v = w_in[:, 1024:1536][:, hs]
    cs = slice(hg * 128, (hg + 1) * 128)
    xr = w_in[:, 1536:1792][:, cs]
    xg = w_in[:, 1792:2048][:, cs]
    cval = w_in[:, 2048:2304]
    cgate = w_in[:, 2304:2560]
    wall = np.ascontiguousarray(np.concatenate([q, k, xr, xg, cval, cgate, v], axis=1), dtype=f)
    vec = np.zeros((128, NV), f)
    vec[:, V_GQ] = np.tile(inp['q_norm_g'][l], 2)
    vec[:, V_GK] = np.tile(inp['k_norm_g'][l], 2)
    for kk in range(4):
        vec[:, V_LCW + kk] = inp['lru_conv_w'][l][kk, cs]
    vec[:, V_LCB] = inp['lru_conv_b'][l][cs]
    vec[:, V_BA] = inp['lru_ba'][l][cs]
    vec[:, V_BX] = inp['lru_bx'][l][cs]
    vec[:, V_LAM] = inp['lru_lambda'][l][cs]
    for t in range(2):
        ts_ = slice(t * 128, (t + 1) * 128)
        vec[:, V_DWB + t] = inp['conf_dw_b'][l][ts_]
        vec[:, V_LNG + t] = inp['conf_ln_g'][l][ts_]
        vec[:, V_LNB + t] = inp['conf_ln_b'][l][ts_]
        for kk in range(31):
            vec[:, V_DWW + t * 31 + kk] = inp['conf_dw_w'][l][kk, ts_]
    vec[:, V_PWB] = inp['conf_pw_b'][l][cs]
    g1 = np.ascontiguousarray(np.asarray(inp['norm1_g'][l]).reshape(8, 128).T, dtype=f)
    wab = np.zeros((128, 2, 128), f)
    for i in range(2):
        blk = hg * 2 + i
        wab[i * 64:(i + 1) * 64, 0, i * 64:(i + 1) * 64] = inp['lru_wa'][l][blk]
        wab[i * 64:(i + 1) * 64, 1, i * 64:(i + 1) * 64] = inp['lru_wx'][l][blk]
    pw = np.asarray(inp['conf_pw_w'][l])[:, cs]
    pww = np.ascontiguousarray(pw.reshape(2, 128, 128).transpose(1, 0, 2), dtype=f)
    return dict(wall=wall, vec=vec, g1=g1, wab=wab, pww=pww)


CAP = 640
NSLOT = 32 * CAP
NROWS = NSLOT + 128
TRASH = float(NSLOT)


def build_ffn(nc, gs, NT, xin, yG, msk, wout, gout, g2bd, wr, wg, wu, wd, xout, Xs, Ys, xmid, post=None):
    NTT = NT // 128
    stage = 4
    dbg = False
    with ExitStack() as st:
        C = Ctx(nc, st, gs)
        S = C.S
        sb, ps = C.sb, C.ps
        mks = sb("mks", [128, 2], F32)
        ysc = [[sb(f"ysc{i}_{k}", [128, 8, 128], BF16) for k in range(2)] for i in range(2)]
        Woutb = sb("Woutb", [128, 8, 1024], BF16)
        wstg = [sb(f"wstg{i}", [128, 1024], F32) for i in range(2)]
        gos = sb("gos", [128, 8], F32)
        g2b = sb("g2b", [128, 1024], F32)
        wrs = sb("wrs", [128, 8, 36], F32)
        identf = sb("identf", [128, 128], F32)
        identb = sb("identb", [128, 128], BF16)
        onec = sb("onec", [128, 2], BF16)
        ustr = sb("ustr", [128, 128], BF16)
        ones128 = sb("ones128", [128, 128], BF16)
        basef = sb("basef", [128, 32], F32)
        zt = sb("zt", [128, 4, 1024], BF16)
        gates = sb("gates", [128, NTT, 2], F32)
        dsti = sb("dsti", [128, NTT * 2], I32)
        Macc = sb("Macc", [128, 32], F32)
        Maccb = sb("Maccb", [128, 32], BF16)
        xt = [sb(f"xt{i}", [128, 1024], F32) for i in range(2)]
        ys = [sb(f"ys{i}", [128, 8, 128], BF16) for i in range(2)]
        ysq = sb("ysq", [128, 8, 128], BF16)
        x1 = [sb(f"x1_{i}", [128, 1024], F32) for i in range(2)]
        junk = sb("junk", [128, 1024], BF16)
        h2f = sb("h2f", [128, 1024], F32)
        h2b = [sb(f"h2b{i}", [128, 1024], BF16) for i in range(2)]
        h2T = sb("h2T", [128, 8, 128], F32)
        sm = [sb(f"sm{i}", [128, 16], F32) for i in range(2)]
        lg = sb("lg", [128, 36], F32)
        r1 = sb("r1", [128, 224], F32)
        Mb = sb("Mb", [128, 32], BF16)
        dstf = sb("dstf", [128, 2], F32)
        wgb = [sb(f"wgb{i}", [128, 8, 512], BF16) for i in range(2)]
        wub = [sb(f"wub{i}", [128, 8, 512], BF16) for i in range(2)]
        wdb = [sb(f"wdb{i}", [128, 4, 1024], BF16) for i in range(2)]
        NST = CAP // 128
        xe = [sb(f"xe{i}", [128, NST, 1024], BF16) for i in range(2)]
        xeT = sb("xeT", [128, 8, CAP], BF16)
        sgl = [sb(f"sgl{i}", [128, CAP], F32) for i in range(2)]
        aT = sb("aT", [128, 4, CAP], BF16)
        yo = [sb(f"yo{i}", [128, NST, 1024], BF16) for i in range(2)]
        yg = [[sb(f"yg{i}_{k}", [128, 1024], BF16) for k in range(2)] for i in range(2)]
        pM = ps("pM", [128, 512], F32)
        pP = [ps(f"pP{i}", [128, 512], F32) for i in range(2)]
        pTf = ps("pTf", [128, 1024], F32)
        pUU = ps("pUU", [128, 1024], F32)
        pTb = ps("pTb", [128, 1024], BF16)

        C.dma('sp', gos[:], gout, [], ['gos'], 'c0')
        C.dma('sp', mks[:], msk, [], ['mks'], 'c3')
        C.dma('sp', g2b[:], g2bd, [], ['g2b'], 'c1')
        C.dma('sp', wrs[:], wr.rearrange("(c p) n -> p c n", p=128), [], ['wrs'], 'c2')
        wov = wout.rearrange("(c p) n -> p c n", p=128)
        for c in range(8):
            b = c % 2
            C.dma('sp', wstg[b][:], wov[:, c, :], [], [f'wstg{b}'], f'wstg{b}')
            C.ts('dve' if b else 'pool', Woutb[:, c, :], wstg[b][:], gos[:, c:c + 1], None, ALU.mult, None,
                 [f'wstg{b}', 'gos'], [('Wo', c)])
        WoK = [('Wo', c) for c in range(8)]
        C.memset('pool', identf[:], 0.0, ['identf'])
        C.aselect(identf[:], identf[:], [[-1, 128]], ALU.not_equal, 1.0, 0, 1, ['identf'], ['identf'])
        C.cp('pool', identb[:], identf[:], ['identf'], ['identb'])
        C.memset('pool', onec[:], 1.0, ['onec'])
        C.memset('pool', ones128[:], 1.0, ['ones128'])
        C.memset('pool', ustr[:], 1.0, ['ustr'])
        C.aselect(ustr[:], ustr[:], [[1, 128]], ALU.is_ge, 0.0, -1, -1, ['ustr'], ['ustr'])
        S.add('pool', lambda e: e.iota(basef[:], pattern=[[CAP, 32]], base=0, channel_multiplier=0,
                                       allow_small_or_imprecise_dtypes=True), w=['basef'])
        C.memset('pool', zt[:], 0.0, ['zt'])
        C.memset('pool', Macc[:], 0.0, ['Macc'])
        C.memset('pool', Maccb[:], 0.0, ['Maccb'])
        Xv = Xs.rearrange("(n p) d -> p n d", p=128)
        nrt = NROWS // 128
        zi = 0
        for n0 in range(0, nrt, 4):
            n1 = min(nrt, n0 + 4)
            C.dma('sp', Xv[:, n0:n1, :], zt[:, 0:n1 - n0, :], ['zt'], [], f'z{zi % 2}', cw=['Xs'])
            zi += 1
        C.dma('sp', Ys[NSLOT:NROWS, :], zt[:, 0, :], ['zt'], [], 'zy', cw=['Ys'])
        S.add('sp', lambda e: e.nop(), r=[], w=['Xs'])

        xinv = xin.rearrange("(n p) d -> n p d", p=128)
        xmv = xmid.rearrange("(n p) d -> n p d", p=128)
        xov = xout.rearrange("(n p) d -> n p d", p=128)
        yv = yG.rearrange("(c p) t -> p c t", p=128)

        def loads(i):
            b = i % 2
            C.dma('sp', xt[b][:], xinv[i], [], [f'xt{b}'], f'lx{b}')
            for hh in range(2):
                C.dma('sp', ysc[b][hh][:], yv[:, :, hh * NT + i * 128:hh * NT + (i + 1) * 128], [], [f'ysc{b}{hh}'], f'ly{b}{hh}')
            C.ts('dve', ys[b][:], ysc[b][0][:], mks[:, 0:1], None, ALU.mult, None, [f'ysc{b}0', 'mks'], [f'ys{b}'])
            C.stt(ys[b][:], ysc[b][1][:], mks[:, 1:2], ys[b][:], ALU.mult, ALU.add, [f'ysc{b}1', 'mks', f'ys{b}'], [f'ys{b}'])

        loads(0)
        GRP = [([0, 1, 2, 3], 512.0), ([4, 5], 256.0), ([6, 7], 256.0)]
        for i in range(NTT):
            b = i % 2
            if i + 1 < NTT:
                loads(i + 1)
            s = sm[b]
            C.act(ysq[:], ys[b][:], AF.Square, [f'ys{b}'], ['ysq'])
            for gi, (cl_, n) in enumerate(GRP):
                for c in cl_:
                    C.mm(pM[:, gi:gi + 1], ysq[:, c, :], onec[:, 0:1], c == cl_[0], c == cl_[-1], ['ysq', 'onec'], ['pM'])
            C.act(s[:, 0:1], pM[:, 0:1], AF.Sqrt, ['pM'], [f's0{b}'], scale=1.0 / 512.0, bias=EPS)
            C.act(s[:, 1:3], pM[:, 1:3], AF.Sqrt, ['pM'], [f's1{b}'], scale=1.0 / 256.0, bias=EPS)
            C.recip(s[:, 3:6], s[:, 0:3], [f's0{b}', f's1{b}'], [f'rg{b}'])
            k = 0
            for half in range(2):
                for gi, (cl_, n) in enumerate(GRP):
                    bk = k % 2
                    k += 1
                    for c in cl_:
                        C.mm(pP[bk][:], ys[b][:, c, :], Woutb[:, c, half * 512:(half + 1) * 512], c == cl_[0], c == cl_[-1],
                             [f'ys{b}'] + WoK, [f'pP{bk}'])
                    src = xt[b] if gi == 0 else x1[b]
                    C.stt(x1[b][:, half * 512:(half + 1) * 512], pP[bk][:], s[:, 3 + gi:4 + gi], src[:, half * 512:(half + 1) * 512],
                          ALU.mult, ALU.add, [f'pP{bk}', f'rg{b}', f'xt{b}', ('x1', b, half)], [('x1', b, half)])
            x1k = [('x1', b, 0), ('x1', b, 1)]
            C.dma('sp', xmv[i], x1[b][:], x1k, [], f'sx{b}', cw=['xmid'])
            C.act(junk[:], x1[b][:], AF.Square, x1k, [f'ss{b}'], accum=s[:, 6:7])
            C.act(s[:, 7:8], s[:, 6:7], AF.Sqrt, [f'ss{b}'], [f'rt{b}'], scale=1.0 / 1024.0, bias=EPS)
            C.recip(s[:, 8:9], s[:, 7:8], [f'rt{b}'], [f'r2{b}'])
            C.stt(h2f[:], x1[b][:], s[:, 8:9], g2b[:], ALU.mult, ALU.mult, x1k + [f'r2{b}', 'g2b'], ['h2f'])
            C.cp('act', h2b[b][:], h2f[:], ['h2f'], [f'h2b{b}'])
            for c in range(8):
                C.tr(pTf[:, c * 128:(c + 1) * 128], h2f[:, c * 128:(c + 1) * 128], identf[:], ['h2f', 'identf'], ['pTf'])
            C.cp('dve', h2T[:].rearrange("p c t -> p (c t)"), pTf[:], ['pTf'], ['h2T'])
            for c in range(8):
                C.mm(pM[:, 8:44], h2T[:, c, :], wrs[:, c, :], c == 0, c == 7, ['h2T', 'wrs'], ['pM'])
            C.cp('act', lg[:], pM[:, 8:44], ['pM'], ['lg'])
            R = 'r1'
            S.add('dve', lambda e, s=s: e.tensor_reduce(out=s[:, 9:10], in_=lg[:, 0:4], axis=AX.X, op=ALU.max), r=['lg'], w=[f'mx{b}'])
            C.ts('dve', r1[:, 0:4], lg[:, 0:4], s[:, 9:10], None, ALU.is_equal, None, ['lg', f'mx{b}'], ['ohg'])
            C.ts('dve', s[:, 10:11], s[:, 9:10], -1.0, None, ALU.mult, None, [f'mx{b}'], [f'nmx{b}'])
            C.act(r1[:, 4:8], lg[:, 0:4], AF.Exp, ['lg', f'nmx{b}'], ['ec', f'sc{b}'], bias=s[:, 10:11], accum=s[:, 11:12])
            C.recip(s[:, 12:13], s[:, 11:12], [f'sc{b}'], [f'wgrp{b}'])
            C.ts('dve', r1[:, 8:12], r1[:, 0:4], -1.0, 1e30, ALU.add, ALU.mult, ['ohg'], ['pen'])
            for gq in range(4):
                C.ts('dve', r1[:, 16 + gq * 8:24 + gq * 8], lg[:, 4 + gq * 8:12 + gq * 8], r1[:, 8 + gq:9 + gq], None, ALU.add, None,
                     ['lg', 'pen'], [('msk', gq)])
            mk = [('msk', gq) for gq in range(4)]
            S.add('dve', lambda e: e.max(out=r1[:, 48:56], in_=r1[:, 16:48]), r=mk, w=['top8'])
            C.ts('dve', r1[:, 56:88], r1[:, 16:48], r1[:, 48:49], None, ALU.is_equal, None, mk + ['top8'], ['oh1'])
            C.ts('dve', r1[:, 88:120], r1[:, 16:48], r1[:, 49:50], None, ALU.is_equal, None, mk + ['top8'], ['oh2'])
            C.tt('dve', s[:, 13:14], r1[:, 49:50], r1[:, 48:49], ALU.subtract, ['top8'], [f'dd{b}'])
            C.act(s[:, 14:15], s[:, 13:14], AF.Exp, [f'dd{b}'], [f'ee{b}'])
            C.ts('dve', s[:, 15:16], s[:, 14:15], 1.0, None, ALU.add, None, [f'ee{b}'], [f'den{b}'])
            C.recip(s[:, 15:16], s[:, 15:16], [f'den{b}'], [f'den{b}'])
            C.tt('dve', gates[:, i, 0:1], s[:, 15:16], s[:, 12:13], ALU.mult, [f'den{b}', f'wgrp{b}'], [('g1', i)])
            C.tt('dve', gates[:, i, 1:2], gates[:, i, 0:1], s[:, 14:15], ALU.mult, [('g1', i), f'ee{b}'], [('g2', i)])
            C.tt('dve', r1[:, 120:152], r1[:, 56:88], r1[:, 88:120], ALU.add, ['oh1', 'oh2'], ['Mf'])
            C.cp('dve', Mb[:], r1[:, 120:152], ['Mf'], ['Mb'])
            C.mm(pM[:, 64:96], ustr[:], Mb[:], True, i == 0, ['ustr', 'Mb'], ['pM'])
            if i > 0:
                C.mm(pM[:, 64:96], ones128[:], Maccb[:], False, True, ['ones128', 'Maccb'], ['pM'])
            C.tt('pool', Macc[:], Macc[:], r1[:, 120:152], ALU.add, ['Macc', 'Mf'], ['Macc'])
            C.cp('pool', Maccb[:], Macc[:], ['Macc'], ['Maccb'])
            SLT = r1[:, 152:184]
            OKM = r1[:, 184:216]
            C.tt('dve', SLT, pM[:, 64:96], basef[:], ALU.add, ['pM', 'basef'], ['slot'])
            C.ts('dve', OKM, pM[:, 64:96], float(CAP), None, ALU.is_lt, None, ['pM'], ['okm'])
            C.ts('dve', SLT, SLT, -TRASH, None, ALU.add, None, ['slot'], ['slot'])
            C.tt('dve', SLT, SLT, OKM, ALU.mult, ['slot', 'okm'], ['slot'])
            C.ts('dve', SLT, SLT, TRASH, None, ALU.add, None, ['slot'], ['slot'])
            C.tt('dve', r1[:, 56:88], r1[:, 56:88], SLT, ALU.mult, ['oh1', 'slot'], ['oh1'])
            C.tt('dve', r1[:, 88:120], r1[:, 88:120], SLT, ALU.mult, ['oh2', 'slot'], ['oh2'])
            S.add('dve', lambda e: e.reduce_sum(out=dstf[:, 0:1], in_=r1[:, 56:88], axis=AX.X), r=['oh1'], w=['dstf0'])
            S.add('dve', lambda e: e.reduce_sum(out=dstf[:, 1:2], in_=r1[:, 88:120], axis=AX.X), r=['oh2'], w=['dstf1'])
            C.ts('dve', dstf[:], dstf[:], 0.0, float(NROWS - 1), ALU.max, ALU.min, ['dstf0', 'dstf1'], ['dstf0', 'dstf1'])
            C.cp('dve', dsti[:, 2 * i:2 * i + 2], dstf[:], ['dstf0', 'dstf1'], [('dsti', i)])
            for k2 in range(2 if stage >= 2 else 0):
                S.add('pool', lambda e, i=i, k2=k2, b=b: e.indirect_dma_start(
                    out=Xs[:, :], out_offset=bass.IndirectOffsetOnAxis(ap=dsti[:, 2 * i + k2:2 * i + k2 + 1], axis=0),
                    in_=h2b[b][:], in_offset=None, oob_is_err=False),
                    r=[f'h2b{b}', ('dsti', i)], cw=['Xs'], tag=f'sc{b}{k2}')

        S.add('dve', lambda e: e.memset(junk[:, 0:8], 0.0), r=['pTf'], w=['pTfg', 'pTfu'])
        def wloads(e_):
            b = e_ % 2
            C.dma('pool', wgb[b][:], wg[e_].rearrange("(c p) f -> p c f", p=128), [], [f'wgb{b}'], f'wg{b}')
            C.dma('pool', wub[b][:], wu[e_].rearrange("(c p) f -> p c f", p=128), [], [f'wub{b}'], f'wu{b}')
            C.dma('pool', wdb[b][:], wd[e_].rearrange("(c p) f -> p c f", p=128), [], [f'wdb{b}'], f'wd{b}')

        def xloads(e_):
            b = e_ % 2
            C.dma('sp', xe[b][:], Xs[e_ * CAP:(e_ + 1) * CAP, :].rearrange("(t p) d -> p t d", p=128), ['Xs'], [f'xe{b}'], f'xe{b}')

        if stage >= 3:
            wloads(0)
            xloads(0)
        dk = 0
        for e_ in range(32 if stage >= 3 else 0):
            b = e_ % 2
            if e_ + 1 < 32:
                wloads(e_ + 1)
                xloads(e_ + 1)
            for stt_ in range(NST):
                for c in range(8):
                    C.tr(pTb[:, c * 128:(c + 1) * 128], xe[b][:, stt_, c * 128:(c + 1) * 128], identb[:], [f'xe{b}', 'identb'], ['pTb'])
                C.cp('act' if stt_ % 2 else 'dve', xeT[:, :, stt_ * 128:(stt_ + 1) * 128], pTb[:].rearrange("p (c t) -> p c t", c=8),
                     ['pTb'], [('xeT', stt_)])
            xk = [('xeT', t_) for t_ in range(NST)]
            for fc in range(4):
                for (a0, a1) in ((0, 512), (512, CAP)):
                    for c in range(8):
                        C.mm(pTf[:, a0:a1], wgb[b][:, c, fc * 128:(fc + 1) * 128], xeT[:, c, a0:a1], c == 0, c == 7, [f'wgb{b}'] + xk, ['pTfg'])
                for (a0, a1) in ((0, 512), (512, CAP)):
                    for c in range(8):
                        C.mm(pUU[:, a0:a1], wub[b][:, c, fc * 128:(fc + 1) * 128], xeT[:, c, a0:a1], c == 0, c == 7, [f'wub{b}'] + xk, ['pTfu'])
                C.act(sgl[fc % 2][:], pTf[:, 0:CAP], AF.Silu, ['pTfg'], [f'sgl{fc % 2}'])
                C.tt('dve', aT[:, fc, :], pUU[:, 0:CAP], sgl[fc % 2][:], ALU.mult, ['pTfu', f'sgl{fc % 2}'], [('aT', fc)])
            ak = [('aT', fc) for fc in range(4)]
            for stt_ in range(NST):
                for half in range(2):
                    bk = dk % 2
                    dk += 1
                    for fc in range(4):
                        C.mm(pP[bk][:], aT[:, fc, stt_ * 128:(stt_ + 1) * 128], wdb[b][:, fc, half * 512:(half + 1) * 512],
                             fc == 0, fc == 3, ak + [f'wdb{b}'], [f'pP{bk}'])
                    C.cp('act' if dk % 2 else 'dve', yo[b][:, stt_, half * 512:(half + 1) * 512], pP[bk][:], [f'pP{bk}'],
                         [('yo', b, stt_, half)])
            yk = [('yo', b, t_, h_) for t_ in range(NST) for h_ in range(2)]
            C.dma('sp', Ys[e_ * CAP:(e_ + 1) * CAP, :].rearrange("(t p) d -> p t d", p=128), yo[b][:], yk, [], f'yo{b}', cw=['Ys'])

        def gloads(i):
            b = i % 2
            C.dma('sp', xt[b][:], xmv[i], ['xmid'], [f'xt{b}'], f'lx{b}')
            for k2 in range(2 if stage >= 4 else 0):
                S.add('pool', lambda e, i=i, k2=k2, b=b: e.indirect_dma_start(
                    out=yg[b][k2][:], out_offset=None, in_=Ys[:, :],
                    in_offset=bass.IndirectOffsetOnAxis(ap=dsti[:, 2 * i + k2:2 * i + k2 + 1], axis=0),
                    oob_is_err=False),
                    r=['Ys', ('dsti', i)], w=[f'yg{b}{k2}'], tag=f'gy{b}{k2}')

        gloads(0)
        for i in range(NTT):
            b = i % 2
            if i + 1 < NTT:
                gloads(i + 1)
            if stage < 4:
                C.dma('sp', xov[i], xt[b][:], [f'xt{b}'], [], f'so{b}')
                continue
            C.stt(x1[b][:], yg[b][0][:], gates[:, i, 0:1], xt[b][:], ALU.mult, ALU.add,
                  [f'yg{b}0', ('g1', i), f'xt{b}'], [('x1', b, 0), ('x1', b, 1)])
            C.stt(x1[b][:], yg[b][1][:], gates[:, i, 1:2], x1[b][:], ALU.mult, ALU.add,
                  [f'yg{b}1', ('g2', i), ('x1', b, 0), ('x1', b, 1)], [('x1', b, 0), ('x1', b, 1)])
            C.dma('sp', xov[i], x1[b][:], [('x1', b, 0), ('x1', b, 1)], [], f'so{b}', cw=['xout'])
        if post is not None:
            post(C)
        info = S.emit()
    return info


STD_OF_YG = [0, 2, 1, 3, 4, 5, 6, 7]


def prep_ffn_inputs(inp, l):
    f = np.float32
    wr = np.ascontiguousarray(np.concatenate([inp['router_coarse'][l], inp['router_fine'][l]], axis=1), dtype=f)
    wout = np.asarray(inp['w_out'][l], dtype=f).reshape(8, 128, 1024)[STD_OF_YG].reshape(1024, 1024)
    gout = np.asarray(inp['out_norm_g'][l], dtype=f).reshape(8, 128)[STD_OF_YG].T
    return dict(
        wout=np.ascontiguousarray(wout),
        gout=np.ascontiguousarray(gout),
        g2bd=np.ascontiguousarray(np.broadcast_to(np.asarray(inp['norm2_g'][l])[None, :], (128, 1024)), dtype=f),
        wr=wr,
        wg=np.ascontiguousarray(inp['exp_w_gate'][l], dtype=f),
        wu=np.ascontiguousarray(inp['exp_w_up'][l], dtype=f),
        wd=np.ascontiguousarray(inp['exp_w_down'][l], dtype=f),
    )


MIX_KEYS = dict(wall=[1024, NW], vec=[128, NV], g1=[128, 8], wab=[128, 2, 128], pww=[128, 2, 128])
FFN_KEYS = dict(wout=[1024, 1024], gout=[128, 8], g2bd=[128, 1024], wr=[1024, 36], wg=[32, 1024, 512],
                wu=[32, 1024, 512], wd=[32, 512, 1024])
RG_PAIRS = [[0, 1], [2, 3], [4, 5], [6, 7]]


def build_fused(SL, depth=2):
    NT = SL // 2
    nc = bass.Bass("TRN2", target_bir_lowering=False)

    def din(name, shape, dt=F32):
        return nc.dram_tensor(name, shape, dt, kind="ExternalInput").ap()

    x_full = din("x_full", [SL, 1024])
    xin0 = din("xin0", [NT, 1024])
    msk = din("msk", [128, 2])
    W = []
    for l in range(depth):
        d = {k: din(f"{k}{l}", shp) for k, shp in MIX_KEYS.items()}
        d.update({k: din(f"{k}{l}", shp) for k, shp in FFN_KEYS.items()})
        W.append(d)
    xout = nc.dram_tensor("xout", [NT, 1024], F32, kind="ExternalOutput").ap()
    yT = nc.dram_tensor("yT_i", [512, SL], BF16, kind="Internal").ap()
    yG = nc.dram_tensor("yG_i", [1024, SL], BF16, kind="Internal").ap()
    Xs = nc.dram_tensor("Xs_i", [NROWS, 1024], BF16, kind="Internal").ap()
    Ys = nc.dram_tensor("Ys_i", [NROWS, 1024], BF16, kind="Internal").ap()
    xmid = nc.dram_tensor("xmid_i", [NT, 1024], F32, kind="Internal").ap()
    xo = nc.dram_tensor("xo_i", [NT, 1024], F32, kind="Internal").ap()
    xG = nc.dram_tensor("xG_i", [SL, 1024], F32, kind="Internal").ap()
    with ExitStack() as outer:
        gs = GSync(nc, outer)
        for l in range(depth):
            last = l == depth - 1
            w = W[l]

            def post_m(C):
                for k in range(4):
                    C.S.add('pool', lambda e, k=k: e.collective_compute(
                        "AllGather", ALU.bypass, replica_groups=RG_PAIRS,
                        ins=[yT[k * 128:(k + 1) * 128, :]], outs=[yG[k * 256:(k + 1) * 256, :]]),
                        r=['yT'], cw=['yG'], tag='cc', inc=1)

            def xtile(n):
                p = n * 128
                r_, q = p // NT, p % NT
                row = (q // 512) * 1024 + r_ * 512 + q % 512
                return xG[row:row + 128, :]

            build_mixer(nc, gs, SL, x_full if l == 0 else xG, w['wall'], w['vec'], w['g1'], w['wab'], w['pww'], yT, post=post_m,
                        xtile=None if l == 0 else xtile)
            nc.all_engine_barrier()

            def post_f(C, last=last):
                if not last:
                    for k in range(NT // 512):
                        C.S.add('pool', lambda e, k=k: e.collective_compute(
                            "AllGather", ALU.bypass, replica_groups=RG_PAIRS,
                            ins=[xo[k * 512:(k + 1) * 512, :]], outs=[xG[k * 1024:(k + 1) * 1024, :]]),
                            r=['xout'], cw=['xG'], tag='cc', inc=1)

            build_ffn(nc, gs, NT, xin0 if l == 0 else xo, yG, msk, w['wout'], w['gout'], w['g2bd'], w['wr'], w['wg'], w['wu'],
                      w['wd'], xout if last else xo, Xs, Ys, xmid, post=post_f)
            if not last:
                nc.all_engine_barrier()
    return nc


_NC_CACHE = {}


def run_fused(inp, X):
    B, SL, D = X.shape
    NT = SL // 2
    depth = np.asarray(inp['w_in']).shape[0]
    key = (SL, depth)
    if key not in _NC_CACHE:
        _NC_CACHE[key] = build_fused(SL, depth)
    nc = _NC_CACHE[key]
    fw = [prep_ffn_inputs(inp, l) for l in range(depth)]
    maps = []
    for c in range(8):
        b, h = c // 2, c % 2
        m = {'x_full': np.ascontiguousarray(X[b]), 'xin0': np.ascontiguousarray(X[b, h * NT:(h + 1) * NT])}
        mk = np.zeros((128, 2), np.float32)
        mk[:, h] = 1.0
        m['msk'] = mk
        for l in range(depth):
            for k, v in prep_mixer_inputs(inp, l, b, h).items():
                m[f'{k}{l}'] = v
            for k, v in fw[l].items():
                m[f'{k}{l}'] = v
        maps.append(m)
    res = run_bass_kernel_spmd(nc, maps, core_ids=list(range(8)))
    out = np.empty_like(X)
    for c in range(8):
        b, h = c // 2, c % 2
        out[b, h * NT:(h + 1) * NT] = np.asarray(res.results[c]['xout'])
    return out


def kernel(**inp):
    X = np.ascontiguousarray(np.asarray(inp['x'], dtype=np.float32))
    return run_fused(inp, X)
```
